# Optimizing a Trainium2 kernel written in Bass

```python
import math
import jax, jax.numpy as jnp
from jax import lax
import numpy as np

D_MODEL = 1024
BATCH = 16
SEQ = 2048
DEPTH = 1

MEM_LEN = 256
ATT_HEADS = 8
ATT_HD = 64
ATT_W = ATT_HEADS * ATT_HD
MOBA_BLOCK = 256
MOBA_TOPK = 3
MOBA_QCHUNK = 16
SSD_W = D_MODEL
SSD_HD = 64
SSD_HEADS = SSD_W // SSD_HD
SSD_GROUPS = 4
SSD_HPG = SSD_HEADS // SSD_GROUPS
SSD_STATE = 128
SSD_CONV = 4
SSD_CHUNK = 256
SSD_XBC = SSD_W + 2 * SSD_GROUPS * SSD_STATE
MEM_HEADS = 4
MEM_HD = 128
MEM_W = MEM_HEADS * MEM_HD
MIX_W = ATT_W + SSD_W + MEM_W
IN_SPLITS = (ATT_W, ATT_W, ATT_W, ATT_W, SSD_W, SSD_XBC, SSD_HEADS, MEM_W, MEM_W)
IN_W = sum(IN_SPLITS)
EPS = 1e-6

kernel_name = "hybrid_moba_ssd_memxattn_block"


def rmsnorm(x, w, eps=EPS):
    xf = x.astype(jnp.float32)
    y = xf * lax.rsqrt(jnp.mean(xf * xf, axis=-1, keepdims=True) + eps)
    return (y * w.astype(jnp.float32)).astype(x.dtype)


def moba_attention(q, k, v):
    bsz, t_len, nh, hd = q.shape
    nb = -(-t_len // MOBA_BLOCK)
    tp = nb * MOBA_BLOCK
    kk = max(1, min(MOBA_TOPK, nb - 1))
    pad = ((0, 0), (0, tp - t_len), (0, 0), (0, 0))
    q, k, v = [jnp.pad(a, pad).transpose(0, 2, 1, 3) for a in (q, k, v)]
    kb = k.reshape(bsz, nh, nb, MOBA_BLOCK, hd)
    vb = v.reshape(bsz, nh, nb, MOBA_BLOCK, hd)
    kmean = jnp.mean(kb.astype(jnp.float32), axis=3)
    qblk = jnp.arange(tp) // MOBA_BLOCK
    gate = jnp.einsum('bhtd,bhnd->bhtn', q.astype(jnp.float32), kmean)
    past = jnp.arange(nb)[None, :] < qblk[:, None]
    gate = jnp.where(past, gate, -jnp.inf)
    _, idx = lax.top_k(gate, kk)
    valid = idx < qblk[:, None]

    nq = tp // MOBA_QCHUNK

    def chunks(a):
        return jnp.moveaxis(a.reshape(bsz, nh, nq, MOBA_QCHUNK, *a.shape[3:]), 2, 0)

    slopes = jnp.exp2(-8.0 * jnp.arange(1, nh + 1, dtype=jnp.float32) / nh)
    scale = hd ** -0.5
    bi = jnp.arange(bsz)[:, None, None, None]
    hi = jnp.arange(nh)[None, :, None, None]
    offs = jnp.arange(MOBA_BLOCK)

    def one_chunk(args):
        c, qc, ic, vc = args
        t = c * MOBA_QCHUNK + jnp.arange(MOBA_QCHUNK)
        ob = (c * MOBA_QCHUNK) // MOBA_BLOCK
        k_own = lax.dynamic_index_in_dim(kb, ob, axis=2, keepdims=False)
        v_own = lax.dynamic_index_in_dim(vb, ob, axis=2, keepdims=False)
        k_sel = kb[bi, hi, ic]
        v_sel = vb[bi, hi, ic]
        s_sel = jnp.einsum('bhqd,bhqjsd->bhqjs', qc, k_sel).astype(jnp.float32) * scale
        pos_sel = ic[..., None] * MOBA_BLOCK + offs
        s_sel = s_sel - slopes[:, None, None, None] * (t[:, None, None] - pos_sel).astype(jnp.float32)
        s_sel = jnp.where(vc[..., None], s_sel, -jnp.inf)
        s_own = jnp.einsum('bhqd,bhsd->bhqs', qc, k_own).astype(jnp.float32) * scale
        dist = t[:, None] - (ob * MOBA_BLOCK + offs)[None, :]
        s_own = jnp.where(dist >= 0, s_own - slopes[:, None, None] * dist.astype(jnp.float32), -jnp.inf)
        logits = jnp.concatenate([s_sel.reshape(bsz, nh, MOBA_QCHUNK, kk * MOBA_BLOCK), s_own], axis=-1)
        p = jax.nn.softmax(logits, axis=-1).astype(qc.dtype)
        p_sel = p[..., :kk * MOBA_BLOCK].reshape(bsz, nh, MOBA_QCHUNK, kk, MOBA_BLOCK)
        p_own = p[..., kk * MOBA_BLOCK:]
        return (jnp.einsum('bhqjs,bhqjsd->bhqd', p_sel, v_sel)
                + jnp.einsum('bhqs,bhsd->bhqd', p_own, v_own))

    out = lax.map(one_chunk, (jnp.arange(nq), chunks(q), chunks(idx), chunks(valid)))
    out = jnp.moveaxis(out, 0, 2).reshape(bsz, nh, tp, hd)[:, :, :t_len]
    return out.transpose(0, 2, 1, 3).reshape(bsz, t_len, nh * hd)


def ssd_chunked(xs, dt, a, bm, cm):
    bsz, t_len = xs.shape[:2]
    nc = t_len // SSD_CHUNK
    xs = xs.reshape(bsz, nc, SSD_CHUNK, SSD_GROUPS, SSD_HPG, SSD_HD)
    dt = dt.reshape(bsz, nc, SSD_CHUNK, SSD_GROUPS, SSD_HPG)
    bm = bm.reshape(bsz, nc, SSD_CHUNK, SSD_GROUPS, SSD_STATE)
    cm = cm.reshape(bsz, nc, SSD_CHUNK, SSD_GROUPS, SSD_STATE)
    da = jnp.moveaxis(dt * a, 2, -1)
    cs = jnp.cumsum(da, axis=-1)
    causal = jnp.tril(jnp.ones((SSD_CHUNK, SSD_CHUNK), dtype=bool))
    seg = cs[..., :, None] - cs[..., None, :]
    lmat = jnp.exp(jnp.where(causal, seg, -jnp.inf))
    xdt = xs * dt[..., None]
    cb = jnp.einsum('bclgn,bcsgn->bcgls', cm, bm)
    y_diag = jnp.einsum('bcgkls,bcsgkp->bclgkp', cb[:, :, :, None] * lmat, xdt)
    decay = jnp.exp(cs[..., -1:] - cs)
    states = jnp.einsum('bclgn,bcgkl,bclgkp->bcgkpn', bm, decay, xdt)
    chunk_decay = jnp.exp(cs[..., -1])

    def step(h, inp):
        st, dec = inp
        return dec[..., None, None] * h + st, h

    h0 = jnp.zeros((bsz, SSD_GROUPS, SSD_HPG, SSD_HD, SSD_STATE), dtype=states.dtype)
    _, prev = lax.scan(step, h0, (jnp.moveaxis(states, 1, 0), jnp.moveaxis(chunk_decay, 1, 0)))
    prev = jnp.moveaxis(prev, 0, 1)
    y_off = jnp.einsum('bclgn,bcgkpn,bcgkl->bclgkp', cm, prev, jnp.exp(cs))
    return (y_diag + y_off).reshape(bsz, t_len, SSD_GROUPS, SSD_HPG, SSD_HD)


def ssd_branch(xbc_raw, dt_raw, z, conv_w, conv_b, dt_bias, a_log, d_skip, norm_w):
    bsz, t_len, _ = xbc_raw.shape
    conv = lax.conv_general_dilated(xbc_raw, conv_w[:, None, :], window_strides=(1,),
                                    padding=[(SSD_CONV - 1, 0)],
                                    dimension_numbers=('NWC', 'WIO', 'NWC'),
                                    feature_group_count=SSD_XBC)
    xbc = jax.nn.silu(conv + conv_b)
    xs, bm, cm = jnp.split(xbc, [SSD_W, SSD_W + SSD_GROUPS * SSD_STATE], axis=-1)
    dt = jax.nn.softplus((dt_raw + dt_bias).astype(jnp.float32))
    a = -jnp.exp(a_log.astype(jnp.float32)).reshape(SSD_GROUPS, SSD_HPG)
    xs = xs.reshape(bsz, t_len, SSD_GROUPS, SSD_HPG, SSD_HD)
    dt = dt.reshape(bsz, t_len, SSD_GROUPS, SSD_HPG)
    bm = bm.reshape(bsz, t_len, SSD_GROUPS, SSD_STATE)
    cm = cm.reshape(bsz, t_len, SSD_GROUPS, SSD_STATE)
    tp = -(-t_len // SSD_CHUNK) * SSD_CHUNK
    padt = lambda arr: jnp.pad(arr, ((0, 0), (0, tp - t_len)) + ((0, 0),) * (arr.ndim - 2))
    y = ssd_chunked(padt(xs), padt(dt), a, padt(bm), padt(cm))[:, :t_len]
    y = (y + d_skip.reshape(SSD_GROUPS, SSD_HPG)[..., None] * xs).astype(xbc_raw.dtype)
    g = (y.reshape(bsz, t_len, SSD_W) * jax.nn.silu(z)).reshape(bsz, t_len, SSD_GROUPS, SSD_W // SSD_GROUPS)
    return rmsnorm(g, norm_w.reshape(SSD_GROUPS, SSD_W // SSD_GROUPS)).reshape(bsz, t_len, SSD_W)


def memory_attention(q, mem_n, w_kv):
    bsz, t_len, _ = q.shape
    kv = mem_n @ w_kv
    k, v = jnp.split(kv, 2, axis=-1)
    k = k.reshape(bsz, -1, MEM_HEADS, MEM_HD)
    v = v.reshape(bsz, -1, MEM_HEADS, MEM_HD)
    q = q.reshape(bsz, t_len, MEM_HEADS, MEM_HD)
    s = jnp.einsum('bthd,bmhd->bhtm', q, k).astype(jnp.float32) * (MEM_HD ** -0.5)
    p = jax.nn.softmax(s, axis=-1).astype(v.dtype)
    return jnp.einsum('bhtm,bmhd->bthd', p, v).reshape(bsz, t_len, MEM_W)


def setup_inputs(seed: int = 0) -> dict:
    key = jax.random.key(seed)
    ks = jax.random.split(key, 16)
    f32 = jnp.float32
    x = jax.random.normal(ks[0], (BATCH, SEQ, D_MODEL), f32)
    mem = jax.random.normal(ks[1], (BATCH, MEM_LEN, D_MODEL), f32)
    norm_w = 1.0 + 0.02 * jax.random.normal(ks[2], (DEPTH, D_MODEL), f32)
    w_in = jax.random.normal(ks[3], (DEPTH, D_MODEL, IN_W), f32) * D_MODEL ** -0.5
    conv_w = jax.random.normal(ks[4], (DEPTH, SSD_CONV, SSD_XBC), f32) * SSD_CONV ** -0.5
    conv_b = 0.02 * jax.random.normal(ks[5], (DEPTH, SSD_XBC), f32)
    u = jax.random.uniform(ks[6], (DEPTH, SSD_HEADS), f32)
    dt0 = jnp.exp(u * (math.log(0.1) - math.log(0.001)) + math.log(0.001))
    dt_bias = dt0 + jnp.log(-jnp.expm1(-dt0))
    a_log = jnp.log(jax.random.uniform(ks[7], (DEPTH, SSD_HEADS), f32, 1.0, 16.0))
    d_skip = 1.0 + 0.1 * jax.random.normal(ks[8], (DEPTH, SSD_HEADS), f32)
    ssd_norm_w = 1.0 + 0.02 * jax.random.normal(ks[9], (DEPTH, SSD_W), f32)
    mem_norm_w = 1.0 + 0.02 * jax.random.normal(ks[10], (DEPTH, D_MODEL), f32)
    w_mem_kv = jax.random.normal(ks[11], (DEPTH, D_MODEL, 2 * MEM_W), f32) * D_MODEL ** -0.5
    w_out = jax.random.normal(ks[12], (DEPTH, MIX_W, D_MODEL), f32) * MIX_W ** -0.5
    final_norm_w = 1.0 + 0.02 * jax.random.normal(ks[13], (D_MODEL,), f32)
    return {"x": x, "mem": mem, "norm_w": norm_w, "w_in": w_in, "conv_w": conv_w,
            "conv_b": conv_b, "dt_bias": dt_bias, "a_log": a_log, "d_skip": d_skip,
            "ssd_norm_w": ssd_norm_w, "mem_norm_w": mem_norm_w, "w_mem_kv": w_mem_kv,
            "w_out": w_out, "final_norm_w": final_norm_w}


def reference(x, mem, norm_w, w_in, conv_w, conv_b, dt_bias, a_log, d_skip,
              ssd_norm_w, mem_norm_w, w_mem_kv, w_out, final_norm_w):
    bsz, t_len, _ = x.shape
    offsets = list(np.cumsum(IN_SPLITS)[:-1])
    h = x
    for l in range(DEPTH):
        u = rmsnorm(h, norm_w[l])
        proj = u @ w_in[l]
        q_a, k_a, v_a, g_a, z_s, xbc_s, dt_s, q_m, g_m = jnp.split(proj, offsets, axis=-1)
        shp = (bsz, t_len, ATT_HEADS, ATT_HD)
        o_att = moba_attention(q_a.reshape(shp), k_a.reshape(shp), v_a.reshape(shp)) * jax.nn.silu(g_a)
        o_ssd = ssd_branch(xbc_s, dt_s, z_s, conv_w[l], conv_b[l], dt_bias[l], a_log[l],
                           d_skip[l], ssd_norm_w[l])
        mem_n = rmsnorm(mem, mem_norm_w[l])
        o_mem = memory_attention(q_m, mem_n, w_mem_kv[l]) * jax.nn.silu(g_m)
        mixed = jnp.concatenate([o_att, o_ssd, o_mem], axis=-1)
        h = h + mixed @ w_out[l]
    return rmsnorm(h, final_norm_w)
```

```python
from contextlib import ExitStack
import concourse.bass as bass
import concourse.mybir as mybir

F32 = mybir.dt.float32
BF16 = mybir.dt.bfloat16
I32 = mybir.dt.int32
AF = mybir.ActivationFunctionType
ALU = mybir.AluOpType
AX = mybir.AxisListType
ESZ = {F32: 4, BF16: 2, I32: 4}


class View:
    __slots__ = ("ap", "arena", "rngs", "pr")

    def __init__(self, ap, arena, rngs, pr=(0, 128)):
        self.ap, self.arena, self.rngs, self.pr = ap, arena, rngs, pr

    def re(self, s, **kw):
        return View(self.ap.rearrange(s, **kw), self.arena, self.rngs, self.pr)

    def bc(self, shape):
        return View(self.ap.broadcast_to(shape), self.arena, self.rngs, self.pr)

    def __getitem__(self, key):
        return View(self.ap[key], self.arena, self.rngs, self.pr)


class Arena:
    def __init__(self, name, t, dtype, ncols, const=False):
        self.name, self.t, self.dtype, self.ncols = name, t, dtype, ncols
        self.esz = ESZ[dtype]
        self.w = []
        self.r = []
        self.const = const
        self.alt = {dtype: t}
        self.psum = False
        self.qlast = [dict() for _ in range(4)]

    def v(self, lo=0, n=None, p0=0, p1=128):
        if n is None:
            n = self.ncols - lo
        assert 0 <= lo and lo + n <= self.ncols, (self.name, lo, n, self.ncols)
        return View(self.t[p0:p1, lo:lo + n], self, [(lo * self.esz, (lo + n) * self.esz)], (p0, p1))

    def v3(self, dtype, nk, stride, lo, n, p0=0, p1=128):
        if dtype not in self.alt:
            self.alt[dtype] = self.t.bitcast(dtype)
        e = ESZ[dtype]
        assert ((nk - 1) * stride + lo + n) * e <= self.ncols * self.esz
        total = self.ncols * self.esz // e
        o = max(0, lo + nk * stride - total)
        assert o <= stride - n and o <= lo, (self.name, lo, nk, stride, n, total)
        ws = lo - o
        ap = self.alt[dtype][p0:p1, ws:ws + nk * stride].rearrange("p (k s) -> p k s", s=stride)[:, :, o:o + n]
        return View(ap, self, [((k * stride + lo) * e, (k * stride + lo + n) * e) for k in range(nk)], (p0, p1))

    def vb(self, dtype, lo, n, p0=0, p1=128):
        if dtype not in self.alt:
            self.alt[dtype] = self.t.bitcast(dtype)
        e = ESZ[dtype]
        assert (lo + n) * e <= self.ncols * self.esz
        return View(self.alt[dtype][p0:p1, lo:lo + n], self, [(lo * e, (lo + n) * e)], (p0, p1))


class Sub:
    def __init__(self, arena, dtype, boff, n):
        self.a, self.dt, self.e, self.n = arena, dtype, ESZ[dtype], n
        assert boff % self.e == 0
        self.base = boff // self.e

    def v(self, lo=0, n=None, p0=0, p1=128):
        if n is None:
            n = self.n - lo
        assert 0 <= lo and lo + n <= self.n, (lo, n, self.n)
        return self.a.vb(self.dt, self.base + lo, n, p0, p1)

    def v3(self, nk, stride, lo, n, p0=0, p1=128):
        assert (nk - 1) * stride + lo + n <= self.n
        return self.a.v3(self.dt, nk, stride, self.base + lo, n, p0, p1)


class Bump:
    def __init__(self, arena, boff, size):
        self.a, self.off, self.end = arena, boff, boff + size

    def get(self, dtype, n):
        self.off = (self.off + 3) // 4 * 4
        s = Sub(self.a, dtype, self.off, n)
        self.off += n * ESZ[dtype]
        assert self.off <= self.end, ("bump overflow", self.off, self.end)
        return s


def D(ap):
    return View(ap, None, [])


COMPUTE = ("pe", "act", "dve", "pool")


class Prog:
    def __init__(self, nc, ndma=40):
        self.nc = nc
        self.ops = {e: [] for e in COMPUTE + ("sp",)}
        self.count = {e: 0 for e in COMPUTE}
        self.waited = {e: {} for e in COMPUTE + ("sp",)}
        self.ndma = ndma
        self.dma_cnt = [0] * ndma
        self.dma_next = 0
        self.nops = 0

    def op(self, eng, fn, reads=(), writes=(), dma=False):
        deps = {}

        def add(tok):
            k, v = tok
            if deps.get(k, 0) < v:
                deps[k] = v

        for vw in reads:
            a = vw.arena
            if a is None:
                continue
            for (lo, hi) in vw.rngs:
                for (l, h, t) in a.w:
                    if l < hi and lo < h:
                        add(t)
        for vw in writes:
            a = vw.arena
            if a is None:
                continue
            assert not a.const, a.name
            for (lo, hi) in vw.rngs:
                for (l, h, t) in a.w:
                    if l < hi and lo < h:
                        add(t)
                for (l, h, t) in a.r:
                    if l < hi and lo < h:
                        add(t)
        for vw in list(reads) + list(writes):
            a = vw.arena
            if a is None or not a.psum:
                continue
            for q in range(vw.pr[0] // 32, (vw.pr[1] + 31) // 32):
                for e2, t in a.qlast[q].items():
                    if e2 != eng:
                        add(t)
        if dma:
            i = self.dma_next
            self.dma_next = (i + 1) % self.ndma
            if self.dma_cnt[i] > 0:
                add((("d", i), self.dma_cnt[i]))
            self.dma_cnt[i] += 16
            tok = (("d", i), self.dma_cnt[i])
            inc = (("d", i), 16)
        else:
            self.count[eng] += 1
            tok = (eng, self.count[eng])
            inc = (eng, 1)
        waits = []
        wd = self.waited[eng]
        for k, v in deps.items():
            if k == eng and eng == "pe":
                continue
            if wd.get(k, 0) >= v:
                continue
            wd[k] = v
            waits.append((k, v))
        self.ops[eng].append((waits, fn, inc))
        self.nops += 1
        for vw in list(reads) + list(writes):
            a = vw.arena
            if a is None or not a.psum:
                continue
            for q in range(vw.pr[0] // 32, (vw.pr[1] + 31) // 32):
                a.qlast[q][eng] = tok
        for vw in reads:
            a = vw.arena
            if a is None or a.const:
                continue
            for (lo, hi) in vw.rngs:
                if not dma:
                    a.r = [x for x in a.r if not (x[2][0] == eng and lo <= x[0] and x[1] <= hi)]
                a.r.append((lo, hi, tok))
        for vw in writes:
            a = vw.arena
            if a is None:
                continue
            for (lo, hi) in vw.rngs:
                a.w = [x for x in a.w if not (lo <= x[0] and x[1] <= hi)]
                a.r = [x for x in a.r if not (lo <= x[0] and x[1] <= hi)]
                a.w.append((lo, hi, tok))
        return tok

    def freeze(self, arena):
        arena.const = True
        arena.r = []

    def matmul(self, out, lhsT, rhs, start=True, stop=True, **kw):
        return self.op("pe", lambda e: e.matmul(out.ap, lhsT.ap, rhs.ap, start=start, stop=stop, **kw),
                       [lhsT, rhs] + ([] if start else [out]), [out])

    def transpose(self, out, in_, ident):
        return self.op("pe", lambda e: e.transpose(out.ap, in_.ap, ident.ap), [in_, ident], [out])

    def act(self, out, in_, func, bias=None, scale=None, accum_out=None):
        rd = [in_]
        kw = {}
        if bias is not None:
            if isinstance(bias, View):
                rd.append(bias)
                kw["bias"] = bias.ap
            else:
                kw["bias"] = bias
        if scale is not None:
            if isinstance(scale, View):
                rd.append(scale)
                kw["scale"] = scale.ap
            else:
                kw["scale"] = scale
        wr = [out]
        if accum_out is not None:
            wr.append(accum_out)
            kw["accum_out"] = accum_out.ap
        return self.op("act", lambda e: e.activation(out.ap, in_.ap, func, **kw), rd, wr)

    def tt(self, out, in0, in1, op, eng="dve"):
        return self.op(eng, lambda e: e.tensor_tensor(out.ap, in0.ap, in1.ap, op), [in0, in1], [out])

    def ts(self, out, in0, s1, s2, op0, op1=None, eng="dve", accum_out=None):
        rd = [in0]
        a1 = s1
        a2 = s2
        if isinstance(s1, View):
            rd.append(s1)
            a1 = s1.ap
        if isinstance(s2, View):
            rd.append(s2)
            a2 = s2.ap
        kw = {}
        if op1 is not None:
            kw["op1"] = op1
        wr = [out]
        if accum_out is not None:
            kw["accum_out"] = accum_out.ap
            wr.append(accum_out)
        return self.op(eng, lambda e: e.tensor_scalar(out.ap, in0.ap, a1, a2, op0, **kw), rd, wr)

    def stt(self, out, in0, scalar, in1, op0, op1):
        rd = [in0, in1]
        sc = scalar
        if isinstance(scalar, View):
            rd.append(scalar)
            sc = scalar.ap
        return self.op("dve", lambda e: e.scalar_tensor_tensor(out.ap, in0.ap, sc, in1.ap, op0, op1), rd, [out])

    def copy(self, out, in_, eng="dve"):
        if eng == "act":
            return self.op("act", lambda e: e.copy(out.ap, in_.ap), [in_], [out])
        return self.op(eng, lambda e: e.tensor_copy(out.ap, in_.ap), [in_], [out])

    def reduce(self, out, in_, op, axis=AX.X, eng="dve"):
        return self.op(eng, lambda e: e.tensor_reduce(out.ap, in_.ap, axis, op), [in_], [out])

    def recip(self, out, in_):
        return self.op("dve", lambda e: e.reciprocal(out.ap, in_.ap), [in_], [out])

    def memset(self, out, val, eng="dve"):
        return self.op(eng, lambda e: e.memset(out.ap, val), [], [out])

    def scan(self, out, d0, d1, initial, op0, op1):
        rd = [d0, d1]
        ini = initial
        if isinstance(initial, View):
            rd.append(initial)
            ini = initial.ap
        return self.op("dve", lambda e: e.tensor_tensor_scan(out.ap, d0.ap, d1.ap, ini, op0, op1), rd, [out])

    def affine_select(self, out, in_, pattern, cmp, fill, base, cm):
        return self.op("pool", lambda e: e.affine_select(out.ap, in_.ap, pattern, cmp, fill, base=base,
                                                         channel_multiplier=cm), [in_], [out])

    def iota(self, out, pattern, base, cm):
        return self.op("pool", lambda e: e.iota(out.ap, pattern, base=base, channel_multiplier=cm), [], [out])

    def dma(self, out, in_, q="sp", **kw):
        if q == "pool":
            kw.setdefault("max_dma_last_dim", 4096)
        return self.op(q, lambda e: e.dma_start(out=out.ap, in_=in_.ap, **kw), [in_], [out], dma=True)

    def emit(self):
        nc = self.nc
        with ExitStack() as es:
            sems = {}
            for e in COMPUTE:
                sems[e] = es.enter_context(nc.semaphore("s_" + e))
            for i in range(self.ndma):
                sems[("d", i)] = es.enter_context(nc.semaphore("s_d%d" % i))
            fin = []
            for i in range(self.ndma):
                if self.dma_cnt[i] > 0:
                    fin.append((("d", i), self.dma_cnt[i]))
            for e in COMPUTE:
                if self.count[e] > 0:
                    fin.append((e, self.count[e]))
            block = es.enter_context(nc.Block())

            def run(stream, final=None):
                def f(eng):
                    for waits, fn, inc in stream:
                        for k, v in waits:
                            eng.wait_ge(sems[k], v)
                        ins = fn(eng)
                        ins.then_inc(sems[inc[0]], inc[1])
                    if final:
                        for k, v in final:
                            eng.wait_ge(sems[k], v)
                return f

            block.tensor(run(self.ops["pe"]))
            block.scalar(run(self.ops["act"]))
            block.vector(run(self.ops["dve"]))
            block.gpsimd(run(self.ops["pool"]))
            block.sync(run(self.ops["sp"], fin))
from concourse.bass_utils import run_bass_kernel_spmd
import numpy as np

T = 2048
DM = 1024
NSEQ = 2
EPS = 1e-6
BIG = 30000.0
C_Q, C_K, C_V, C_G, C_Z, C_XBC, C_DT, C_QM, C_GM = 0, 512, 1024, 1536, 2048, 3072, 5120, 5136, 5648
CB_ID, CB_TRI, CB_MNEG, CB_ONES, CB_EH, NCB = 0, 128, 256, 384, 512, 2560
CF_ID, CF_ONES, CF_PRM, CF_CAP, CF_CMK, CF_OWN, CF_MISC, NCF = 0, 128, 256, 1424, 1488, 1552, 1616, 1624
P_NW, P_MNW, P_SNW, P_CVB, P_CVW, P_DSK, P_DTB, P_ALOG, P_FNW = 0, 8, 16, 24, 40, 104, 112, 128, 144
NPRM = 1168


def host_consts():
    cb = np.zeros((128, NCB), np.float32)
    cb[:, CB_ID:CB_ID + 128] = np.eye(128)
    s = np.arange(128)[:, None]
    t = np.arange(128)[None, :]
    cb[:, CB_TRI:CB_TRI + 128] = (t >= s)
    cb[:, CB_MNEG:CB_MNEG + 128] = np.where(t < s, -BIG, 0.0)
    cb[:, CB_ONES:CB_ONES + 128] = 1.0
    for h in range(16):
        cb[h, CB_EH + h * 128: CB_EH + (h + 1) * 128] = 1.0
    cf = np.zeros((128, NCF), np.float32)
    cf[:, CF_ID:CF_ID + 128] = np.eye(128)
    cf[:, CF_ONES:CF_ONES + 128] = 1.0
    cap = np.zeros((8, 8), np.float32)
    cmk = np.zeros((8, 8), np.float32)
    own = np.zeros((8, 8), np.float32)
    for ti in range(8):
        qb = 4 + ti // 2
        for j in range(8):
            cap[ti, j] = 1e30 if j < qb else -1e30
            cmk[ti, j] = 1.0 if j < qb else 0.0
            own[ti, j] = 1.0 if j == qb else 0.0
    cf[:, CF_CAP:CF_CAP + 64] = cap.reshape(-1)[None, :]
    cf[:, CF_CMK:CF_CMK + 64] = cmk.reshape(-1)[None, :]
    cf[:, CF_OWN:CF_OWN + 64] = own.reshape(-1)[None, :]
    cf[:, CF_MISC] = EPS
    cf[:, CF_MISC + 1] = 1.0
    kc = np.zeros((128, T), np.float32)
    pos = np.arange(T)
    for r in range(8):
        kc[64 + r] = (pos // 256 == r)
    kc[96] = pos // 16
    kc[97] = pos % 16
    kc[98] = 1.0
    kc[99] = 1.0
    qc = np.zeros((8, 4, T), np.float32)
    for h in range(8):
        sl = 2.0 ** (-8.0 * (h + 1) / 8)
        qc[h, 0] = 16 * sl * 8
        qc[h, 1] = sl * 8
        qc[h, 2] = -16 * sl * 8 * (pos // 16)
        qc[h, 3] = -sl * 8 * (pos % 16)
    return cb, cf, kc, qc


def host_params(norm_w, conv_w, conv_b, dt_bias, a_log, d_skip, ssd_norm_w, mem_norm_w, final_norm_w):
    prm = np.zeros((128, NPRM), np.float32)
    prm[:, P_NW:P_NW + 8] = norm_w.reshape(8, 128).T
    prm[:, P_MNW:P_MNW + 8] = mem_norm_w.reshape(8, 128).T
    prm[:, P_SNW:P_SNW + 8] = ssd_norm_w.reshape(8, 128).T
    prm[:, P_CVB:P_CVB + 16] = conv_b.reshape(16, 128).T
    prm[:, P_CVW:P_CVW + 64] = conv_w.T.reshape(16, 128, 4).transpose(1, 0, 2).reshape(128, 64)
    prm[:, P_DSK:P_DSK + 8] = np.repeat(d_skip, 64).reshape(8, 128).T
    prm[:, P_DTB:P_DTB + 16] = dt_bias[None, :]
    prm[:, P_ALOG:P_ALOG + 16] = a_log[None, :]
    prm[:, P_FNW:P_FNW + 1024] = final_norm_w[None, :]
    return prm
def build(nseq=NSEQ, dump=None, phases="ASBME"):
    nc = bass.Bass("TRN2", target_bir_lowering=False)
    x_d = nc.dram_tensor("x", [nseq, T, DM], F32, kind="ExternalInput").ap()
    mem_d = nc.dram_tensor("mem", [nseq, 256, DM], F32, kind="ExternalInput").ap()
    win_d = nc.dram_tensor("w_in", [DM, 6160], F32, kind="ExternalInput").ap()
    wkv_d = nc.dram_tensor("w_kv", [DM, 1024], F32, kind="ExternalInput").ap()
    wout_d = nc.dram_tensor("w_out", [2048, DM], F32, kind="ExternalInput").ap()
    cb_d = nc.dram_tensor("cstb", [128, NCB], F32, kind="ExternalInput").ap()
    cf_d = nc.dram_tensor("cstf", [128, NCF], F32, kind="ExternalInput").ap()
    kc_d = nc.dram_tensor("kconst", [128, T], F32, kind="ExternalInput").ap()
    qc_d = nc.dram_tensor("qconst", [8, 4, T], F32, kind="ExternalInput").ap()
    out_d = nc.dram_tensor("out", [nseq, T, DM], F32, kind="ExternalOutput").ap()
    dumps = {}
    win3 = win_d.rearrange("(k p) c -> p k c", p=128)
    wkv3 = wkv_d.rearrange("(k p) c -> p k c", p=128)
    wout3 = wout_d.rearrange("(k p) c -> p k c", p=128)

    with ExitStack() as es:
        def sb(name, n, dt):
            t = es.enter_context(nc.sbuf_tensor(name, [128, n], dt))
            return Arena(name, t, dt, n)

        UT = sb("UT", 8 * T, BF16)
        MIX = sb("MIX", 16 * T, BF16)
        WB = sb("WB", 8 * 3088, BF16)
        WKSZ = 47616
        WK = sb("WK", WKSZ // 2, BF16)
        CB = sb("CB", NCB, BF16)
        CF = sb("CF", NCF, F32)
        SM = sb("SM", 64, F32)
        PS = []
        for i in range(8):
            t = es.enter_context(nc.psum_tensor("ps%d" % i, [128, 512], F32))
            PS.append(Arena("ps%d" % i, t, F32, 512))
            PS[-1].psum = True
        p = Prog(nc)

        def dbg(name, view, shape):
            if dump is None or name not in dump:
                return
            d = nc.dram_tensor("dbg_" + name, list(shape), view.ap.dtype, kind="ExternalOutput").ap()
            dumps[name] = d
            p.dma(D(d), view)

        p.dma(CB.v(), D(cb_d), q="pool")
        p.dma(CF.v(), D(cf_d))
        ident = CB.v(CB_ID, 128)
        tri = CB.v(CB_TRI, 128)
        mneg = CB.v(CB_MNEG, 128)
        ones = CB.v(CB_ONES, 128)
        identf = CF.v(CF_ID, 128)
        epsv = CF.v(CF_MISC, 1)
        onev = CF.v(CF_MISC + 1, 1)

        def prm(off, n, p0=0, p1=128):
            return CF.v(CF_PRM + off, n, p0, p1)

        AB = SM.v(0, 16)
        p.act(AB, prm(P_ALOG, 16), AF.Exp)
        p.ts(AB, AB, -1.0, None, ALU.mult)

        pj_i = [0]

        def rms_transpose(src_tile, ntiles, nwoff, dst_fn, wk):
            XT = [wk.get(F32, 1024) for _ in range(2)]
            XS = [wk.get(BF16, 1024) for _ in range(2)]
            JNK = wk.get(BF16, 1024)
            SSQ = wk.get(F32, 16)
            RSTD = wk.get(F32, 16)
            for i in range(ntiles):
                xt = XT[i % 2]
                p.dma(xt.v(), D(src_tile(i)))
                p.act(JNK.v(), xt.v(), AF.Square, accum_out=SSQ.v(i, 1))
                p.act(RSTD.v(i, 1), SSQ.v(i, 1), AF.Sqrt, scale=1.0 / DM, bias=epsv)
                p.recip(RSTD.v(i, 1), RSTD.v(i, 1))
                xs = XS[i % 2]
                p.ts(xs.v(), xt.v(), RSTD.v(i, 1), None, ALU.mult)
                pb = PS[i % 2]
                for k in range(8):
                    p.transpose(pb.vb(BF16, k * 128, 128), xs.v(k * 128, 128), ident)
                p.tt(dst_fn(i), pb.vb(BF16, 0, 1024).re("p (k t) -> p k t", k=8),
                     prm(nwoff, 8).re("p (k o) -> p k o", o=1).bc([128, 8, 128]), ALU.mult)

        def utv(k, t0, n):
            return UT.v(k * T + t0, n)

        def proj_fm(nw, c0, ncols, t0, n, ps_view):
            for k in range(8):
                p.matmul(ps_view, WB.v(k * nw + c0, ncols), utv(k, t0, n), start=(k == 0), stop=(k == 7))

        def phase_ssd(b):
            NW = 3088
            for k in range(8):
                p.dma(WB.v(k * NW, NW), D(win3[:, k, C_Z:C_Z + NW]), q="pool")
            b1 = Bump(MIX, 0, 16384)
            b2 = Bump(MIX, 12 * T * 2, 16384)
            wk = Bump(WK, 0, WKSZ)
            XPRE = b1.get(BF16, 16 * 259)
            XST = b1.get(BF16, 8 * 256)
            BMT = b1.get(BF16, 4 * 256)
            LT = [b1.get(BF16, 384) for _ in range(2)]
            SZ = b2.get(BF16, 8 * 256)
            PREV = b2.get(F32, 1024)
            XDD = b2.get(BF16, 2 * 1024)
            CMT = b2.get(BF16, 4 * 256)
            BTOK = b2.get(BF16, 2 * 512)
            DIAG = wk.get(BF16, 64 * 128)
            CBT = wk.get(BF16, 4 * 384)
            L0 = [wk.get(BF16, 256) for _ in range(2)]
            MT = [wk.get(BF16, 384) for _ in range(2)]
            GT = [wk.get(BF16, 256) for _ in range(2)]
            GG = [wk.get(F32, 256) for _ in range(4)]
            GSQ = [wk.get(BF16, 256) for _ in range(4)]
            RS = wk.get(F32, 256)
            XDT = wk.get(BF16, 2 * 2048)
            PREVB = wk.get(BF16, 2048)
            DTR = wk.get(F32, 32)
            DT = wk.get(F32, 32)
            DA = wk.get(F32, 256)
            NCS = wk.get(F32, 32)
            DEC = wk.get(F32, 32)
            W2 = wk.get(F32, 32)
            CST = wk.get(F32, 256)
            CSH = wk.get(BF16, 256)
            CSL = wk.get(BF16, 256)
            DECT = wk.get(F32, 256)
            DG16 = wk.get(F32, 16)
            CDB = wk.get(F32, 16)

            for zt in (XDT, PREVB, DA, CST, CSH, CSL, DECT, DG16):
                p.memset(zt.v(), 0.0, eng="pool")
            for j in range(64):
                p.ts(DIAG.v(j * 128, 128), ident, prm(P_CVW + j, 1), None, ALU.mult,
                     eng=("pool" if j % 2 else "dve"))
            p.memset(XPRE.v3(16, 259, 0, 3), 0.0)
            p.memset(PREV.v(), 0.0)

            for c in range(8):
                t0 = c * 256
                for zc in range(8):
                    ps = PS[zc % 2].v((zc // 2 % 2) * 256, 256)
                    proj_fm(NW, zc * 128, 128, t0, 256, ps)
                    p.act(SZ.v(zc * 256, 256), ps, AF.Silu)
                if c > 0:
                    p.copy(XPRE.v3(16, 259, 0, 3), XPRE.v3(16, 259, 256, 3), eng="pool")
                for cc in range(16):
                    ps = PS[cc % 2].v((cc // 2 % 2) * 256, 256)
                    proj_fm(NW, 1024 + cc * 128, 128, t0, 256, ps)
                    p.copy(XPRE.v(cc * 259 + 3, 256), ps, eng=("dve" if cc % 2 else "act"))
                for lt in range(2):
                    for k in range(8):
                        p.matmul(PS[5].v(384 + lt * 16, 16), utv(k, t0 + lt * 128, 128), WB.v(k * NW + 3072, 16),
                                 start=(k == 0), stop=(k == 7))
                p.tt(DTR.v(), PS[5].v(384, 32).re("p (l h) -> p l h", l=2),
                     prm(P_DTB, 16).re("p (o h) -> p o h", o=1).bc([128, 2, 16]), ALU.add)
                p.act(DTR.v(), DTR.v(), AF.Exp)
                p.act(DT.v(), DTR.v(), AF.Ln, bias=onev)
                p.tt(DA.v3(2, 128, 0, 16), DT.v().re("p (l h) -> p l h", l=2),
                     SM.v(0, 16).re("p (o h) -> p o h", o=1).bc([128, 2, 16]), ALU.mult)
                for lt in range(2):
                    p.transpose(PS[6].v(lt * 128, 128), DA.v(lt * 128, 128), identf)
                p.scan(CST.v(0, 256, 0, 16), CF.v(CF_ONES, 1, 0, 16).bc([16, 256]), PS[6].v(0, 256, 0, 16), 0.0, ALU.mult, ALU.add)
                p.copy(CSH.v(0, 256, 0, 16), CST.v(0, 256, 0, 16))
                p.tt(CSL.v(0, 256, 0, 16), CST.v(0, 256, 0, 16), CSH.v(0, 256, 0, 16), ALU.subtract)
                p.act(DECT.v(0, 256, 0, 16), CST.v(0, 256, 0, 16), AF.Exp, scale=-1.0, bias=CST.v(255, 1, 0, 16))
                for lt in range(2):
                    p.transpose(PS[7].v(lt * 128, 128), CST.v(lt * 128, 128), identf)
                    p.transpose(PS[7].v(256 + lt * 128, 128), DECT.v(lt * 128, 128), identf)
                p.ts(NCS.v().re("p (l h) -> p l h", l=2), PS[7].v3(F32, 2, 128, 0, 16), -1.0, None, ALU.mult)
                p.copy(DEC.v().re("p (l h) -> p l h", l=2), PS[7].v3(F32, 2, 128, 256, 16))
                p.tt(W2.v(), DT.v(), DEC.v(), ALU.mult)
                if c < 7:
                    p.ts(DG16.v(0, 16, 0, 16), CF.v(CF_ID, 16, 0, 16), CST.v(255, 1, 0, 16), None, ALU.mult)
                    p.matmul(PS[5].v(480, 16), CF.v(CF_ONES, 128), DG16.v())
                    p.act(CDB.v(), PS[5].v(480, 16), AF.Exp)
                for cc in range(16):
                    ps = PS[cc % 2].v((cc // 2 % 2) * 256, 256)
                    for k in range(4):
                        p.matmul(ps, DIAG.v((cc * 4 + k) * 128, 128), XPRE.v(cc * 259 + k, 256),
                                 start=(k == 0), stop=(k == 3))
                    if cc < 8:
                        dst = XST.v(cc * 256, 256)
                    elif cc < 12:
                        dst = BMT.v((cc - 8) * 256, 256)
                    else:
                        dst = CMT.v((cc - 12) * 256, 256)
                    p.act(dst, ps, AF.Silu, bias=prm(P_CVB + cc, 1))
                for lt in range(2):
                    pb = PS[6]
                    for cc in range(8):
                        p.transpose(pb.vb(BF16, cc * 128, 128), XST.v(cc * 256 + lt * 128, 128), ident)
                    src = pb.vb(BF16, 0, 1024).re("p (h q) -> p h q", h=16)
                    for hh in range(2):
                        p.tt(XDT.v(lt * 2048, 2048).re("p (c e q) -> p c e q", c=8, e=2)[:, :, hh, hh * 64:hh * 64 + 64],
                             src.re("p (c e) q -> p c e q", e=2)[:, :, hh, :],
                             DT.v(lt * 16, 16).re("p (c e o) -> p c e o", e=2, o=1)[:, :, hh, :].bc([128, 8, 64]),
                             ALU.mult)
                    p.tt(XDD.v(lt * 1024, 1024).re("p (h q) -> p h q", h=16), src,
                         W2.v(lt * 16, 16).re("p (h o) -> p h o", o=1).bc([128, 16, 64]), ALU.mult)
                for lt in range(2):
                    pb = PS[7]
                    for g in range(4):
                        p.transpose(pb.vb(BF16, g * 128, 128), BMT.v(g * 256 + lt * 128, 128), ident)
                    p.copy(BTOK.v(lt * 512, 512), pb.vb(BF16, 0, 512), eng="act")
                for g in range(4):
                    ps = PS[5]
                    p.matmul(ps.v(0, 256), BMT.v(g * 256, 128), CMT.v(g * 256, 256))
                    p.matmul(ps.v(256, 128), BMT.v(g * 256 + 128, 128), CMT.v(g * 256 + 128, 128))
                    p.copy(CBT.v(g * 384, 384), ps.v(0, 384), eng=("dve" if g % 2 else "act"))
                for h in range(16):
                    g = h // 4
                    pr = h % 2
                    pc = h // 2
                    sg = PS[2 + h % 2]
                    eh = CB.v(CB_EH + h * 128, 128)
                    csh = lambda lo, n: CSH.v(lo, n)
                    csl = lambda lo, n: CSL.v(lo, n)
                    p.matmul(sg.v(0, 256), eh, csh(0, 256), start=True, stop=False)
                    p.matmul(sg.v(0, 256), eh, csl(0, 256), start=False, stop=True)
                    p.matmul(sg.v(256, 128), eh, csh(0, 128), start=True, stop=False)
                    p.matmul(sg.v(256, 128), eh, csl(0, 128), start=False, stop=False)
                    p.matmul(sg.v(256, 128), ident, mneg, start=False, stop=True)
                    p.matmul(sg.v(384, 128), eh, csh(128, 128), start=True, stop=False)
                    p.matmul(sg.v(384, 128), eh, csl(128, 128), start=False, stop=False)
                    p.matmul(sg.v(384, 128), ident, mneg, start=False, stop=True)
                    l0 = L0[h % 2]
                    lt_ = LT[h % 2]
                    mt = MT[h % 2]
                    gt = GT[h % 2]
                    if c > 0:
                        p.act(l0.v(), sg.v(0, 256), AF.Exp)
                    p.act(lt_.v(0, 128), sg.v(256, 128), AF.Exp, bias=NCS.v(h, 1))
                    p.act(lt_.v(128, 128), sg.v(128, 128), AF.Exp, bias=NCS.v(h, 1))
                    p.act(lt_.v(256, 128), sg.v(384, 128), AF.Exp, bias=NCS.v(16 + h, 1))
                    p.tt(mt.v(), lt_.v(), CBT.v(g * 384, 384), ALU.mult)
                    if c > 0:
                        p.tt(gt.v(), l0.v(), CMT.v(g * 256, 256), ALU.mult, eng="pool")
                    yt = PS[4].v((pc % 2) * 256, 256)
                    yt2 = PS[4].v((pc % 2) * 256 + 128, 128)
                    p.matmul(yt, XDT.v(h * 128, 128), mt.v(0, 256), start=(pr == 0), stop=False)
                    p.matmul(yt2, XDT.v(2048 + h * 128, 128), mt.v(256, 128), start=False, stop=(c == 0 and pr == 1))
                    if c > 0:
                        p.matmul(yt, PREVB.v(h * 128, 128), gt.v(), start=False, stop=(pr == 1))
                    if pr == 1:
                        ytp = PS[4].v((pc % 2) * 256, 256)
                        gg = GG[pc % 4]
                        p.stt(gg.v(), XST.v(pc * 256, 256), prm(P_DSK + pc, 1), ytp, ALU.mult, ALU.add)
                        p.tt(gg.v(), gg.v(), SZ.v(pc * 256, 256), ALU.mult)
                        p.act(GSQ[pc % 4].v(), gg.v(), AF.Square)
                    if h % 4 == 3:
                        pcs = [pc - 1, pc]
                        ss = PS[7].v(256, 256)
                        p.matmul(ss, ones, GSQ[pcs[0] % 4].v(), start=True, stop=False)
                        p.matmul(ss, ones, GSQ[pcs[1] % 4].v(), start=False, stop=True)
                        p.act(RS.v(), ss, AF.Sqrt, scale=1.0 / 256, bias=epsv)
                        p.recip(RS.v(), RS.v())
                        for q in pcs:
                            p.stt(MIX.v((4 + q) * T + t0, 256), GG[q % 4].v(), prm(P_SNW + q, 1), RS.v(),
                                  ALU.mult, ALU.mult)
                if c < 7:
                    for g in range(4):
                        st = PS[7].v(0, 256)
                        for lt in range(2):
                            p.matmul(st, BTOK.v(lt * 512 + g * 128, 128), XDD.v(lt * 1024 + g * 256, 256),
                                     start=(lt == 0), stop=(lt == 1))
                        pv = PREV.v(g * 256, 256)
                        p.tt(pv.re("p (h q) -> p h q", h=4), pv.re("p (h q) -> p h q", h=4),
                             CDB.v(g * 4, 4).re("p (h o) -> p h o", o=1).bc([128, 4, 64]), ALU.mult)
                        p.tt(pv, pv, st, ALU.add)
                        for hh in range(2):
                            p.copy(PREVB.v(g * 512, 512).re("p (c e q) -> p c e q", c=2, e=2)[:, :, hh, hh * 64:hh * 64 + 64],
                                   pv.re("p (c e q) -> p c e q", c=2, e=2)[:, :, hh, :], eng="pool")
            dbg("ssd", MIX.v(4 * T, 8 * T), [128, 8 * T])

        def attn_core(kt_list_fn, nq, kv_lhsT, q_rhs, v_lhsT, scale, PTs, epilogue, diag_fn=None, den_lhsT=None):
            pass

        def phase_attn(b):
            NW = 2048
            for k in range(8):
                p.dma(WB.v(k * NW, NW), D(win3[:, k, 0:NW]), q="pool")
            wk = Bump(WK, 0, WKSZ)
            QK = [wk.get(BF16, T) for _ in range(4)]
            VA = wk.get(BF16, 16 * 128)
            VB = wk.get(BF16, 16 * 128)
            SG = wk.get(BF16, T)
            PT = [wk.get(BF16, 512) for _ in range(3)]
            R = wk.get(F32, 512)
            T1 = wk.get(F32, 512)
            PENB = wk.get(BF16, 8 * 128)
            GM = wk.get(F32, 64)
            CMP = wk.get(F32, 512)
            RANK = wk.get(F32, 64)
            KSUM = wk.get(F32, 8)
            KMT = wk.get(BF16, 8)
            p.memset(KMT.v(), 0.0, eng="pool")
            for i in range(2):
                p.memset(QK[i].v(), 0.0, eng="pool")
                p.dma(QK[2 + i].v(), D(kc_d), q="pool")
            p.memset(VA.v(), 1.0, eng="pool")
            p.memset(VB.v(), 1.0, eng="pool")
            p.memset(PENB.v(), 0.0, eng="pool")
            pt_i = 0
            for hp in range(1 if "p" in phases else 4):
                for hh in range(2):
                    p.dma(QK[hh].v(0, T, 96, 100), D(qc_d[2 * hp + hh]), q="pool")
                if "a" in phases:
                    continue
                for which, c0 in ((0, C_Q), (1, C_K)):
                    for tq in range(4):
                        ps = PS[2 + tq % 2]
                        proj_fm(NW, c0 + hp * 128, 128, tq * 512, 512, ps.v())
                        p.copy(QK[2 * which].v(tq * 512, 512, 0, 64), ps.v(0, 512, 0, 64), eng="act")
                        p.copy(QK[2 * which + 1].v(tq * 512, 512, 0, 64), ps.v(0, 512, 64, 128), eng="dve")
                if "b" in phases:
                    continue
                for tq in range(4):
                    ps = PS[2 + tq % 2]
                    proj_fm(NW, C_G + hp * 128, 128, tq * 512, 512, ps.v())
                    p.act(SG.v(tq * 512, 512), ps.v(), AF.Silu)
                if "c" in phases:
                    continue
                for tg in range(4):
                    ps = PS[2 + tg % 2]
                    for j in range(4):
                        ti = tg * 4 + j
                        for k in range(8):
                            p.matmul(ps.v(j * 128, 128), utv(k, ti * 128, 128), WB.v(k * NW + C_V + hp * 128, 128),
                                     start=(k == 0), stop=(k == 7))
                    src = ps.v().re("p (j c) -> p j c", j=4)
                    p.copy(VA.v3(4, 128, tg * 512, 64), src[:, :, 0:64], eng="act")
                    p.copy(VB.v3(4, 128, tg * 512 + 64, 64), src[:, :, 64:128], eng="dve")
                if "1" in phases:
                    continue
                for hh in range(2):
                    h = 2 * hp + hh
                    Q = QK[hh]
                    K = QK[2 + hh]
                    V = VA if hh == 0 else VB
                    p.reduce(KSUM.v(0, 8, 0, 64), K.v(0, T, 0, 64).re("p (j s) -> p j s", j=8), ALU.add)
                    p.ts(KMT.v(0, 8, 0, 64), KSUM.v(0, 8, 0, 64), 1.0 / 256, None, ALU.mult)
                    gps = PS[7]
                    for ti in range(8):
                        p.matmul(gps.v(ti * 8, 8), Q.v(1024 + ti * 128, 128), KMT.v())
                    p.tt(GM.v(), gps.v(0, 64), CF.v(CF_CAP, 64), ALU.min)
                    g3 = GM.v().re("p (t j) -> p t j", t=8)
                    p.tt(CMP.v().re("p (t j k) -> p t j k", t=8, j=8),
                         g3.re("p t (o k) -> p t o k", o=1).bc([128, 8, 8, 8]),
                         g3.re("p t (j o) -> p t j o", o=1).bc([128, 8, 8, 8]), ALU.is_gt)
                    p.reduce(RANK.v(), CMP.v().re("p (a k) -> p a k", k=8), ALU.add)
                    p.ts(RANK.v(), RANK.v(), 3.0, None, ALU.is_lt)
                    p.tt(RANK.v(), RANK.v(), CF.v(CF_CMK, 64), ALU.mult)
                    p.tt(RANK.v(), RANK.v(), CF.v(CF_OWN, 64), ALU.add)
                    p.ts(PENB.v3(8, 128, 64, 8), RANK.v().re("p (t j) -> p t j", t=8), -1.0, BIG, ALU.add, ALU.mult)
                    pps = PS[7]
                    for ti in range(8):
                        p.transpose(pps.vb(BF16, ti * 128, 128), PENB.v(ti * 128, 128), ident)
                    p.copy(Q.v(1024, 1024, 64, 72), pps.vb(BF16, 0, 1024, 64, 72), eng="act")
                    if "2" in phases:
                        continue
                    for qt in range(1 if "q" in phases else 4):
                        tq0 = qt * 512
                        po = PS[qt % 2]
                        nkt = 4 * qt + 4
                        for kt in range(nkt):
                            s0 = kt * 128
                            qlo = max(tq0, s0)
                            n = tq0 + 512 - qlo
                            sp = PS[4 + pt_i % 3]
                            ptile = PT[pt_i % 3]
                            pt_i += 1
                            p.matmul(sp.v(0, n), K.v(s0, 128), Q.v(qlo, n))
                            p.act(ptile.v(0, n), sp.v(0, n), AF.Exp, scale=0.125)
                            if s0 >= tq0 and "m" not in phases:
                                p.tt(ptile.v(0, 128), ptile.v(0, 128), tri, ALU.mult, eng="pool")
                            p.matmul(po.v(qlo - tq0, n), V.v(kt * 128, 128), ptile.v(0, n),
                                     start=(kt == 0), stop=(kt == nkt - 1))
                        if "e" in phases:
                            continue
                        o0, d0 = (0, 64) if hh == 0 else (64, 0)
                        p.recip(R.v(0, 512, o0, o0 + 64), po.v(0, 512, d0, d0 + 64))
                        p.tt(T1.v(0, 512, o0, o0 + 64), R.v(0, 512, o0, o0 + 64), SG.v(tq0, 512, o0, o0 + 64),
                             ALU.mult, eng="pool")
                        p.tt(MIX.v(hp * T + tq0, 512, o0, o0 + 64), po.v(0, 512, o0, o0 + 64),
                             T1.v(0, 512, o0, o0 + 64), ALU.mult)
            dbg("att", MIX.v(0, 4 * T), [128, 4 * T])

        def phase_mem(b):
            NW = 1024
            for k in range(8):
                p.dma(WB.v(k * NW, NW), D(win3[:, k, C_QM:C_QM + NW]), q="pool")
                p.dma(WB.v(8192 + k * NW, NW), D(wkv3[:, k, :]), q="pool")
            wk = Bump(WK, 0, WKSZ)
            MEMT = wk.get(BF16, 8 * 256)
            KM = wk.get(BF16, 4 * 256)
            VM = wk.get(BF16, 2 * 512)
            QM = [wk.get(BF16, T) for _ in range(2)]
            SGM = [wk.get(BF16, T) for _ in range(2)]
            PT = [wk.get(BF16, 512) for _ in range(2)]
            R = wk.get(F32, 512)
            T1 = wk.get(F32, 512)
            rms_transpose(lambda i: mem_d[b, i * 128:(i + 1) * 128, :], 2, P_MNW,
                          lambda i: MEMT.v3(8, 256, i * 128, 128), wk)
            for h in range(4):
                ps = PS[2 + h % 2]
                for k in range(8):
                    p.matmul(ps.v(0, 256), WB.v(8192 + k * NW + h * 128, 128), MEMT.v(k * 256, 256),
                             start=(k == 0), stop=(k == 7))
                p.copy(KM.v(h * 256, 256), ps.v(0, 256), eng="act")
            for mt in range(2):
                ps = PS[2 + mt % 2]
                for k in range(8):
                    p.matmul(ps.v(), MEMT.v(k * 256 + mt * 128, 128), WB.v(8192 + k * NW + 512, 512),
                             start=(k == 0), stop=(k == 7))
                p.copy(VM.v(mt * 512, 512), ps.v(), eng="dve")
            pt_i = 0
            for h in range(4):
                qm = QM[h % 2]
                sgm = SGM[h % 2]
                for tq in range(4):
                    ps = PS[2 + tq % 2]
                    proj_fm(NW, h * 128, 128, tq * 512, 512, ps.v())
                    p.copy(qm.v(tq * 512, 512), ps.v(), eng="act")
                for tq in range(4):
                    ps = PS[2 + tq % 2]
                    proj_fm(NW, 512 + h * 128, 128, tq * 512, 512, ps.v())
                    p.act(sgm.v(tq * 512, 512), ps.v(), AF.Silu)
                for qt in range(4):
                    tq0 = qt * 512
                    po = PS[qt % 2]
                    den = PS[6 + qt % 2]
                    for mt in range(2):
                        sp = PS[4 + pt_i % 2]
                        ptile = PT[pt_i % 2]
                        pt_i += 1
                        p.matmul(sp.v(), KM.v(h * 256 + mt * 128, 128), qm.v(tq0, 512))
                        p.act(ptile.v(), sp.v(), AF.Exp, scale=128 ** -0.5)
                        p.matmul(po.v(), VM.v(mt * 512 + h * 128, 128), ptile.v(), start=(mt == 0), stop=(mt == 1))
                        p.matmul(den.v(), ones, ptile.v(), start=(mt == 0), stop=(mt == 1))
                    p.recip(R.v(), den.v())
                    p.tt(T1.v(), R.v(), sgm.v(tq0, 512), ALU.mult, eng="pool")
                    p.tt(MIX.v((12 + h) * T + tq0, 512), po.v(), T1.v(), ALU.mult)
            dbg("memo", MIX.v(12 * T, 4 * T), [128, 4 * T])

        def phase_out(b):
            NW = 1024
            for k in range(16):
                p.dma(WB.v(k * NW, NW), D(wout3[:, k, :]), q="pool")
            wk = Bump(WK, 0, WKSZ)
            XT = [wk.get(F32, 1024) for _ in range(2)]
            HT = [wk.get(F32, 1024) for _ in range(2)]
            OT = [wk.get(F32, 1024) for _ in range(2)]
            JNK = wk.get(BF16, 1024)
            SSQ = wk.get(F32, 16)
            RSTD = wk.get(F32, 16)
            for i in range(16):
                xt = XT[i % 2]
                ht = HT[i % 2]
                ot = OT[i % 2]
                p.dma(xt.v(), D(x_d[b, i * 128:(i + 1) * 128, :]))
                for nh in range(2):
                    ps = PS[(2 * i + nh) % 4]
                    for m in range(16):
                        p.matmul(ps.v(), MIX.v(m * T + i * 128, 128), WB.v(m * NW + nh * 512, 512),
                                 start=(m == 0), stop=(m == 15))
                    p.tt(ht.v(nh * 512, 512), ps.v(), xt.v(nh * 512, 512), ALU.add)
                p.act(JNK.v(), ht.v(), AF.Square, accum_out=SSQ.v(i, 1))
                p.act(RSTD.v(i, 1), SSQ.v(i, 1), AF.Sqrt, scale=1.0 / DM, bias=epsv)
                p.recip(RSTD.v(i, 1), RSTD.v(i, 1))
                p.stt(ot.v(), ht.v(), RSTD.v(i, 1), prm(P_FNW, 1024), ALU.mult, ALU.mult)
                p.dma(D(out_d[b, i * 128:(i + 1) * 128, :]), ot.v())

        for b in range(nseq):
            wkA = Bump(WK, 16384, WKSZ - 16384)
            rms_transpose(lambda i: x_d[b, i * 128:(i + 1) * 128, :], 16, P_NW,
                          lambda i: UT.v3(BF16, 8, T, i * 128, 128), wkA)
            if b == 0:
                dbg("ut", UT.v(), [128, 8 * T])
            if "S" in phases:
                phase_ssd(b)
            if "B" in phases:
                phase_attn(b)
            if "M" in phases:
                phase_mem(b)
            if "E" in phases:
                phase_out(b)
        print("ops:", p.nops, {e: len(v) for e, v in p.ops.items()})
        p.emit()
    return nc, dumps


def kernel(x, mem, norm_w, w_in, conv_w, conv_b, dt_bias, a_log, d_skip, ssd_norm_w, mem_norm_w, w_mem_kv,
           w_out, final_norm_w):
    f = lambda a: np.ascontiguousarray(np.asarray(a, dtype=np.float32))
    x, mem = f(x), f(mem)
    cb, cf, kc, qc = host_consts()
    cf[:, CF_PRM:CF_PRM + NPRM] = host_params(f(norm_w)[0], f(conv_w)[0], f(conv_b)[0], f(dt_bias)[0], f(a_log)[0],
                                              f(d_skip)[0], f(ssd_norm_w)[0], f(mem_norm_w)[0], f(final_norm_w))
    nc, _ = build()
    shared = {"w_in": f(w_in)[0], "w_kv": f(w_mem_kv)[0], "w_out": f(w_out)[0], "cstb": cb, "cstf": cf,
              "kconst": kc, "qconst": qc}
    in_maps = []
    for c in range(8):
        m = dict(shared)
        m["x"] = x[c * NSEQ:(c + 1) * NSEQ]
        m["mem"] = mem[c * NSEQ:(c + 1) * NSEQ]
        in_maps.append(m)
    res = run_bass_kernel_spmd(nc, in_maps, core_ids=list(range(8)))
    return np.concatenate([r["out"] for r in res.results], axis=0)
```

```python
from contextlib import ExitStack
import concourse.bass as bass
import concourse.mybir as mybir

F32 = mybir.dt.float32
BF16 = mybir.dt.bfloat16
I32 = mybir.dt.int32
AF = mybir.ActivationFunctionType
ALU = mybir.AluOpType
AX = mybir.AxisListType
ESZ = {F32: 4, BF16: 2, I32: 4}


class View:
    __slots__ = ("ap", "arena", "rngs", "pr")

    def __init__(self, ap, arena, rngs, pr=(0, 128)):
        self.ap, self.arena, self.rngs, self.pr = ap, arena, rngs, pr

    def re(self, s, **kw):
        return View(self.ap.rearrange(s, **kw), self.arena, self.rngs, self.pr)

    def bc(self, shape):
        return View(self.ap.broadcast_to(shape), self.arena, self.rngs, self.pr)

    def __getitem__(self, key):
        return View(self.ap[key], self.arena, self.rngs, self.pr)


class Arena:
    def __init__(self, name, t, dtype, ncols, const=False):
        self.name, self.t, self.dtype, self.ncols = name, t, dtype, ncols
        self.esz = ESZ[dtype]
        self.w = []
        self.r = []
        self.const = const
        self.alt = {dtype: t}
        self.psum = False
        self.qlast = [dict() for _ in range(4)]

    def v(self, lo=0, n=None, p0=0, p1=128):
        if n is None:
            n = self.ncols - lo
        assert 0 <= lo and lo + n <= self.ncols, (self.name, lo, n, self.ncols)
        return View(self.t[p0:p1, lo:lo + n], self, [(lo * self.esz, (lo + n) * self.esz)], (p0, p1))

    def v3(self, dtype, nk, stride, lo, n, p0=0, p1=128):
        if dtype not in self.alt:
            self.alt[dtype] = self.t.bitcast(dtype)
        e = ESZ[dtype]
        assert ((nk - 1) * stride + lo + n) * e <= self.ncols * self.esz
        total = self.ncols * self.esz // e
        o = max(0, lo + nk * stride - total)
        assert o <= stride - n and o <= lo, (self.name, lo, nk, stride, n, total)
        ws = lo - o
        ap = self.alt[dtype][p0:p1, ws:ws + nk * stride].rearrange("p (k s) -> p k s", s=stride)[:, :, o:o + n]
        return View(ap, self, [((k * stride + lo) * e, (k * stride + lo + n) * e) for k in range(nk)], (p0, p1))

    def vb(self, dtype, lo, n, p0=0, p1=128):
        if dtype not in self.alt:
            self.alt[dtype] = self.t.bitcast(dtype)
        e = ESZ[dtype]
        assert (lo + n) * e <= self.ncols * self.esz
        return View(self.alt[dtype][p0:p1, lo:lo + n], self, [(lo * e, (lo + n) * e)], (p0, p1))


class Sub:
    def __init__(self, arena, dtype, boff, n):
        self.a, self.dt, self.e, self.n = arena, dtype, ESZ[dtype], n
        assert boff % self.e == 0
        self.base = boff // self.e

    def v(self, lo=0, n=None, p0=0, p1=128):
        if n is None:
            n = self.n - lo
        assert 0 <= lo and lo + n <= self.n, (lo, n, self.n)
        return self.a.vb(self.dt, self.base + lo, n, p0, p1)

    def v3(self, nk, stride, lo, n, p0=0, p1=128):
        assert (nk - 1) * stride + lo + n <= self.n
        return self.a.v3(self.dt, nk, stride, self.base + lo, n, p0, p1)


class Bump:
    def __init__(self, arena, boff, size):
        self.a, self.off, self.end = arena, boff, boff + size

    def get(self, dtype, n):
        self.off = (self.off + 3) // 4 * 4
        s = Sub(self.a, dtype, self.off, n)
        self.off += n * ESZ[dtype]
        assert self.off <= self.end, ("bump overflow", self.off, self.end)
        return s


def D(ap):
    return View(ap, None, [])


COMPUTE = ("pe", "act", "dve", "pool")


class Prog:
    def __init__(self, nc, ndma=40):
        self.nc = nc
        self.ops = {e: [] for e in COMPUTE + ("sp",)}
        self.count = {e: 0 for e in COMPUTE}
        self.waited = {e: {} for e in COMPUTE + ("sp",)}
        self.ndma = ndma
        self.dma_cnt = [0] * ndma
        self.dma_next = 0
        self.nops = 0

    def op(self, eng, fn, reads=(), writes=(), dma=False):
        deps = {}

        def add(tok):
            k, v = tok
            if deps.get(k, 0) < v:
                deps[k] = v

        for vw in reads:
            a = vw.arena
            if a is None:
                continue
            for (lo, hi) in vw.rngs:
                for (l, h, t) in a.w:
                    if l < hi and lo < h:
                        add(t)
        for vw in writes:
            a = vw.arena
            if a is None:
                continue
            assert not a.const, a.name
            for (lo, hi) in vw.rngs:
                for (l, h, t) in a.w:
                    if l < hi and lo < h:
                        add(t)
                for (l, h, t) in a.r:
                    if l < hi and lo < h:
                        add(t)
        for vw in list(reads) + list(writes):
            a = vw.arena
            if a is None or not a.psum:
                continue
            for q in range(vw.pr[0] // 32, (vw.pr[1] + 31) // 32):
                for e2, t in a.qlast[q].items():
                    if e2 != eng:
                        add(t)
        if dma:
            i = self.dma_next
            self.dma_next = (i + 1) % self.ndma
            if self.dma_cnt[i] > 0:
                add((("d", i), self.dma_cnt[i]))
            self.dma_cnt[i] += 16
            tok = (("d", i), self.dma_cnt[i])
            inc = (("d", i), 16)
        else:
            self.count[eng] += 1
            tok = (eng, self.count[eng])
            inc = (eng, 1)
        waits = []
        wd = self.waited[eng]
        for k, v in deps.items():
            if k == eng and eng == "pe":
                continue
            if wd.get(k, 0) >= v:
                continue
            wd[k] = v
            waits.append((k, v))
        self.ops[eng].append((waits, fn, inc))
        self.nops += 1
        for vw in list(reads) + list(writes):
            a = vw.arena
            if a is None or not a.psum:
                continue
            for q in range(vw.pr[0] // 32, (vw.pr[1] + 31) // 32):
                a.qlast[q][eng] = tok
        for vw in reads:
            a = vw.arena
            if a is None or a.const:
                continue
            for (lo, hi) in vw.rngs:
                if not dma:
                    a.r = [x for x in a.r if not (x[2][0] == eng and lo <= x[0] and x[1] <= hi)]
                a.r.append((lo, hi, tok))
        for vw in writes:
            a = vw.arena
            if a is None:
                continue
            for (lo, hi) in vw.rngs:
                a.w = [x for x in a.w if not (lo <= x[0] and x[1] <= hi)]
                a.r = [x for x in a.r if not (lo <= x[0] and x[1] <= hi)]
                a.w.append((lo, hi, tok))
        return tok

    def freeze(self, arena):
        arena.const = True
        arena.r = []

    def matmul(self, out, lhsT, rhs, start=True, stop=True, **kw):
        return self.op("pe", lambda e: e.matmul(out.ap, lhsT.ap, rhs.ap, start=start, stop=stop, **kw),
                       [lhsT, rhs] + ([] if start else [out]), [out])

    def transpose(self, out, in_, ident):
        return self.op("pe", lambda e: e.transpose(out.ap, in_.ap, ident.ap), [in_, ident], [out])

    def act(self, out, in_, func, bias=None, scale=None, accum_out=None):
        rd = [in_]
        kw = {}
        if bias is not None:
            if isinstance(bias, View):
                rd.append(bias)
                kw["bias"] = bias.ap
            else:
                kw["bias"] = bias
        if scale is not None:
            if isinstance(scale, View):
                rd.append(scale)
                kw["scale"] = scale.ap
            else:
                kw["scale"] = scale
        wr = [out]
        if accum_out is not None:
            wr.append(accum_out)
            kw["accum_out"] = accum_out.ap
        return self.op("act", lambda e: e.activation(out.ap, in_.ap, func, **kw), rd, wr)

    def tt(self, out, in0, in1, op, eng="dve"):
        return self.op(eng, lambda e: e.tensor_tensor(out.ap, in0.ap, in1.ap, op), [in0, in1], [out])

    def ts(self, out, in0, s1, s2, op0, op1=None, eng="dve", accum_out=None):
        rd = [in0]
        a1 = s1
        a2 = s2
        if isinstance(s1, View):
            rd.append(s1)
            a1 = s1.ap
        if isinstance(s2, View):
            rd.append(s2)
            a2 = s2.ap
        kw = {}
        if op1 is not None:
            kw["op1"] = op1
        wr = [out]
        if accum_out is not None:
            kw["accum_out"] = accum_out.ap
            wr.append(accum_out)
        return self.op(eng, lambda e: e.tensor_scalar(out.ap, in0.ap, a1, a2, op0, **kw), rd, wr)

    def stt(self, out, in0, scalar, in1, op0, op1):
        rd = [in0, in1]
        sc = scalar
        if isinstance(scalar, View):
            rd.append(scalar)
            sc = scalar.ap
        return self.op("dve", lambda e: e.scalar_tensor_tensor(out.ap, in0.ap, sc, in1.ap, op0, op1), rd, [out])

    def copy(self, out, in_, eng="dve"):
        if eng == "act":
            return self.op("act", lambda e: e.copy(out.ap, in_.ap), [in_], [out])
        return self.op(eng, lambda e: e.tensor_copy(out.ap, in_.ap), [in_], [out])

    def reduce(self, out, in_, op, axis=AX.X, eng="dve"):
        return self.op(eng, lambda e: e.tensor_reduce(out.ap, in_.ap, axis, op), [in_], [out])

    def recip(self, out, in_):
        return self.op("dve", lambda e: e.reciprocal(out.ap, in_.ap), [in_], [out])

    def memset(self, out, val, eng="dve"):
        return self.op(eng, lambda e: e.memset(out.ap, val), [], [out])

    def scan(self, out, d0, d1, initial, op0, op1):
        rd = [d0, d1]
        ini = initial
        if isinstance(initial, View):
            rd.append(initial)
            ini = initial.ap
        return self.op("dve", lambda e: e.tensor_tensor_scan(out.ap, d0.ap, d1.ap, ini, op0, op1), rd, [out])

    def affine_select(self, out, in_, pattern, cmp, fill, base, cm):
        return self.op("pool", lambda e: e.affine_select(out.ap, in_.ap, pattern, cmp, fill, base=base,
                                                         channel_multiplier=cm), [in_], [out])

    def iota(self, out, pattern, base, cm):
        return self.op("pool", lambda e: e.iota(out.ap, pattern, base=base, channel_multiplier=cm), [], [out])

    def dma(self, out, in_, q="sp", **kw):
        if q == "pool":
            kw.setdefault("max_dma_last_dim", 4096)
        return self.op(q, lambda e: e.dma_start(out=out.ap, in_=in_.ap, **kw), [in_], [out], dma=True)

    def emit(self):
        nc = self.nc
        with ExitStack() as es:
            sems = {}
            for e in COMPUTE:
                sems[e] = es.enter_context(nc.semaphore("s_" + e))
            for i in range(self.ndma):
                sems[("d", i)] = es.enter_context(nc.semaphore("s_d%d" % i))
            fin = []
            for i in range(self.ndma):
                if self.dma_cnt[i] > 0:
                    fin.append((("d", i), self.dma_cnt[i]))
            for e in COMPUTE:
                if self.count[e] > 0:
                    fin.append((e, self.count[e]))
            block = es.enter_context(nc.Block())

            def run(stream, final=None):
                def f(eng):
                    for waits, fn, inc in stream:
                        for k, v in waits:
                            eng.wait_ge(sems[k], v)
                        ins = fn(eng)
                        ins.then_inc(sems[inc[0]], inc[1])
                    if final:
                        for k, v in final:
                            eng.wait_ge(sems[k], v)
                return f

            block.tensor(run(self.ops["pe"]))
            block.scalar(run(self.ops["act"]))
            block.vector(run(self.ops["dve"]))
            block.gpsimd(run(self.ops["pool"]))
            block.sync(run(self.ops["sp"], fin))
from concourse.bass_utils import run_bass_kernel_spmd
import numpy as np

T = 2048
DM = 1024
NSEQ = 2
EPS = 1e-6
BIG = 30000.0
C_Q, C_K, C_V, C_G, C_Z, C_XBC, C_DT, C_QM, C_GM = 0, 512, 1024, 1536, 2048, 3072, 5120, 5136, 5648
CB_ID, CB_TRI, CB_MNEG, CB_ONES, CB_EH, NCB = 0, 128, 256, 384, 512, 2560
CF_ID, CF_ONES, CF_PRM, CF_CAP, CF_CMK, CF_OWN, CF_MISC, NCF = 0, 128, 256, 1424, 1488, 1552, 1616, 1624
P_NW, P_MNW, P_SNW, P_CVB, P_CVW, P_DSK, P_DTB, P_ALOG, P_FNW = 0, 8, 16, 24, 40, 104, 112, 128, 144
NPRM = 1168


def host_consts():
    cb = np.zeros((128, NCB), np.float32)
    cb[:, CB_ID:CB_ID + 128] = np.eye(128)
    s = np.arange(128)[:, None]
    t = np.arange(128)[None, :]
    cb[:, CB_TRI:CB_TRI + 128] = (t >= s)
    cb[:, CB_MNEG:CB_MNEG + 128] = np.where(t < s, -BIG, 0.0)
    cb[:, CB_ONES:CB_ONES + 128] = 1.0
    for h in range(16):
        cb[h, CB_EH + h * 128: CB_EH + (h + 1) * 128] = 1.0
    cf = np.zeros((128, NCF), np.float32)
    cf[:, CF_ID:CF_ID + 128] = np.eye(128)
    cf[:, CF_ONES:CF_ONES + 128] = 1.0
    cap = np.zeros((8, 8), np.float32)
    cmk = np.zeros((8, 8), np.float32)
    own = np.zeros((8, 8), np.float32)
    for ti in range(8):
        qb = 4 + ti // 2
        for j in range(8):
            cap[ti, j] = 1e30 if j < qb else -1e30
            cmk[ti, j] = 1.0 if j < qb else 0.0
            own[ti, j] = 1.0 if j == qb else 0.0
    cf[:, CF_CAP:CF_CAP + 64] = cap.reshape(-1)[None, :]
    cf[:, CF_CMK:CF_CMK + 64] = cmk.reshape(-1)[None, :]
    cf[:, CF_OWN:CF_OWN + 64] = own.reshape(-1)[None, :]
    cf[:, CF_MISC] = EPS
    cf[:, CF_MISC + 1] = 1.0
    kc = np.zeros((128, T), np.float32)
    pos = np.arange(T)
    for r in range(8):
        kc[64 + r] = (pos // 256 == r)
    kc[96] = pos // 16
    kc[97] = pos % 16
    kc[98] = 1.0
    kc[99] = 1.0
    qc = np.zeros((8, 4, T), np.float32)
    for h in range(8):
        sl = 2.0 ** (-8.0 * (h + 1) / 8)
        qc[h, 0] = 16 * sl * 8
        qc[h, 1] = sl * 8
        qc[h, 2] = -16 * sl * 8 * (pos // 16)
        qc[h, 3] = -sl * 8 * (pos % 16)
    return cb, cf, kc, qc


def host_params(norm_w, conv_w, conv_b, dt_bias, a_log, d_skip, ssd_norm_w, mem_norm_w, final_norm_w):
    prm = np.zeros((128, NPRM), np.float32)
    prm[:, P_NW:P_NW + 8] = norm_w.reshape(8, 128).T
    prm[:, P_MNW:P_MNW + 8] = mem_norm_w.reshape(8, 128).T
    prm[:, P_SNW:P_SNW + 8] = ssd_norm_w.reshape(8, 128).T
    prm[:, P_CVB:P_CVB + 16] = conv_b.reshape(16, 128).T
    prm[:, P_CVW:P_CVW + 64] = conv_w.T.reshape(16, 128, 4).transpose(1, 0, 2).reshape(128, 64)
    prm[:, P_DSK:P_DSK + 8] = np.repeat(d_skip, 64).reshape(8, 128).T
    prm[:, P_DTB:P_DTB + 16] = dt_bias[None, :]
    prm[:, P_ALOG:P_ALOG + 16] = a_log[None, :]
    prm[:, P_FNW:P_FNW + 1024] = final_norm_w[None, :]
    return prm
def build(nseq=NSEQ, dump=None, phases="ASBME"):
    nc = bass.Bass("TRN2", target_bir_lowering=False)
    x_d = nc.dram_tensor("x", [nseq, T, DM], F32, kind="ExternalInput").ap()
    mem_d = nc.dram_tensor("mem", [nseq, 256, DM], F32, kind="ExternalInput").ap()
    win_d = nc.dram_tensor("w_in", [DM, 6160], F32, kind="ExternalInput").ap()
    wkv_d = nc.dram_tensor("w_kv", [DM, 1024], F32, kind="ExternalInput").ap()
    wout_d = nc.dram_tensor("w_out", [2048, DM], F32, kind="ExternalInput").ap()
    cb_d = nc.dram_tensor("cstb", [128, NCB], F32, kind="ExternalInput").ap()
    cf_d = nc.dram_tensor("cstf", [128, NCF], F32, kind="ExternalInput").ap()
    kc_d = nc.dram_tensor("kconst", [128, T], F32, kind="ExternalInput").ap()
    qc_d = nc.dram_tensor("qconst", [8, 4, T], F32, kind="ExternalInput").ap()
    out_d = nc.dram_tensor("out", [nseq, T, DM], F32, kind="ExternalOutput").ap()
    dumps = {}
    win3 = win_d.rearrange("(k p) c -> p k c", p=128)
    wkv3 = wkv_d.rearrange("(k p) c -> p k c", p=128)
    wout3 = wout_d.rearrange("(k p) c -> p k c", p=128)

    with ExitStack() as es:
        def sb(name, n, dt):
            t = es.enter_context(nc.sbuf_tensor(name, [128, n], dt))
            return Arena(name, t, dt, n)

        UT = sb("UT", 8 * T, BF16)
        MIX = sb("MIX", 16 * T, BF16)
        WB = sb("WB", 8 * 3088, BF16)
        WKSZ = 47616
        WK = sb("WK", WKSZ // 2, BF16)
        CB = sb("CB", NCB, BF16)
        CF = sb("CF", NCF, F32)
        SM = sb("SM", 64, F32)
        PS = []
        for i in range(8):
            t = es.enter_context(nc.psum_tensor("ps%d" % i, [128, 512], F32))
            PS.append(Arena("ps%d" % i, t, F32, 512))
            PS[-1].psum = True
        p = Prog(nc)

        def dbg(name, view, shape):
            if dump is None or name not in dump:
                return
            d = nc.dram_tensor("dbg_" + name, list(shape), view.ap.dtype, kind="ExternalOutput").ap()
            dumps[name] = d
            p.dma(D(d), view)

        p.dma(CB.v(), D(cb_d), q="pool")
        p.dma(CF.v(), D(cf_d))
        ident = CB.v(CB_ID, 128)
        tri = CB.v(CB_TRI, 128)
        mneg = CB.v(CB_MNEG, 128)
        ones = CB.v(CB_ONES, 128)
        identf = CF.v(CF_ID, 128)
        epsv = CF.v(CF_MISC, 1)
        onev = CF.v(CF_MISC + 1, 1)

        def prm(off, n, p0=0, p1=128):
            return CF.v(CF_PRM + off, n, p0, p1)

        AB = SM.v(0, 16)
        p.act(AB, prm(P_ALOG, 16), AF.Exp)
        p.ts(AB, AB, -1.0, None, ALU.mult)

        pj_i = [0]

        def rms_transpose(src_tile, ntiles, nwoff, dst_fn, wk):
            XT = [wk.get(F32, 1024) for _ in range(2)]
            XS = [wk.get(BF16, 1024) for _ in range(2)]
            JNK = wk.get(BF16, 1024)
            SSQ = wk.get(F32, 16)
            RSTD = wk.get(F32, 16)
            for i in range(ntiles):
                xt = XT[i % 2]
                p.dma(xt.v(), D(src_tile(i)))
                p.act(JNK.v(), xt.v(), AF.Square, accum_out=SSQ.v(i, 1))
                p.act(RSTD.v(i, 1), SSQ.v(i, 1), AF.Ln, scale=1.0 / DM, bias=epsv)
                p.act(RSTD.v(i, 1), RSTD.v(i, 1), AF.Exp, scale=-0.5)
                xs = XS[i % 2]
                p.ts(xs.v(), xt.v(), RSTD.v(i, 1), None, ALU.mult)
                pb = PS[i % 2]
                for k in range(8):
                    p.transpose(pb.vb(BF16, k * 128, 128), xs.v(k * 128, 128), ident)
                p.tt(dst_fn(i), pb.vb(BF16, 0, 1024).re("p (k t) -> p k t", k=8),
                     prm(nwoff, 8).re("p (k o) -> p k o", o=1).bc([128, 8, 128]), ALU.mult)

        def utv(k, t0, n):
            return UT.v(k * T + t0, n)

        def proj_fm(nw, c0, ncols, t0, n, ps_view):
            for k in range(8):
                p.matmul(ps_view, WB.v(k * nw + c0, ncols), utv(k, t0, n), start=(k == 0), stop=(k == 7))

        def phase_ssd(b):
            NW = 3088
            for k in range(8):
                p.dma(WB.v(k * NW, NW), D(win3[:, k, C_Z:C_Z + NW]), q="pool")
            b1 = Bump(MIX, 0, 16384)
            b2 = Bump(MIX, 12 * T * 2, 16384)
            wk = Bump(WK, 0, WKSZ)
            XPRE = b1.get(BF16, 16 * 259)
            XST = b1.get(BF16, 8 * 256)
            BMT = b1.get(BF16, 4 * 256)
            LT = [b1.get(BF16, 384) for _ in range(2)]
            SZ = b2.get(BF16, 8 * 256)
            PREV = b2.get(F32, 1024)
            XDD = b2.get(BF16, 2 * 1024)
            CMT = b2.get(BF16, 4 * 256)
            BTOK = b2.get(BF16, 2 * 512)
            DIAG = wk.get(BF16, 64 * 128)
            CBT = wk.get(BF16, 4 * 384)
            L0 = [wk.get(BF16, 256) for _ in range(2)]
            MT = [wk.get(BF16, 384) for _ in range(2)]
            GT = [wk.get(BF16, 256) for _ in range(2)]
            GG = [wk.get(F32, 256) for _ in range(4)]
            GSQ = [wk.get(BF16, 256) for _ in range(4)]
            RS = wk.get(F32, 256)
            XDT = wk.get(BF16, 2 * 2048)
            PREVB = wk.get(BF16, 2048)
            DTR = wk.get(F32, 32)
            DT = wk.get(F32, 32)
            DA = wk.get(F32, 256)
            NCS = wk.get(F32, 32)
            DEC = wk.get(F32, 32)
            W2 = wk.get(F32, 32)
            CST = wk.get(F32, 256)
            CSH = wk.get(BF16, 256)
            CSL = wk.get(BF16, 256)
            DECT = wk.get(F32, 256)
            DG16 = wk.get(F32, 16)
            CDB = wk.get(F32, 16)

            for zt in (XDT, PREVB, DA, CST, CSH, CSL, DECT, DG16):
                p.memset(zt.v(), 0.0, eng="pool")
            for j in range(64):
                p.ts(DIAG.v(j * 128, 128), ident, prm(P_CVW + j, 1), None, ALU.mult,
                     eng=("pool" if j % 2 else "dve"))
            p.memset(XPRE.v3(16, 259, 0, 3), 0.0)
            p.memset(PREV.v(), 0.0)

            for c in range(8):
                t0 = c * 256
                for zc in range(8):
                    ps = PS[zc % 2].v((zc // 2 % 2) * 256, 256)
                    proj_fm(NW, zc * 128, 128, t0, 256, ps)
                    p.act(SZ.v(zc * 256, 256), ps, AF.Silu)
                if c > 0:
                    p.copy(XPRE.v3(16, 259, 0, 3), XPRE.v3(16, 259, 256, 3), eng="pool")
                for cc in range(16):
                    ps = PS[cc % 2].v((cc // 2 % 2) * 256, 256)
                    proj_fm(NW, 1024 + cc * 128, 128, t0, 256, ps)
                    p.copy(XPRE.v(cc * 259 + 3, 256), ps, eng=("dve" if cc % 2 else "act"))
                for lt in range(2):
                    for k in range(8):
                        p.matmul(PS[5].v(384 + lt * 16, 16), utv(k, t0 + lt * 128, 128), WB.v(k * NW + 3072, 16),
                                 start=(k == 0), stop=(k == 7))
                p.tt(DTR.v(), PS[5].v(384, 32).re("p (l h) -> p l h", l=2),
                     prm(P_DTB, 16).re("p (o h) -> p o h", o=1).bc([128, 2, 16]), ALU.add)
                p.act(DTR.v(), DTR.v(), AF.Exp)
                p.act(DT.v(), DTR.v(), AF.Ln, bias=onev)
                p.tt(DA.v3(2, 128, 0, 16), DT.v().re("p (l h) -> p l h", l=2),
                     SM.v(0, 16).re("p (o h) -> p o h", o=1).bc([128, 2, 16]), ALU.mult)
                for lt in range(2):
                    p.transpose(PS[6].v(lt * 128, 128), DA.v(lt * 128, 128), identf)
                p.scan(CST.v(0, 256, 0, 16), CF.v(CF_ONES, 1, 0, 16).bc([16, 256]), PS[6].v(0, 256, 0, 16), 0.0, ALU.mult, ALU.add)
                p.copy(CSH.v(0, 256, 0, 16), CST.v(0, 256, 0, 16))
                p.tt(CSL.v(0, 256, 0, 16), CST.v(0, 256, 0, 16), CSH.v(0, 256, 0, 16), ALU.subtract)
                p.act(DECT.v(0, 256, 0, 16), CST.v(0, 256, 0, 16), AF.Exp, scale=-1.0, bias=CST.v(255, 1, 0, 16))
                for lt in range(2):
                    p.transpose(PS[7].v(lt * 128, 128), CST.v(lt * 128, 128), identf)
                    p.transpose(PS[7].v(256 + lt * 128, 128), DECT.v(lt * 128, 128), identf)
                p.ts(NCS.v().re("p (l h) -> p l h", l=2), PS[7].v3(F32, 2, 128, 0, 16), -1.0, None, ALU.mult)
                p.copy(DEC.v().re("p (l h) -> p l h", l=2), PS[7].v3(F32, 2, 128, 256, 16))
                p.tt(W2.v(), DT.v(), DEC.v(), ALU.mult)
                if c < 7:
                    p.ts(DG16.v(0, 16, 0, 16), CF.v(CF_ID, 16, 0, 16), CST.v(255, 1, 0, 16), None, ALU.mult)
                    p.matmul(PS[5].v(480, 16), CF.v(CF_ONES, 128), DG16.v())
                    p.act(CDB.v(), PS[5].v(480, 16), AF.Exp)
                for cc in range(16):
                    ps = PS[cc % 2].v((cc // 2 % 2) * 256, 256)
                    for k in range(4):
                        p.matmul(ps, DIAG.v((cc * 4 + k) * 128, 128), XPRE.v(cc * 259 + k, 256),
                                 start=(k == 0), stop=(k == 3))
                    if cc < 8:
                        dst = XST.v(cc * 256, 256)
                    elif cc < 12:
                        dst = BMT.v((cc - 8) * 256, 256)
                    else:
                        dst = CMT.v((cc - 12) * 256, 256)
                    p.act(dst, ps, AF.Silu, bias=prm(P_CVB + cc, 1))
                for lt in range(2):
                    pb = PS[6]
                    for cc in range(8):
                        p.transpose(pb.vb(BF16, cc * 128, 128), XST.v(cc * 256 + lt * 128, 128), ident)
                    src = pb.vb(BF16, 0, 1024).re("p (h q) -> p h q", h=16)
                    for hh in range(2):
                        p.tt(XDT.v(lt * 2048, 2048).re("p (c e q) -> p c e q", c=8, e=2)[:, :, hh, hh * 64:hh * 64 + 64],
                             src.re("p (c e) q -> p c e q", e=2)[:, :, hh, :],
                             DT.v(lt * 16, 16).re("p (c e o) -> p c e o", e=2, o=1)[:, :, hh, :].bc([128, 8, 64]),
                             ALU.mult)
                    p.tt(XDD.v(lt * 1024, 1024).re("p (h q) -> p h q", h=16), src,
                         W2.v(lt * 16, 16).re("p (h o) -> p h o", o=1).bc([128, 16, 64]), ALU.mult)
                for lt in range(2):
                    pb = PS[7]
                    for g in range(4):
                        p.transpose(pb.vb(BF16, g * 128, 128), BMT.v(g * 256 + lt * 128, 128), ident)
                    p.copy(BTOK.v(lt * 512, 512), pb.vb(BF16, 0, 512), eng="act")
                for g in range(4):
                    ps = PS[5]
                    p.matmul(ps.v(0, 256), BMT.v(g * 256, 128), CMT.v(g * 256, 256))
                    p.matmul(ps.v(256, 128), BMT.v(g * 256 + 128, 128), CMT.v(g * 256 + 128, 128))
                    p.copy(CBT.v(g * 384, 384), ps.v(0, 384), eng=("dve" if g % 2 else "act"))
                def seg_stage(h):
                    g = h // 4
                    sg = PS[2 + h % 2]
                    eh = CB.v(CB_EH + h * 128, 128)
                    csh = lambda lo, n: CSH.v(lo, n)
                    csl = lambda lo, n: CSL.v(lo, n)
                    p.matmul(sg.v(0, 256), eh, csh(0, 256), start=True, stop=False)
                    p.matmul(sg.v(0, 256), eh, csl(0, 256), start=False, stop=True)
                    p.matmul(sg.v(256, 128), eh, csh(0, 128), start=True, stop=False)
                    p.matmul(sg.v(256, 128), eh, csl(0, 128), start=False, stop=False)
                    p.matmul(sg.v(256, 128), ident, mneg, start=False, stop=True)
                    p.matmul(sg.v(384, 128), eh, csh(128, 128), start=True, stop=False)
                    p.matmul(sg.v(384, 128), eh, csl(128, 128), start=False, stop=False)
                    p.matmul(sg.v(384, 128), ident, mneg, start=False, stop=True)
                    l0 = L0[h % 2]
                    lt_ = LT[h % 2]
                    mt = MT[h % 2]
                    gt = GT[h % 2]
                    if c > 0:
                        p.act(l0.v(), sg.v(0, 256), AF.Exp)
                    p.act(lt_.v(0, 128), sg.v(256, 128), AF.Exp, bias=NCS.v(h, 1))
                    p.act(lt_.v(128, 128), sg.v(128, 128), AF.Exp, bias=NCS.v(h, 1))
                    p.act(lt_.v(256, 128), sg.v(384, 128), AF.Exp, bias=NCS.v(16 + h, 1))
                    p.tt(mt.v(), lt_.v(), CBT.v(g * 384, 384), ALU.mult)
                    if c > 0:
                        p.tt(gt.v(), l0.v(), CMT.v(g * 256, 256), ALU.mult, eng="pool")

                def y_stage(h):
                    g = h // 4
                    pr = h % 2
                    pc = h // 2
                    mt = MT[h % 2]
                    gt = GT[h % 2]
                    ybank = PS[4 + 2 * (pc % 2)]
                    yt = ybank.v(0, 256)
                    yt2 = ybank.v(128, 128)
                    p.matmul(yt, XDT.v(h * 128, 128), mt.v(0, 256), start=(pr == 0), stop=False)
                    p.matmul(yt2, XDT.v(2048 + h * 128, 128), mt.v(256, 128), start=False, stop=(c == 0 and pr == 1))
                    if c > 0:
                        p.matmul(yt, PREVB.v(h * 128, 128), gt.v(), start=False, stop=(pr == 1))
                    if pr == 1:
                        gg = GG[pc % 4]
                        p.stt(gg.v(), XST.v(pc * 256, 256), prm(P_DSK + pc, 1), yt, ALU.mult, ALU.add)
                        p.tt(gg.v(), gg.v(), SZ.v(pc * 256, 256), ALU.mult)
                        p.act(GSQ[pc % 4].v(), gg.v(), AF.Square)
                    if h % 4 == 3:
                        pcs = [pc - 1, pc]
                        ss = PS[7].v(256, 256)
                        p.matmul(ss, ones, GSQ[pcs[0] % 4].v(), start=True, stop=False)
                        p.matmul(ss, ones, GSQ[pcs[1] % 4].v(), start=False, stop=True)
                        p.act(RS.v(), ss, AF.Ln, scale=1.0 / 256, bias=epsv)
                        p.act(RS.v(), RS.v(), AF.Exp, scale=-0.5)
                        for q in pcs:
                            p.stt(MIX.v((4 + q) * T + t0, 256), GG[q % 4].v(), prm(P_SNW + q, 1), RS.v(),
                                  ALU.mult, ALU.mult)

                for h in range(17):
                    if h < 16:
                        seg_stage(h)
                    if h >= 1:
                        y_stage(h - 1)
                if c < 7:
                    for g in range(4):
                        st = PS[7].v(0, 256)
                        for lt in range(2):
                            p.matmul(st, BTOK.v(lt * 512 + g * 128, 128), XDD.v(lt * 1024 + g * 256, 256),
                                     start=(lt == 0), stop=(lt == 1))
                        pv = PREV.v(g * 256, 256)
                        p.tt(pv.re("p (h q) -> p h q", h=4), pv.re("p (h q) -> p h q", h=4),
                             CDB.v(g * 4, 4).re("p (h o) -> p h o", o=1).bc([128, 4, 64]), ALU.mult)
                        p.tt(pv, pv, st, ALU.add)
                        for hh in range(2):
                            p.copy(PREVB.v(g * 512, 512).re("p (c e q) -> p c e q", c=2, e=2)[:, :, hh, hh * 64:hh * 64 + 64],
                                   pv.re("p (c e q) -> p c e q", c=2, e=2)[:, :, hh, :], eng="pool")
            dbg("ssd", MIX.v(4 * T, 8 * T), [128, 8 * T])

        def attn_core(kt_list_fn, nq, kv_lhsT, q_rhs, v_lhsT, scale, PTs, epilogue, diag_fn=None, den_lhsT=None):
            pass

        def phase_attn(b):
            NW = 2048
            for k in range(8):
                p.dma(WB.v(k * NW, NW), D(win3[:, k, 0:NW]), q="pool")
            wk = Bump(WK, 0, WKSZ)
            QK = [wk.get(BF16, T) for _ in range(4)]
            VA = wk.get(BF16, 16 * 128)
            VB = wk.get(BF16, 16 * 128)
            SG = wk.get(BF16, T)
            PT = [wk.get(BF16, 512) for _ in range(3)]
            R = [wk.get(F32, 512) for _ in range(2)]
            T1 = [wk.get(F32, 512) for _ in range(2)]
            PENB = wk.get(BF16, 8 * 128)
            GM = wk.get(F32, 64)
            CMP = wk.get(F32, 512)
            RANK = wk.get(F32, 64)
            KSUM = wk.get(F32, 8)
            KMT = wk.get(BF16, 8)
            p.memset(KMT.v(), 0.0, eng="pool")
            for i in range(2):
                p.memset(QK[i].v(), 0.0, eng="pool")
                p.dma(QK[2 + i].v(), D(kc_d), q="pool")
            p.memset(VA.v(), 1.0, eng="pool")
            p.memset(VB.v(), 1.0, eng="pool")
            p.memset(PENB.v(), 0.0, eng="pool")
            pt_i = 0
            for hp in range(1 if "p" in phases else 4):
                for hh in range(2):
                    p.dma(QK[hh].v(0, T, 96, 100), D(qc_d[2 * hp + hh]), q="pool")
                if "a" in phases:
                    continue
                for which, c0 in ((0, C_Q), (1, C_K)):
                    for tq in range(4):
                        ps = PS[2 + tq % 2]
                        proj_fm(NW, c0 + hp * 128, 128, tq * 512, 512, ps.v())
                        p.copy(QK[2 * which].v(tq * 512, 512, 0, 64), ps.v(0, 512, 0, 64), eng="act")
                        p.copy(QK[2 * which + 1].v(tq * 512, 512, 0, 64), ps.v(0, 512, 64, 128), eng="dve")
                if "b" in phases:
                    continue
                for tq in range(4):
                    ps = PS[2 + tq % 2]
                    proj_fm(NW, C_G + hp * 128, 128, tq * 512, 512, ps.v())
                    p.act(SG.v(tq * 512, 512), ps.v(), AF.Silu)
                if "c" in phases:
                    continue
                for tg in range(4):
                    ps = PS[2 + tg % 2]
                    for j in range(4):
                        ti = tg * 4 + j
                        for k in range(8):
                            p.matmul(ps.v(j * 128, 128), utv(k, ti * 128, 128), WB.v(k * NW + C_V + hp * 128, 128),
                                     start=(k == 0), stop=(k == 7))
                    src = ps.v().re("p (j c) -> p j c", j=4)
                    p.copy(VA.v3(4, 128, tg * 512, 64), src[:, :, 0:64], eng="act")
                    p.copy(VB.v3(4, 128, tg * 512 + 64, 64), src[:, :, 64:128], eng="dve")
                if "1" in phases:
                    continue
                for hh in range(2):
                    h = 2 * hp + hh
                    Q = QK[hh]
                    K = QK[2 + hh]
                    V = VA if hh == 0 else VB
                    p.reduce(KSUM.v(0, 8, 0, 64), K.v(0, T, 0, 64).re("p (j s) -> p j s", j=8), ALU.add)
                    p.ts(KMT.v(0, 8, 0, 64), KSUM.v(0, 8, 0, 64), 1.0 / 256, None, ALU.mult)
                    gps = PS[7]
                    for ti in range(8):
                        p.matmul(gps.v(ti * 8, 8), Q.v(1024 + ti * 128, 128), KMT.v())
                    p.tt(GM.v(), gps.v(0, 64), CF.v(CF_CAP, 64), ALU.min)
                    g3 = GM.v().re("p (t j) -> p t j", t=8)
                    p.tt(CMP.v().re("p (t j k) -> p t j k", t=8, j=8),
                         g3.re("p t (o k) -> p t o k", o=1).bc([128, 8, 8, 8]),
                         g3.re("p t (j o) -> p t j o", o=1).bc([128, 8, 8, 8]), ALU.is_gt)
                    p.reduce(RANK.v(), CMP.v().re("p (a k) -> p a k", k=8), ALU.add)
                    p.ts(RANK.v(), RANK.v(), 3.0, None, ALU.is_lt)
                    p.tt(RANK.v(), RANK.v(), CF.v(CF_CMK, 64), ALU.mult)
                    p.tt(RANK.v(), RANK.v(), CF.v(CF_OWN, 64), ALU.add)
                    p.ts(PENB.v3(8, 128, 64, 8), RANK.v().re("p (t j) -> p t j", t=8), -1.0, BIG, ALU.add, ALU.mult)
                    pps = PS[7]
                    for ti in range(8):
                        p.transpose(pps.vb(BF16, ti * 128, 128), PENB.v(ti * 128, 128), ident)
                    p.copy(Q.v(1024, 1024, 64, 72), pps.vb(BF16, 0, 1024, 64, 72), eng="act")
                    tiles = [(qt, kt) for qt in range(4) for kt in range(4 * qt + 4)]
                    o0, d0 = (0, 64) if hh == 0 else (64, 0)

                    def emit_qk(idx):
                        qt, kt = tiles[idx]
                        tq0 = qt * 512
                        s0 = kt * 128
                        qlo = max(tq0, s0)
                        n = tq0 + 512 - qlo
                        gi = pt_base + idx
                        sp = PS[4 + gi % 3]
                        ptile = PT[gi % 3]
                        p.matmul(sp.v(0, n), K.v(s0, 128), Q.v(qlo, n))
                        p.act(ptile.v(0, n), sp.v(0, n), AF.Exp, scale=0.125)
                        if s0 >= tq0:
                            p.tt(ptile.v(0, 128), ptile.v(0, 128), tri, ALU.mult, eng="pool")

                    def emit_pv(idx):
                        qt, kt = tiles[idx]
                        tq0 = qt * 512
                        s0 = kt * 128
                        qlo = max(tq0, s0)
                        n = tq0 + 512 - qlo
                        gi = pt_base + idx
                        nkt = 4 * qt + 4
                        po = PS[qt % 2]
                        ptile = PT[gi % 3]
                        p.matmul(po.v(qlo - tq0, n), V.v(kt * 128, 128), ptile.v(0, n),
                                 start=(kt == 0), stop=(kt == nkt - 1))
                        if kt == nkt - 1:
                            r_ = R[qt % 2]
                            t1_ = T1[qt % 2]
                            p.recip(r_.v(0, 512, o0, o0 + 64), po.v(0, 512, d0, d0 + 64))
                            p.tt(t1_.v(0, 512, o0, o0 + 64), r_.v(0, 512, o0, o0 + 64), SG.v(tq0, 512, o0, o0 + 64),
                                 ALU.mult, eng="pool")
                            p.tt(MIX.v(hp * T + tq0, 512, o0, o0 + 64), po.v(0, 512, o0, o0 + 64),
                                 t1_.v(0, 512, o0, o0 + 64), ALU.mult)

                    pt_base = pt_i
                    LA = 2
                    for idx in range(len(tiles) + LA):
                        if idx < len(tiles):
                            emit_qk(idx)
                        if idx >= LA:
                            emit_pv(idx - LA)
                    pt_i += len(tiles)
            dbg("att", MIX.v(0, 4 * T), [128, 4 * T])

        def phase_mem(b):
            NW = 1024
            for k in range(8):
                p.dma(WB.v(k * NW, NW), D(win3[:, k, C_QM:C_QM + NW]), q="pool")
                p.dma(WB.v(8192 + k * NW, NW), D(wkv3[:, k, :]), q="pool")
            wk = Bump(WK, 0, WKSZ)
            MEMT = wk.get(BF16, 8 * 256)
            KM = wk.get(BF16, 4 * 256)
            VM = wk.get(BF16, 2 * 512)
            QM = [wk.get(BF16, T) for _ in range(2)]
            SGM = [wk.get(BF16, T) for _ in range(2)]
            PT = [wk.get(BF16, 512) for _ in range(2)]
            R = wk.get(F32, 512)
            T1 = wk.get(F32, 512)
            rms_transpose(lambda i: mem_d[b, i * 128:(i + 1) * 128, :], 2, P_MNW,
                          lambda i: MEMT.v3(8, 256, i * 128, 128), wk)
            for h in range(4):
                ps = PS[2 + h % 2]
                for k in range(8):
                    p.matmul(ps.v(0, 256), WB.v(8192 + k * NW + h * 128, 128), MEMT.v(k * 256, 256),
                             start=(k == 0), stop=(k == 7))
                p.copy(KM.v(h * 256, 256), ps.v(0, 256), eng="act")
            for mt in range(2):
                ps = PS[2 + mt % 2]
                for k in range(8):
                    p.matmul(ps.v(), MEMT.v(k * 256 + mt * 128, 128), WB.v(8192 + k * NW + 512, 512),
                             start=(k == 0), stop=(k == 7))
                p.copy(VM.v(mt * 512, 512), ps.v(), eng="dve")
            pt_i = 0
            for h in range(4):
                qm = QM[h % 2]
                sgm = SGM[h % 2]
                for tq in range(4):
                    ps = PS[2 + tq % 2]
                    proj_fm(NW, h * 128, 128, tq * 512, 512, ps.v())
                    p.copy(qm.v(tq * 512, 512), ps.v(), eng="act")
                for tq in range(4):
                    ps = PS[2 + tq % 2]
                    proj_fm(NW, 512 + h * 128, 128, tq * 512, 512, ps.v())
                    p.act(sgm.v(tq * 512, 512), ps.v(), AF.Silu)
                for qt in range(4):
                    tq0 = qt * 512
                    po = PS[qt % 2]
                    den = PS[6 + qt % 2]
                    for mt in range(2):
                        sp = PS[4 + pt_i % 2]
                        ptile = PT[pt_i % 2]
                        pt_i += 1
                        p.matmul(sp.v(), KM.v(h * 256 + mt * 128, 128), qm.v(tq0, 512))
                        p.act(ptile.v(), sp.v(), AF.Exp, scale=128 ** -0.5)
                        p.matmul(po.v(), VM.v(mt * 512 + h * 128, 128), ptile.v(), start=(mt == 0), stop=(mt == 1))
                        p.matmul(den.v(), ones, ptile.v(), start=(mt == 0), stop=(mt == 1))
                    p.recip(R.v(), den.v())
                    p.tt(T1.v(), R.v(), sgm.v(tq0, 512), ALU.mult, eng="pool")
                    p.tt(MIX.v((12 + h) * T + tq0, 512), po.v(), T1.v(), ALU.mult)
            dbg("memo", MIX.v(12 * T, 4 * T), [128, 4 * T])

        def phase_out(b):
            NW = 1024
            for k in range(16):
                p.dma(WB.v(k * NW, NW), D(wout3[:, k, :]), q="pool")
            wk = Bump(WK, 0, WKSZ)
            XT = [wk.get(F32, 1024) for _ in range(2)]
            HT = [wk.get(F32, 1024) for _ in range(2)]
            OT = [wk.get(F32, 1024) for _ in range(2)]
            JNK = wk.get(BF16, 1024)
            SSQ = wk.get(F32, 16)
            RSTD = wk.get(F32, 16)
            for i in range(16):
                xt = XT[i % 2]
                ht = HT[i % 2]
                ot = OT[i % 2]
                p.dma(xt.v(), D(x_d[b, i * 128:(i + 1) * 128, :]))
                for nh in range(2):
                    ps = PS[(2 * i + nh) % 4]
                    for m in range(16):
                        p.matmul(ps.v(), MIX.v(m * T + i * 128, 128), WB.v(m * NW + nh * 512, 512),
                                 start=(m == 0), stop=(m == 15))
                    p.tt(ht.v(nh * 512, 512), ps.v(), xt.v(nh * 512, 512), ALU.add)
                p.act(JNK.v(), ht.v(), AF.Square, accum_out=SSQ.v(i, 1))
                p.act(RSTD.v(i, 1), SSQ.v(i, 1), AF.Ln, scale=1.0 / DM, bias=epsv)
                p.act(RSTD.v(i, 1), RSTD.v(i, 1), AF.Exp, scale=-0.5)
                p.stt(ot.v(), ht.v(), RSTD.v(i, 1), prm(P_FNW, 1024), ALU.mult, ALU.mult)
                p.dma(D(out_d[b, i * 128:(i + 1) * 128, :]), ot.v())

        for b in range(nseq):
            wkA = Bump(WK, 16384, WKSZ - 16384)
            rms_transpose(lambda i: x_d[b, i * 128:(i + 1) * 128, :], 16, P_NW,
                          lambda i: UT.v3(BF16, 8, T, i * 128, 128), wkA)
            if b == 0:
                dbg("ut", UT.v(), [128, 8 * T])
            if "S" in phases:
                phase_ssd(b)
            if "B" in phases:
                phase_attn(b)
            if "M" in phases:
                phase_mem(b)
            if "E" in phases:
                phase_out(b)
        print("ops:", p.nops, {e: len(v) for e, v in p.ops.items()})
        p.emit()
    return nc, dumps


def kernel(x, mem, norm_w, w_in, conv_w, conv_b, dt_bias, a_log, d_skip, ssd_norm_w, mem_norm_w, w_mem_kv,
           w_out, final_norm_w):
    f = lambda a: np.ascontiguousarray(np.asarray(a, dtype=np.float32))
    x, mem = f(x), f(mem)
    cb, cf, kc, qc = host_consts()
    cf[:, CF_PRM:CF_PRM + NPRM] = host_params(f(norm_w)[0], f(conv_w)[0], f(conv_b)[0], f(dt_bias)[0], f(a_log)[0],
                                              f(d_skip)[0], f(ssd_norm_w)[0], f(mem_norm_w)[0], f(final_norm_w))
    nc, _ = build()
    shared = {"w_in": f(w_in)[0], "w_kv": f(w_mem_kv)[0], "w_out": f(w_out)[0], "cstb": cb, "cstf": cf,
              "kconst": kc, "qconst": qc}
    in_maps = []
    for c in range(8):
        m = dict(shared)
        m["x"] = x[c * NSEQ:(c + 1) * NSEQ]
        m["mem"] = mem[c * NSEQ:(c + 1) * NSEQ]
        in_maps.append(m)
    res = run_bass_kernel_spmd(nc, in_maps, core_ids=list(range(8)))
    return np.concatenate([r["out"] for r in res.results], axis=0)
```

```python
from contextlib import ExitStack
import concourse.bass as bass
import concourse.mybir as mybir

F32 = mybir.dt.float32
BF16 = mybir.dt.bfloat16
I32 = mybir.dt.int32
AF = mybir.ActivationFunctionType
ALU = mybir.AluOpType
AX = mybir.AxisListType
ESZ = {F32: 4, BF16: 2, I32: 4}


class View:
    __slots__ = ("ap", "arena", "rngs", "pr")

    def __init__(self, ap, arena, rngs, pr=(0, 128)):
        self.ap, self.arena, self.rngs, self.pr = ap, arena, rngs, pr

    def re(self, s, **kw):
        return View(self.ap.rearrange(s, **kw), self.arena, self.rngs, self.pr)

    def bc(self, shape):
        return View(self.ap.broadcast_to(shape), self.arena, self.rngs, self.pr)

    def __getitem__(self, key):
        return View(self.ap[key], self.arena, self.rngs, self.pr)


class Arena:
    def __init__(self, name, t, dtype, ncols, const=False):
        self.name, self.t, self.dtype, self.ncols = name, t, dtype, ncols
        self.esz = ESZ[dtype]
        self.w = []
        self.r = []
        self.const = const
        self.alt = {dtype: t}
        self.psum = False
        self.qlast = [dict() for _ in range(4)]

    def v(self, lo=0, n=None, p0=0, p1=128):
        if n is None:
            n = self.ncols - lo
        assert 0 <= lo and lo + n <= self.ncols, (self.name, lo, n, self.ncols)
        return View(self.t[p0:p1, lo:lo + n], self, [(lo * self.esz, (lo + n) * self.esz)], (p0, p1))

    def v3(self, dtype, nk, stride, lo, n, p0=0, p1=128):
        if dtype not in self.alt:
            self.alt[dtype] = self.t.bitcast(dtype)
        e = ESZ[dtype]
        assert ((nk - 1) * stride + lo + n) * e <= self.ncols * self.esz
        total = self.ncols * self.esz // e
        o = max(0, lo + nk * stride - total)
        assert o <= stride - n and o <= lo, (self.name, lo, nk, stride, n, total)
        ws = lo - o
        ap = self.alt[dtype][p0:p1, ws:ws + nk * stride].rearrange("p (k s) -> p k s", s=stride)[:, :, o:o + n]
        return View(ap, self, [((k * stride + lo) * e, (k * stride + lo + n) * e) for k in range(nk)], (p0, p1))

    def vb(self, dtype, lo, n, p0=0, p1=128):
        if dtype not in self.alt:
            self.alt[dtype] = self.t.bitcast(dtype)
        e = ESZ[dtype]
        assert (lo + n) * e <= self.ncols * self.esz
        return View(self.alt[dtype][p0:p1, lo:lo + n], self, [(lo * e, (lo + n) * e)], (p0, p1))


class Sub:
    def __init__(self, arena, dtype, boff, n):
        self.a, self.dt, self.e, self.n = arena, dtype, ESZ[dtype], n
        assert boff % self.e == 0
        self.base = boff // self.e

    def v(self, lo=0, n=None, p0=0, p1=128):
        if n is None:
            n = self.n - lo
        assert 0 <= lo and lo + n <= self.n, (lo, n, self.n)
        return self.a.vb(self.dt, self.base + lo, n, p0, p1)

    def v3(self, nk, stride, lo, n, p0=0, p1=128):
        assert (nk - 1) * stride + lo + n <= self.n
        return self.a.v3(self.dt, nk, stride, self.base + lo, n, p0, p1)


class Bump:
    def __init__(self, arena, boff, size):
        self.a, self.off, self.end = arena, boff, boff + size

    def get(self, dtype, n):
        self.off = (self.off + 3) // 4 * 4
        s = Sub(self.a, dtype, self.off, n)
        self.off += n * ESZ[dtype]
        assert self.off <= self.end, ("bump overflow", self.off, self.end)
        return s


def D(ap):
    return View(ap, None, [])


COMPUTE = ("pe", "act", "dve", "pool")


class Prog:
    def __init__(self, nc, ndma=40):
        self.nc = nc
        self.ops = {e: [] for e in COMPUTE + ("sp",)}
        self.count = {e: 0 for e in COMPUTE}
        self.waited = {e: {} for e in COMPUTE + ("sp",)}
        self.ndma = ndma
        self.dma_cnt = [0] * ndma
        self.dma_next = 0
        self.nops = 0

    def op(self, eng, fn, reads=(), writes=(), dma=False):
        deps = {}

        def add(tok):
            k, v = tok
            if deps.get(k, 0) < v:
                deps[k] = v

        for vw in reads:
            a = vw.arena
            if a is None:
                continue
            for (lo, hi) in vw.rngs:
                for (l, h, t) in a.w:
                    if l < hi and lo < h:
                        add(t)
        for vw in writes:
            a = vw.arena
            if a is None:
                continue
            assert not a.const, a.name
            for (lo, hi) in vw.rngs:
                for (l, h, t) in a.w:
                    if l < hi and lo < h:
                        add(t)
                for (l, h, t) in a.r:
                    if l < hi and lo < h:
                        add(t)
        for vw in list(reads) + list(writes):
            a = vw.arena
            if a is None or not a.psum:
                continue
            for q in range(vw.pr[0] // 32, (vw.pr[1] + 31) // 32):
                for e2, t in a.qlast[q].items():
                    if e2 != eng:
                        add(t)
        if dma:
            i = self.dma_next
            self.dma_next = (i + 1) % self.ndma
            if self.dma_cnt[i] > 0:
                add((("d", i), self.dma_cnt[i]))
            self.dma_cnt[i] += 16
            tok = (("d", i), self.dma_cnt[i])
            inc = (("d", i), 16)
        else:
            self.count[eng] += 1
            tok = (eng, self.count[eng])
            inc = (eng, 1)
        waits = []
        wd = self.waited[eng]
        for k, v in deps.items():
            if k == eng and eng == "pe":
                continue
            if wd.get(k, 0) >= v:
                continue
            wd[k] = v
            waits.append((k, v))
        self.ops[eng].append((waits, fn, inc))
        self.nops += 1
        for vw in list(reads) + list(writes):
            a = vw.arena
            if a is None or not a.psum:
                continue
            for q in range(vw.pr[0] // 32, (vw.pr[1] + 31) // 32):
                a.qlast[q][eng] = tok
        for vw in reads:
            a = vw.arena
            if a is None or a.const:
                continue
            for (lo, hi) in vw.rngs:
                if not dma:
                    a.r = [x for x in a.r if not (x[2][0] == eng and lo <= x[0] and x[1] <= hi)]
                a.r.append((lo, hi, tok))
        for vw in writes:
            a = vw.arena
            if a is None:
                continue
            for (lo, hi) in vw.rngs:
                a.w = [x for x in a.w if not (lo <= x[0] and x[1] <= hi)]
                a.r = [x for x in a.r if not (lo <= x[0] and x[1] <= hi)]
                a.w.append((lo, hi, tok))
        return tok

    def freeze(self, arena):
        arena.const = True
        arena.r = []

    def matmul(self, out, lhsT, rhs, start=True, stop=True, **kw):
        return self.op("pe", lambda e: e.matmul(out.ap, lhsT.ap, rhs.ap, start=start, stop=stop, **kw),
                       [lhsT, rhs] + ([] if start else [out]), [out])

    def transpose(self, out, in_, ident):
        return self.op("pe", lambda e: e.transpose(out.ap, in_.ap, ident.ap), [in_, ident], [out])

    def act(self, out, in_, func, bias=None, scale=None, accum_out=None):
        rd = [in_]
        kw = {}
        if bias is not None:
            if isinstance(bias, View):
                rd.append(bias)
                kw["bias"] = bias.ap
            else:
                kw["bias"] = bias
        if scale is not None:
            if isinstance(scale, View):
                rd.append(scale)
                kw["scale"] = scale.ap
            else:
                kw["scale"] = scale
        wr = [out]
        if accum_out is not None:
            wr.append(accum_out)
            kw["accum_out"] = accum_out.ap
        return self.op("act", lambda e: e.activation(out.ap, in_.ap, func, **kw), rd, wr)

    def tt(self, out, in0, in1, op, eng="dve"):
        return self.op(eng, lambda e: e.tensor_tensor(out.ap, in0.ap, in1.ap, op), [in0, in1], [out])

    def ts(self, out, in0, s1, s2, op0, op1=None, eng="dve", accum_out=None):
        rd = [in0]
        a1 = s1
        a2 = s2
        if isinstance(s1, View):
            rd.append(s1)
            a1 = s1.ap
        if isinstance(s2, View):
            rd.append(s2)
            a2 = s2.ap
        kw = {}
        if op1 is not None:
            kw["op1"] = op1
        wr = [out]
        if accum_out is not None:
            kw["accum_out"] = accum_out.ap
            wr.append(accum_out)
        return self.op(eng, lambda e: e.tensor_scalar(out.ap, in0.ap, a1, a2, op0, **kw), rd, wr)

    def stt(self, out, in0, scalar, in1, op0, op1):
        rd = [in0, in1]
        sc = scalar
        if isinstance(scalar, View):
            rd.append(scalar)
            sc = scalar.ap
        return self.op("dve", lambda e: e.scalar_tensor_tensor(out.ap, in0.ap, sc, in1.ap, op0, op1), rd, [out])

    def copy(self, out, in_, eng="dve"):
        if eng == "act":
            return self.op("act", lambda e: e.copy(out.ap, in_.ap), [in_], [out])
        return self.op(eng, lambda e: e.tensor_copy(out.ap, in_.ap), [in_], [out])

    def reduce(self, out, in_, op, axis=AX.X, eng="dve"):
        return self.op(eng, lambda e: e.tensor_reduce(out.ap, in_.ap, axis, op), [in_], [out])

    def recip(self, out, in_):
        return self.op("dve", lambda e: e.reciprocal(out.ap, in_.ap), [in_], [out])

    def memset(self, out, val, eng="dve"):
        return self.op(eng, lambda e: e.memset(out.ap, val), [], [out])

    def scan(self, out, d0, d1, initial, op0, op1):
        rd = [d0, d1]
        ini = initial
        if isinstance(initial, View):
            rd.append(initial)
            ini = initial.ap
        return self.op("dve", lambda e: e.tensor_tensor_scan(out.ap, d0.ap, d1.ap, ini, op0, op1), rd, [out])

    def affine_select(self, out, in_, pattern, cmp, fill, base, cm):
        return self.op("pool", lambda e: e.affine_select(out.ap, in_.ap, pattern, cmp, fill, base=base,
                                                         channel_multiplier=cm), [in_], [out])

    def iota(self, out, pattern, base, cm):
        return self.op("pool", lambda e: e.iota(out.ap, pattern, base=base, channel_multiplier=cm), [], [out])

    def dma(self, out, in_, q="sp", **kw):
        if q == "pool":
            kw.setdefault("max_dma_last_dim", 4096)
        return self.op(q, lambda e: e.dma_start(out=out.ap, in_=in_.ap, **kw), [in_], [out], dma=True)

    def emit(self):
        nc = self.nc
        with ExitStack() as es:
            sems = {}
            for e in COMPUTE:
                sems[e] = es.enter_context(nc.semaphore("s_" + e))
            for i in range(self.ndma):
                sems[("d", i)] = es.enter_context(nc.semaphore("s_d%d" % i))
            fin = []
            for i in range(self.ndma):
                if self.dma_cnt[i] > 0:
                    fin.append((("d", i), self.dma_cnt[i]))
            for e in COMPUTE:
                if self.count[e] > 0:
                    fin.append((e, self.count[e]))
            block = es.enter_context(nc.Block())

            def run(stream, final=None):
                def f(eng):
                    for waits, fn, inc in stream:
                        for k, v in waits:
                            eng.wait_ge(sems[k], v)
                        ins = fn(eng)
                        ins.then_inc(sems[inc[0]], inc[1])
                    if final:
                        for k, v in final:
                            eng.wait_ge(sems[k], v)
                return f

            block.tensor(run(self.ops["pe"]))
            block.scalar(run(self.ops["act"]))
            block.vector(run(self.ops["dve"]))
            block.gpsimd(run(self.ops["pool"]))
            block.sync(run(self.ops["sp"], fin))
from concourse.bass_utils import run_bass_kernel_spmd
import numpy as np

T = 2048
DM = 1024
NSEQ = 2
EPS = 1e-6
BIG = 30000.0
C_Q, C_K, C_V, C_G, C_Z, C_XBC, C_DT, C_QM, C_GM = 0, 512, 1024, 1536, 2048, 3072, 5120, 5136, 5648
CB_ID, CB_TRI, CB_MNEG, CB_ONES, CB_EH, NCB = 0, 128, 256, 384, 512, 2560
CF_ID, CF_ONES, CF_PRM, CF_CAP, CF_CMK, CF_OWN, CF_MISC, NCF = 0, 128, 256, 1424, 1488, 1552, 1616, 1624
P_NW, P_MNW, P_SNW, P_CVB, P_CVW, P_DSK, P_DTB, P_ALOG, P_FNW = 0, 8, 16, 24, 40, 104, 112, 128, 144
NPRM = 1168


def host_consts():
    cb = np.zeros((128, NCB), np.float32)
    cb[:, CB_ID:CB_ID + 128] = np.eye(128)
    s = np.arange(128)[:, None]
    t = np.arange(128)[None, :]
    cb[:, CB_TRI:CB_TRI + 128] = (t >= s)
    cb[:, CB_MNEG:CB_MNEG + 128] = np.where(t < s, -BIG, 0.0)
    cb[:, CB_ONES:CB_ONES + 128] = 1.0
    for h in range(16):
        cb[h, CB_EH + h * 128: CB_EH + (h + 1) * 128] = 1.0
    cf = np.zeros((128, NCF), np.float32)
    cf[:, CF_ID:CF_ID + 128] = np.eye(128)
    cf[:, CF_ONES:CF_ONES + 128] = 1.0
    cap = np.zeros((8, 8), np.float32)
    cmk = np.zeros((8, 8), np.float32)
    own = np.zeros((8, 8), np.float32)
    for ti in range(8):
        qb = 4 + ti // 2
        for j in range(8):
            cap[ti, j] = 1e30 if j < qb else -1e30
            cmk[ti, j] = 1.0 if j < qb else 0.0
            own[ti, j] = 1.0 if j == qb else 0.0
    cf[:, CF_CAP:CF_CAP + 64] = cap.reshape(-1)[None, :]
    cf[:, CF_CMK:CF_CMK + 64] = cmk.reshape(-1)[None, :]
    cf[:, CF_OWN:CF_OWN + 64] = own.reshape(-1)[None, :]
    cf[:, CF_MISC] = EPS
    cf[:, CF_MISC + 1] = 1.0
    kc = np.zeros((128, T), np.float32)
    pos = np.arange(T)
    for r in range(8):
        kc[64 + r] = (pos // 256 == r)
    kc[96] = pos // 16
    kc[97] = pos % 16
    kc[98] = 1.0
    kc[99] = 1.0
    qc = np.zeros((8, 4, T), np.float32)
    for h in range(8):
        sl = 2.0 ** (-8.0 * (h + 1) / 8)
        qc[h, 0] = 16 * sl * 8
        qc[h, 1] = sl * 8
        qc[h, 2] = -16 * sl * 8 * (pos // 16)
        qc[h, 3] = -sl * 8 * (pos % 16)
    return cb, cf, kc, qc


def host_params(norm_w, conv_w, conv_b, dt_bias, a_log, d_skip, ssd_norm_w, mem_norm_w, final_norm_w):
    prm = np.zeros((128, NPRM), np.float32)
    prm[:, P_NW:P_NW + 8] = norm_w.reshape(8, 128).T
    prm[:, P_MNW:P_MNW + 8] = mem_norm_w.reshape(8, 128).T
    prm[:, P_SNW:P_SNW + 8] = ssd_norm_w.reshape(8, 128).T
    prm[:, P_CVB:P_CVB + 16] = conv_b.reshape(16, 128).T
    prm[:, P_CVW:P_CVW + 64] = conv_w.T.reshape(16, 128, 4).transpose(1, 0, 2).reshape(128, 64)
    prm[:, P_DSK:P_DSK + 8] = np.repeat(d_skip, 64).reshape(8, 128).T
    prm[:, P_DTB:P_DTB + 16] = dt_bias[None, :]
    prm[:, P_ALOG:P_ALOG + 16] = a_log[None, :]
    prm[:, P_FNW:P_FNW + 1024] = final_norm_w[None, :]
    return prm
def build(nseq=NSEQ, dump=None, phases="ASBME"):
    nc = bass.Bass("TRN2", target_bir_lowering=False)
    x_d = nc.dram_tensor("x", [nseq, T, DM], F32, kind="ExternalInput").ap()
    mem_d = nc.dram_tensor("mem", [nseq, 256, DM], F32, kind="ExternalInput").ap()
    win_d = nc.dram_tensor("w_in", [DM, 6160], F32, kind="ExternalInput").ap()
    wkv_d = nc.dram_tensor("w_kv", [DM, 1024], F32, kind="ExternalInput").ap()
    wout_d = nc.dram_tensor("w_out", [2048, DM], F32, kind="ExternalInput").ap()
    cb_d = nc.dram_tensor("cstb", [128, NCB], F32, kind="ExternalInput").ap()
    cf_d = nc.dram_tensor("cstf", [128, NCF], F32, kind="ExternalInput").ap()
    kc_d = nc.dram_tensor("kconst", [128, T], F32, kind="ExternalInput").ap()
    qc_d = nc.dram_tensor("qconst", [8, 4, T], F32, kind="ExternalInput").ap()
    out_d = nc.dram_tensor("out", [nseq, T, DM], F32, kind="ExternalOutput").ap()
    dumps = {}
    wsi_d = nc.dram_tensor("wsc_in", [DM, 6160], BF16, kind="Internal").ap()
    wskv_d = nc.dram_tensor("wsc_kv", [DM, 1024], BF16, kind="Internal").ap()
    wso_d = nc.dram_tensor("wsc_out", [2048, DM], BF16, kind="Internal").ap()
    wsi3 = wsi_d.rearrange("(k p) c -> p k c", p=128)
    wskv3 = wskv_d.rearrange("(k p) c -> p k c", p=128)
    wso3 = wso_d.rearrange("(k p) c -> p k c", p=128)
    win3 = win_d.rearrange("(k p) c -> p k c", p=128)
    wkv3 = wkv_d.rearrange("(k p) c -> p k c", p=128)
    wout3 = wout_d.rearrange("(k p) c -> p k c", p=128)

    with ExitStack() as es:
        def sb(name, n, dt):
            t = es.enter_context(nc.sbuf_tensor(name, [128, n], dt))
            return Arena(name, t, dt, n)

        UT = sb("UT", 8 * T, BF16)
        MIX = sb("MIX", 16 * T, BF16)
        WB = sb("WB", 8 * 3088, BF16)
        WKSZ = 47616
        WK = sb("WK", WKSZ // 2, BF16)
        CB = sb("CB", NCB, BF16)
        CF = sb("CF", NCF, F32)
        SM = sb("SM", 64, F32)
        PS = []
        for i in range(8):
            t = es.enter_context(nc.psum_tensor("ps%d" % i, [128, 512], F32))
            PS.append(Arena("ps%d" % i, t, F32, 512))
            PS[-1].psum = True
        p = Prog(nc)

        def dbg(name, view, shape):
            if dump is None or name not in dump:
                return
            d = nc.dram_tensor("dbg_" + name, list(shape), view.ap.dtype, kind="ExternalOutput").ap()
            dumps[name] = d
            p.dma(D(d), view)

        p.dma(CB.v(), D(cb_d), q="pool")
        p.dma(CF.v(), D(cf_d))
        ident = CB.v(CB_ID, 128)
        tri = CB.v(CB_TRI, 128)
        mneg = CB.v(CB_MNEG, 128)
        ones = CB.v(CB_ONES, 128)
        identf = CF.v(CF_ID, 128)
        epsv = CF.v(CF_MISC, 1)
        onev = CF.v(CF_MISC + 1, 1)

        def prm(off, n, p0=0, p1=128):
            return CF.v(CF_PRM + off, n, p0, p1)

        AB = SM.v(0, 16)
        p.act(AB, prm(P_ALOG, 16), AF.Exp)
        p.ts(AB, AB, -1.0, None, ALU.mult)

        SC = {n: Arena("sc_" + n, None, BF16, 1) for n in ("attn", "mem", "ssd", "kv", "out")}
        converted = [False]

        def scv(ap, name, k):
            return View(ap, SC[name], [(k, k + 1)])

        def convert_all():
            for name, src3, dst3, c0, nw, nk in (("attn", win3, wsi3, 0, 2048, 8), ("kv", wkv3, wskv3, 0, 1024, 8),
                                                 ("mem", win3, wsi3, C_QM, 1024, 8), ("out", wout3, wso3, 0, 1024, 16),
                                                 ("ssd", win3, wsi3, C_Z, 3088, 8)):
                for k in range(nk):
                    p.dma(scv(dst3[:, k, c0:c0 + nw], name, k), D(src3[:, k, c0:c0 + nw]), q="pool")
            converted[0] = True

        def load_w(name, dst3, src3, c0, nw, nk, wb_off=0):
            for k in range(nk):
                if converted[0]:
                    p.dma(WB.v(wb_off + k * nw, nw), scv(dst3[:, k, c0:c0 + nw], name, k), q="sp")
                else:
                    p.dma(WB.v(wb_off + k * nw, nw), D(src3[:, k, c0:c0 + nw]), q="pool")

        pj_i = [0]

        def rms_transpose(src_tile, ntiles, nwoff, dst_fn, wk):
            XT = [wk.get(F32, 1024) for _ in range(2)]
            XS = [wk.get(BF16, 1024) for _ in range(2)]
            JNK = wk.get(BF16, 1024)
            SSQ = wk.get(F32, 16)
            RSTD = wk.get(F32, 16)
            for i in range(ntiles):
                xt = XT[i % 2]
                p.dma(xt.v(), D(src_tile(i)))
                p.act(JNK.v(), xt.v(), AF.Square, accum_out=SSQ.v(i, 1))
                p.act(RSTD.v(i, 1), SSQ.v(i, 1), AF.Ln, scale=1.0 / DM, bias=epsv)
                p.act(RSTD.v(i, 1), RSTD.v(i, 1), AF.Exp, scale=-0.5)
                xs = XS[i % 2]
                p.ts(xs.v(), xt.v(), RSTD.v(i, 1), None, ALU.mult)
                pb = PS[i % 2]
                for k in range(8):
                    p.transpose(pb.vb(BF16, k * 128, 128), xs.v(k * 128, 128), ident)
                p.tt(dst_fn(i), pb.vb(BF16, 0, 1024).re("p (k t) -> p k t", k=8),
                     prm(nwoff, 8).re("p (k o) -> p k o", o=1).bc([128, 8, 128]), ALU.mult)

        def utv(k, t0, n):
            return UT.v(k * T + t0, n)

        def proj_fm(nw, c0, ncols, t0, n, ps_view):
            for k in range(8):
                p.matmul(ps_view, WB.v(k * nw + c0, ncols), utv(k, t0, n), start=(k == 0), stop=(k == 7))

        def phase_ssd(b):
            NW = 3088
            load_w("ssd", wsi3, win3, C_Z, NW, 8)
            if not converted[0]:
                convert_all()
            b1 = Bump(MIX, 0, 16384)
            b2 = Bump(MIX, 12 * T * 2, 16384)
            wk = Bump(WK, 0, WKSZ)
            XPRE = b1.get(BF16, 16 * 259)
            XST = b1.get(BF16, 8 * 256)
            BMT = b1.get(BF16, 4 * 256)
            LT = [b1.get(BF16, 384) for _ in range(2)]
            SZ = b2.get(BF16, 8 * 256)
            PREV = b2.get(F32, 1024)
            XDD = b2.get(BF16, 2 * 1024)
            CMT = b2.get(BF16, 4 * 256)
            BTOK = b2.get(BF16, 2 * 512)
            DIAG = wk.get(BF16, 64 * 128)
            CBT = wk.get(BF16, 4 * 384)
            L0 = [wk.get(BF16, 256) for _ in range(2)]
            MT = [wk.get(BF16, 384) for _ in range(2)]
            GT = [wk.get(BF16, 256) for _ in range(2)]
            GG = [wk.get(F32, 256) for _ in range(4)]
            GSQ = [wk.get(BF16, 256) for _ in range(4)]
            RS = wk.get(F32, 256)
            XDT = wk.get(BF16, 2 * 2048)
            PREVB = wk.get(BF16, 2048)
            DTR = wk.get(F32, 32)
            DT = wk.get(F32, 32)
            DA = wk.get(F32, 256)
            NCS = wk.get(F32, 32)
            DEC = wk.get(F32, 32)
            W2 = wk.get(F32, 32)
            CST = wk.get(F32, 256)
            CSH = wk.get(BF16, 256)
            CSL = wk.get(BF16, 256)
            DECT = wk.get(F32, 256)
            DG16 = wk.get(F32, 16)
            CDB = wk.get(F32, 16)

            for zt in (XDT, PREVB, DA, CST, CSH, CSL, DECT, DG16):
                p.memset(zt.v(), 0.0, eng="pool")
            for j in range(64):
                p.ts(DIAG.v(j * 128, 128), ident, prm(P_CVW + j, 1), None, ALU.mult,
                     eng=("pool" if j % 2 else "dve"))
            p.memset(XPRE.v3(16, 259, 0, 3), 0.0)
            p.memset(PREV.v(), 0.0)

            for c in range(8):
                t0 = c * 256

                def zgroup(zc):
                    ps = PS[zc % 2].v((zc // 2 % 2) * 256, 256)
                    proj_fm(NW, zc * 128, 128, t0, 256, ps)
                    p.copy(SZ.v(zc * 256, 256), ps, eng="dve")

                def xgroup(cc):
                    ps = PS[cc % 2].v((cc // 2 % 2) * 256, 256)
                    proj_fm(NW, 1024 + cc * 128, 128, t0, 256, ps)
                    p.copy(XPRE.v(cc * 259 + 3, 256), ps, eng=("dve" if cc % 2 else "act"))

                if c > 0:
                    p.copy(XPRE.v3(16, 259, 0, 3), XPRE.v3(16, 259, 256, 3), eng="pool")
                for lt in range(2):
                    for k in range(8):
                        p.matmul(PS[5].v(384 + lt * 16, 16), utv(k, t0 + lt * 128, 128), WB.v(k * NW + 3072, 16),
                                 start=(k == 0), stop=(k == 7))
                p.tt(DTR.v(), PS[5].v(384, 32).re("p (l h) -> p l h", l=2),
                     prm(P_DTB, 16).re("p (o h) -> p o h", o=1).bc([128, 2, 16]), ALU.add)
                p.act(DTR.v(), DTR.v(), AF.Exp)
                p.act(DT.v(), DTR.v(), AF.Ln, bias=onev)
                p.tt(DA.v3(2, 128, 0, 16), DT.v().re("p (l h) -> p l h", l=2),
                     SM.v(0, 16).re("p (o h) -> p o h", o=1).bc([128, 2, 16]), ALU.mult)
                for zc in range(6):
                    zgroup(zc)
                for lt in range(2):
                    p.transpose(PS[6].v(lt * 128, 128), DA.v(lt * 128, 128), identf)
                p.scan(CST.v(0, 256, 0, 16), CF.v(CF_ONES, 1, 0, 16).bc([16, 256]), PS[6].v(0, 256, 0, 16), 0.0,
                       ALU.mult, ALU.add)
                p.copy(CSH.v(0, 256, 0, 16), CST.v(0, 256, 0, 16))
                p.tt(CSL.v(0, 256, 0, 16), CST.v(0, 256, 0, 16), CSH.v(0, 256, 0, 16), ALU.subtract)
                p.act(DECT.v(0, 256, 0, 16), CST.v(0, 256, 0, 16), AF.Exp, scale=-1.0, bias=CST.v(255, 1, 0, 16))
                for zc in range(6, 8):
                    zgroup(zc)
                for cc in range(8, 12):
                    xgroup(cc)
                for lt in range(2):
                    p.transpose(PS[7].v(lt * 128, 128), CST.v(lt * 128, 128), identf)
                    p.transpose(PS[7].v(256 + lt * 128, 128), DECT.v(lt * 128, 128), identf)
                p.ts(NCS.v().re("p (l h) -> p l h", l=2), PS[7].v3(F32, 2, 128, 0, 16), -1.0, None, ALU.mult)
                p.copy(DEC.v().re("p (l h) -> p l h", l=2), PS[7].v3(F32, 2, 128, 256, 16))
                p.tt(W2.v(), DT.v(), DEC.v(), ALU.mult)
                if c < 7:
                    p.ts(DG16.v(0, 16, 0, 16), CF.v(CF_ID, 16, 0, 16), CST.v(255, 1, 0, 16), None, ALU.mult)
                    p.matmul(PS[5].v(480, 16), CF.v(CF_ONES, 128), DG16.v())
                    p.act(CDB.v(), PS[5].v(480, 16), AF.Exp)
                for cc in list(range(12, 16)) + list(range(0, 8)):
                    xgroup(cc)

                def conv(cc):
                    ps = PS[cc % 2].v((cc // 2 % 2) * 256, 256)
                    for k in range(4):
                        p.matmul(ps, DIAG.v((cc * 4 + k) * 128, 128), XPRE.v(cc * 259 + k, 256),
                                 start=(k == 0), stop=(k == 3))
                    if cc < 8:
                        dst = XST.v(cc * 256, 256)
                    elif cc < 12:
                        dst = BMT.v((cc - 8) * 256, 256)
                    else:
                        dst = CMT.v((cc - 12) * 256, 256)
                    p.act(dst, ps, AF.Silu, bias=prm(P_CVB + cc, 1))

                for cc in range(8, 16):
                    conv(cc)
                for lt in range(2):
                    pb = PS[7]
                    for g in range(4):
                        p.transpose(pb.vb(BF16, g * 128, 128), BMT.v(g * 256 + lt * 128, 128), ident)
                    p.copy(BTOK.v(lt * 512, 512), pb.vb(BF16, 0, 512), eng="dve")
                for g in range(4):
                    ps = PS[5]
                    p.matmul(ps.v(0, 256), BMT.v(g * 256, 128), CMT.v(g * 256, 256))
                    p.matmul(ps.v(256, 128), BMT.v(g * 256 + 128, 128), CMT.v(g * 256 + 128, 128))
                    p.copy(CBT.v(g * 384, 384), ps.v(0, 384), eng="dve")
                for cc in range(0, 8):
                    conv(cc)
                p.act(SZ.v(), SZ.v(), AF.Silu)
                for lt in range(2):
                    pb = PS[6]
                    for cc in range(8):
                        p.transpose(pb.vb(BF16, cc * 128, 128), XST.v(cc * 256 + lt * 128, 128), ident)
                    src = pb.vb(BF16, 0, 1024).re("p (h q) -> p h q", h=16)
                    for hh in range(2):
                        p.tt(XDT.v(lt * 2048, 2048).re("p (c e q) -> p c e q", c=8, e=2)[:, :, hh, hh * 64:hh * 64 + 64],
                             src.re("p (c e) q -> p c e q", e=2)[:, :, hh, :],
                             DT.v(lt * 16, 16).re("p (c e o) -> p c e o", e=2, o=1)[:, :, hh, :].bc([128, 8, 64]),
                             ALU.mult)
                    p.tt(XDD.v(lt * 1024, 1024).re("p (h q) -> p h q", h=16), src,
                         W2.v(lt * 16, 16).re("p (h o) -> p h o", o=1).bc([128, 16, 64]), ALU.mult)
                def seg_stage(h):
                    g = h // 4
                    sg = PS[2 + h % 2]
                    eh = CB.v(CB_EH + h * 128, 128)
                    csh = lambda lo, n: CSH.v(lo, n)
                    csl = lambda lo, n: CSL.v(lo, n)
                    p.matmul(sg.v(0, 256), eh, csh(0, 256), start=True, stop=False)
                    p.matmul(sg.v(0, 256), eh, csl(0, 256), start=False, stop=True)
                    p.matmul(sg.v(256, 128), eh, csh(0, 128), start=True, stop=False)
                    p.matmul(sg.v(256, 128), eh, csl(0, 128), start=False, stop=False)
                    p.matmul(sg.v(256, 128), ident, mneg, start=False, stop=True)
                    p.matmul(sg.v(384, 128), eh, csh(128, 128), start=True, stop=False)
                    p.matmul(sg.v(384, 128), eh, csl(128, 128), start=False, stop=False)
                    p.matmul(sg.v(384, 128), ident, mneg, start=False, stop=True)
                    l0 = L0[h % 2]
                    lt_ = LT[h % 2]
                    mt = MT[h % 2]
                    gt = GT[h % 2]
                    if c > 0:
                        p.act(l0.v(), sg.v(0, 256), AF.Exp)
                    p.act(lt_.v(0, 128), sg.v(256, 128), AF.Exp, bias=NCS.v(h, 1))
                    p.act(lt_.v(128, 128), sg.v(128, 128), AF.Exp, bias=NCS.v(h, 1))
                    p.act(lt_.v(256, 128), sg.v(384, 128), AF.Exp, bias=NCS.v(16 + h, 1))
                    p.tt(mt.v(), lt_.v(), CBT.v(g * 384, 384), ALU.mult)
                    if c > 0:
                        p.tt(gt.v(), l0.v(), CMT.v(g * 256, 256), ALU.mult, eng="pool")

                def y_stage(h):
                    g = h // 4
                    pr = h % 2
                    pc = h // 2
                    mt = MT[h % 2]
                    gt = GT[h % 2]
                    ybank = PS[4 + 2 * (pc % 2)]
                    yt = ybank.v(0, 256)
                    yt2 = ybank.v(128, 128)
                    p.matmul(yt, XDT.v(h * 128, 128), mt.v(0, 256), start=(pr == 0), stop=False)
                    p.matmul(yt2, XDT.v(2048 + h * 128, 128), mt.v(256, 128), start=False, stop=(c == 0 and pr == 1))
                    if c > 0:
                        p.matmul(yt, PREVB.v(h * 128, 128), gt.v(), start=False, stop=(pr == 1))
                    if pr == 1:
                        gg = GG[pc % 4]
                        p.stt(gg.v(), XST.v(pc * 256, 256), prm(P_DSK + pc, 1), yt, ALU.mult, ALU.add)
                        p.tt(gg.v(), gg.v(), SZ.v(pc * 256, 256), ALU.mult)
                        p.act(GSQ[pc % 4].v(), gg.v(), AF.Square)
                def norm_stage(g):
                        pc = 2 * g + 1
                        pcs = [pc - 1, pc]
                        ss = PS[7].v(256, 256)
                        p.matmul(ss, ones, GSQ[pcs[0] % 4].v(), start=True, stop=False)
                        p.matmul(ss, ones, GSQ[pcs[1] % 4].v(), start=False, stop=True)
                        p.act(RS.v(), ss, AF.Ln, scale=1.0 / 256, bias=epsv)
                        p.act(RS.v(), RS.v(), AF.Exp, scale=-0.5)
                        for q in pcs:
                            p.stt(MIX.v((4 + q) * T + t0, 256), GG[q % 4].v(), prm(P_SNW + q, 1), RS.v(),
                                  ALU.mult, ALU.mult)

                for h in range(18):
                    if h < 16:
                        seg_stage(h)
                    if 1 <= h <= 16:
                        y_stage(h - 1)
                    if h >= 2 and (h - 2) % 4 == 3:
                        norm_stage((h - 2) // 4)
                if c < 7:
                    for g in range(4):
                        st = PS[7].v(0, 256)
                        for lt in range(2):
                            p.matmul(st, BTOK.v(lt * 512 + g * 128, 128), XDD.v(lt * 1024 + g * 256, 256),
                                     start=(lt == 0), stop=(lt == 1))
                        pv = PREV.v(g * 256, 256)
                        p.tt(pv.re("p (h q) -> p h q", h=4), pv.re("p (h q) -> p h q", h=4),
                             CDB.v(g * 4, 4).re("p (h o) -> p h o", o=1).bc([128, 4, 64]), ALU.mult)
                        p.tt(pv, pv, st, ALU.add)
                        for hh in range(2):
                            p.copy(PREVB.v(g * 512, 512).re("p (c e q) -> p c e q", c=2, e=2)[:, :, hh, hh * 64:hh * 64 + 64],
                                   pv.re("p (c e q) -> p c e q", c=2, e=2)[:, :, hh, :], eng="pool")
            dbg("ssd", MIX.v(4 * T, 8 * T), [128, 8 * T])

        def attn_core(kt_list_fn, nq, kv_lhsT, q_rhs, v_lhsT, scale, PTs, epilogue, diag_fn=None, den_lhsT=None):
            pass

        def phase_attn(b):
            NW = 2048
            load_w("attn", wsi3, win3, 0, NW, 8)
            wk = Bump(WK, 0, WKSZ)
            QK = [wk.get(BF16, T) for _ in range(4)]
            VA = wk.get(BF16, 16 * 128)
            VB = wk.get(BF16, 16 * 128)
            SG = wk.get(BF16, T)
            PT = [wk.get(BF16, 512) for _ in range(3)]
            R = [wk.get(F32, 512) for _ in range(2)]
            T1 = [wk.get(F32, 512) for _ in range(2)]
            PENB = wk.get(BF16, 8 * 128)
            GM = wk.get(F32, 64)
            CMP = wk.get(F32, 512)
            RANK = wk.get(F32, 64)
            KSUM = wk.get(F32, 8)
            KMT = wk.get(BF16, 8)
            p.memset(KMT.v(), 0.0, eng="pool")
            for i in range(2):
                p.memset(QK[i].v(), 0.0, eng="pool")
                p.dma(QK[2 + i].v(), D(kc_d), q="pool")
            p.memset(VA.v(), 1.0, eng="pool")
            p.memset(VB.v(), 1.0, eng="pool")
            p.memset(PENB.v(), 0.0, eng="pool")
            pt_i = 0
            for hp in range(1 if "p" in phases else 4):
                for hh in range(2):
                    p.dma(QK[hh].v(0, T, 96, 100), D(qc_d[2 * hp + hh]), q="pool")
                if "a" in phases:
                    continue
                for which, c0 in ((0, C_Q), (1, C_K)):
                    for tq in range(4):
                        ps = PS[2 + tq % 2]
                        proj_fm(NW, c0 + hp * 128, 128, tq * 512, 512, ps.v())
                        p.copy(QK[2 * which].v(tq * 512, 512, 0, 64), ps.v(0, 512, 0, 64), eng="act")
                        p.copy(QK[2 * which + 1].v(tq * 512, 512, 0, 64), ps.v(0, 512, 64, 128), eng="dve")
                if "b" in phases:
                    continue
                for tq in range(4):
                    ps = PS[2 + tq % 2]
                    proj_fm(NW, C_G + hp * 128, 128, tq * 512, 512, ps.v())
                    p.act(SG.v(tq * 512, 512), ps.v(), AF.Silu)
                if "c" in phases:
                    continue
                for tg in range(4):
                    ps = PS[2 + tg % 2]
                    for j in range(4):
                        ti = tg * 4 + j
                        for k in range(8):
                            p.matmul(ps.v(j * 128, 128), utv(k, ti * 128, 128), WB.v(k * NW + C_V + hp * 128, 128),
                                     start=(k == 0), stop=(k == 7))
                    src = ps.v().re("p (j c) -> p j c", j=4)
                    p.copy(VA.v3(4, 128, tg * 512, 64), src[:, :, 0:64], eng="act")
                    p.copy(VB.v3(4, 128, tg * 512 + 64, 64), src[:, :, 64:128], eng="dve")
                if "1" in phases:
                    continue
                for hh in range(2):
                    h = 2 * hp + hh
                    Q = QK[hh]
                    K = QK[2 + hh]
                    V = VA if hh == 0 else VB
                    p.reduce(KSUM.v(0, 8, 0, 64), K.v(0, T, 0, 64).re("p (j s) -> p j s", j=8), ALU.add)
                    p.ts(KMT.v(0, 8, 0, 64), KSUM.v(0, 8, 0, 64), 1.0 / 256, None, ALU.mult)
                    gps = PS[7]
                    for ti in range(8):
                        p.matmul(gps.v(ti * 8, 8), Q.v(1024 + ti * 128, 128), KMT.v())
                    p.tt(GM.v(), gps.v(0, 64), CF.v(CF_CAP, 64), ALU.min)
                    g3 = GM.v().re("p (t j) -> p t j", t=8)
                    p.tt(CMP.v().re("p (t j k) -> p t j k", t=8, j=8),
                         g3.re("p t (o k) -> p t o k", o=1).bc([128, 8, 8, 8]),
                         g3.re("p t (j o) -> p t j o", o=1).bc([128, 8, 8, 8]), ALU.is_gt)
                    p.reduce(RANK.v(), CMP.v().re("p (a k) -> p a k", k=8), ALU.add)
                    p.ts(RANK.v(), RANK.v(), 3.0, None, ALU.is_lt)
                    p.tt(RANK.v(), RANK.v(), CF.v(CF_CMK, 64), ALU.mult)
                    p.tt(RANK.v(), RANK.v(), CF.v(CF_OWN, 64), ALU.add)
                    p.ts(PENB.v3(8, 128, 64, 8), RANK.v().re("p (t j) -> p t j", t=8), -1.0, BIG, ALU.add, ALU.mult)
                    pps = PS[7]
                    for ti in range(8):
                        p.transpose(pps.vb(BF16, ti * 128, 128), PENB.v(ti * 128, 128), ident)
                    p.copy(Q.v(1024, 1024, 64, 72), pps.vb(BF16, 0, 1024, 64, 72), eng="act")
                    tiles = [(qt, kt) for qt in range(4) for kt in range(4 * qt + 4)]
                    o0, d0 = (0, 64) if hh == 0 else (64, 0)

                    def emit_qk(idx):
                        qt, kt = tiles[idx]
                        tq0 = qt * 512
                        s0 = kt * 128
                        qlo = max(tq0, s0)
                        n = tq0 + 512 - qlo
                        gi = pt_base + idx
                        sp = PS[4 + gi % 3]
                        ptile = PT[gi % 3]
                        p.matmul(sp.v(0, n), K.v(s0, 128), Q.v(qlo, n))
                        p.act(ptile.v(0, n), sp.v(0, n), AF.Exp, scale=0.125)
                        if s0 >= tq0:
                            p.tt(ptile.v(0, 128), ptile.v(0, 128), tri, ALU.mult, eng="pool")

                    def emit_pv(idx):
                        qt, kt = tiles[idx]
                        tq0 = qt * 512
                        s0 = kt * 128
                        qlo = max(tq0, s0)
                        n = tq0 + 512 - qlo
                        gi = pt_base + idx
                        nkt = 4 * qt + 4
                        po = PS[qt % 2]
                        ptile = PT[gi % 3]
                        p.matmul(po.v(qlo - tq0, n), V.v(kt * 128, 128), ptile.v(0, n),
                                 start=(kt == 0), stop=(kt == nkt - 1))
                        if kt == nkt - 1:
                            r_ = R[qt % 2]
                            t1_ = T1[qt % 2]
                            p.recip(r_.v(0, 512, o0, o0 + 64), po.v(0, 512, d0, d0 + 64))
                            p.tt(t1_.v(0, 512, o0, o0 + 64), r_.v(0, 512, o0, o0 + 64), SG.v(tq0, 512, o0, o0 + 64),
                                 ALU.mult, eng="pool")
                            p.tt(MIX.v(hp * T + tq0, 512, o0, o0 + 64), po.v(0, 512, o0, o0 + 64),
                                 t1_.v(0, 512, o0, o0 + 64), ALU.mult)

                    pt_base = pt_i
                    LA = 2
                    for idx in range(len(tiles) + LA):
                        if idx < len(tiles):
                            emit_qk(idx)
                        if idx >= LA:
                            emit_pv(idx - LA)
                    pt_i += len(tiles)
            dbg("att", MIX.v(0, 4 * T), [128, 4 * T])

        def phase_mem(b):
            NW = 1024
            load_w("mem", wsi3, win3, C_QM, NW, 8)
            load_w("kv", wskv3, wkv3, 0, NW, 8, wb_off=8192)
            wk = Bump(WK, 0, WKSZ)
            MEMT = wk.get(BF16, 8 * 256)
            KM = wk.get(BF16, 4 * 256)
            VM = wk.get(BF16, 2 * 512)
            QM = [wk.get(BF16, T) for _ in range(2)]
            SGM = [wk.get(BF16, T) for _ in range(2)]
            PT = [wk.get(BF16, 512) for _ in range(2)]
            R = wk.get(F32, 512)
            T1 = wk.get(F32, 512)
            rms_transpose(lambda i: mem_d[b, i * 128:(i + 1) * 128, :], 2, P_MNW,
                          lambda i: MEMT.v3(8, 256, i * 128, 128), wk)
            for h in range(4):
                ps = PS[2 + h % 2]
                for k in range(8):
                    p.matmul(ps.v(0, 256), WB.v(8192 + k * NW + h * 128, 128), MEMT.v(k * 256, 256),
                             start=(k == 0), stop=(k == 7))
                p.copy(KM.v(h * 256, 256), ps.v(0, 256), eng="act")
            for mt in range(2):
                ps = PS[2 + mt % 2]
                for k in range(8):
                    p.matmul(ps.v(), MEMT.v(k * 256 + mt * 128, 128), WB.v(8192 + k * NW + 512, 512),
                             start=(k == 0), stop=(k == 7))
                p.copy(VM.v(mt * 512, 512), ps.v(), eng="dve")
            pt_i = 0
            for h in range(4):
                qm = QM[h % 2]
                sgm = SGM[h % 2]
                for tq in range(4):
                    ps = PS[2 + tq % 2]
                    proj_fm(NW, h * 128, 128, tq * 512, 512, ps.v())
                    p.copy(qm.v(tq * 512, 512), ps.v(), eng="act")
                for tq in range(4):
                    ps = PS[2 + tq % 2]
                    proj_fm(NW, 512 + h * 128, 128, tq * 512, 512, ps.v())
                    p.act(sgm.v(tq * 512, 512), ps.v(), AF.Silu)
                for qt in range(4):
                    tq0 = qt * 512
                    po = PS[qt % 2]
                    den = PS[6 + qt % 2]
                    for mt in range(2):
                        sp = PS[4 + pt_i % 2]
                        ptile = PT[pt_i % 2]
                        pt_i += 1
                        p.matmul(sp.v(), KM.v(h * 256 + mt * 128, 128), qm.v(tq0, 512))
                        p.act(ptile.v(), sp.v(), AF.Exp, scale=128 ** -0.5)
                        p.matmul(po.v(), VM.v(mt * 512 + h * 128, 128), ptile.v(), start=(mt == 0), stop=(mt == 1))
                        p.matmul(den.v(), ones, ptile.v(), start=(mt == 0), stop=(mt == 1))
                    p.recip(R.v(), den.v())
                    p.tt(T1.v(), R.v(), sgm.v(tq0, 512), ALU.mult, eng="pool")
                    p.tt(MIX.v((12 + h) * T + tq0, 512), po.v(), T1.v(), ALU.mult)
            dbg("memo", MIX.v(12 * T, 4 * T), [128, 4 * T])

        def phase_out(b):
            NW = 1024
            load_w("out", wso3, wout3, 0, NW, 16)
            wk = Bump(WK, 0, WKSZ)
            XT = [wk.get(F32, 1024) for _ in range(2)]
            HT = [wk.get(F32, 1024) for _ in range(2)]
            OT = [wk.get(F32, 1024) for _ in range(2)]
            JNK = wk.get(BF16, 1024)
            SSQ = wk.get(F32, 16)
            RSTD = wk.get(F32, 16)
            for i in range(16):
                xt = XT[i % 2]
                ht = HT[i % 2]
                ot = OT[i % 2]
                p.dma(xt.v(), D(x_d[b, i * 128:(i + 1) * 128, :]))
                for nh in range(2):
                    ps = PS[(2 * i + nh) % 4]
                    for m in range(16):
                        p.matmul(ps.v(), MIX.v(m * T + i * 128, 128), WB.v(m * NW + nh * 512, 512),
                                 start=(m == 0), stop=(m == 15))
                    p.tt(ht.v(nh * 512, 512), ps.v(), xt.v(nh * 512, 512), ALU.add)
                p.act(JNK.v(), ht.v(), AF.Square, accum_out=SSQ.v(i, 1))
                p.act(RSTD.v(i, 1), SSQ.v(i, 1), AF.Ln, scale=1.0 / DM, bias=epsv)
                p.act(RSTD.v(i, 1), RSTD.v(i, 1), AF.Exp, scale=-0.5)
                p.stt(ot.v(), ht.v(), RSTD.v(i, 1), prm(P_FNW, 1024), ALU.mult, ALU.mult)
                p.dma(D(out_d[b, i * 128:(i + 1) * 128, :]), ot.v())

        for b in range(nseq):
            wkA = Bump(WK, 16384, WKSZ - 16384)
            rms_transpose(lambda i: x_d[b, i * 128:(i + 1) * 128, :], 16, P_NW,
                          lambda i: UT.v3(BF16, 8, T, i * 128, 128), wkA)
            if b == 0:
                dbg("ut", UT.v(), [128, 8 * T])
            if "S" in phases:
                phase_ssd(b)
            if "B" in phases:
                phase_attn(b)
            if "M" in phases:
                phase_mem(b)
            if "E" in phases:
                phase_out(b)
        print("ops:", p.nops, {e: len(v) for e, v in p.ops.items()})
        p.emit()
    return nc, dumps


def kernel(x, mem, norm_w, w_in, conv_w, conv_b, dt_bias, a_log, d_skip, ssd_norm_w, mem_norm_w, w_mem_kv,
           w_out, final_norm_w):
    f = lambda a: np.ascontiguousarray(np.asarray(a, dtype=np.float32))
    x, mem = f(x), f(mem)
    cb, cf, kc, qc = host_consts()
    cf[:, CF_PRM:CF_PRM + NPRM] = host_params(f(norm_w)[0], f(conv_w)[0], f(conv_b)[0], f(dt_bias)[0], f(a_log)[0],
                                              f(d_skip)[0], f(ssd_norm_w)[0], f(mem_norm_w)[0], f(final_norm_w))
    nc, _ = build()
    shared = {"w_in": f(w_in)[0], "w_kv": f(w_mem_kv)[0], "w_out": f(w_out)[0], "cstb": cb, "cstf": cf,
              "kconst": kc, "qconst": qc}
    in_maps = []
    for c in range(8):
        m = dict(shared)
        m["x"] = x[c * NSEQ:(c + 1) * NSEQ]
        m["mem"] = mem[c * NSEQ:(c + 1) * NSEQ]
        in_maps.append(m)
    res = run_bass_kernel_spmd(nc, in_maps, core_ids=list(range(8)))
    return np.concatenate([r["out"] for r in res.results], axis=0)
```

```python
from contextlib import ExitStack
import concourse.bass as bass
import concourse.mybir as mybir

F32 = mybir.dt.float32
BF16 = mybir.dt.bfloat16
I32 = mybir.dt.int32
AF = mybir.ActivationFunctionType
ALU = mybir.AluOpType
AX = mybir.AxisListType
ESZ = {F32: 4, BF16: 2, I32: 4}


class View:
    __slots__ = ("ap", "arena", "rngs", "pr")

    def __init__(self, ap, arena, rngs, pr=(0, 128)):
        self.ap, self.arena, self.rngs, self.pr = ap, arena, rngs, pr

    def re(self, s, **kw):
        return View(self.ap.rearrange(s, **kw), self.arena, self.rngs, self.pr)

    def bc(self, shape):
        return View(self.ap.broadcast_to(shape), self.arena, self.rngs, self.pr)

    def __getitem__(self, key):
        return View(self.ap[key], self.arena, self.rngs, self.pr)


class Arena:
    def __init__(self, name, t, dtype, ncols, const=False):
        self.name, self.t, self.dtype, self.ncols = name, t, dtype, ncols
        self.esz = ESZ[dtype]
        self.w = []
        self.r = []
        self.const = const
        self.alt = {dtype: t}
        self.psum = False
        self.qlast = [dict() for _ in range(4)]

    def v(self, lo=0, n=None, p0=0, p1=128):
        if n is None:
            n = self.ncols - lo
        assert 0 <= lo and lo + n <= self.ncols, (self.name, lo, n, self.ncols)
        return View(self.t[p0:p1, lo:lo + n], self, [(lo * self.esz, (lo + n) * self.esz)], (p0, p1))

    def v3(self, dtype, nk, stride, lo, n, p0=0, p1=128):
        if dtype not in self.alt:
            self.alt[dtype] = self.t.bitcast(dtype)
        e = ESZ[dtype]
        assert ((nk - 1) * stride + lo + n) * e <= self.ncols * self.esz
        total = self.ncols * self.esz // e
        o = max(0, lo + nk * stride - total)
        assert o <= stride - n and o <= lo, (self.name, lo, nk, stride, n, total)
        ws = lo - o
        ap = self.alt[dtype][p0:p1, ws:ws + nk * stride].rearrange("p (k s) -> p k s", s=stride)[:, :, o:o + n]
        return View(ap, self, [((k * stride + lo) * e, (k * stride + lo + n) * e) for k in range(nk)], (p0, p1))

    def vb(self, dtype, lo, n, p0=0, p1=128):
        if dtype not in self.alt:
            self.alt[dtype] = self.t.bitcast(dtype)
        e = ESZ[dtype]
        assert (lo + n) * e <= self.ncols * self.esz
        return View(self.alt[dtype][p0:p1, lo:lo + n], self, [(lo * e, (lo + n) * e)], (p0, p1))


class Sub:
    def __init__(self, arena, dtype, boff, n):
        self.a, self.dt, self.e, self.n = arena, dtype, ESZ[dtype], n
        assert boff % self.e == 0
        self.base = boff // self.e

    def v(self, lo=0, n=None, p0=0, p1=128):
        if n is None:
            n = self.n - lo
        assert 0 <= lo and lo + n <= self.n, (lo, n, self.n)
        return self.a.vb(self.dt, self.base + lo, n, p0, p1)

    def v3(self, nk, stride, lo, n, p0=0, p1=128):
        assert (nk - 1) * stride + lo + n <= self.n
        return self.a.v3(self.dt, nk, stride, self.base + lo, n, p0, p1)


class Bump:
    def __init__(self, arena, boff, size):
        self.a, self.off, self.end = arena, boff, boff + size

    def get(self, dtype, n):
        self.off = (self.off + 3) // 4 * 4
        s = Sub(self.a, dtype, self.off, n)
        self.off += n * ESZ[dtype]
        assert self.off <= self.end, ("bump overflow", self.off, self.end)
        return s


def D(ap):
    return View(ap, None, [])


COMPUTE = ("pe", "act", "dve", "pool")


class Prog:
    def __init__(self, nc, ndma=40):
        self.nc = nc
        self.ops = {e: [] for e in COMPUTE + ("sp",)}
        self.count = {e: 0 for e in COMPUTE}
        self.waited = {e: {} for e in COMPUTE + ("sp",)}
        self.ndma = ndma
        self.dma_cnt = [0] * ndma
        self.dma_next = 0
        self.nops = 0

    def op(self, eng, fn, reads=(), writes=(), dma=False):
        deps = {}

        def add(tok):
            k, v = tok
            if deps.get(k, 0) < v:
                deps[k] = v

        for vw in reads:
            a = vw.arena
            if a is None:
                continue
            for (lo, hi) in vw.rngs:
                for (l, h, t) in a.w:
                    if l < hi and lo < h:
                        add(t)
        for vw in writes:
            a = vw.arena
            if a is None:
                continue
            assert not a.const, a.name
            for (lo, hi) in vw.rngs:
                for (l, h, t) in a.w:
                    if l < hi and lo < h:
                        add(t)
                for (l, h, t) in a.r:
                    if l < hi and lo < h:
                        add(t)
        for vw in list(reads) + list(writes):
            a = vw.arena
            if a is None or not a.psum:
                continue
            for q in range(vw.pr[0] // 32, (vw.pr[1] + 31) // 32):
                for e2, t in a.qlast[q].items():
                    if e2 != eng:
                        add(t)
        if dma:
            i = self.dma_next
            self.dma_next = (i + 1) % self.ndma
            if self.dma_cnt[i] > 0:
                add((("d", i), self.dma_cnt[i]))
            self.dma_cnt[i] += 16
            tok = (("d", i), self.dma_cnt[i])
            inc = (("d", i), 16)
        else:
            self.count[eng] += 1
            tok = (eng, self.count[eng])
            inc = (eng, 1)
        waits = []
        wd = self.waited[eng]
        for k, v in deps.items():
            if k == eng and eng == "pe":
                continue
            if wd.get(k, 0) >= v:
                continue
            wd[k] = v
            waits.append((k, v))
        self.ops[eng].append((waits, fn, inc))
        self.nops += 1
        for vw in list(reads) + list(writes):
            a = vw.arena
            if a is None or not a.psum:
                continue
            for q in range(vw.pr[0] // 32, (vw.pr[1] + 31) // 32):
                a.qlast[q][eng] = tok
        for vw in reads:
            a = vw.arena
            if a is None or a.const:
                continue
            for (lo, hi) in vw.rngs:
                if not dma:
                    a.r = [x for x in a.r if not (x[2][0] == eng and lo <= x[0] and x[1] <= hi)]
                a.r.append((lo, hi, tok))
        for vw in writes:
            a = vw.arena
            if a is None:
                continue
            for (lo, hi) in vw.rngs:
                a.w = [x for x in a.w if not (lo <= x[0] and x[1] <= hi)]
                a.r = [x for x in a.r if not (lo <= x[0] and x[1] <= hi)]
                a.w.append((lo, hi, tok))
        return tok

    def freeze(self, arena):
        arena.const = True
        arena.r = []

    def matmul(self, out, lhsT, rhs, start=True, stop=True, **kw):
        return self.op("pe", lambda e: e.matmul(out.ap, lhsT.ap, rhs.ap, start=start, stop=stop, **kw),
                       [lhsT, rhs] + ([] if start else [out]), [out])

    def transpose(self, out, in_, ident):
        return self.op("pe", lambda e: e.transpose(out.ap, in_.ap, ident.ap), [in_, ident], [out])

    def act(self, out, in_, func, bias=None, scale=None, accum_out=None):
        rd = [in_]
        kw = {}
        if bias is not None:
            if isinstance(bias, View):
                rd.append(bias)
                kw["bias"] = bias.ap
            else:
                kw["bias"] = bias
        if scale is not None:
            if isinstance(scale, View):
                rd.append(scale)
                kw["scale"] = scale.ap
            else:
                kw["scale"] = scale
        wr = [out]
        if accum_out is not None:
            wr.append(accum_out)
            kw["accum_out"] = accum_out.ap
        return self.op("act", lambda e: e.activation(out.ap, in_.ap, func, **kw), rd, wr)

    def tt(self, out, in0, in1, op, eng="dve"):
        return self.op(eng, lambda e: e.tensor_tensor(out.ap, in0.ap, in1.ap, op), [in0, in1], [out])

    def ts(self, out, in0, s1, s2, op0, op1=None, eng="dve", accum_out=None):
        rd = [in0]
        a1 = s1
        a2 = s2
        if isinstance(s1, View):
            rd.append(s1)
            a1 = s1.ap
        if isinstance(s2, View):
            rd.append(s2)
            a2 = s2.ap
        kw = {}
        if op1 is not None:
            kw["op1"] = op1
        wr = [out]
        if accum_out is not None:
            kw["accum_out"] = accum_out.ap
            wr.append(accum_out)
        return self.op(eng, lambda e: e.tensor_scalar(out.ap, in0.ap, a1, a2, op0, **kw), rd, wr)

    def stt(self, out, in0, scalar, in1, op0, op1):
        rd = [in0, in1]
        sc = scalar
        if isinstance(scalar, View):
            rd.append(scalar)
            sc = scalar.ap
        return self.op("dve", lambda e: e.scalar_tensor_tensor(out.ap, in0.ap, sc, in1.ap, op0, op1), rd, [out])

    def copy(self, out, in_, eng="dve"):
        if eng == "act":
            return self.op("act", lambda e: e.copy(out.ap, in_.ap), [in_], [out])
        return self.op(eng, lambda e: e.tensor_copy(out.ap, in_.ap), [in_], [out])

    def reduce(self, out, in_, op, axis=AX.X, eng="dve"):
        return self.op(eng, lambda e: e.tensor_reduce(out.ap, in_.ap, axis, op), [in_], [out])

    def recip(self, out, in_):
        return self.op("dve", lambda e: e.reciprocal(out.ap, in_.ap), [in_], [out])

    def memset(self, out, val, eng="dve"):
        return self.op(eng, lambda e: e.memset(out.ap, val), [], [out])

    def scan(self, out, d0, d1, initial, op0, op1):
        rd = [d0, d1]
        ini = initial
        if isinstance(initial, View):
            rd.append(initial)
            ini = initial.ap
        return self.op("dve", lambda e: e.tensor_tensor_scan(out.ap, d0.ap, d1.ap, ini, op0, op1), rd, [out])

    def affine_select(self, out, in_, pattern, cmp, fill, base, cm):
        return self.op("pool", lambda e: e.affine_select(out.ap, in_.ap, pattern, cmp, fill, base=base,
                                                         channel_multiplier=cm), [in_], [out])

    def iota(self, out, pattern, base, cm):
        return self.op("pool", lambda e: e.iota(out.ap, pattern, base=base, channel_multiplier=cm), [], [out])

    def dma(self, out, in_, q="sp", **kw):
        if q == "pool":
            kw.setdefault("max_dma_last_dim", 4096)
        return self.op(q, lambda e: e.dma_start(out=out.ap, in_=in_.ap, **kw), [in_], [out], dma=True)

    def emit(self):
        nc = self.nc
        with ExitStack() as es:
            sems = {}
            for e in COMPUTE:
                sems[e] = es.enter_context(nc.semaphore("s_" + e))
            for i in range(self.ndma):
                sems[("d", i)] = es.enter_context(nc.semaphore("s_d%d" % i))
            fin = []
            for i in range(self.ndma):
                if self.dma_cnt[i] > 0:
                    fin.append((("d", i), self.dma_cnt[i]))
            for e in COMPUTE:
                if self.count[e] > 0:
                    fin.append((e, self.count[e]))
            block = es.enter_context(nc.Block())

            def run(stream, final=None):
                def f(eng):
                    for waits, fn, inc in stream:
                        for k, v in waits:
                            eng.wait_ge(sems[k], v)
                        ins = fn(eng)
                        ins.then_inc(sems[inc[0]], inc[1])
                    if final:
                        for k, v in final:
                            eng.wait_ge(sems[k], v)
                return f

            block.tensor(run(self.ops["pe"]))
            block.scalar(run(self.ops["act"]))
            block.vector(run(self.ops["dve"]))
            block.gpsimd(run(self.ops["pool"]))
            block.sync(run(self.ops["sp"], fin))
from concourse.bass_utils import run_bass_kernel_spmd
import numpy as np

T = 2048
DM = 1024
NSEQ = 2
EPS = 1e-6
BIG = 30000.0
C_Q, C_K, C_V, C_G, C_Z, C_XBC, C_DT, C_QM, C_GM = 0, 512, 1024, 1536, 2048, 3072, 5120, 5136, 5648
CB_ID, CB_TRI, CB_MNEG, CB_ONES, CB_EH, NCB = 0, 128, 256, 384, 512, 2560
CF_ID, CF_ONES, CF_PRM, CF_CAP, CF_CMK, CF_OWN, CF_MISC, NCF = 0, 128, 256, 1424, 1488, 1552, 1616, 1624
P_NW, P_MNW, P_SNW, P_CVB, P_CVW, P_DSK, P_DTB, P_ALOG, P_FNW = 0, 8, 16, 24, 40, 104, 112, 128, 144
NPRM = 1168


def host_consts():
    cb = np.zeros((128, NCB), np.float32)
    cb[:, CB_ID:CB_ID + 128] = np.eye(128)
    s = np.arange(128)[:, None]
    t = np.arange(128)[None, :]
    cb[:, CB_TRI:CB_TRI + 128] = (t >= s)
    cb[:, CB_MNEG:CB_MNEG + 128] = np.where(t < s, -BIG, 0.0)
    cb[:, CB_ONES:CB_ONES + 128] = 1.0
    for h in range(16):
        cb[h, CB_EH + h * 128: CB_EH + (h + 1) * 128] = 1.0
    cf = np.zeros((128, NCF), np.float32)
    cf[:, CF_ID:CF_ID + 128] = np.eye(128)
    cf[:, CF_ONES:CF_ONES + 128] = 1.0
    cap = np.zeros((8, 8), np.float32)
    cmk = np.zeros((8, 8), np.float32)
    own = np.zeros((8, 8), np.float32)
    for ti in range(8):
        qb = 4 + ti // 2
        for j in range(8):
            cap[ti, j] = 1e30 if j < qb else -1e30
            cmk[ti, j] = 1.0 if j < qb else 0.0
            own[ti, j] = 1.0 if j == qb else 0.0
    cf[:, CF_CAP:CF_CAP + 64] = cap.reshape(-1)[None, :]
    cf[:, CF_CMK:CF_CMK + 64] = cmk.reshape(-1)[None, :]
    cf[:, CF_OWN:CF_OWN + 64] = own.reshape(-1)[None, :]
    cf[:, CF_MISC] = EPS
    cf[:, CF_MISC + 1] = 1.0
    kc = np.zeros((128, T), np.float32)
    pos = np.arange(T)
    for r in range(8):
        kc[64 + r] = (pos // 256 == r)
    kc[96] = pos // 16
    kc[97] = pos % 16
    kc[98] = 1.0
    kc[99] = 1.0
    qc = np.zeros((8, 4, T), np.float32)
    for h in range(8):
        sl = 2.0 ** (-8.0 * (h + 1) / 8)
        qc[h, 0] = 16 * sl * 8
        qc[h, 1] = sl * 8
        qc[h, 2] = -16 * sl * 8 * (pos // 16)
        qc[h, 3] = -sl * 8 * (pos % 16)
    return cb, cf, kc, qc


def host_params(norm_w, conv_w, conv_b, dt_bias, a_log, d_skip, ssd_norm_w, mem_norm_w, final_norm_w):
    prm = np.zeros((128, NPRM), np.float32)
    prm[:, P_NW:P_NW + 8] = norm_w.reshape(8, 128).T
    prm[:, P_MNW:P_MNW + 8] = mem_norm_w.reshape(8, 128).T
    prm[:, P_SNW:P_SNW + 8] = ssd_norm_w.reshape(8, 128).T
    prm[:, P_CVB:P_CVB + 16] = conv_b.reshape(16, 128).T
    prm[:, P_CVW:P_CVW + 64] = conv_w.T.reshape(16, 128, 4).transpose(1, 0, 2).reshape(128, 64)
    prm[:, P_DSK:P_DSK + 8] = np.repeat(d_skip, 64).reshape(8, 128).T
    prm[:, P_DTB:P_DTB + 16] = dt_bias[None, :]
    prm[:, P_ALOG:P_ALOG + 16] = a_log[None, :]
    prm[:, P_FNW:P_FNW + 1024] = final_norm_w[None, :]
    return prm
def build(nseq=NSEQ, dump=None, phases="ASBME"):
    nc = bass.Bass("TRN2", target_bir_lowering=False)
    x_d = nc.dram_tensor("x", [nseq, T, DM], F32, kind="ExternalInput").ap()
    mem_d = nc.dram_tensor("mem", [nseq, 256, DM], F32, kind="ExternalInput").ap()
    win_d = nc.dram_tensor("w_in", [DM, 6160], F32, kind="ExternalInput").ap()
    wkv_d = nc.dram_tensor("w_kv", [DM, 1024], F32, kind="ExternalInput").ap()
    wout_d = nc.dram_tensor("w_out", [2048, DM], F32, kind="ExternalInput").ap()
    cb_d = nc.dram_tensor("cstb", [128, NCB], F32, kind="ExternalInput").ap()
    cf_d = nc.dram_tensor("cstf", [128, NCF], F32, kind="ExternalInput").ap()
    kc_d = nc.dram_tensor("kconst", [128, T], F32, kind="ExternalInput").ap()
    qc_d = nc.dram_tensor("qconst", [8, 4, T], F32, kind="ExternalInput").ap()
    out_d = nc.dram_tensor("out", [nseq, T, DM], F32, kind="ExternalOutput").ap()
    dumps = {}
    wsi_d = nc.dram_tensor("wsc_in", [DM, 6160], BF16, kind="Internal").ap()
    wskv_d = nc.dram_tensor("wsc_kv", [DM, 1024], BF16, kind="Internal").ap()
    wso_d = nc.dram_tensor("wsc_out", [2048, DM], BF16, kind="Internal").ap()
    wsi3 = wsi_d.rearrange("(k p) c -> p k c", p=128)
    wskv3 = wskv_d.rearrange("(k p) c -> p k c", p=128)
    wso3 = wso_d.rearrange("(k p) c -> p k c", p=128)
    win3 = win_d.rearrange("(k p) c -> p k c", p=128)
    wkv3 = wkv_d.rearrange("(k p) c -> p k c", p=128)
    wout3 = wout_d.rearrange("(k p) c -> p k c", p=128)

    with ExitStack() as es:
        def sb(name, n, dt):
            t = es.enter_context(nc.sbuf_tensor(name, [128, n], dt))
            return Arena(name, t, dt, n)

        UT = sb("UT", 8 * T, BF16)
        MIX = sb("MIX", 16 * T, BF16)
        WB = sb("WB", 8 * 3088, BF16)
        WKSZ = 47616
        WK = sb("WK", WKSZ // 2, BF16)
        CB = sb("CB", NCB, BF16)
        CF = sb("CF", NCF, F32)
        SM = sb("SM", 64, F32)
        PS = []
        for i in range(8):
            t = es.enter_context(nc.psum_tensor("ps%d" % i, [128, 512], F32))
            PS.append(Arena("ps%d" % i, t, F32, 512))
            PS[-1].psum = True
        p = Prog(nc)

        def dbg(name, view, shape):
            if dump is None or name not in dump:
                return
            d = nc.dram_tensor("dbg_" + name, list(shape), view.ap.dtype, kind="ExternalOutput").ap()
            dumps[name] = d
            p.dma(D(d), view)

        p.dma(CB.v(), D(cb_d), q="pool")
        p.dma(CF.v(), D(cf_d))
        ident = CB.v(CB_ID, 128)
        tri = CB.v(CB_TRI, 128)
        mneg = CB.v(CB_MNEG, 128)
        ones = CB.v(CB_ONES, 128)
        identf = CF.v(CF_ID, 128)
        epsv = CF.v(CF_MISC, 1)
        onev = CF.v(CF_MISC + 1, 1)

        def prm(off, n, p0=0, p1=128):
            return CF.v(CF_PRM + off, n, p0, p1)

        AB = SM.v(0, 16)
        p.act(AB, prm(P_ALOG, 16), AF.Exp)
        p.ts(AB, AB, -1.0, None, ALU.mult)

        SC = {n: Arena("sc_" + n, None, BF16, 1) for n in ("attn", "mem", "ssd", "kv", "out")}
        converted = [False]

        def scv(ap, name, k):
            return View(ap, SC[name], [(k, k + 1)])

        def convert_all():
            for name, src3, dst3, c0, nw, nk in (("attn", win3, wsi3, 0, 2048, 8), ("kv", wkv3, wskv3, 0, 1024, 8),
                                                 ("mem", win3, wsi3, C_QM, 1024, 8), ("out", wout3, wso3, 0, 1024, 16),
                                                 ("ssd", win3, wsi3, C_Z, 3088, 8)):
                for k in range(nk):
                    p.dma(scv(dst3[:, k, c0:c0 + nw], name, k), D(src3[:, k, c0:c0 + nw]), q="pool")
            converted[0] = True

        def load_w(name, dst3, src3, c0, nw, nk, wb_off=0):
            for k in range(nk):
                if converted[0]:
                    p.dma(WB.v(wb_off + k * nw, nw), scv(dst3[:, k, c0:c0 + nw], name, k), q="sp")
                else:
                    p.dma(WB.v(wb_off + k * nw, nw), D(src3[:, k, c0:c0 + nw]), q="pool")

        pj_i = [0]

        def rms_transpose(src_tile, ntiles, nwoff, dst_fn, wk):
            XT = [wk.get(F32, 1024) for _ in range(2)]
            XS = [wk.get(BF16, 1024) for _ in range(2)]
            JNK = wk.get(BF16, 1024)
            SSQ = wk.get(F32, 16)
            RSTD = wk.get(F32, 16)
            for i in range(ntiles):
                xt = XT[i % 2]
                p.dma(xt.v(), D(src_tile(i)))
                p.act(JNK.v(), xt.v(), AF.Square, accum_out=SSQ.v(i, 1))
                p.act(RSTD.v(i, 1), SSQ.v(i, 1), AF.Ln, scale=1.0 / DM, bias=epsv)
                p.act(RSTD.v(i, 1), RSTD.v(i, 1), AF.Exp, scale=-0.5)
                xs = XS[i % 2]
                p.ts(xs.v(), xt.v(), RSTD.v(i, 1), None, ALU.mult)
                pb = PS[i % 2]
                for k in range(8):
                    p.transpose(pb.vb(BF16, k * 128, 128), xs.v(k * 128, 128), ident)
                p.tt(dst_fn(i), pb.vb(BF16, 0, 1024).re("p (k t) -> p k t", k=8),
                     prm(nwoff, 8).re("p (k o) -> p k o", o=1).bc([128, 8, 128]), ALU.mult)

        def utv(k, t0, n):
            return UT.v(k * T + t0, n)

        def proj_fm(nw, c0, ncols, t0, n, ps_view):
            for k in range(8):
                p.matmul(ps_view, WB.v(k * nw + c0, ncols), utv(k, t0, n), start=(k == 0), stop=(k == 7))

        def phase_ssd(b):
            NW = 3088
            load_w("ssd", wsi3, win3, C_Z, NW, 8)
            if not converted[0]:
                convert_all()
            b1 = Bump(MIX, 0, 16384)
            b2 = Bump(MIX, 12 * T * 2, 16384)
            wk = Bump(WK, 0, WKSZ)
            XPRE = b1.get(BF16, 16 * 259)
            XST = b1.get(BF16, 8 * 256)
            BMT = b1.get(BF16, 4 * 256)
            LT = [b1.get(BF16, 384) for _ in range(2)]
            SZ = b2.get(BF16, 8 * 256)
            PREV = b2.get(F32, 1024)
            XDD = b2.get(BF16, 2 * 1024)
            CMT = b2.get(BF16, 4 * 256)
            BTOK = b2.get(BF16, 2 * 512)
            DIAG = wk.get(BF16, 64 * 128)
            CBT = wk.get(BF16, 4 * 384)
            L0 = [wk.get(BF16, 256) for _ in range(2)]
            MT = [wk.get(BF16, 384) for _ in range(2)]
            GT = [wk.get(BF16, 256) for _ in range(2)]
            GG = [wk.get(F32, 256) for _ in range(4)]
            GSQ = [wk.get(BF16, 256) for _ in range(4)]
            RS = wk.get(F32, 256)
            XDT = wk.get(BF16, 2 * 2048)
            PREVB = wk.get(BF16, 2048)
            DTR = wk.get(F32, 32)
            DT = wk.get(F32, 32)
            DA = wk.get(F32, 256)
            NCS = wk.get(F32, 32)
            DEC = wk.get(F32, 32)
            W2 = wk.get(F32, 32)
            CST = wk.get(F32, 256)
            CSH = wk.get(BF16, 256)
            CSL = wk.get(BF16, 256)
            DECT = wk.get(F32, 256)
            DG16 = wk.get(F32, 16)
            CDB = wk.get(F32, 16)

            for zt in (XDT, PREVB, DA, CST, CSH, CSL, DECT, DG16):
                p.memset(zt.v(), 0.0, eng="pool")
            for j in range(64):
                p.ts(DIAG.v(j * 128, 128), ident, prm(P_CVW + j, 1), None, ALU.mult,
                     eng=("pool" if j % 2 else "dve"))
            p.memset(XPRE.v3(16, 259, 0, 3), 0.0)
            p.memset(PREV.v(), 0.0)

            for c in range(8):
                t0 = c * 256

                def zgroup(zc):
                    ps = PS[zc % 2].v((zc // 2 % 2) * 256, 256)
                    proj_fm(NW, zc * 128, 128, t0, 256, ps)
                    p.copy(SZ.v(zc * 256, 256), ps, eng="dve")

                def xgroup(cc):
                    ps = PS[cc % 2].v((cc // 2 % 2) * 256, 256)
                    proj_fm(NW, 1024 + cc * 128, 128, t0, 256, ps)
                    p.copy(XPRE.v(cc * 259 + 3, 256), ps, eng=("dve" if cc % 2 else "act"))

                if c > 0:
                    p.copy(XPRE.v3(16, 259, 0, 3), XPRE.v3(16, 259, 256, 3), eng="pool")
                for lt in range(2):
                    for k in range(8):
                        p.matmul(PS[5].v(384 + lt * 16, 16), utv(k, t0 + lt * 128, 128), WB.v(k * NW + 3072, 16),
                                 start=(k == 0), stop=(k == 7))
                p.tt(DTR.v(), PS[5].v(384, 32).re("p (l h) -> p l h", l=2),
                     prm(P_DTB, 16).re("p (o h) -> p o h", o=1).bc([128, 2, 16]), ALU.add)
                p.act(DTR.v(), DTR.v(), AF.Exp)
                p.act(DT.v(), DTR.v(), AF.Ln, bias=onev)
                p.tt(DA.v3(2, 128, 0, 16), DT.v().re("p (l h) -> p l h", l=2),
                     SM.v(0, 16).re("p (o h) -> p o h", o=1).bc([128, 2, 16]), ALU.mult)
                for zc in range(6):
                    zgroup(zc)
                for lt in range(2):
                    p.transpose(PS[6].v(lt * 128, 128), DA.v(lt * 128, 128), identf)
                p.scan(CST.v(0, 256, 0, 16), CF.v(CF_ONES, 1, 0, 16).bc([16, 256]), PS[6].v(0, 256, 0, 16), 0.0,
                       ALU.mult, ALU.add)
                p.copy(CSH.v(0, 256, 0, 16), CST.v(0, 256, 0, 16))
                p.tt(CSL.v(0, 256, 0, 16), CST.v(0, 256, 0, 16), CSH.v(0, 256, 0, 16), ALU.subtract)
                p.act(DECT.v(0, 256, 0, 16), CST.v(0, 256, 0, 16), AF.Exp, scale=-1.0, bias=CST.v(255, 1, 0, 16))
                for zc in range(6, 8):
                    zgroup(zc)
                for cc in range(8, 12):
                    xgroup(cc)
                for lt in range(2):
                    p.transpose(PS[7].v(lt * 128, 128), CST.v(lt * 128, 128), identf)
                    p.transpose(PS[7].v(256 + lt * 128, 128), DECT.v(lt * 128, 128), identf)
                p.ts(NCS.v().re("p (l h) -> p l h", l=2), PS[7].v3(F32, 2, 128, 0, 16), -1.0, None, ALU.mult)
                p.copy(DEC.v().re("p (l h) -> p l h", l=2), PS[7].v3(F32, 2, 128, 256, 16))
                p.tt(W2.v(), DT.v(), DEC.v(), ALU.mult)
                if c < 7:
                    p.ts(DG16.v(0, 16, 0, 16), CF.v(CF_ID, 16, 0, 16), CST.v(255, 1, 0, 16), None, ALU.mult)
                    p.matmul(PS[5].v(480, 16), CF.v(CF_ONES, 128), DG16.v())
                    p.act(CDB.v(), PS[5].v(480, 16), AF.Exp)
                for cc in list(range(12, 16)) + list(range(0, 8)):
                    xgroup(cc)

                def conv(cc):
                    ps = PS[cc % 2].v((cc // 2 % 2) * 256, 256)
                    for k in range(4):
                        p.matmul(ps, DIAG.v((cc * 4 + k) * 128, 128), XPRE.v(cc * 259 + k, 256),
                                 start=(k == 0), stop=(k == 3))
                    if cc < 8:
                        dst = XST.v(cc * 256, 256)
                    elif cc < 12:
                        dst = BMT.v((cc - 8) * 256, 256)
                    else:
                        dst = CMT.v((cc - 12) * 256, 256)
                    p.act(dst, ps, AF.Silu, bias=prm(P_CVB + cc, 1))

                for cc in range(8, 16):
                    conv(cc)
                for lt in range(2):
                    pb = PS[7]
                    for g in range(4):
                        p.transpose(pb.vb(BF16, g * 128, 128), BMT.v(g * 256 + lt * 128, 128), ident)
                    p.copy(BTOK.v(lt * 512, 512), pb.vb(BF16, 0, 512), eng="dve")
                for g in range(4):
                    ps = PS[5]
                    p.matmul(ps.v(0, 256), BMT.v(g * 256, 128), CMT.v(g * 256, 256))
                    p.matmul(ps.v(256, 128), BMT.v(g * 256 + 128, 128), CMT.v(g * 256 + 128, 128))
                    p.copy(CBT.v(g * 384, 384), ps.v(0, 384), eng="dve")
                for cc in range(0, 8):
                    conv(cc)
                p.act(SZ.v(), SZ.v(), AF.Silu)
                for lt in range(2):
                    pb = PS[6]
                    for cc in range(8):
                        p.transpose(pb.vb(BF16, cc * 128, 128), XST.v(cc * 256 + lt * 128, 128), ident)
                    src = pb.vb(BF16, 0, 1024).re("p (h q) -> p h q", h=16)
                    for hh in range(2):
                        p.tt(XDT.v(lt * 2048, 2048).re("p (c e q) -> p c e q", c=8, e=2)[:, :, hh, hh * 64:hh * 64 + 64],
                             src.re("p (c e) q -> p c e q", e=2)[:, :, hh, :],
                             DT.v(lt * 16, 16).re("p (c e o) -> p c e o", e=2, o=1)[:, :, hh, :].bc([128, 8, 64]),
                             ALU.mult)
                    p.tt(XDD.v(lt * 1024, 1024).re("p (h q) -> p h q", h=16), src,
                         W2.v(lt * 16, 16).re("p (h o) -> p h o", o=1).bc([128, 16, 64]), ALU.mult)
                def seg_stage(h):
                    g = h // 4
                    sg = PS[2 + h % 2]
                    eh = CB.v(CB_EH + h * 128, 128)
                    csh = lambda lo, n: CSH.v(lo, n)
                    csl = lambda lo, n: CSL.v(lo, n)
                    p.matmul(sg.v(0, 256), eh, csh(0, 256), start=True, stop=False)
                    p.matmul(sg.v(0, 256), eh, csl(0, 256), start=False, stop=True)
                    p.matmul(sg.v(256, 128), eh, csh(0, 128), start=True, stop=False)
                    p.matmul(sg.v(256, 128), eh, csl(0, 128), start=False, stop=False)
                    p.matmul(sg.v(256, 128), ident, mneg, start=False, stop=True)
                    p.matmul(sg.v(384, 128), eh, csh(128, 128), start=True, stop=False)
                    p.matmul(sg.v(384, 128), eh, csl(128, 128), start=False, stop=False)
                    p.matmul(sg.v(384, 128), ident, mneg, start=False, stop=True)
                    l0 = L0[h % 2]
                    lt_ = LT[h % 2]
                    mt = MT[h % 2]
                    gt = GT[h % 2]
                    if c > 0:
                        p.act(l0.v(), sg.v(0, 256), AF.Exp)
                    p.act(lt_.v(0, 128), sg.v(256, 128), AF.Exp, bias=NCS.v(h, 1))
                    p.act(lt_.v(128, 128), sg.v(128, 128), AF.Exp, bias=NCS.v(h, 1))
                    p.act(lt_.v(256, 128), sg.v(384, 128), AF.Exp, bias=NCS.v(16 + h, 1))
                    p.tt(mt.v(), lt_.v(), CBT.v(g * 384, 384), ALU.mult)
                    if c > 0:
                        p.tt(gt.v(), l0.v(), CMT.v(g * 256, 256), ALU.mult, eng="pool")

                def y_stage(h):
                    g = h // 4
                    pr = h % 2
                    pc = h // 2
                    mt = MT[h % 2]
                    gt = GT[h % 2]
                    ybank = PS[4 + 2 * (pc % 2)]
                    yt = ybank.v(0, 256)
                    yt2 = ybank.v(128, 128)
                    p.matmul(yt, XDT.v(h * 128, 128), mt.v(0, 256), start=(pr == 0), stop=False)
                    p.matmul(yt2, XDT.v(2048 + h * 128, 128), mt.v(256, 128), start=False, stop=(c == 0 and pr == 1))
                    if c > 0:
                        p.matmul(yt, PREVB.v(h * 128, 128), gt.v(), start=False, stop=(pr == 1))
                    if pr == 1:
                        gg = GG[pc % 4]
                        p.stt(gg.v(), XST.v(pc * 256, 256), prm(P_DSK + pc, 1), yt, ALU.mult, ALU.add)
                        p.tt(gg.v(), gg.v(), SZ.v(pc * 256, 256), ALU.mult)
                        p.act(GSQ[pc % 4].v(), gg.v(), AF.Square)
                def norm_stage(g):
                        pc = 2 * g + 1
                        pcs = [pc - 1, pc]
                        ss = PS[7].v(256, 256)
                        p.matmul(ss, ones, GSQ[pcs[0] % 4].v(), start=True, stop=False)
                        p.matmul(ss, ones, GSQ[pcs[1] % 4].v(), start=False, stop=True)
                        p.act(RS.v(), ss, AF.Ln, scale=1.0 / 256, bias=epsv)
                        p.act(RS.v(), RS.v(), AF.Exp, scale=-0.5)
                        for q in pcs:
                            p.stt(MIX.v((4 + q) * T + t0, 256), GG[q % 4].v(), prm(P_SNW + q, 1), RS.v(),
                                  ALU.mult, ALU.mult)

                for h in range(18):
                    if h < 16:
                        seg_stage(h)
                    if 1 <= h <= 16:
                        y_stage(h - 1)
                    if h >= 2 and (h - 2) % 4 == 3:
                        norm_stage((h - 2) // 4)
                if c < 7:
                    for g in range(4):
                        st = PS[7].v(0, 256)
                        for lt in range(2):
                            p.matmul(st, BTOK.v(lt * 512 + g * 128, 128), XDD.v(lt * 1024 + g * 256, 256),
                                     start=(lt == 0), stop=(lt == 1))
                        pv = PREV.v(g * 256, 256)
                        p.tt(pv.re("p (h q) -> p h q", h=4), pv.re("p (h q) -> p h q", h=4),
                             CDB.v(g * 4, 4).re("p (h o) -> p h o", o=1).bc([128, 4, 64]), ALU.mult)
                        p.tt(pv, pv, st, ALU.add)
                        for hh in range(2):
                            p.copy(PREVB.v(g * 512, 512).re("p (c e q) -> p c e q", c=2, e=2)[:, :, hh, hh * 64:hh * 64 + 64],
                                   pv.re("p (c e q) -> p c e q", c=2, e=2)[:, :, hh, :], eng="pool")
            dbg("ssd", MIX.v(4 * T, 8 * T), [128, 8 * T])

        def attn_core(kt_list_fn, nq, kv_lhsT, q_rhs, v_lhsT, scale, PTs, epilogue, diag_fn=None, den_lhsT=None):
            pass

        def phase_attn(b):
            NW = 2048
            load_w("attn", wsi3, win3, 0, NW, 8)
            wk = Bump(WK, 0, WKSZ)
            bm = Bump(MIX, 12 * T * 2, 16384)
            bw = Bump(WB, 8 * NW * 2, WB.ncols * 2 - 8 * NW * 2)
            sets = [dict(QK=[wk.get(BF16, T) for _ in range(4)], VA=wk.get(BF16, 2048), VB=wk.get(BF16, 2048),
                         SG=wk.get(BF16, T)),
                    dict(QK=[bm.get(BF16, T) for _ in range(4)], VA=bw.get(BF16, 2048), VB=bw.get(BF16, 2048),
                         SG=bw.get(BF16, T))]
            PT = [wk.get(BF16, 512) for _ in range(3)]
            R = [wk.get(F32, 512) for _ in range(2)]
            T1 = [wk.get(F32, 512) for _ in range(2)]
            PENB = wk.get(BF16, 8 * 128)
            GM = wk.get(F32, 64)
            CMP = wk.get(F32, 512)
            RANK = wk.get(F32, 64)
            KSUM = wk.get(F32, 8)
            KMT = wk.get(BF16, 8)
            p.memset(KMT.v(), 0.0, eng="pool")
            p.memset(PENB.v(), 0.0, eng="pool")
            for S in sets:
                for i in range(2):
                    p.memset(S["QK"][i].v(), 0.0, eng="pool")
                    p.dma(S["QK"][2 + i].v(), D(kc_d), q="pool")
                p.memset(S["VA"].v(), 1.0, eng="pool")
                p.memset(S["VB"].v(), 1.0, eng="pool")

            def make_thunks(hp, S):
                QK, VA, VB, SG = S["QK"], S["VA"], S["VB"], S["SG"]
                th = []

                def t_qc():
                    for hh in range(2):
                        p.dma(QK[hh].v(0, T, 96, 100), D(qc_d[2 * hp + hh]), q="pool")
                th.append(t_qc)
                for which, c0 in ((0, C_Q), (1, C_K)):
                    for tq in range(4):
                        def f(which=which, c0=c0, tq=tq):
                            ps = PS[2 + tq % 2]
                            proj_fm(NW, c0 + hp * 128, 128, tq * 512, 512, ps.v())
                            p.copy(QK[2 * which].v(tq * 512, 512, 0, 64), ps.v(0, 512, 0, 64), eng="act")
                            p.copy(QK[2 * which + 1].v(tq * 512, 512, 0, 64), ps.v(0, 512, 64, 128), eng="dve")
                        th.append(f)

                def gating(hh):
                    Q = QK[hh]
                    K = QK[2 + hh]
                    p.reduce(KSUM.v(0, 8, 0, 64), K.v(0, T, 0, 64).re("p (j s) -> p j s", j=8), ALU.add)
                    p.ts(KMT.v(0, 8, 0, 64), KSUM.v(0, 8, 0, 64), 1.0 / 256, None, ALU.mult)
                    gps = PS[7]
                    for ti in range(8):
                        p.matmul(gps.v(ti * 8, 8), Q.v(1024 + ti * 128, 128), KMT.v())
                    p.tt(GM.v(), gps.v(0, 64), CF.v(CF_CAP, 64), ALU.min)
                    g3 = GM.v().re("p (t j) -> p t j", t=8)
                    p.tt(CMP.v().re("p (t j k) -> p t j k", t=8, j=8),
                         g3.re("p t (o k) -> p t o k", o=1).bc([128, 8, 8, 8]),
                         g3.re("p t (j o) -> p t j o", o=1).bc([128, 8, 8, 8]), ALU.is_gt)
                    p.reduce(RANK.v(), CMP.v().re("p (a k) -> p a k", k=8), ALU.add)
                    p.ts(RANK.v(), RANK.v(), 3.0, None, ALU.is_lt)
                    p.tt(RANK.v(), RANK.v(), CF.v(CF_CMK, 64), ALU.mult)
                    p.tt(RANK.v(), RANK.v(), CF.v(CF_OWN, 64), ALU.add)
                    p.ts(PENB.v3(8, 128, 64, 8), RANK.v().re("p (t j) -> p t j", t=8), -1.0, BIG, ALU.add, ALU.mult)
                    pps = PS[7]
                    for ti in range(8):
                        p.transpose(pps.vb(BF16, ti * 128, 128), PENB.v(ti * 128, 128), ident)
                    p.copy(Q.v(1024, 1024, 64, 72), pps.vb(BF16, 0, 1024, 64, 72), eng="dve")

                for tq in range(4):
                    def f(tq=tq):
                        ps = PS[2 + tq % 2]
                        proj_fm(NW, C_G + hp * 128, 128, tq * 512, 512, ps.v())
                        p.copy(SG.v(tq * 512, 512), ps.v(), eng="dve")
                    th.append(f)
                th.append(lambda: gating(0))
                for tg in range(4):
                    def f(tg=tg):
                        ps = PS[2 + tg % 2]
                        for j in range(4):
                            ti = tg * 4 + j
                            for k in range(8):
                                p.matmul(ps.v(j * 128, 128), utv(k, ti * 128, 128),
                                         WB.v(k * NW + C_V + hp * 128, 128), start=(k == 0), stop=(k == 7))
                        src = ps.v().re("p (j c) -> p j c", j=4)
                        p.copy(VA.v3(4, 128, tg * 512, 64), src[:, :, 0:64], eng="dve")
                        p.copy(VB.v3(4, 128, tg * 512 + 64, 64), src[:, :, 64:128], eng="dve")
                    th.append(f)
                th.append(lambda: gating(1))
                th.append(lambda: p.act(SG.v(), SG.v(), AF.Silu))
                return th

            def attention(hp, hh, S, bg, pt_base):
                Q = S["QK"][hh]
                K = S["QK"][2 + hh]
                V = S["VA"] if hh == 0 else S["VB"]
                SG = S["SG"]
                tiles = [(qt, kt) for qt in range(4) for kt in range(4 * qt + 4)]
                o0, d0 = (0, 64) if hh == 0 else (64, 0)

                def geom(idx):
                    qt, kt = tiles[idx]
                    tq0 = qt * 512
                    s0 = kt * 128
                    qlo = max(tq0, s0)
                    return qt, kt, tq0, s0, qlo, tq0 + 512 - qlo

                def emit_qk(idx):
                    qt, kt, tq0, s0, qlo, n = geom(idx)
                    gi = pt_base + idx
                    sp = PS[4 + gi % 3]
                    ptile = PT[gi % 3]
                    p.matmul(sp.v(0, n), K.v(s0, 128), Q.v(qlo, n))
                    p.act(ptile.v(0, n), sp.v(0, n), AF.Exp, scale=0.125)
                    if s0 >= tq0:
                        p.tt(ptile.v(0, 128), ptile.v(0, 128), tri, ALU.mult, eng="pool")

                def emit_pv(idx):
                    qt, kt, tq0, s0, qlo, n = geom(idx)
                    gi = pt_base + idx
                    nkt = 4 * qt + 4
                    po = PS[qt % 2]
                    ptile = PT[gi % 3]
                    p.matmul(po.v(qlo - tq0, n), V.v(kt * 128, 128), ptile.v(0, n),
                             start=(kt == 0), stop=(kt == nkt - 1))
                    if kt == nkt - 1:
                        r_ = R[qt % 2]
                        t1_ = T1[qt % 2]
                        p.recip(r_.v(0, 512, o0, o0 + 64), po.v(0, 512, d0, d0 + 64))
                        p.tt(t1_.v(0, 512, o0, o0 + 64), r_.v(0, 512, o0, o0 + 64), SG.v(tq0, 512, o0, o0 + 64),
                             ALU.mult, eng="pool")
                        p.tt(MIX.v(hp * T + tq0, 512, o0, o0 + 64), po.v(0, 512, o0, o0 + 64),
                             t1_.v(0, 512, o0, o0 + 64), ALU.mult)

                LA = 2
                for idx in range(len(tiles) + LA):
                    if idx < len(tiles):
                        emit_qk(idx)
                    if idx >= LA:
                        emit_pv(idx - LA)
                    if bg and idx % 4 == 3:
                        bg.pop(0)()
                return pt_base + len(tiles)

            pt_i = 0
            for f in make_thunks(0, sets[0]):
                f()
            for hp in range(4):
                bg = make_thunks(hp + 1, sets[(hp + 1) % 2]) if hp < 3 else []
                for hh in range(2):
                    pt_i = attention(hp, hh, sets[hp % 2], bg, pt_i)
                while bg:
                    bg.pop(0)()
            dbg("att", MIX.v(0, 4 * T), [128, 4 * T])

        def phase_mem(b):
            NW = 1024
            load_w("mem", wsi3, win3, C_QM, NW, 8)
            load_w("kv", wskv3, wkv3, 0, NW, 8, wb_off=8192)
            wk = Bump(WK, 0, WKSZ)
            MEMT = wk.get(BF16, 8 * 256)
            KM = wk.get(BF16, 4 * 256)
            VM = wk.get(BF16, 2 * 512)
            QM = [wk.get(BF16, T) for _ in range(2)]
            SGM = [wk.get(BF16, T) for _ in range(2)]
            PT = [wk.get(BF16, 512) for _ in range(2)]
            R = wk.get(F32, 512)
            T1 = wk.get(F32, 512)
            rms_transpose(lambda i: mem_d[b, i * 128:(i + 1) * 128, :], 2, P_MNW,
                          lambda i: MEMT.v3(8, 256, i * 128, 128), wk)
            for h in range(4):
                ps = PS[2 + h % 2]
                for k in range(8):
                    p.matmul(ps.v(0, 256), WB.v(8192 + k * NW + h * 128, 128), MEMT.v(k * 256, 256),
                             start=(k == 0), stop=(k == 7))
                p.copy(KM.v(h * 256, 256), ps.v(0, 256), eng="act")
            for mt in range(2):
                ps = PS[2 + mt % 2]
                for k in range(8):
                    p.matmul(ps.v(), MEMT.v(k * 256 + mt * 128, 128), WB.v(8192 + k * NW + 512, 512),
                             start=(k == 0), stop=(k == 7))
                p.copy(VM.v(mt * 512, 512), ps.v(), eng="dve")
            pt_i = 0
            for h in range(4):
                qm = QM[h % 2]
                sgm = SGM[h % 2]
                for tq in range(4):
                    ps = PS[2 + tq % 2]
                    proj_fm(NW, h * 128, 128, tq * 512, 512, ps.v())
                    p.copy(qm.v(tq * 512, 512), ps.v(), eng="act")
                for tq in range(4):
                    ps = PS[2 + tq % 2]
                    proj_fm(NW, 512 + h * 128, 128, tq * 512, 512, ps.v())
                    p.act(sgm.v(tq * 512, 512), ps.v(), AF.Silu)
                for qt in range(4):
                    tq0 = qt * 512
                    po = PS[qt % 2]
                    den = PS[6 + qt % 2]
                    for mt in range(2):
                        sp = PS[4 + pt_i % 2]
                        ptile = PT[pt_i % 2]
                        pt_i += 1
                        p.matmul(sp.v(), KM.v(h * 256 + mt * 128, 128), qm.v(tq0, 512))
                        p.act(ptile.v(), sp.v(), AF.Exp, scale=128 ** -0.5)
                        p.matmul(po.v(), VM.v(mt * 512 + h * 128, 128), ptile.v(), start=(mt == 0), stop=(mt == 1))
                        p.matmul(den.v(), ones, ptile.v(), start=(mt == 0), stop=(mt == 1))
                    p.recip(R.v(), den.v())
                    p.tt(T1.v(), R.v(), sgm.v(tq0, 512), ALU.mult, eng="pool")
                    p.tt(MIX.v((12 + h) * T + tq0, 512), po.v(), T1.v(), ALU.mult)
            dbg("memo", MIX.v(12 * T, 4 * T), [128, 4 * T])

        def phase_out(b):
            NW = 1024
            load_w("out", wso3, wout3, 0, NW, 16)
            wk = Bump(WK, 0, WKSZ)
            XT = [wk.get(F32, 1024) for _ in range(2)]
            HT = [wk.get(F32, 1024) for _ in range(2)]
            OT = [wk.get(F32, 1024) for _ in range(2)]
            JNK = wk.get(BF16, 1024)
            SSQ = wk.get(F32, 16)
            RSTD = wk.get(F32, 16)
            for i in range(16):
                xt = XT[i % 2]
                ht = HT[i % 2]
                ot = OT[i % 2]
                p.dma(xt.v(), D(x_d[b, i * 128:(i + 1) * 128, :]))
                for nh in range(2):
                    ps = PS[(2 * i + nh) % 4]
                    for m in range(16):
                        p.matmul(ps.v(), MIX.v(m * T + i * 128, 128), WB.v(m * NW + nh * 512, 512),
                                 start=(m == 0), stop=(m == 15))
                    p.tt(ht.v(nh * 512, 512), ps.v(), xt.v(nh * 512, 512), ALU.add)
                p.act(JNK.v(), ht.v(), AF.Square, accum_out=SSQ.v(i, 1))
                p.act(RSTD.v(i, 1), SSQ.v(i, 1), AF.Ln, scale=1.0 / DM, bias=epsv)
                p.act(RSTD.v(i, 1), RSTD.v(i, 1), AF.Exp, scale=-0.5)
                p.stt(ot.v(), ht.v(), RSTD.v(i, 1), prm(P_FNW, 1024), ALU.mult, ALU.mult)
                p.dma(D(out_d[b, i * 128:(i + 1) * 128, :]), ot.v())

        for b in range(nseq):
            wkA = Bump(WK, 16384, WKSZ - 16384)
            rms_transpose(lambda i: x_d[b, i * 128:(i + 1) * 128, :], 16, P_NW,
                          lambda i: UT.v3(BF16, 8, T, i * 128, 128), wkA)
            if b == 0:
                dbg("ut", UT.v(), [128, 8 * T])
            if "S" in phases:
                phase_ssd(b)
            if "B" in phases:
                phase_attn(b)
            if "M" in phases:
                phase_mem(b)
            if "E" in phases:
                phase_out(b)
        print("ops:", p.nops, {e: len(v) for e, v in p.ops.items()})
        p.emit()
    return nc, dumps


def kernel(x, mem, norm_w, w_in, conv_w, conv_b, dt_bias, a_log, d_skip, ssd_norm_w, mem_norm_w, w_mem_kv,
           w_out, final_norm_w):
    f = lambda a: np.ascontiguousarray(np.asarray(a, dtype=np.float32))
    x, mem = f(x), f(mem)
    cb, cf, kc, qc = host_consts()
    cf[:, CF_PRM:CF_PRM + NPRM] = host_params(f(norm_w)[0], f(conv_w)[0], f(conv_b)[0], f(dt_bias)[0], f(a_log)[0],
                                              f(d_skip)[0], f(ssd_norm_w)[0], f(mem_norm_w)[0], f(final_norm_w))
    nc, _ = build()
    shared = {"w_in": f(w_in)[0], "w_kv": f(w_mem_kv)[0], "w_out": f(w_out)[0], "cstb": cb, "cstf": cf,
              "kconst": kc, "qconst": qc}
    in_maps = []
    for c in range(8):
        m = dict(shared)
        m["x"] = x[c * NSEQ:(c + 1) * NSEQ]
        m["mem"] = mem[c * NSEQ:(c + 1) * NSEQ]
        in_maps.append(m)
    res = run_bass_kernel_spmd(nc, in_maps, core_ids=list(range(8)))
    return np.concatenate([r["out"] for r in res.results], axis=0)
```

```python
from contextlib import ExitStack
import concourse.bass as bass
import concourse.mybir as mybir

F32 = mybir.dt.float32
BF16 = mybir.dt.bfloat16
I32 = mybir.dt.int32
AF = mybir.ActivationFunctionType
ALU = mybir.AluOpType
AX = mybir.AxisListType
ESZ = {F32: 4, BF16: 2, I32: 4}


class View:
    __slots__ = ("ap", "arena", "rngs", "pr")

    def __init__(self, ap, arena, rngs, pr=(0, 128)):
        self.ap, self.arena, self.rngs, self.pr = ap, arena, rngs, pr

    def re(self, s, **kw):
        return View(self.ap.rearrange(s, **kw), self.arena, self.rngs, self.pr)

    def bc(self, shape):
        return View(self.ap.broadcast_to(shape), self.arena, self.rngs, self.pr)

    def __getitem__(self, key):
        return View(self.ap[key], self.arena, self.rngs, self.pr)


class Arena:
    def __init__(self, name, t, dtype, ncols, const=False):
        self.name, self.t, self.dtype, self.ncols = name, t, dtype, ncols
        self.esz = ESZ[dtype]
        self.w = []
        self.r = []
        self.const = const
        self.alt = {dtype: t}
        self.psum = False
        self.qlast = [dict() for _ in range(4)]

    def v(self, lo=0, n=None, p0=0, p1=128):
        if n is None:
            n = self.ncols - lo
        assert 0 <= lo and lo + n <= self.ncols, (self.name, lo, n, self.ncols)
        return View(self.t[p0:p1, lo:lo + n], self, [(lo * self.esz, (lo + n) * self.esz)], (p0, p1))

    def v3(self, dtype, nk, stride, lo, n, p0=0, p1=128):
        if dtype not in self.alt:
            self.alt[dtype] = self.t.bitcast(dtype)
        e = ESZ[dtype]
        assert ((nk - 1) * stride + lo + n) * e <= self.ncols * self.esz
        total = self.ncols * self.esz // e
        o = max(0, lo + nk * stride - total)
        assert o <= stride - n and o <= lo, (self.name, lo, nk, stride, n, total)
        ws = lo - o
        ap = self.alt[dtype][p0:p1, ws:ws + nk * stride].rearrange("p (k s) -> p k s", s=stride)[:, :, o:o + n]
        return View(ap, self, [((k * stride + lo) * e, (k * stride + lo + n) * e) for k in range(nk)], (p0, p1))

    def vb(self, dtype, lo, n, p0=0, p1=128):
        if dtype not in self.alt:
            self.alt[dtype] = self.t.bitcast(dtype)
        e = ESZ[dtype]
        assert (lo + n) * e <= self.ncols * self.esz
        return View(self.alt[dtype][p0:p1, lo:lo + n], self, [(lo * e, (lo + n) * e)], (p0, p1))


class Sub:
    def __init__(self, arena, dtype, boff, n):
        self.a, self.dt, self.e, self.n = arena, dtype, ESZ[dtype], n
        assert boff % self.e == 0
        self.base = boff // self.e

    def v(self, lo=0, n=None, p0=0, p1=128):
        if n is None:
            n = self.n - lo
        assert 0 <= lo and lo + n <= self.n, (lo, n, self.n)
        return self.a.vb(self.dt, self.base + lo, n, p0, p1)

    def v3(self, nk, stride, lo, n, p0=0, p1=128):
        assert (nk - 1) * stride + lo + n <= self.n
        return self.a.v3(self.dt, nk, stride, self.base + lo, n, p0, p1)


class Bump:
    def __init__(self, arena, boff, size):
        self.a, self.off, self.end = arena, boff, boff + size

    def get(self, dtype, n):
        self.off = (self.off + 3) // 4 * 4
        s = Sub(self.a, dtype, self.off, n)
        self.off += n * ESZ[dtype]
        assert self.off <= self.end, ("bump overflow", self.off, self.end)
        return s


def D(ap):
    return View(ap, None, [])


COMPUTE = ("pe", "act", "dve", "pool")


class Prog:
    def __init__(self, nc, ndma=40):
        self.nc = nc
        self.ops = {e: [] for e in COMPUTE + ("sp",)}
        self.count = {e: 0 for e in COMPUTE}
        self.waited = {e: {} for e in COMPUTE + ("sp",)}
        self.ndma = ndma
        self.dma_cnt = [0] * ndma
        self.dma_next = 0
        self.nops = 0

    def op(self, eng, fn, reads=(), writes=(), dma=False):
        deps = {}

        def add(tok):
            k, v = tok
            if deps.get(k, 0) < v:
                deps[k] = v

        for vw in reads:
            a = vw.arena
            if a is None:
                continue
            for (lo, hi) in vw.rngs:
                for (l, h, t) in a.w:
                    if l < hi and lo < h:
                        add(t)
        for vw in writes:
            a = vw.arena
            if a is None:
                continue
            assert not a.const, a.name
            for (lo, hi) in vw.rngs:
                for (l, h, t) in a.w:
                    if l < hi and lo < h:
                        add(t)
                for (l, h, t) in a.r:
                    if l < hi and lo < h:
                        add(t)
        for vw in list(reads) + list(writes):
            a = vw.arena
            if a is None or not a.psum:
                continue
            for q in range(vw.pr[0] // 32, (vw.pr[1] + 31) // 32):
                for e2, t in a.qlast[q].items():
                    if e2 != eng:
                        add(t)
        if dma:
            i = self.dma_next
            self.dma_next = (i + 1) % self.ndma
            if self.dma_cnt[i] > 0:
                add((("d", i), self.dma_cnt[i]))
            self.dma_cnt[i] += 16
            tok = (("d", i), self.dma_cnt[i])
            inc = (("d", i), 16)
        else:
            self.count[eng] += 1
            tok = (eng, self.count[eng])
            inc = (eng, 1)
        waits = []
        wd = self.waited[eng]
        for k, v in deps.items():
            if k == eng and eng == "pe":
                continue
            if wd.get(k, 0) >= v:
                continue
            wd[k] = v
            waits.append((k, v))
        self.ops[eng].append((waits, fn, inc))
        self.nops += 1
        for vw in list(reads) + list(writes):
            a = vw.arena
            if a is None or not a.psum:
                continue
            for q in range(vw.pr[0] // 32, (vw.pr[1] + 31) // 32):
                a.qlast[q][eng] = tok
        for vw in reads:
            a = vw.arena
            if a is None or a.const:
                continue
            for (lo, hi) in vw.rngs:
                if not dma:
                    a.r = [x for x in a.r if not (x[2][0] == eng and lo <= x[0] and x[1] <= hi)]
                a.r.append((lo, hi, tok))
        for vw in writes:
            a = vw.arena
            if a is None:
                continue
            for (lo, hi) in vw.rngs:
                a.w = [x for x in a.w if not (lo <= x[0] and x[1] <= hi)]
                a.r = [x for x in a.r if not (lo <= x[0] and x[1] <= hi)]
                a.w.append((lo, hi, tok))
        return tok

    def freeze(self, arena):
        arena.const = True
        arena.r = []

    def matmul(self, out, lhsT, rhs, start=True, stop=True, **kw):
        return self.op("pe", lambda e: e.matmul(out.ap, lhsT.ap, rhs.ap, start=start, stop=stop, **kw),
                       [lhsT, rhs] + ([] if start else [out]), [out])

    def transpose(self, out, in_, ident):
        return self.op("pe", lambda e: e.transpose(out.ap, in_.ap, ident.ap), [in_, ident], [out])

    def act(self, out, in_, func, bias=None, scale=None, accum_out=None):
        rd = [in_]
        kw = {}
        if bias is not None:
            if isinstance(bias, View):
                rd.append(bias)
                kw["bias"] = bias.ap
            else:
                kw["bias"] = bias
        if scale is not None:
            if isinstance(scale, View):
                rd.append(scale)
                kw["scale"] = scale.ap
            else:
                kw["scale"] = scale
        wr = [out]
        if accum_out is not None:
            wr.append(accum_out)
            kw["accum_out"] = accum_out.ap
        return self.op("act", lambda e: e.activation(out.ap, in_.ap, func, **kw), rd, wr)

    def tt(self, out, in0, in1, op, eng="dve"):
        return self.op(eng, lambda e: e.tensor_tensor(out.ap, in0.ap, in1.ap, op), [in0, in1], [out])

    def ts(self, out, in0, s1, s2, op0, op1=None, eng="dve", accum_out=None):
        rd = [in0]
        a1 = s1
        a2 = s2
        if isinstance(s1, View):
            rd.append(s1)
            a1 = s1.ap
        if isinstance(s2, View):
            rd.append(s2)
            a2 = s2.ap
        kw = {}
        if op1 is not None:
            kw["op1"] = op1
        wr = [out]
        if accum_out is not None:
            kw["accum_out"] = accum_out.ap
            wr.append(accum_out)
        return self.op(eng, lambda e: e.tensor_scalar(out.ap, in0.ap, a1, a2, op0, **kw), rd, wr)

    def stt(self, out, in0, scalar, in1, op0, op1):
        rd = [in0, in1]
        sc = scalar
        if isinstance(scalar, View):
            rd.append(scalar)
            sc = scalar.ap
        return self.op("dve", lambda e: e.scalar_tensor_tensor(out.ap, in0.ap, sc, in1.ap, op0, op1), rd, [out])

    def copy(self, out, in_, eng="dve"):
        if eng == "act":
            return self.op("act", lambda e: e.copy(out.ap, in_.ap), [in_], [out])
        return self.op(eng, lambda e: e.tensor_copy(out.ap, in_.ap), [in_], [out])

    def reduce(self, out, in_, op, axis=AX.X, eng="dve"):
        return self.op(eng, lambda e: e.tensor_reduce(out.ap, in_.ap, axis, op), [in_], [out])

    def recip(self, out, in_):
        return self.op("dve", lambda e: e.reciprocal(out.ap, in_.ap), [in_], [out])

    def memset(self, out, val, eng="dve"):
        return self.op(eng, lambda e: e.memset(out.ap, val), [], [out])

    def scan(self, out, d0, d1, initial, op0, op1):
        rd = [d0, d1]
        ini = initial
        if isinstance(initial, View):
            rd.append(initial)
            ini = initial.ap
        return self.op("dve", lambda e: e.tensor_tensor_scan(out.ap, d0.ap, d1.ap, ini, op0, op1), rd, [out])

    def affine_select(self, out, in_, pattern, cmp, fill, base, cm):
        return self.op("pool", lambda e: e.affine_select(out.ap, in_.ap, pattern, cmp, fill, base=base,
                                                         channel_multiplier=cm), [in_], [out])

    def iota(self, out, pattern, base, cm):
        return self.op("pool", lambda e: e.iota(out.ap, pattern, base=base, channel_multiplier=cm), [], [out])

    def dma(self, out, in_, q="sp", **kw):
        if q == "pool":
            kw.setdefault("max_dma_last_dim", 4096)
        return self.op(q, lambda e: e.dma_start(out=out.ap, in_=in_.ap, **kw), [in_], [out], dma=True)

    def emit(self):
        nc = self.nc
        with ExitStack() as es:
            sems = {}
            for e in COMPUTE:
                sems[e] = es.enter_context(nc.semaphore("s_" + e))
            for i in range(self.ndma):
                sems[("d", i)] = es.enter_context(nc.semaphore("s_d%d" % i))
            fin = []
            for i in range(self.ndma):
                if self.dma_cnt[i] > 0:
                    fin.append((("d", i), self.dma_cnt[i]))
            for e in COMPUTE:
                if self.count[e] > 0:
                    fin.append((e, self.count[e]))
            block = es.enter_context(nc.Block())

            def run(stream, final=None):
                def f(eng):
                    for waits, fn, inc in stream:
                        for k, v in waits:
                            eng.wait_ge(sems[k], v)
                        ins = fn(eng)
                        ins.then_inc(sems[inc[0]], inc[1])
                    if final:
                        for k, v in final:
                            eng.wait_ge(sems[k], v)
                return f

            block.tensor(run(self.ops["pe"]))
            block.scalar(run(self.ops["act"]))
            block.vector(run(self.ops["dve"]))
            block.gpsimd(run(self.ops["pool"]))
            block.sync(run(self.ops["sp"], fin))
from concourse.bass_utils import run_bass_kernel_spmd
import numpy as np

T = 2048
DM = 1024
NSEQ = 2
EPS = 1e-6
BIG = 30000.0
C_Q, C_K, C_V, C_G, C_Z, C_XBC, C_DT, C_QM, C_GM = 0, 512, 1024, 1536, 2048, 3072, 5120, 5136, 5648
CB_ID, CB_TRI, CB_MNEG, CB_ONES, CB_EH, NCB = 0, 128, 256, 384, 512, 2560
CF_ID, CF_ONES, CF_PRM, CF_CAP, CF_CMK, CF_OWN, CF_MISC, NCF = 0, 128, 256, 1424, 1488, 1552, 1616, 1624
P_NW, P_MNW, P_SNW, P_CVB, P_CVW, P_DSK, P_DTB, P_ALOG, P_FNW = 0, 8, 16, 24, 40, 104, 112, 128, 144
NPRM = 1168


def host_consts():
    cb = np.zeros((128, NCB), np.float32)
    cb[:, CB_ID:CB_ID + 128] = np.eye(128)
    s = np.arange(128)[:, None]
    t = np.arange(128)[None, :]
    cb[:, CB_TRI:CB_TRI + 128] = (t >= s)
    cb[:, CB_MNEG:CB_MNEG + 128] = np.where(t < s, -BIG, 0.0)
    cb[:, CB_ONES:CB_ONES + 128] = 1.0
    for h in range(16):
        cb[h, CB_EH + h * 128: CB_EH + (h + 1) * 128] = 1.0
    cf = np.zeros((128, NCF), np.float32)
    cf[:, CF_ID:CF_ID + 128] = np.eye(128)
    cf[:, CF_ONES:CF_ONES + 128] = 1.0
    cap = np.zeros((8, 8), np.float32)
    cmk = np.zeros((8, 8), np.float32)
    own = np.zeros((8, 8), np.float32)
    for ti in range(8):
        qb = 4 + ti // 2
        for j in range(8):
            cap[ti, j] = 1e30 if j < qb else -1e30
            cmk[ti, j] = 1.0 if j < qb else 0.0
            own[ti, j] = 1.0 if j == qb else 0.0
    cf[:, CF_CAP:CF_CAP + 64] = cap.reshape(-1)[None, :]
    cf[:, CF_CMK:CF_CMK + 64] = cmk.reshape(-1)[None, :]
    cf[:, CF_OWN:CF_OWN + 64] = own.reshape(-1)[None, :]
    cf[:, CF_MISC] = EPS
    cf[:, CF_MISC + 1] = 1.0
    kc = np.zeros((128, T), np.float32)
    pos = np.arange(T)
    for r in range(8):
        kc[64 + r] = (pos // 256 == r)
    kc[96] = pos // 16
    kc[97] = pos % 16
    kc[98] = 1.0
    kc[99] = 1.0
    qc = np.zeros((8, 4, T), np.float32)
    for h in range(8):
        sl = 2.0 ** (-8.0 * (h + 1) / 8)
        qc[h, 0] = 16 * sl * 8
        qc[h, 1] = sl * 8
        qc[h, 2] = -16 * sl * 8 * (pos // 16)
        qc[h, 3] = -sl * 8 * (pos % 16)
    return cb, cf, kc, qc


def host_params(norm_w, conv_w, conv_b, dt_bias, a_log, d_skip, ssd_norm_w, mem_norm_w, final_norm_w):
    prm = np.zeros((128, NPRM), np.float32)
    prm[:, P_NW:P_NW + 8] = norm_w.reshape(8, 128).T
    prm[:, P_MNW:P_MNW + 8] = mem_norm_w.reshape(8, 128).T
    prm[:, P_SNW:P_SNW + 8] = ssd_norm_w.reshape(8, 128).T
    prm[:, P_CVB:P_CVB + 16] = conv_b.reshape(16, 128).T
    prm[:, P_CVW:P_CVW + 64] = conv_w.T.reshape(16, 128, 4).transpose(1, 0, 2).reshape(128, 64)
    prm[:, P_DSK:P_DSK + 8] = np.repeat(d_skip, 64).reshape(8, 128).T
    prm[:, P_DTB:P_DTB + 16] = dt_bias[None, :]
    prm[:, P_ALOG:P_ALOG + 16] = a_log[None, :]
    prm[:, P_FNW:P_FNW + 1024] = final_norm_w[None, :]
    return prm
def build(nseq=NSEQ, dump=None, phases="ASBME"):
    nc = bass.Bass("TRN2", target_bir_lowering=False)
    x_d = nc.dram_tensor("x", [nseq, T, DM], F32, kind="ExternalInput").ap()
    mem_d = nc.dram_tensor("mem", [nseq, 256, DM], F32, kind="ExternalInput").ap()
    win_d = nc.dram_tensor("w_in", [DM, 6160], F32, kind="ExternalInput").ap()
    wkv_d = nc.dram_tensor("w_kv", [DM, 1024], F32, kind="ExternalInput").ap()
    wout_d = nc.dram_tensor("w_out", [2048, DM], F32, kind="ExternalInput").ap()
    cb_d = nc.dram_tensor("cstb", [128, NCB], F32, kind="ExternalInput").ap()
    cf_d = nc.dram_tensor("cstf", [128, NCF], F32, kind="ExternalInput").ap()
    kc_d = nc.dram_tensor("kconst", [128, T], F32, kind="ExternalInput").ap()
    qc_d = nc.dram_tensor("qconst", [8, 4, T], F32, kind="ExternalInput").ap()
    out_d = nc.dram_tensor("out", [nseq, T, DM], F32, kind="ExternalOutput").ap()
    dumps = {}
    wsi_d = nc.dram_tensor("wsc_in", [DM, 6160], BF16, kind="Internal").ap()
    wskv_d = nc.dram_tensor("wsc_kv", [DM, 1024], BF16, kind="Internal").ap()
    wso_d = nc.dram_tensor("wsc_out", [2048, DM], BF16, kind="Internal").ap()
    wsi3 = wsi_d.rearrange("(k p) c -> p k c", p=128)
    wskv3 = wskv_d.rearrange("(k p) c -> p k c", p=128)
    wso3 = wso_d.rearrange("(k p) c -> p k c", p=128)
    win3 = win_d.rearrange("(k p) c -> p k c", p=128)
    wkv3 = wkv_d.rearrange("(k p) c -> p k c", p=128)
    wout3 = wout_d.rearrange("(k p) c -> p k c", p=128)

    with ExitStack() as es:
        def sb(name, n, dt):
            t = es.enter_context(nc.sbuf_tensor(name, [128, n], dt))
            return Arena(name, t, dt, n)

        UT = sb("UT", 8 * T, BF16)
        MIX = sb("MIX", 16 * T, BF16)
        WB = sb("WB", 8 * 3088, BF16)
        WKSZ = 47616
        WK = sb("WK", WKSZ // 2, BF16)
        CB = sb("CB", NCB, BF16)
        CF = sb("CF", NCF, F32)
        SM = sb("SM", 64, F32)
        PS = []
        for i in range(8):
            t = es.enter_context(nc.psum_tensor("ps%d" % i, [128, 512], F32))
            PS.append(Arena("ps%d" % i, t, F32, 512))
            PS[-1].psum = True
        p = Prog(nc)

        def dbg(name, view, shape):
            if dump is None or name not in dump:
                return
            d = nc.dram_tensor("dbg_" + name, list(shape), view.ap.dtype, kind="ExternalOutput").ap()
            dumps[name] = d
            p.dma(D(d), view)

        p.dma(CB.v(), D(cb_d), q="pool")
        p.dma(CF.v(), D(cf_d))
        ident = CB.v(CB_ID, 128)
        tri = CB.v(CB_TRI, 128)
        mneg = CB.v(CB_MNEG, 128)
        ones = CB.v(CB_ONES, 128)
        identf = CF.v(CF_ID, 128)
        epsv = CF.v(CF_MISC, 1)
        onev = CF.v(CF_MISC + 1, 1)

        def prm(off, n, p0=0, p1=128):
            return CF.v(CF_PRM + off, n, p0, p1)

        AB = SM.v(0, 16)
        p.act(AB, prm(P_ALOG, 16), AF.Exp)
        p.ts(AB, AB, -1.0, None, ALU.mult)

        SC = {n: Arena("sc_" + n, None, BF16, 1) for n in ("attn", "mem", "ssd", "kv", "out")}
        converted = [False]

        def scv(ap, name, k):
            return View(ap, SC[name], [(k, k + 1)])

        def convert_all():
            for name, src3, dst3, c0, nw, nk in (("attn", win3, wsi3, 0, 2048, 8), ("kv", wkv3, wskv3, 0, 1024, 8),
                                                 ("mem", win3, wsi3, C_QM, 1024, 8), ("out", wout3, wso3, 0, 1024, 16),
                                                 ("ssd", win3, wsi3, C_Z, 3088, 8)):
                for k in range(nk):
                    p.dma(scv(dst3[:, k, c0:c0 + nw], name, k), D(src3[:, k, c0:c0 + nw]), q="pool")
            converted[0] = True

        def load_w(name, dst3, src3, c0, nw, nk, wb_off=0):
            for k in range(nk):
                if converted[0]:
                    p.dma(WB.v(wb_off + k * nw, nw), scv(dst3[:, k, c0:c0 + nw], name, k), q="sp")
                else:
                    p.dma(WB.v(wb_off + k * nw, nw), D(src3[:, k, c0:c0 + nw]), q="pool")

        pj_i = [0]

        def rms_transpose(src_tile, ntiles, nwoff, dst_fn, wk):
            XT = [wk.get(F32, 1024) for _ in range(2)]
            XS = [wk.get(BF16, 1024) for _ in range(2)]
            JNK = wk.get(BF16, 1024)
            SSQ = wk.get(F32, 16)
            RSTD = wk.get(F32, 16)
            for i in range(ntiles):
                xt = XT[i % 2]
                p.dma(xt.v(), D(src_tile(i)))
                p.act(JNK.v(), xt.v(), AF.Square, accum_out=SSQ.v(i, 1))
                p.act(RSTD.v(i, 1), SSQ.v(i, 1), AF.Ln, scale=1.0 / DM, bias=epsv)
                p.act(RSTD.v(i, 1), RSTD.v(i, 1), AF.Exp, scale=-0.5)
                xs = XS[i % 2]
                p.ts(xs.v(), xt.v(), RSTD.v(i, 1), None, ALU.mult)
                pb = PS[i % 2]
                for k in range(8):
                    p.transpose(pb.vb(BF16, k * 128, 128), xs.v(k * 128, 128), ident)
                p.tt(dst_fn(i), pb.vb(BF16, 0, 1024).re("p (k t) -> p k t", k=8),
                     prm(nwoff, 8).re("p (k o) -> p k o", o=1).bc([128, 8, 128]), ALU.mult)

        def utv(k, t0, n):
            return UT.v(k * T + t0, n)

        def proj_fm(nw, c0, ncols, t0, n, ps_view):
            for k in range(8):
                p.matmul(ps_view, WB.v(k * nw + c0, ncols), utv(k, t0, n), start=(k == 0), stop=(k == 7))

        def phase_ssd(b):
            NW = 3088
            load_w("ssd", wsi3, win3, C_Z, NW, 8)
            if not converted[0]:
                convert_all()
            b1 = Bump(MIX, 0, 16384)
            b2 = Bump(MIX, 12 * T * 2, 16384)
            wk = Bump(WK, 0, WKSZ)
            XPRE = b1.get(BF16, 16 * 259)
            XST = b1.get(BF16, 8 * 256)
            BMT = b1.get(BF16, 4 * 256)
            LT = [b1.get(BF16, 384) for _ in range(2)]
            SZ = b2.get(BF16, 8 * 256)
            PREV = b2.get(F32, 1024)
            XDD = b2.get(BF16, 2 * 1024)
            CMT = b2.get(BF16, 4 * 256)
            BTOK = b2.get(BF16, 2 * 512)
            DIAG = wk.get(BF16, 64 * 128)
            CBT = wk.get(BF16, 4 * 384)
            L0 = [wk.get(BF16, 256) for _ in range(2)]
            MT = [wk.get(BF16, 384) for _ in range(2)]
            GT = [wk.get(BF16, 256) for _ in range(2)]
            GG = [wk.get(F32, 256) for _ in range(4)]
            GSQ = [wk.get(BF16, 256) for _ in range(4)]
            RS = wk.get(F32, 256)
            XDT = wk.get(BF16, 2 * 2048)
            PREVB = wk.get(BF16, 2048)
            DTR = wk.get(F32, 32)
            DT = wk.get(F32, 32)
            DA = wk.get(F32, 256)
            NCS = wk.get(F32, 32)
            DEC = wk.get(F32, 32)
            W2 = wk.get(F32, 32)
            CST = wk.get(F32, 256)
            CSH = wk.get(BF16, 256)
            CSL = wk.get(BF16, 256)
            DECT = wk.get(F32, 256)
            DG16 = wk.get(F32, 16)
            CDB = wk.get(F32, 16)

            for zt in (XDT, PREVB, DA, CST, CSH, CSL, DECT, DG16):
                p.memset(zt.v(), 0.0, eng="pool")
            for j in range(64):
                p.ts(DIAG.v(j * 128, 128), ident, prm(P_CVW + j, 1), None, ALU.mult,
                     eng=("pool" if j % 2 else "dve"))
            p.memset(XPRE.v3(16, 259, 0, 3), 0.0)
            p.memset(PREV.v(), 0.0)

            for c in range(8):
                t0 = c * 256

                def zgroup(zc):
                    ps = PS[zc % 2].v((zc // 2 % 2) * 256, 256)
                    proj_fm(NW, zc * 128, 128, t0, 256, ps)
                    p.copy(SZ.v(zc * 256, 256), ps, eng="dve")

                def xgroup(cc):
                    ps = PS[cc % 2].v((cc // 2 % 2) * 256, 256)
                    proj_fm(NW, 1024 + cc * 128, 128, t0, 256, ps)
                    p.copy(XPRE.v(cc * 259 + 3, 256), ps, eng=("dve" if cc % 2 else "act"))

                if c > 0:
                    p.copy(XPRE.v3(16, 259, 0, 3), XPRE.v3(16, 259, 256, 3), eng="pool")
                for lt in range(2):
                    for k in range(8):
                        p.matmul(PS[5].v(384 + lt * 16, 16), utv(k, t0 + lt * 128, 128), WB.v(k * NW + 3072, 16),
                                 start=(k == 0), stop=(k == 7))
                p.tt(DTR.v(), PS[5].v(384, 32).re("p (l h) -> p l h", l=2),
                     prm(P_DTB, 16).re("p (o h) -> p o h", o=1).bc([128, 2, 16]), ALU.add)
                p.act(DTR.v(), DTR.v(), AF.Exp)
                p.act(DT.v(), DTR.v(), AF.Ln, bias=onev)
                p.tt(DA.v3(2, 128, 0, 16), DT.v().re("p (l h) -> p l h", l=2),
                     SM.v(0, 16).re("p (o h) -> p o h", o=1).bc([128, 2, 16]), ALU.mult)
                for zc in range(6):
                    zgroup(zc)
                for lt in range(2):
                    p.transpose(PS[6].v(lt * 128, 128), DA.v(lt * 128, 128), identf)
                p.scan(CST.v(0, 256, 0, 16), CF.v(CF_ONES, 1, 0, 16).bc([16, 256]), PS[6].v(0, 256, 0, 16), 0.0,
                       ALU.mult, ALU.add)
                p.copy(CSH.v(0, 256, 0, 16), CST.v(0, 256, 0, 16))
                p.tt(CSL.v(0, 256, 0, 16), CST.v(0, 256, 0, 16), CSH.v(0, 256, 0, 16), ALU.subtract)
                p.act(DECT.v(0, 256, 0, 16), CST.v(0, 256, 0, 16), AF.Exp, scale=-1.0, bias=CST.v(255, 1, 0, 16))
                for zc in range(6, 8):
                    zgroup(zc)
                for cc in range(8, 12):
                    xgroup(cc)
                for lt in range(2):
                    p.transpose(PS[7].v(lt * 128, 128), CST.v(lt * 128, 128), identf)
                    p.transpose(PS[7].v(256 + lt * 128, 128), DECT.v(lt * 128, 128), identf)
                p.ts(NCS.v().re("p (l h) -> p l h", l=2), PS[7].v3(F32, 2, 128, 0, 16), -1.0, None, ALU.mult)
                p.copy(DEC.v().re("p (l h) -> p l h", l=2), PS[7].v3(F32, 2, 128, 256, 16))
                p.tt(W2.v(), DT.v(), DEC.v(), ALU.mult)
                if c < 7:
                    p.ts(DG16.v(0, 16, 0, 16), CF.v(CF_ID, 16, 0, 16), CST.v(255, 1, 0, 16), None, ALU.mult)
                    p.matmul(PS[5].v(480, 16), CF.v(CF_ONES, 128), DG16.v())
                    p.act(CDB.v(), PS[5].v(480, 16), AF.Exp)
                for cc in list(range(12, 16)) + list(range(0, 8)):
                    xgroup(cc)

                def conv(cc):
                    ps = PS[cc % 2].v((cc // 2 % 2) * 256, 256)
                    for k in range(4):
                        p.matmul(ps, DIAG.v((cc * 4 + k) * 128, 128), XPRE.v(cc * 259 + k, 256),
                                 start=(k == 0), stop=(k == 3))
                    if cc < 8:
                        dst = XST.v(cc * 256, 256)
                    elif cc < 12:
                        dst = BMT.v((cc - 8) * 256, 256)
                    else:
                        dst = CMT.v((cc - 12) * 256, 256)
                    p.act(dst, ps, AF.Silu, bias=prm(P_CVB + cc, 1))

                for cc in range(8, 16):
                    conv(cc)
                for lt in range(2):
                    pb = PS[7]
                    for g in range(4):
                        p.transpose(pb.vb(BF16, g * 128, 128), BMT.v(g * 256 + lt * 128, 128), ident)
                    p.copy(BTOK.v(lt * 512, 512), pb.vb(BF16, 0, 512), eng="dve")
                for g in range(4):
                    ps = PS[5]
                    p.matmul(ps.v(0, 256), BMT.v(g * 256, 128), CMT.v(g * 256, 256))
                    p.matmul(ps.v(256, 128), BMT.v(g * 256 + 128, 128), CMT.v(g * 256 + 128, 128))
                    p.copy(CBT.v(g * 384, 384), ps.v(0, 384), eng="dve")
                for cc in range(0, 8):
                    conv(cc)
                p.act(SZ.v(), SZ.v(), AF.Silu)
                for lt in range(2):
                    pb = PS[6]
                    for cc in range(8):
                        p.transpose(pb.vb(BF16, cc * 128, 128), XST.v(cc * 256 + lt * 128, 128), ident)
                    src = pb.vb(BF16, 0, 1024).re("p (h q) -> p h q", h=16)
                    for hh in range(2):
                        p.tt(XDT.v(lt * 2048, 2048).re("p (c e q) -> p c e q", c=8, e=2)[:, :, hh, hh * 64:hh * 64 + 64],
                             src.re("p (c e) q -> p c e q", e=2)[:, :, hh, :],
                             DT.v(lt * 16, 16).re("p (c e o) -> p c e o", e=2, o=1)[:, :, hh, :].bc([128, 8, 64]),
                             ALU.mult)
                    p.tt(XDD.v(lt * 1024, 1024).re("p (h q) -> p h q", h=16), src,
                         W2.v(lt * 16, 16).re("p (h o) -> p h o", o=1).bc([128, 16, 64]), ALU.mult)
                def seg_stage(h):
                    g = h // 4
                    sg = PS[2 + h % 2]
                    eh = CB.v(CB_EH + h * 128, 128)
                    csh = lambda lo, n: CSH.v(lo, n)
                    csl = lambda lo, n: CSL.v(lo, n)
                    p.matmul(sg.v(0, 256), eh, csh(0, 256), start=True, stop=False)
                    p.matmul(sg.v(0, 256), eh, csl(0, 256), start=False, stop=True)
                    p.matmul(sg.v(256, 128), eh, csh(0, 128), start=True, stop=False)
                    p.matmul(sg.v(256, 128), eh, csl(0, 128), start=False, stop=False)
                    p.matmul(sg.v(256, 128), ident, mneg, start=False, stop=True)
                    p.matmul(sg.v(384, 128), eh, csh(128, 128), start=True, stop=False)
                    p.matmul(sg.v(384, 128), eh, csl(128, 128), start=False, stop=False)
                    p.matmul(sg.v(384, 128), ident, mneg, start=False, stop=True)
                    l0 = L0[h % 2]
                    lt_ = LT[h % 2]
                    mt = MT[h % 2]
                    gt = GT[h % 2]
                    if c > 0:
                        p.act(l0.v(), sg.v(0, 256), AF.Exp)
                    p.act(lt_.v(0, 128), sg.v(256, 128), AF.Exp, bias=NCS.v(h, 1))
                    p.act(lt_.v(128, 128), sg.v(128, 128), AF.Exp, bias=NCS.v(h, 1))
                    p.act(lt_.v(256, 128), sg.v(384, 128), AF.Exp, bias=NCS.v(16 + h, 1))
                    p.tt(mt.v(), lt_.v(), CBT.v(g * 384, 384), ALU.mult)
                    if c > 0:
                        p.tt(gt.v(), l0.v(), CMT.v(g * 256, 256), ALU.mult, eng="pool")

                def y_stage(h):
                    g = h // 4
                    pr = h % 2
                    pc = h // 2
                    mt = MT[h % 2]
                    gt = GT[h % 2]
                    ybank = PS[4 + 2 * (pc % 2)]
                    yt = ybank.v(0, 256)
                    yt2 = ybank.v(128, 128)
                    p.matmul(yt, XDT.v(h * 128, 128), mt.v(0, 256), start=(pr == 0), stop=False)
                    p.matmul(yt2, XDT.v(2048 + h * 128, 128), mt.v(256, 128), start=False, stop=(c == 0 and pr == 1))
                    if c > 0:
                        p.matmul(yt, PREVB.v(h * 128, 128), gt.v(), start=False, stop=(pr == 1))
                    if pr == 1:
                        gg = GG[pc % 4]
                        p.stt(gg.v(), XST.v(pc * 256, 256), prm(P_DSK + pc, 1), yt, ALU.mult, ALU.add)
                        p.tt(gg.v(), gg.v(), SZ.v(pc * 256, 256), ALU.mult)
                        p.act(GSQ[pc % 4].v(), gg.v(), AF.Square)
                def norm_stage(g):
                        pc = 2 * g + 1
                        pcs = [pc - 1, pc]
                        ss = PS[7].v(256, 256)
                        p.matmul(ss, ones, GSQ[pcs[0] % 4].v(), start=True, stop=False)
                        p.matmul(ss, ones, GSQ[pcs[1] % 4].v(), start=False, stop=True)
                        p.act(RS.v(), ss, AF.Ln, scale=1.0 / 256, bias=epsv)
                        p.act(RS.v(), RS.v(), AF.Exp, scale=-0.5)
                        for q in pcs:
                            p.stt(MIX.v((4 + q) * T + t0, 256), GG[q % 4].v(), prm(P_SNW + q, 1), RS.v(),
                                  ALU.mult, ALU.mult)

                for h in range(18):
                    if h < 16:
                        seg_stage(h)
                    if 1 <= h <= 16:
                        y_stage(h - 1)
                    if h >= 2 and (h - 2) % 4 == 3:
                        norm_stage((h - 2) // 4)
                if c < 7:
                    for g in range(4):
                        st = PS[7].v(0, 256)
                        for lt in range(2):
                            p.matmul(st, BTOK.v(lt * 512 + g * 128, 128), XDD.v(lt * 1024 + g * 256, 256),
                                     start=(lt == 0), stop=(lt == 1))
                        pv = PREV.v(g * 256, 256)
                        p.tt(pv.re("p (h q) -> p h q", h=4), pv.re("p (h q) -> p h q", h=4),
                             CDB.v(g * 4, 4).re("p (h o) -> p h o", o=1).bc([128, 4, 64]), ALU.mult)
                        p.tt(pv, pv, st, ALU.add)
                        for hh in range(2):
                            p.copy(PREVB.v(g * 512, 512).re("p (c e q) -> p c e q", c=2, e=2)[:, :, hh, hh * 64:hh * 64 + 64],
                                   pv.re("p (c e q) -> p c e q", c=2, e=2)[:, :, hh, :], eng="pool")
            dbg("ssd", MIX.v(4 * T, 8 * T), [128, 8 * T])

        def attn_core(kt_list_fn, nq, kv_lhsT, q_rhs, v_lhsT, scale, PTs, epilogue, diag_fn=None, den_lhsT=None):
            pass

        def phase_attn(b):
            NW = 2048
            load_w("attn", wsi3, win3, 0, NW, 8)
            wk = Bump(WK, 0, WKSZ)
            bm = Bump(MIX, 12 * T * 2, 16384)
            bw = Bump(WB, 8 * NW * 2, WB.ncols * 2 - 8 * NW * 2)
            sets = [dict(QK=[wk.get(BF16, T) for _ in range(4)], VA=wk.get(BF16, 2048), VB=wk.get(BF16, 2048),
                         SG=wk.get(BF16, T)),
                    dict(QK=[bm.get(BF16, T) for _ in range(4)], VA=bw.get(BF16, 2048), VB=bw.get(BF16, 2048),
                         SG=bw.get(BF16, T))]
            PT = [wk.get(BF16, 512) for _ in range(3)]
            R = [wk.get(F32, 512) for _ in range(2)]
            T1 = [wk.get(F32, 512) for _ in range(2)]
            PENB = wk.get(BF16, 8 * 128)
            GM = wk.get(F32, 64)
            CMP = wk.get(F32, 512)
            RANK = wk.get(F32, 64)
            KSUM = wk.get(F32, 8)
            KMT = wk.get(BF16, 8)
            p.memset(KMT.v(), 0.0, eng="pool")
            p.memset(PENB.v(), 0.0, eng="pool")
            for S in sets:
                for i in range(2):
                    p.memset(S["QK"][i].v(), 0.0, eng="pool")
                    p.dma(S["QK"][2 + i].v(), D(kc_d), q="pool")
                p.memset(S["VA"].v(), 1.0, eng="pool")
                p.memset(S["VB"].v(), 1.0, eng="pool")

            def make_thunks(hp, S):
                QK, VA, VB, SG = S["QK"], S["VA"], S["VB"], S["SG"]
                th = []

                def t_qc():
                    for hh in range(2):
                        p.dma(QK[hh].v(0, T, 96, 100), D(qc_d[2 * hp + hh]), q="pool")
                th.append(t_qc)
                def half_proj(c0, tq, half, evac):
                    def f():
                        ps = PS[2 + tq % 2]
                        if half == 1:
                            for k in range(8):
                                p.matmul(ps.v(), WB.v(k * NW + c0, 128), utv(k, tq * 512, 512), start=(k == 0), stop=(k == 7))
                            evac(ps)
                    return f

                for which, c0 in ((0, C_Q), (1, C_K)):
                    for tq in range(4):
                        def ev(ps, which=which, tq=tq):
                            p.copy(QK[2 * which].v(tq * 512, 512, 0, 64), ps.v(0, 512, 0, 64), eng="act")
                            p.copy(QK[2 * which + 1].v(tq * 512, 512, 0, 64), ps.v(0, 512, 64, 128), eng="dve")
                        th.append(half_proj(c0 + hp * 128, tq, 0, ev))
                        th.append(half_proj(c0 + hp * 128, tq, 1, ev))

                def gating(hh):
                    Q = QK[hh]
                    K = QK[2 + hh]
                    p.reduce(KSUM.v(0, 8, 0, 64), K.v(0, T, 0, 64).re("p (j s) -> p j s", j=8), ALU.add)
                    p.ts(KMT.v(0, 8, 0, 64), KSUM.v(0, 8, 0, 64), 1.0 / 256, None, ALU.mult)
                    gps = PS[7]
                    for ti in range(8):
                        p.matmul(gps.v(ti * 8, 8), Q.v(1024 + ti * 128, 128), KMT.v())
                    p.tt(GM.v(), gps.v(0, 64), CF.v(CF_CAP, 64), ALU.min)
                    g3 = GM.v().re("p (t j) -> p t j", t=8)
                    p.tt(CMP.v().re("p (t j k) -> p t j k", t=8, j=8),
                         g3.re("p t (o k) -> p t o k", o=1).bc([128, 8, 8, 8]),
                         g3.re("p t (j o) -> p t j o", o=1).bc([128, 8, 8, 8]), ALU.is_gt)
                    p.reduce(RANK.v(), CMP.v().re("p (a k) -> p a k", k=8), ALU.add)
                    p.ts(RANK.v(), RANK.v(), 3.0, None, ALU.is_lt)
                    p.tt(RANK.v(), RANK.v(), CF.v(CF_CMK, 64), ALU.mult)
                    p.tt(RANK.v(), RANK.v(), CF.v(CF_OWN, 64), ALU.add)
                    p.ts(PENB.v3(8, 128, 64, 8), RANK.v().re("p (t j) -> p t j", t=8), -1.0, BIG, ALU.add, ALU.mult)
                    pps = PS[7]
                    for ti in range(8):
                        p.transpose(pps.vb(BF16, ti * 128, 128), PENB.v(ti * 128, 128), ident)
                    p.copy(Q.v(1024, 1024, 64, 72), pps.vb(BF16, 0, 1024, 64, 72), eng="dve")

                for tq in range(4):
                    def ev(ps, tq=tq):
                        p.copy(SG.v(tq * 512, 512), ps.v(), eng="dve")
                    th.append(half_proj(C_G + hp * 128, tq, 0, ev))
                    th.append(half_proj(C_G + hp * 128, tq, 1, ev))
                th.append(lambda: gating(0))
                for tg in range(4):
                    for j in range(4):
                        def f(tg=tg, j=j):
                            ps = PS[2 + tg % 2]
                            ti = tg * 4 + j
                            for k in range(8):
                                p.matmul(ps.v(j * 128, 128), utv(k, ti * 128, 128),
                                         WB.v(k * NW + C_V + hp * 128, 128), start=(k == 0), stop=(k == 7))
                            if j == 3:
                                src = ps.v().re("p (j c) -> p j c", j=4)
                                p.copy(VA.v3(4, 128, tg * 512, 64), src[:, :, 0:64], eng="dve")
                                p.copy(VB.v3(4, 128, tg * 512 + 64, 64), src[:, :, 64:128], eng="dve")
                        th.append(f)
                th.append(lambda: gating(1))
                th.append(lambda: p.act(SG.v(), SG.v(), AF.Silu))
                return th

            def attention(hp, hh, S, bg, pt_base):
                Q = S["QK"][hh]
                K = S["QK"][2 + hh]
                V = S["VA"] if hh == 0 else S["VB"]
                SG = S["SG"]
                tiles = [(qt, kt) for qt in range(4) for kt in range(4 * qt + 4)]
                o0, d0 = (0, 64) if hh == 0 else (64, 0)

                def geom(idx):
                    qt, kt = tiles[idx]
                    tq0 = qt * 512
                    s0 = kt * 128
                    qlo = max(tq0, s0)
                    return qt, kt, tq0, s0, qlo, tq0 + 512 - qlo

                def emit_qk(idx):
                    qt, kt, tq0, s0, qlo, n = geom(idx)
                    gi = pt_base + idx
                    sp = PS[4 + gi % 3]
                    ptile = PT[gi % 3]
                    p.matmul(sp.v(0, n), K.v(s0, 128), Q.v(qlo, n))
                    p.act(ptile.v(0, n), sp.v(0, n), AF.Exp, scale=0.125)
                    if s0 >= tq0:
                        p.tt(ptile.v(0, 128), ptile.v(0, 128), tri, ALU.mult, eng="pool")

                def emit_pv(idx):
                    qt, kt, tq0, s0, qlo, n = geom(idx)
                    gi = pt_base + idx
                    nkt = 4 * qt + 4
                    po = PS[qt % 2]
                    ptile = PT[gi % 3]
                    p.matmul(po.v(qlo - tq0, n), V.v(kt * 128, 128), ptile.v(0, n),
                             start=(kt == 0), stop=(kt == nkt - 1))
                    if kt == nkt - 1:
                        r_ = R[qt % 2]
                        t1_ = T1[qt % 2]
                        p.recip(r_.v(0, 512, o0, o0 + 64), po.v(0, 512, d0, d0 + 64))
                        p.tt(t1_.v(0, 512, o0, o0 + 64), r_.v(0, 512, o0, o0 + 64), SG.v(tq0, 512, o0, o0 + 64),
                             ALU.mult, eng="pool")
                        p.tt(MIX.v(hp * T + tq0, 512, o0, o0 + 64), po.v(0, 512, o0, o0 + 64),
                             t1_.v(0, 512, o0, o0 + 64), ALU.mult)

                LA = 2
                for idx in range(len(tiles) + LA):
                    if idx < len(tiles):
                        emit_qk(idx)
                    if idx >= LA:
                        emit_pv(idx - LA)
                    if bg and idx % 2 == 1:
                        bg.pop(0)()
                return pt_base + len(tiles)

            pt_i = 0
            for f in make_thunks(0, sets[0]):
                f()
            for hp in range(4):
                bg = make_thunks(hp + 1, sets[(hp + 1) % 2]) if hp < 3 else []
                for hh in range(2):
                    pt_i = attention(hp, hh, sets[hp % 2], bg, pt_i)
                while bg:
                    bg.pop(0)()
            dbg("att", MIX.v(0, 4 * T), [128, 4 * T])

        def phase_mem(b):
            NW = 1024
            load_w("mem", wsi3, win3, C_QM, NW, 8)
            load_w("kv", wskv3, wkv3, 0, NW, 8, wb_off=8192)
            wk = Bump(WK, 0, WKSZ)
            MEMT = wk.get(BF16, 8 * 256)
            KM = wk.get(BF16, 4 * 256)
            VM = wk.get(BF16, 2 * 512)
            QM = [wk.get(BF16, T) for _ in range(2)]
            SGM = [wk.get(BF16, T) for _ in range(2)]
            PT = [wk.get(BF16, 512) for _ in range(2)]
            R = wk.get(F32, 512)
            T1 = wk.get(F32, 512)
            rms_transpose(lambda i: mem_d[b, i * 128:(i + 1) * 128, :], 2, P_MNW,
                          lambda i: MEMT.v3(8, 256, i * 128, 128), wk)
            for h in range(4):
                ps = PS[2 + h % 2]
                for k in range(8):
                    p.matmul(ps.v(0, 256), WB.v(8192 + k * NW + h * 128, 128), MEMT.v(k * 256, 256),
                             start=(k == 0), stop=(k == 7))
                p.copy(KM.v(h * 256, 256), ps.v(0, 256), eng="act")
            for mt in range(2):
                ps = PS[2 + mt % 2]
                for k in range(8):
                    p.matmul(ps.v(), MEMT.v(k * 256 + mt * 128, 128), WB.v(8192 + k * NW + 512, 512),
                             start=(k == 0), stop=(k == 7))
                p.copy(VM.v(mt * 512, 512), ps.v(), eng="dve")
            pt_i = 0
            for h in range(4):
                qm = QM[h % 2]
                sgm = SGM[h % 2]
                for tq in range(4):
                    ps = PS[2 + tq % 2]
                    proj_fm(NW, h * 128, 128, tq * 512, 512, ps.v())
                    p.copy(qm.v(tq * 512, 512), ps.v(), eng="act")
                for tq in range(4):
                    ps = PS[2 + tq % 2]
                    proj_fm(NW, 512 + h * 128, 128, tq * 512, 512, ps.v())
                    p.act(sgm.v(tq * 512, 512), ps.v(), AF.Silu)
                for qt in range(4):
                    tq0 = qt * 512
                    po = PS[qt % 2]
                    den = PS[6 + qt % 2]
                    for mt in range(2):
                        sp = PS[4 + pt_i % 2]
                        ptile = PT[pt_i % 2]
                        pt_i += 1
                        p.matmul(sp.v(), KM.v(h * 256 + mt * 128, 128), qm.v(tq0, 512))
                        p.act(ptile.v(), sp.v(), AF.Exp, scale=128 ** -0.5)
                        p.matmul(po.v(), VM.v(mt * 512 + h * 128, 128), ptile.v(), start=(mt == 0), stop=(mt == 1))
                        p.matmul(den.v(), ones, ptile.v(), start=(mt == 0), stop=(mt == 1))
                    p.recip(R.v(), den.v())
                    p.tt(T1.v(), R.v(), sgm.v(tq0, 512), ALU.mult, eng="pool")
                    p.tt(MIX.v((12 + h) * T + tq0, 512), po.v(), T1.v(), ALU.mult)
            dbg("memo", MIX.v(12 * T, 4 * T), [128, 4 * T])

        def phase_out(b):
            NW = 1024
            load_w("out", wso3, wout3, 0, NW, 16)
            wk = Bump(WK, 0, WKSZ)
            XT = [wk.get(F32, 1024) for _ in range(2)]
            HT = [wk.get(F32, 1024) for _ in range(2)]
            OT = [wk.get(F32, 1024) for _ in range(2)]
            JNK = wk.get(BF16, 1024)
            SSQ = wk.get(F32, 16)
            RSTD = wk.get(F32, 16)
            for i in range(16):
                xt = XT[i % 2]
                ht = HT[i % 2]
                ot = OT[i % 2]
                p.dma(xt.v(), D(x_d[b, i * 128:(i + 1) * 128, :]))
                for nh in range(2):
                    ps = PS[(2 * i + nh) % 4]
                    for m in range(16):
                        p.matmul(ps.v(), MIX.v(m * T + i * 128, 128), WB.v(m * NW + nh * 512, 512),
                                 start=(m == 0), stop=(m == 15))
                    p.tt(ht.v(nh * 512, 512), ps.v(), xt.v(nh * 512, 512), ALU.add)
                p.act(JNK.v(), ht.v(), AF.Square, accum_out=SSQ.v(i, 1))
                p.act(RSTD.v(i, 1), SSQ.v(i, 1), AF.Ln, scale=1.0 / DM, bias=epsv)
                p.act(RSTD.v(i, 1), RSTD.v(i, 1), AF.Exp, scale=-0.5)
                p.stt(ot.v(), ht.v(), RSTD.v(i, 1), prm(P_FNW, 1024), ALU.mult, ALU.mult)
                p.dma(D(out_d[b, i * 128:(i + 1) * 128, :]), ot.v())

        for b in range(nseq):
            wkA = Bump(WK, 16384, WKSZ - 16384)
            rms_transpose(lambda i: x_d[b, i * 128:(i + 1) * 128, :], 16, P_NW,
                          lambda i: UT.v3(BF16, 8, T, i * 128, 128), wkA)
            if b == 0:
                dbg("ut", UT.v(), [128, 8 * T])
            if "S" in phases:
                phase_ssd(b)
            if "B" in phases:
                phase_attn(b)
            if "M" in phases:
                phase_mem(b)
            if "E" in phases:
                phase_out(b)
        print("ops:", p.nops, {e: len(v) for e, v in p.ops.items()})
        p.emit()
    return nc, dumps


def kernel(x, mem, norm_w, w_in, conv_w, conv_b, dt_bias, a_log, d_skip, ssd_norm_w, mem_norm_w, w_mem_kv,
           w_out, final_norm_w):
    f = lambda a: np.ascontiguousarray(np.asarray(a, dtype=np.float32))
    x, mem = f(x), f(mem)
    cb, cf, kc, qc = host_consts()
    cf[:, CF_PRM:CF_PRM + NPRM] = host_params(f(norm_w)[0], f(conv_w)[0], f(conv_b)[0], f(dt_bias)[0], f(a_log)[0],
                                              f(d_skip)[0], f(ssd_norm_w)[0], f(mem_norm_w)[0], f(final_norm_w))
    nc, _ = build()
    shared = {"w_in": f(w_in)[0], "w_kv": f(w_mem_kv)[0], "w_out": f(w_out)[0], "cstb": cb, "cstf": cf,
              "kconst": kc, "qconst": qc}
    in_maps = []
    for c in range(8):
        m = dict(shared)
        m["x"] = x[c * NSEQ:(c + 1) * NSEQ]
        m["mem"] = mem[c * NSEQ:(c + 1) * NSEQ]
        in_maps.append(m)
    res = run_bass_kernel_spmd(nc, in_maps, core_ids=list(range(8)))
    return np.concatenate([r["out"] for r in res.results], axis=0)
```

```python
from contextlib import ExitStack
import concourse.bass as bass
import concourse.mybir as mybir

F32 = mybir.dt.float32
BF16 = mybir.dt.bfloat16
I32 = mybir.dt.int32
AF = mybir.ActivationFunctionType
ALU = mybir.AluOpType
AX = mybir.AxisListType
ESZ = {F32: 4, BF16: 2, I32: 4}


class View:
    __slots__ = ("ap", "arena", "rngs", "pr")

    def __init__(self, ap, arena, rngs, pr=(0, 128)):
        self.ap, self.arena, self.rngs, self.pr = ap, arena, rngs, pr

    def re(self, s, **kw):
        return View(self.ap.rearrange(s, **kw), self.arena, self.rngs, self.pr)

    def bc(self, shape):
        return View(self.ap.broadcast_to(shape), self.arena, self.rngs, self.pr)

    def __getitem__(self, key):
        return View(self.ap[key], self.arena, self.rngs, self.pr)


class Arena:
    def __init__(self, name, t, dtype, ncols, const=False):
        self.name, self.t, self.dtype, self.ncols = name, t, dtype, ncols
        self.esz = ESZ[dtype]
        self.w = []
        self.r = []
        self.const = const
        self.alt = {dtype: t}
        self.psum = False
        self.qlast = [dict() for _ in range(4)]

    def v(self, lo=0, n=None, p0=0, p1=128):
        if n is None:
            n = self.ncols - lo
        assert 0 <= lo and lo + n <= self.ncols, (self.name, lo, n, self.ncols)
        return View(self.t[p0:p1, lo:lo + n], self, [(lo * self.esz, (lo + n) * self.esz)], (p0, p1))

    def v3(self, dtype, nk, stride, lo, n, p0=0, p1=128):
        if dtype not in self.alt:
            self.alt[dtype] = self.t.bitcast(dtype)
        e = ESZ[dtype]
        assert ((nk - 1) * stride + lo + n) * e <= self.ncols * self.esz
        total = self.ncols * self.esz // e
        o = max(0, lo + nk * stride - total)
        assert o <= stride - n and o <= lo, (self.name, lo, nk, stride, n, total)
        ws = lo - o
        ap = self.alt[dtype][p0:p1, ws:ws + nk * stride].rearrange("p (k s) -> p k s", s=stride)[:, :, o:o + n]
        return View(ap, self, [((k * stride + lo) * e, (k * stride + lo + n) * e) for k in range(nk)], (p0, p1))

    def vb(self, dtype, lo, n, p0=0, p1=128):
        if dtype not in self.alt:
            self.alt[dtype] = self.t.bitcast(dtype)
        e = ESZ[dtype]
        assert (lo + n) * e <= self.ncols * self.esz
        return View(self.alt[dtype][p0:p1, lo:lo + n], self, [(lo * e, (lo + n) * e)], (p0, p1))


class Sub:
    def __init__(self, arena, dtype, boff, n):
        self.a, self.dt, self.e, self.n = arena, dtype, ESZ[dtype], n
        assert boff % self.e == 0
        self.base = boff // self.e

    def v(self, lo=0, n=None, p0=0, p1=128):
        if n is None:
            n = self.n - lo
        assert 0 <= lo and lo + n <= self.n, (lo, n, self.n)
        return self.a.vb(self.dt, self.base + lo, n, p0, p1)

    def v3(self, nk, stride, lo, n, p0=0, p1=128):
        assert (nk - 1) * stride + lo + n <= self.n
        return self.a.v3(self.dt, nk, stride, self.base + lo, n, p0, p1)


class Bump:
    def __init__(self, arena, boff, size):
        self.a, self.off, self.end = arena, boff, boff + size

    def get(self, dtype, n):
        self.off = (self.off + 3) // 4 * 4
        s = Sub(self.a, dtype, self.off, n)
        self.off += n * ESZ[dtype]
        assert self.off <= self.end, ("bump overflow", self.off, self.end)
        return s


def D(ap):
    return View(ap, None, [])


COMPUTE = ("pe", "act", "dve", "pool")


class Prog:
    def __init__(self, nc, ndma=40):
        self.nc = nc
        self.ops = {e: [] for e in COMPUTE + ("sp",)}
        self.count = {e: 0 for e in COMPUTE}
        self.waited = {e: {} for e in COMPUTE + ("sp",)}
        self.ndma = ndma
        self.dma_cnt = [0] * ndma
        self.dma_next = 0
        self.nops = 0

    def op(self, eng, fn, reads=(), writes=(), dma=False):
        deps = {}

        def add(tok):
            k, v = tok
            if deps.get(k, 0) < v:
                deps[k] = v

        for vw in reads:
            a = vw.arena
            if a is None:
                continue
            for (lo, hi) in vw.rngs:
                for (l, h, t) in a.w:
                    if l < hi and lo < h:
                        add(t)
        for vw in writes:
            a = vw.arena
            if a is None:
                continue
            assert not a.const, a.name
            for (lo, hi) in vw.rngs:
                for (l, h, t) in a.w:
                    if l < hi and lo < h:
                        add(t)
                for (l, h, t) in a.r:
                    if l < hi and lo < h:
                        add(t)
        for vw in list(reads) + list(writes):
            a = vw.arena
            if a is None or not a.psum:
                continue
            for q in range(vw.pr[0] // 32, (vw.pr[1] + 31) // 32):
                for e2, t in a.qlast[q].items():
                    if e2 != eng:
                        add(t)
        if dma:
            i = self.dma_next
            self.dma_next = (i + 1) % self.ndma
            if self.dma_cnt[i] > 0:
                add((("d", i), self.dma_cnt[i]))
            self.dma_cnt[i] += 16
            tok = (("d", i), self.dma_cnt[i])
            inc = (("d", i), 16)
        else:
            self.count[eng] += 1
            tok = (eng, self.count[eng])
            inc = (eng, 1)
        waits = []
        wd = self.waited[eng]
        for k, v in deps.items():
            if k == eng and eng == "pe":
                continue
            if wd.get(k, 0) >= v:
                continue
            wd[k] = v
            waits.append((k, v))
        self.ops[eng].append((waits, fn, inc))
        self.nops += 1
        for vw in list(reads) + list(writes):
            a = vw.arena
            if a is None or not a.psum:
                continue
            for q in range(vw.pr[0] // 32, (vw.pr[1] + 31) // 32):
                a.qlast[q][eng] = tok
        for vw in reads:
            a = vw.arena
            if a is None or a.const:
                continue
            for (lo, hi) in vw.rngs:
                if not dma:
                    a.r = [x for x in a.r if not (x[2][0] == eng and lo <= x[0] and x[1] <= hi)]
                a.r.append((lo, hi, tok))
        for vw in writes:
            a = vw.arena
            if a is None:
                continue
            for (lo, hi) in vw.rngs:
                a.w = [x for x in a.w if not (lo <= x[0] and x[1] <= hi)]
                a.r = [x for x in a.r if not (lo <= x[0] and x[1] <= hi)]
                a.w.append((lo, hi, tok))
        return tok

    def freeze(self, arena):
        arena.const = True
        arena.r = []

    def matmul(self, out, lhsT, rhs, start=True, stop=True, **kw):
        return self.op("pe", lambda e: e.matmul(out.ap, lhsT.ap, rhs.ap, start=start, stop=stop, **kw),
                       [lhsT, rhs] + ([] if start else [out]), [out])

    def transpose(self, out, in_, ident):
        return self.op("pe", lambda e: e.transpose(out.ap, in_.ap, ident.ap), [in_, ident], [out])

    def act(self, out, in_, func, bias=None, scale=None, accum_out=None):
        rd = [in_]
        kw = {}
        if bias is not None:
            if isinstance(bias, View):
                rd.append(bias)
                kw["bias"] = bias.ap
            else:
                kw["bias"] = bias
        if scale is not None:
            if isinstance(scale, View):
                rd.append(scale)
                kw["scale"] = scale.ap
            else:
                kw["scale"] = scale
        wr = [out]
        if accum_out is not None:
            wr.append(accum_out)
            kw["accum_out"] = accum_out.ap
        return self.op("act", lambda e: e.activation(out.ap, in_.ap, func, **kw), rd, wr)

    def tt(self, out, in0, in1, op, eng="dve"):
        return self.op(eng, lambda e: e.tensor_tensor(out.ap, in0.ap, in1.ap, op), [in0, in1], [out])

    def ts(self, out, in0, s1, s2, op0, op1=None, eng="dve", accum_out=None):
        rd = [in0]
        a1 = s1
        a2 = s2
        if isinstance(s1, View):
            rd.append(s1)
            a1 = s1.ap
        if isinstance(s2, View):
            rd.append(s2)
            a2 = s2.ap
        kw = {}
        if op1 is not None:
            kw["op1"] = op1
        wr = [out]
        if accum_out is not None:
            kw["accum_out"] = accum_out.ap
            wr.append(accum_out)
        return self.op(eng, lambda e: e.tensor_scalar(out.ap, in0.ap, a1, a2, op0, **kw), rd, wr)

    def stt(self, out, in0, scalar, in1, op0, op1):
        rd = [in0, in1]
        sc = scalar
        if isinstance(scalar, View):
            rd.append(scalar)
            sc = scalar.ap
        return self.op("dve", lambda e: e.scalar_tensor_tensor(out.ap, in0.ap, sc, in1.ap, op0, op1), rd, [out])

    def copy(self, out, in_, eng="dve"):
        if eng == "act":
            return self.op("act", lambda e: e.copy(out.ap, in_.ap), [in_], [out])
        return self.op(eng, lambda e: e.tensor_copy(out.ap, in_.ap), [in_], [out])

    def reduce(self, out, in_, op, axis=AX.X, eng="dve"):
        return self.op(eng, lambda e: e.tensor_reduce(out.ap, in_.ap, axis, op), [in_], [out])

    def recip(self, out, in_):
        return self.op("dve", lambda e: e.reciprocal(out.ap, in_.ap), [in_], [out])

    def memset(self, out, val, eng="dve"):
        return self.op(eng, lambda e: e.memset(out.ap, val), [], [out])

    def scan(self, out, d0, d1, initial, op0, op1):
        rd = [d0, d1]
        ini = initial
        if isinstance(initial, View):
            rd.append(initial)
            ini = initial.ap
        return self.op("dve", lambda e: e.tensor_tensor_scan(out.ap, d0.ap, d1.ap, ini, op0, op1), rd, [out])

    def affine_select(self, out, in_, pattern, cmp, fill, base, cm):
        return self.op("pool", lambda e: e.affine_select(out.ap, in_.ap, pattern, cmp, fill, base=base,
                                                         channel_multiplier=cm), [in_], [out])

    def iota(self, out, pattern, base, cm):
        return self.op("pool", lambda e: e.iota(out.ap, pattern, base=base, channel_multiplier=cm), [], [out])

    def dma(self, out, in_, q="sp", **kw):
        if q == "pool":
            kw.setdefault("max_dma_last_dim", 4096)
        return self.op(q, lambda e: e.dma_start(out=out.ap, in_=in_.ap, **kw), [in_], [out], dma=True)

    def emit(self):
        nc = self.nc
        with ExitStack() as es:
            sems = {}
            for e in COMPUTE:
                sems[e] = es.enter_context(nc.semaphore("s_" + e))
            for i in range(self.ndma):
                sems[("d", i)] = es.enter_context(nc.semaphore("s_d%d" % i))
            fin = []
            for i in range(self.ndma):
                if self.dma_cnt[i] > 0:
                    fin.append((("d", i), self.dma_cnt[i]))
            for e in COMPUTE:
                if self.count[e] > 0:
                    fin.append((e, self.count[e]))
            block = es.enter_context(nc.Block())

            def run(stream, final=None):
                def f(eng):
                    for waits, fn, inc in stream:
                        for k, v in waits:
                            eng.wait_ge(sems[k], v)
                        ins = fn(eng)
                        ins.then_inc(sems[inc[0]], inc[1])
                    if final:
                        for k, v in final:
                            eng.wait_ge(sems[k], v)
                return f

            block.tensor(run(self.ops["pe"]))
            block.scalar(run(self.ops["act"]))
            block.vector(run(self.ops["dve"]))
            block.gpsimd(run(self.ops["pool"]))
            block.sync(run(self.ops["sp"], fin))
from concourse.bass_utils import run_bass_kernel_spmd
import numpy as np

T = 2048
DM = 1024
NSEQ = 2
EPS = 1e-6
BIG = 30000.0
C_Q, C_K, C_V, C_G, C_Z, C_XBC, C_DT, C_QM, C_GM = 0, 512, 1024, 1536, 2048, 3072, 5120, 5136, 5648
CB_ID, CB_TRI, CB_MNEG, CB_ONES, CB_EH, NCB = 0, 128, 256, 384, 512, 2560
CF_ID, CF_ONES, CF_PRM, CF_CAP, CF_CMK, CF_OWN, CF_MISC, NCF = 0, 128, 256, 1424, 1488, 1552, 1616, 1624
P_NW, P_MNW, P_SNW, P_CVB, P_CVW, P_DSK, P_DTB, P_ALOG, P_FNW = 0, 8, 16, 24, 40, 104, 112, 128, 144
NPRM = 1168


def host_consts():
    cb = np.zeros((128, NCB), np.float32)
    cb[:, CB_ID:CB_ID + 128] = np.eye(128)
    s = np.arange(128)[:, None]
    t = np.arange(128)[None, :]
    cb[:, CB_TRI:CB_TRI + 128] = (t >= s)
    cb[:, CB_MNEG:CB_MNEG + 128] = np.where(t < s, -BIG, 0.0)
    cb[:, CB_ONES:CB_ONES + 128] = 1.0
    for h in range(16):
        cb[h, CB_EH + h * 128: CB_EH + (h + 1) * 128] = 1.0
    cf = np.zeros((128, NCF), np.float32)
    cf[:, CF_ID:CF_ID + 128] = np.eye(128)
    cf[:, CF_ONES:CF_ONES + 128] = 1.0
    cap = np.zeros((8, 8), np.float32)
    cmk = np.zeros((8, 8), np.float32)
    own = np.zeros((8, 8), np.float32)
    for ti in range(8):
        qb = 4 + ti // 2
        for j in range(8):
            cap[ti, j] = 1e30 if j < qb else -1e30
            cmk[ti, j] = 1.0 if j < qb else 0.0
            own[ti, j] = 1.0 if j == qb else 0.0
    cf[:, CF_CAP:CF_CAP + 64] = cap.reshape(-1)[None, :]
    cf[:, CF_CMK:CF_CMK + 64] = cmk.reshape(-1)[None, :]
    cf[:, CF_OWN:CF_OWN + 64] = own.reshape(-1)[None, :]
    cf[:, CF_MISC] = EPS
    cf[:, CF_MISC + 1] = 1.0
    kc = np.zeros((128, T), np.float32)
    pos = np.arange(T)
    for r in range(8):
        kc[64 + r] = (pos // 256 == r)
    kc[96] = pos // 16
    kc[97] = pos % 16
    kc[98] = 1.0
    kc[99] = 1.0
    qc = np.zeros((8, 4, T), np.float32)
    for h in range(8):
        sl = 2.0 ** (-8.0 * (h + 1) / 8)
        qc[h, 0] = 16 * sl * 8
        qc[h, 1] = sl * 8
        qc[h, 2] = -16 * sl * 8 * (pos // 16)
        qc[h, 3] = -sl * 8 * (pos % 16)
    return cb, cf, kc, qc


def host_params(norm_w, conv_w, conv_b, dt_bias, a_log, d_skip, ssd_norm_w, mem_norm_w, final_norm_w):
    prm = np.zeros((128, NPRM), np.float32)
    prm[:, P_NW:P_NW + 8] = norm_w.reshape(8, 128).T
    prm[:, P_MNW:P_MNW + 8] = mem_norm_w.reshape(8, 128).T
    prm[:, P_SNW:P_SNW + 8] = ssd_norm_w.reshape(8, 128).T
    prm[:, P_CVB:P_CVB + 16] = conv_b.reshape(16, 128).T
    prm[:, P_CVW:P_CVW + 64] = conv_w.T.reshape(16, 128, 4).transpose(1, 0, 2).reshape(128, 64)
    prm[:, P_DSK:P_DSK + 8] = np.repeat(d_skip, 64).reshape(8, 128).T
    prm[:, P_DTB:P_DTB + 16] = dt_bias[None, :]
    prm[:, P_ALOG:P_ALOG + 16] = a_log[None, :]
    prm[:, P_FNW:P_FNW + 1024] = final_norm_w[None, :]
    return prm
def build(nseq=NSEQ, dump=None, phases="ASBME"):
    nc = bass.Bass("TRN2", target_bir_lowering=False)
    x_d = nc.dram_tensor("x", [nseq, T, DM], F32, kind="ExternalInput").ap()
    mem_d = nc.dram_tensor("mem", [nseq, 256, DM], F32, kind="ExternalInput").ap()
    win_d = nc.dram_tensor("w_in", [DM, 6160], F32, kind="ExternalInput").ap()
    wkv_d = nc.dram_tensor("w_kv", [DM, 1024], F32, kind="ExternalInput").ap()
    wout_d = nc.dram_tensor("w_out", [2048, DM], F32, kind="ExternalInput").ap()
    cb_d = nc.dram_tensor("cstb", [128, NCB], F32, kind="ExternalInput").ap()
    cf_d = nc.dram_tensor("cstf", [128, NCF], F32, kind="ExternalInput").ap()
    kc_d = nc.dram_tensor("kconst", [128, T], F32, kind="ExternalInput").ap()
    qc_d = nc.dram_tensor("qconst", [8, 4, T], F32, kind="ExternalInput").ap()
    out_d = nc.dram_tensor("out", [nseq, T, DM], F32, kind="ExternalOutput").ap()
    dumps = {}
    wsi_d = nc.dram_tensor("wsc_in", [DM, 6160], BF16, kind="Internal").ap()
    wskv_d = nc.dram_tensor("wsc_kv", [DM, 1024], BF16, kind="Internal").ap()
    wso_d = nc.dram_tensor("wsc_out", [2048, DM], BF16, kind="Internal").ap()
    wsi3 = wsi_d.rearrange("(k p) c -> p k c", p=128)
    wskv3 = wskv_d.rearrange("(k p) c -> p k c", p=128)
    wso3 = wso_d.rearrange("(k p) c -> p k c", p=128)
    win3 = win_d.rearrange("(k p) c -> p k c", p=128)
    wkv3 = wkv_d.rearrange("(k p) c -> p k c", p=128)
    wout3 = wout_d.rearrange("(k p) c -> p k c", p=128)

    with ExitStack() as es:
        def sb(name, n, dt):
            t = es.enter_context(nc.sbuf_tensor(name, [128, n], dt))
            return Arena(name, t, dt, n)

        UT = sb("UT", 8 * T, BF16)
        MIX = sb("MIX", 16 * T, BF16)
        WB = sb("WB", 8 * 3088, BF16)
        WKSZ = 47616
        WK = sb("WK", WKSZ // 2, BF16)
        CB = sb("CB", NCB, BF16)
        CF = sb("CF", NCF, F32)
        SM = sb("SM", 64, F32)
        PS = []
        for i in range(8):
            t = es.enter_context(nc.psum_tensor("ps%d" % i, [128, 512], F32))
            PS.append(Arena("ps%d" % i, t, F32, 512))
            PS[-1].psum = True
        p = Prog(nc)

        def dbg(name, view, shape):
            if dump is None or name not in dump:
                return
            d = nc.dram_tensor("dbg_" + name, list(shape), view.ap.dtype, kind="ExternalOutput").ap()
            dumps[name] = d
            p.dma(D(d), view)

        p.dma(CB.v(), D(cb_d), q="pool")
        p.dma(CF.v(), D(cf_d))
        ident = CB.v(CB_ID, 128)
        tri = CB.v(CB_TRI, 128)
        mneg = CB.v(CB_MNEG, 128)
        ones = CB.v(CB_ONES, 128)
        identf = CF.v(CF_ID, 128)
        epsv = CF.v(CF_MISC, 1)
        onev = CF.v(CF_MISC + 1, 1)

        def prm(off, n, p0=0, p1=128):
            return CF.v(CF_PRM + off, n, p0, p1)

        AB = SM.v(0, 16)
        p.act(AB, prm(P_ALOG, 16), AF.Exp)
        p.ts(AB, AB, -1.0, None, ALU.mult)

        SC = {n: Arena("sc_" + n, None, BF16, 1) for n in ("attn", "mem", "ssd", "kv", "out")}
        converted = [False]

        def scv(ap, name, k):
            return View(ap, SC[name], [(k, k + 1)])

        def convert_all():
            for name, src3, dst3, c0, nw, nk in (("attn", win3, wsi3, 0, 2048, 8), ("kv", wkv3, wskv3, 0, 1024, 8),
                                                 ("mem", win3, wsi3, C_QM, 1024, 8), ("out", wout3, wso3, 0, 1024, 16),
                                                 ("ssd", win3, wsi3, C_Z, 3088, 8)):
                for k in range(nk):
                    p.dma(scv(dst3[:, k, c0:c0 + nw], name, k), D(src3[:, k, c0:c0 + nw]), q="pool")
            converted[0] = True

        def load_w(name, dst3, src3, c0, nw, nk, wb_off=0):
            for k in range(nk):
                if converted[0]:
                    p.dma(WB.v(wb_off + k * nw, nw), scv(dst3[:, k, c0:c0 + nw], name, k), q="sp")
                else:
                    p.dma(WB.v(wb_off + k * nw, nw), D(src3[:, k, c0:c0 + nw]), q="pool")

        pj_i = [0]

        def rms_transpose(src_tile, ntiles, nwoff, dst_fn, wk):
            XT = [wk.get(F32, 1024) for _ in range(2)]
            XS = [wk.get(BF16, 1024) for _ in range(2)]
            JNK = wk.get(BF16, 1024)
            SSQ = wk.get(F32, 16)
            RSTD = wk.get(F32, 16)
            for i in range(ntiles):
                xt = XT[i % 2]
                p.dma(xt.v(), D(src_tile(i)))
                p.act(JNK.v(), xt.v(), AF.Square, accum_out=SSQ.v(i, 1))
                p.act(RSTD.v(i, 1), SSQ.v(i, 1), AF.Ln, scale=1.0 / DM, bias=epsv)
                p.act(RSTD.v(i, 1), RSTD.v(i, 1), AF.Exp, scale=-0.5)
                xs = XS[i % 2]
                p.ts(xs.v(), xt.v(), RSTD.v(i, 1), None, ALU.mult)
                pb = PS[i % 2]
                for k in range(8):
                    p.transpose(pb.vb(BF16, k * 128, 128), xs.v(k * 128, 128), ident)
                p.tt(dst_fn(i), pb.vb(BF16, 0, 1024).re("p (k t) -> p k t", k=8),
                     prm(nwoff, 8).re("p (k o) -> p k o", o=1).bc([128, 8, 128]), ALU.mult)

        def utv(k, t0, n):
            return UT.v(k * T + t0, n)

        def proj_fm(nw, c0, ncols, t0, n, ps_view):
            for k in range(8):
                p.matmul(ps_view, WB.v(k * nw + c0, ncols), utv(k, t0, n), start=(k == 0), stop=(k == 7))

        def phase_ssd(b):
            NW = 3088
            load_w("ssd", wsi3, win3, C_Z, NW, 8)
            b1 = Bump(MIX, 0, 16384)
            b2 = Bump(MIX, 12 * T * 2, 16384)
            wk = Bump(WK, 0, WKSZ)
            XPRE = b1.get(BF16, 16 * 259)
            XST = b1.get(BF16, 8 * 256)
            BMT = b1.get(BF16, 4 * 256)
            LT = [b1.get(BF16, 384) for _ in range(2)]
            SZ = b2.get(BF16, 8 * 256)
            PREV = b2.get(F32, 1024)
            XDD = b2.get(BF16, 2 * 1024)
            CMT = b2.get(BF16, 4 * 256)
            BTOK = b2.get(BF16, 2 * 512)
            DIAG = wk.get(BF16, 64 * 128)
            CBT = wk.get(BF16, 4 * 384)
            L0 = [wk.get(BF16, 256) for _ in range(2)]
            MT = [wk.get(BF16, 384) for _ in range(2)]
            GT = [wk.get(BF16, 256) for _ in range(2)]
            GG = [wk.get(F32, 256) for _ in range(4)]
            GSQ = [wk.get(BF16, 256) for _ in range(4)]
            RS = wk.get(F32, 256)
            XDT = wk.get(BF16, 2 * 2048)
            PREVB = wk.get(BF16, 2048)
            DTR = wk.get(F32, 32)
            DT = wk.get(F32, 32)
            DA = wk.get(F32, 256)
            NCS = wk.get(F32, 32)
            DEC = wk.get(F32, 32)
            W2 = wk.get(F32, 32)
            CST = wk.get(F32, 256)
            CSH = wk.get(BF16, 256)
            CSL = wk.get(BF16, 256)
            DECT = wk.get(F32, 256)
            DG16 = wk.get(F32, 16)
            CDB = wk.get(F32, 16)

            for zt in (XDT, PREVB, DA, CST, CSH, CSL, DECT, DG16):
                p.memset(zt.v(), 0.0, eng="pool")
            for j in range(64):
                p.ts(DIAG.v(j * 128, 128), ident, prm(P_CVW + j, 1), None, ALU.mult,
                     eng=("pool" if j % 2 else "dve"))
            p.memset(XPRE.v3(16, 259, 0, 3), 0.0)
            p.memset(PREV.v(), 0.0)

            for c in range(8):
                t0 = c * 256
                if c == 1 and not converted[0]:
                    convert_all()

                def zgroup(zc):
                    ps = PS[zc % 2].v((zc // 2 % 2) * 256, 256)
                    proj_fm(NW, zc * 128, 128, t0, 256, ps)
                    p.copy(SZ.v(zc * 256, 256), ps, eng="dve")

                def xgroup(cc):
                    ps = PS[cc % 2].v((cc // 2 % 2) * 256, 256)
                    proj_fm(NW, 1024 + cc * 128, 128, t0, 256, ps)
                    p.copy(XPRE.v(cc * 259 + 3, 256), ps, eng=("dve" if cc % 2 else "act"))

                if c > 0:
                    p.copy(XPRE.v3(16, 259, 0, 3), XPRE.v3(16, 259, 256, 3), eng="pool")
                for lt in range(2):
                    for k in range(8):
                        p.matmul(PS[5].v(384 + lt * 16, 16), utv(k, t0 + lt * 128, 128), WB.v(k * NW + 3072, 16),
                                 start=(k == 0), stop=(k == 7))
                p.tt(DTR.v(), PS[5].v(384, 32).re("p (l h) -> p l h", l=2),
                     prm(P_DTB, 16).re("p (o h) -> p o h", o=1).bc([128, 2, 16]), ALU.add)
                p.act(DTR.v(), DTR.v(), AF.Exp)
                p.act(DT.v(), DTR.v(), AF.Ln, bias=onev)
                p.tt(DA.v3(2, 128, 0, 16), DT.v().re("p (l h) -> p l h", l=2),
                     SM.v(0, 16).re("p (o h) -> p o h", o=1).bc([128, 2, 16]), ALU.mult)
                for zc in range(6):
                    zgroup(zc)
                for lt in range(2):
                    p.transpose(PS[6].v(lt * 128, 128), DA.v(lt * 128, 128), identf)
                p.scan(CST.v(0, 256, 0, 16), CF.v(CF_ONES, 1, 0, 16).bc([16, 256]), PS[6].v(0, 256, 0, 16), 0.0,
                       ALU.mult, ALU.add)
                p.copy(CSH.v(0, 256, 0, 16), CST.v(0, 256, 0, 16))
                p.tt(CSL.v(0, 256, 0, 16), CST.v(0, 256, 0, 16), CSH.v(0, 256, 0, 16), ALU.subtract)
                p.act(DECT.v(0, 256, 0, 16), CST.v(0, 256, 0, 16), AF.Exp, scale=-1.0, bias=CST.v(255, 1, 0, 16))
                for zc in range(6, 8):
                    zgroup(zc)
                for cc in range(8, 12):
                    xgroup(cc)
                for lt in range(2):
                    p.transpose(PS[7].v(lt * 128, 128), CST.v(lt * 128, 128), identf)
                    p.transpose(PS[7].v(256 + lt * 128, 128), DECT.v(lt * 128, 128), identf)
                p.ts(NCS.v().re("p (l h) -> p l h", l=2), PS[7].v3(F32, 2, 128, 0, 16), -1.0, None, ALU.mult)
                p.copy(DEC.v().re("p (l h) -> p l h", l=2), PS[7].v3(F32, 2, 128, 256, 16))
                p.tt(W2.v(), DT.v(), DEC.v(), ALU.mult)
                if c < 7:
                    p.ts(DG16.v(0, 16, 0, 16), CF.v(CF_ID, 16, 0, 16), CST.v(255, 1, 0, 16), None, ALU.mult)
                    p.matmul(PS[5].v(480, 16), CF.v(CF_ONES, 128), DG16.v())
                    p.act(CDB.v(), PS[5].v(480, 16), AF.Exp)
                for cc in list(range(12, 16)) + list(range(0, 8)):
                    xgroup(cc)

                def conv(cc):
                    ps = PS[cc % 2].v((cc // 2 % 2) * 256, 256)
                    for k in range(4):
                        p.matmul(ps, DIAG.v((cc * 4 + k) * 128, 128), XPRE.v(cc * 259 + k, 256),
                                 start=(k == 0), stop=(k == 3))
                    if cc < 8:
                        dst = XST.v(cc * 256, 256)
                    elif cc < 12:
                        dst = BMT.v((cc - 8) * 256, 256)
                    else:
                        dst = CMT.v((cc - 12) * 256, 256)
                    p.act(dst, ps, AF.Silu, bias=prm(P_CVB + cc, 1))

                for cc in range(8, 16):
                    conv(cc)
                for lt in range(2):
                    pb = PS[7]
                    for g in range(4):
                        p.transpose(pb.vb(BF16, g * 128, 128), BMT.v(g * 256 + lt * 128, 128), ident)
                    p.copy(BTOK.v(lt * 512, 512), pb.vb(BF16, 0, 512), eng="dve")
                for g in range(4):
                    ps = PS[5]
                    p.matmul(ps.v(0, 256), BMT.v(g * 256, 128), CMT.v(g * 256, 256))
                    p.matmul(ps.v(256, 128), BMT.v(g * 256 + 128, 128), CMT.v(g * 256 + 128, 128))
                    p.copy(CBT.v(g * 384, 384), ps.v(0, 384), eng="dve")
                for cc in range(0, 8):
                    conv(cc)
                p.act(SZ.v(), SZ.v(), AF.Silu)
                for lt in range(2):
                    pb = PS[6]
                    for cc in range(8):
                        p.transpose(pb.vb(BF16, cc * 128, 128), XST.v(cc * 256 + lt * 128, 128), ident)
                    src = pb.vb(BF16, 0, 1024).re("p (h q) -> p h q", h=16)
                    for hh in range(2):
                        p.tt(XDT.v(lt * 2048, 2048).re("p (c e q) -> p c e q", c=8, e=2)[:, :, hh, hh * 64:hh * 64 + 64],
                             src.re("p (c e) q -> p c e q", e=2)[:, :, hh, :],
                             DT.v(lt * 16, 16).re("p (c e o) -> p c e o", e=2, o=1)[:, :, hh, :].bc([128, 8, 64]),
                             ALU.mult)
                    p.tt(XDD.v(lt * 1024, 1024).re("p (h q) -> p h q", h=16), src,
                         W2.v(lt * 16, 16).re("p (h o) -> p h o", o=1).bc([128, 16, 64]), ALU.mult)
                def seg_stage(h):
                    g = h // 4
                    sg = PS[2 + h % 2]
                    eh = CB.v(CB_EH + h * 128, 128)
                    csh = lambda lo, n: CSH.v(lo, n)
                    csl = lambda lo, n: CSL.v(lo, n)
                    p.matmul(sg.v(0, 256), eh, csh(0, 256), start=True, stop=False)
                    p.matmul(sg.v(0, 256), eh, csl(0, 256), start=False, stop=True)
                    p.matmul(sg.v(256, 128), eh, csh(0, 128), start=True, stop=False)
                    p.matmul(sg.v(256, 128), eh, csl(0, 128), start=False, stop=False)
                    p.matmul(sg.v(256, 128), ident, mneg, start=False, stop=True)
                    p.matmul(sg.v(384, 128), eh, csh(128, 128), start=True, stop=False)
                    p.matmul(sg.v(384, 128), eh, csl(128, 128), start=False, stop=False)
                    p.matmul(sg.v(384, 128), ident, mneg, start=False, stop=True)
                    l0 = L0[h % 2]
                    lt_ = LT[h % 2]
                    mt = MT[h % 2]
                    gt = GT[h % 2]
                    if c > 0:
                        p.act(l0.v(), sg.v(0, 256), AF.Exp)
                    p.act(lt_.v(0, 128), sg.v(256, 128), AF.Exp, bias=NCS.v(h, 1))
                    p.act(lt_.v(128, 128), sg.v(128, 128), AF.Exp, bias=NCS.v(h, 1))
                    p.act(lt_.v(256, 128), sg.v(384, 128), AF.Exp, bias=NCS.v(16 + h, 1))
                    p.tt(mt.v(), lt_.v(), CBT.v(g * 384, 384), ALU.mult)
                    if c > 0:
                        p.tt(gt.v(), l0.v(), CMT.v(g * 256, 256), ALU.mult, eng="pool")

                def y_stage(h):
                    g = h // 4
                    pr = h % 2
                    pc = h // 2
                    mt = MT[h % 2]
                    gt = GT[h % 2]
                    ybank = PS[4 + 2 * (pc % 2)]
                    yt = ybank.v(0, 256)
                    yt2 = ybank.v(128, 128)
                    p.matmul(yt, XDT.v(h * 128, 128), mt.v(0, 256), start=(pr == 0), stop=False)
                    p.matmul(yt2, XDT.v(2048 + h * 128, 128), mt.v(256, 128), start=False, stop=(c == 0 and pr == 1))
                    if c > 0:
                        p.matmul(yt, PREVB.v(h * 128, 128), gt.v(), start=False, stop=(pr == 1))
                    if pr == 1:
                        gg = GG[pc % 4]
                        p.stt(gg.v(), XST.v(pc * 256, 256), prm(P_DSK + pc, 1), yt, ALU.mult, ALU.add)
                        p.tt(gg.v(), gg.v(), SZ.v(pc * 256, 256), ALU.mult)
                        p.act(GSQ[pc % 4].v(), gg.v(), AF.Square)
                def norm_stage(g):
                        pc = 2 * g + 1
                        pcs = [pc - 1, pc]
                        ss = PS[7].v(256, 256)
                        p.matmul(ss, ones, GSQ[pcs[0] % 4].v(), start=True, stop=False)
                        p.matmul(ss, ones, GSQ[pcs[1] % 4].v(), start=False, stop=True)
                        p.act(RS.v(), ss, AF.Ln, scale=1.0 / 256, bias=epsv)
                        p.act(RS.v(), RS.v(), AF.Exp, scale=-0.5)
                        for q in pcs:
                            p.stt(MIX.v((4 + q) * T + t0, 256), GG[q % 4].v(), prm(P_SNW + q, 1), RS.v(),
                                  ALU.mult, ALU.mult)

                for h in range(18):
                    if h < 16:
                        seg_stage(h)
                    if 1 <= h <= 16:
                        y_stage(h - 1)
                    if h >= 2 and (h - 2) % 4 == 3:
                        norm_stage((h - 2) // 4)
                if c < 7:
                    for g in range(4):
                        st = PS[7].v(0, 256)
                        for lt in range(2):
                            p.matmul(st, BTOK.v(lt * 512 + g * 128, 128), XDD.v(lt * 1024 + g * 256, 256),
                                     start=(lt == 0), stop=(lt == 1))
                        pv = PREV.v(g * 256, 256)
                        p.tt(pv.re("p (h q) -> p h q", h=4), pv.re("p (h q) -> p h q", h=4),
                             CDB.v(g * 4, 4).re("p (h o) -> p h o", o=1).bc([128, 4, 64]), ALU.mult)
                        p.tt(pv, pv, st, ALU.add)
                        for hh in range(2):
                            p.copy(PREVB.v(g * 512, 512).re("p (c e q) -> p c e q", c=2, e=2)[:, :, hh, hh * 64:hh * 64 + 64],
                                   pv.re("p (c e q) -> p c e q", c=2, e=2)[:, :, hh, :], eng="pool")
            dbg("ssd", MIX.v(4 * T, 8 * T), [128, 8 * T])

        def attn_core(kt_list_fn, nq, kv_lhsT, q_rhs, v_lhsT, scale, PTs, epilogue, diag_fn=None, den_lhsT=None):
            pass

        def phase_attn(b):
            NW = 2048
            load_w("attn", wsi3, win3, 0, NW, 8)
            wk = Bump(WK, 0, WKSZ)
            bm = Bump(MIX, 12 * T * 2, 16384)
            bw = Bump(WB, 8 * NW * 2, WB.ncols * 2 - 8 * NW * 2)
            sets = [dict(QK=[wk.get(BF16, T) for _ in range(4)], VA=wk.get(BF16, 2048), VB=wk.get(BF16, 2048),
                         SG=wk.get(BF16, T)),
                    dict(QK=[bm.get(BF16, T) for _ in range(4)], VA=bw.get(BF16, 2048), VB=bw.get(BF16, 2048),
                         SG=bw.get(BF16, T))]
            PT = [wk.get(BF16, 512) for _ in range(3)]
            R = [wk.get(F32, 512) for _ in range(2)]
            T1 = [wk.get(F32, 512) for _ in range(2)]
            PENB = wk.get(BF16, 8 * 128)
            GM = wk.get(F32, 64)
            CMP = wk.get(F32, 512)
            RANK = wk.get(F32, 64)
            KSUM = wk.get(F32, 8)
            KMT = wk.get(BF16, 8)
            p.memset(KMT.v(), 0.0, eng="pool")
            p.memset(PENB.v(), 0.0, eng="pool")
            for S in sets:
                for i in range(2):
                    p.memset(S["QK"][i].v(), 0.0, eng="pool")
                    p.dma(S["QK"][2 + i].v(), D(kc_d), q="pool")
                p.memset(S["VA"].v(), 1.0, eng="pool")
                p.memset(S["VB"].v(), 1.0, eng="pool")

            def make_thunks(hp, S):
                QK, VA, VB, SG = S["QK"], S["VA"], S["VB"], S["SG"]
                th = []

                def t_qc():
                    for hh in range(2):
                        p.dma(QK[hh].v(0, T, 96, 100), D(qc_d[2 * hp + hh]), q="pool")
                th.append(t_qc)
                def half_proj(c0, tq, half, evac):
                    def f():
                        ps = PS[2 + tq % 2]
                        if half == 1:
                            for k in range(8):
                                p.matmul(ps.v(), WB.v(k * NW + c0, 128), utv(k, tq * 512, 512), start=(k == 0), stop=(k == 7))
                            evac(ps)
                    return f

                for which, c0 in ((0, C_Q), (1, C_K)):
                    for tq in range(4):
                        def ev(ps, which=which, tq=tq):
                            p.copy(QK[2 * which].v(tq * 512, 512, 0, 64), ps.v(0, 512, 0, 64), eng="act")
                            p.copy(QK[2 * which + 1].v(tq * 512, 512, 0, 64), ps.v(0, 512, 64, 128), eng="dve")
                        th.append(half_proj(c0 + hp * 128, tq, 0, ev))
                        th.append(half_proj(c0 + hp * 128, tq, 1, ev))

                def gating(hh):
                    Q = QK[hh]
                    K = QK[2 + hh]
                    p.reduce(KSUM.v(0, 8, 0, 64), K.v(0, T, 0, 64).re("p (j s) -> p j s", j=8), ALU.add)
                    p.ts(KMT.v(0, 8, 0, 64), KSUM.v(0, 8, 0, 64), 1.0 / 256, None, ALU.mult)
                    gps = PS[7]
                    for ti in range(8):
                        p.matmul(gps.v(ti * 8, 8), Q.v(1024 + ti * 128, 128), KMT.v())
                    p.tt(GM.v(), gps.v(0, 64), CF.v(CF_CAP, 64), ALU.min)
                    g3 = GM.v().re("p (t j) -> p t j", t=8)
                    p.tt(CMP.v().re("p (t j k) -> p t j k", t=8, j=8),
                         g3.re("p t (o k) -> p t o k", o=1).bc([128, 8, 8, 8]),
                         g3.re("p t (j o) -> p t j o", o=1).bc([128, 8, 8, 8]), ALU.is_gt)
                    p.reduce(RANK.v(), CMP.v().re("p (a k) -> p a k", k=8), ALU.add)
                    p.ts(RANK.v(), RANK.v(), 3.0, None, ALU.is_lt)
                    p.tt(RANK.v(), RANK.v(), CF.v(CF_CMK, 64), ALU.mult)
                    p.tt(RANK.v(), RANK.v(), CF.v(CF_OWN, 64), ALU.add)
                    p.ts(PENB.v3(8, 128, 64, 8), RANK.v().re("p (t j) -> p t j", t=8), -1.0, BIG, ALU.add, ALU.mult)
                    pps = PS[7]
                    for ti in range(8):
                        p.transpose(pps.vb(BF16, ti * 128, 128), PENB.v(ti * 128, 128), ident)
                    p.copy(Q.v(1024, 1024, 64, 72), pps.vb(BF16, 0, 1024, 64, 72), eng="dve")

                for tq in range(4):
                    def ev(ps, tq=tq):
                        p.copy(SG.v(tq * 512, 512), ps.v(), eng="dve")
                    th.append(half_proj(C_G + hp * 128, tq, 0, ev))
                    th.append(half_proj(C_G + hp * 128, tq, 1, ev))
                th.append(lambda: gating(0))
                for tg in range(4):
                    for j in range(4):
                        def f(tg=tg, j=j):
                            ps = PS[2 + tg % 2]
                            ti = tg * 4 + j
                            for k in range(8):
                                p.matmul(ps.v(j * 128, 128), utv(k, ti * 128, 128),
                                         WB.v(k * NW + C_V + hp * 128, 128), start=(k == 0), stop=(k == 7))
                            if j == 3:
                                src = ps.v().re("p (j c) -> p j c", j=4)
                                p.copy(VA.v3(4, 128, tg * 512, 64), src[:, :, 0:64], eng="dve")
                                p.copy(VB.v3(4, 128, tg * 512 + 64, 64), src[:, :, 64:128], eng="dve")
                        th.append(f)
                th.append(lambda: gating(1))
                th.append(lambda: p.act(SG.v(), SG.v(), AF.Silu))
                return th

            def attention(hp, hh, S, bg, pt_base):
                Q = S["QK"][hh]
                K = S["QK"][2 + hh]
                V = S["VA"] if hh == 0 else S["VB"]
                SG = S["SG"]
                tiles = [(qt, kt) for qt in range(4) for kt in range(4 * qt + 4)]
                o0, d0 = (0, 64) if hh == 0 else (64, 0)

                def geom(idx):
                    qt, kt = tiles[idx]
                    tq0 = qt * 512
                    s0 = kt * 128
                    qlo = max(tq0, s0)
                    return qt, kt, tq0, s0, qlo, tq0 + 512 - qlo

                def emit_qk(idx):
                    qt, kt, tq0, s0, qlo, n = geom(idx)
                    gi = pt_base + idx
                    sp = PS[4 + gi % 3]
                    ptile = PT[gi % 3]
                    p.matmul(sp.v(0, n), K.v(s0, 128), Q.v(qlo, n))
                    p.act(ptile.v(0, n), sp.v(0, n), AF.Exp, scale=0.125)
                    if s0 >= tq0:
                        p.tt(ptile.v(0, 128), ptile.v(0, 128), tri, ALU.mult, eng="pool")

                def emit_pv(idx):
                    qt, kt, tq0, s0, qlo, n = geom(idx)
                    gi = pt_base + idx
                    nkt = 4 * qt + 4
                    po = PS[qt % 2]
                    ptile = PT[gi % 3]
                    p.matmul(po.v(qlo - tq0, n), V.v(kt * 128, 128), ptile.v(0, n),
                             start=(kt == 0), stop=(kt == nkt - 1))
                    if kt == nkt - 1:
                        r_ = R[qt % 2]
                        t1_ = T1[qt % 2]
                        p.recip(r_.v(0, 512, o0, o0 + 64), po.v(0, 512, d0, d0 + 64))
                        p.tt(t1_.v(0, 512, o0, o0 + 64), r_.v(0, 512, o0, o0 + 64), SG.v(tq0, 512, o0, o0 + 64),
                             ALU.mult, eng="pool")
                        p.tt(MIX.v(hp * T + tq0, 512, o0, o0 + 64), po.v(0, 512, o0, o0 + 64),
                             t1_.v(0, 512, o0, o0 + 64), ALU.mult)

                LA = 2
                for idx in range(len(tiles) + LA):
                    if idx < len(tiles):
                        emit_qk(idx)
                    if idx >= LA:
                        emit_pv(idx - LA)
                    if bg and idx % 2 == 1:
                        bg.pop(0)()
                return pt_base + len(tiles)

            pt_i = 0
            for f in make_thunks(0, sets[0]):
                f()
            for hp in range(4):
                bg = make_thunks(hp + 1, sets[(hp + 1) % 2]) if hp < 3 else []
                for hh in range(2):
                    pt_i = attention(hp, hh, sets[hp % 2], bg, pt_i)
                while bg:
                    bg.pop(0)()
            dbg("att", MIX.v(0, 4 * T), [128, 4 * T])

        def phase_mem(b):
            NW = 1024
            load_w("mem", wsi3, win3, C_QM, NW, 8)
            load_w("kv", wskv3, wkv3, 0, NW, 8, wb_off=8192)
            wk = Bump(WK, 0, WKSZ)
            MEMT = wk.get(BF16, 8 * 256)
            KM = wk.get(BF16, 4 * 256)
            VM = wk.get(BF16, 2 * 512)
            QM = [wk.get(BF16, T) for _ in range(2)]
            SGM = [wk.get(BF16, T) for _ in range(2)]
            PT = [wk.get(BF16, 512) for _ in range(2)]
            R = wk.get(F32, 512)
            T1 = wk.get(F32, 512)
            rms_transpose(lambda i: mem_d[b, i * 128:(i + 1) * 128, :], 2, P_MNW,
                          lambda i: MEMT.v3(8, 256, i * 128, 128), wk)
            for h in range(4):
                ps = PS[2 + h % 2]
                for k in range(8):
                    p.matmul(ps.v(0, 256), WB.v(8192 + k * NW + h * 128, 128), MEMT.v(k * 256, 256),
                             start=(k == 0), stop=(k == 7))
                p.copy(KM.v(h * 256, 256), ps.v(0, 256), eng="act")
            for mt in range(2):
                ps = PS[2 + mt % 2]
                for k in range(8):
                    p.matmul(ps.v(), MEMT.v(k * 256 + mt * 128, 128), WB.v(8192 + k * NW + 512, 512),
                             start=(k == 0), stop=(k == 7))
                p.copy(VM.v(mt * 512, 512), ps.v(), eng="dve")
            pt_i = 0
            for h in range(4):
                qm = QM[h % 2]
                sgm = SGM[h % 2]
                for tq in range(4):
                    ps = PS[2 + tq % 2]
                    proj_fm(NW, h * 128, 128, tq * 512, 512, ps.v())
                    p.copy(qm.v(tq * 512, 512), ps.v(), eng="act")
                for tq in range(4):
                    ps = PS[2 + tq % 2]
                    proj_fm(NW, 512 + h * 128, 128, tq * 512, 512, ps.v())
                    p.act(sgm.v(tq * 512, 512), ps.v(), AF.Silu)
                for qt in range(4):
                    tq0 = qt * 512
                    po = PS[qt % 2]
                    den = PS[6 + qt % 2]
                    for mt in range(2):
                        sp = PS[4 + pt_i % 2]
                        ptile = PT[pt_i % 2]
                        pt_i += 1
                        p.matmul(sp.v(), KM.v(h * 256 + mt * 128, 128), qm.v(tq0, 512))
                        p.act(ptile.v(), sp.v(), AF.Exp, scale=128 ** -0.5)
                        p.matmul(po.v(), VM.v(mt * 512 + h * 128, 128), ptile.v(), start=(mt == 0), stop=(mt == 1))
                        p.matmul(den.v(), ones, ptile.v(), start=(mt == 0), stop=(mt == 1))
                    p.recip(R.v(), den.v())
                    p.tt(T1.v(), R.v(), sgm.v(tq0, 512), ALU.mult, eng="pool")
                    p.tt(MIX.v((12 + h) * T + tq0, 512), po.v(), T1.v(), ALU.mult)
            dbg("memo", MIX.v(12 * T, 4 * T), [128, 4 * T])

        def phase_out(b):
            NW = 1024
            load_w("out", wso3, wout3, 0, NW, 16)
            wk = Bump(WK, 0, WKSZ)
            XT = [wk.get(F32, 1024) for _ in range(2)]
            HT = [wk.get(F32, 1024) for _ in range(2)]
            OT = [wk.get(F32, 1024) for _ in range(2)]
            JNK = wk.get(BF16, 1024)
            SSQ = wk.get(F32, 16)
            RSTD = wk.get(F32, 16)
            for i in range(16):
                xt = XT[i % 2]
                ht = HT[i % 2]
                ot = OT[i % 2]
                p.dma(xt.v(), D(x_d[b, i * 128:(i + 1) * 128, :]))
                for nh in range(2):
                    ps = PS[(2 * i + nh) % 4]
                    for m in range(16):
                        p.matmul(ps.v(), MIX.v(m * T + i * 128, 128), WB.v(m * NW + nh * 512, 512),
                                 start=(m == 0), stop=(m == 15))
                    p.tt(ht.v(nh * 512, 512), ps.v(), xt.v(nh * 512, 512), ALU.add)
                p.act(JNK.v(), ht.v(), AF.Square, accum_out=SSQ.v(i, 1))
                p.act(RSTD.v(i, 1), SSQ.v(i, 1), AF.Ln, scale=1.0 / DM, bias=epsv)
                p.act(RSTD.v(i, 1), RSTD.v(i, 1), AF.Exp, scale=-0.5)
                p.stt(ot.v(), ht.v(), RSTD.v(i, 1), prm(P_FNW, 1024), ALU.mult, ALU.mult)
                p.dma(D(out_d[b, i * 128:(i + 1) * 128, :]), ot.v())

        for b in range(nseq):
            wkA = Bump(WK, 16384, WKSZ - 16384)
            rms_transpose(lambda i: x_d[b, i * 128:(i + 1) * 128, :], 16, P_NW,
                          lambda i: UT.v3(BF16, 8, T, i * 128, 128), wkA)
            if b == 0:
                dbg("ut", UT.v(), [128, 8 * T])
            if "S" in phases:
                phase_ssd(b)
            if "B" in phases:
                phase_attn(b)
            if "M" in phases:
                phase_mem(b)
            if "E" in phases:
                phase_out(b)
        print("ops:", p.nops, {e: len(v) for e, v in p.ops.items()})
        p.emit()
    return nc, dumps


def kernel(x, mem, norm_w, w_in, conv_w, conv_b, dt_bias, a_log, d_skip, ssd_norm_w, mem_norm_w, w_mem_kv,
           w_out, final_norm_w):
    f = lambda a: np.ascontiguousarray(np.asarray(a, dtype=np.float32))
    x, mem = f(x), f(mem)
    cb, cf, kc, qc = host_consts()
    cf[:, CF_PRM:CF_PRM + NPRM] = host_params(f(norm_w)[0], f(conv_w)[0], f(conv_b)[0], f(dt_bias)[0], f(a_log)[0],
                                              f(d_skip)[0], f(ssd_norm_w)[0], f(mem_norm_w)[0], f(final_norm_w))
    nc, _ = build()
    shared = {"w_in": f(w_in)[0], "w_kv": f(w_mem_kv)[0], "w_out": f(w_out)[0], "cstb": cb, "cstf": cf,
              "kconst": kc, "qconst": qc}
    in_maps = []
    for c in range(8):
        m = dict(shared)
        m["x"] = x[c * NSEQ:(c + 1) * NSEQ]
        m["mem"] = mem[c * NSEQ:(c + 1) * NSEQ]
        in_maps.append(m)
    res = run_bass_kernel_spmd(nc, in_maps, core_ids=list(range(8)))
    return np.concatenate([r["out"] for r in res.results], axis=0)
```

```python
from contextlib import ExitStack
import concourse.bass as bass
import concourse.mybir as mybir

F32 = mybir.dt.float32
BF16 = mybir.dt.bfloat16
I32 = mybir.dt.int32
AF = mybir.ActivationFunctionType
ALU = mybir.AluOpType
AX = mybir.AxisListType
ESZ = {F32: 4, BF16: 2, I32: 4}


class View:
    __slots__ = ("ap", "arena", "rngs", "pr")

    def __init__(self, ap, arena, rngs, pr=(0, 128)):
        self.ap, self.arena, self.rngs, self.pr = ap, arena, rngs, pr

    def re(self, s, **kw):
        return View(self.ap.rearrange(s, **kw), self.arena, self.rngs, self.pr)

    def bc(self, shape):
        return View(self.ap.broadcast_to(shape), self.arena, self.rngs, self.pr)

    def __getitem__(self, key):
        return View(self.ap[key], self.arena, self.rngs, self.pr)


class Arena:
    def __init__(self, name, t, dtype, ncols, const=False):
        self.name, self.t, self.dtype, self.ncols = name, t, dtype, ncols
        self.esz = ESZ[dtype]
        self.w = []
        self.r = []
        self.const = const
        self.alt = {dtype: t}
        self.psum = False
        self.qlast = [dict() for _ in range(4)]

    def v(self, lo=0, n=None, p0=0, p1=128):
        if n is None:
            n = self.ncols - lo
        assert 0 <= lo and lo + n <= self.ncols, (self.name, lo, n, self.ncols)
        return View(self.t[p0:p1, lo:lo + n], self, [(lo * self.esz, (lo + n) * self.esz)], (p0, p1))

    def v3(self, dtype, nk, stride, lo, n, p0=0, p1=128):
        if dtype not in self.alt:
            self.alt[dtype] = self.t.bitcast(dtype)
        e = ESZ[dtype]
        assert ((nk - 1) * stride + lo + n) * e <= self.ncols * self.esz
        total = self.ncols * self.esz // e
        o = max(0, lo + nk * stride - total)
        assert o <= stride - n and o <= lo, (self.name, lo, nk, stride, n, total)
        ws = lo - o
        ap = self.alt[dtype][p0:p1, ws:ws + nk * stride].rearrange("p (k s) -> p k s", s=stride)[:, :, o:o + n]
        return View(ap, self, [((k * stride + lo) * e, (k * stride + lo + n) * e) for k in range(nk)], (p0, p1))

    def vb(self, dtype, lo, n, p0=0, p1=128):
        if dtype not in self.alt:
            self.alt[dtype] = self.t.bitcast(dtype)
        e = ESZ[dtype]
        assert (lo + n) * e <= self.ncols * self.esz
        return View(self.alt[dtype][p0:p1, lo:lo + n], self, [(lo * e, (lo + n) * e)], (p0, p1))


class Sub:
    def __init__(self, arena, dtype, boff, n):
        self.a, self.dt, self.e, self.n = arena, dtype, ESZ[dtype], n
        assert boff % self.e == 0
        self.base = boff // self.e

    def v(self, lo=0, n=None, p0=0, p1=128):
        if n is None:
            n = self.n - lo
        assert 0 <= lo and lo + n <= self.n, (lo, n, self.n)
        return self.a.vb(self.dt, self.base + lo, n, p0, p1)

    def v3(self, nk, stride, lo, n, p0=0, p1=128):
        assert (nk - 1) * stride + lo + n <= self.n
        return self.a.v3(self.dt, nk, stride, self.base + lo, n, p0, p1)


class Bump:
    def __init__(self, arena, boff, size):
        self.a, self.off, self.end = arena, boff, boff + size

    def get(self, dtype, n):
        self.off = (self.off + 3) // 4 * 4
        s = Sub(self.a, dtype, self.off, n)
        self.off += n * ESZ[dtype]
        assert self.off <= self.end, ("bump overflow", self.off, self.end)
        return s


def D(ap):
    return View(ap, None, [])


COMPUTE = ("pe", "act", "dve", "pool")


class Prog:
    def __init__(self, nc, ndma=40):
        self.nc = nc
        self.ops = {e: [] for e in COMPUTE + ("sp",)}
        self.count = {e: 0 for e in COMPUTE}
        self.waited = {e: {} for e in COMPUTE + ("sp",)}
        self.ndma = ndma
        self.dma_cnt = [0] * ndma
        self.dma_next = 0
        self.npool = 0
        self.nops = 0

    def op(self, eng, fn, reads=(), writes=(), dma=False):
        deps = {}

        def add(tok):
            k, v = tok
            if deps.get(k, 0) < v:
                deps[k] = v

        for vw in reads:
            a = vw.arena
            if a is None:
                continue
            for (lo, hi) in vw.rngs:
                for (l, h, t) in a.w:
                    if l < hi and lo < h:
                        add(t)
        for vw in writes:
            a = vw.arena
            if a is None:
                continue
            assert not a.const, a.name
            for (lo, hi) in vw.rngs:
                for (l, h, t) in a.w:
                    if l < hi and lo < h:
                        add(t)
                for (l, h, t) in a.r:
                    if l < hi and lo < h:
                        add(t)
        for vw in list(reads) + list(writes):
            a = vw.arena
            if a is None or not a.psum:
                continue
            for q in range(vw.pr[0] // 32, (vw.pr[1] + 31) // 32):
                for e2, t in a.qlast[q].items():
                    if e2 != eng:
                        add(t)
        if dma and eng == "pool":
            key = ("p", self.npool)
            self.npool += 1
            tok = (key, 16)
            inc = (key, 16)
        elif dma:
            i = self.dma_next
            self.dma_next = (i + 1) % self.ndma
            if self.dma_cnt[i] > 0:
                add((("d", i), self.dma_cnt[i]))
            self.dma_cnt[i] += 16
            tok = (("d", i), self.dma_cnt[i])
            inc = (("d", i), 16)
        else:
            self.count[eng] += 1
            tok = (eng, self.count[eng])
            inc = (eng, 1)
        waits = []
        wd = self.waited[eng]
        for k, v in deps.items():
            if k == eng and eng == "pe":
                continue
            if wd.get(k, 0) >= v:
                continue
            wd[k] = v
            waits.append((k, v))
        self.ops[eng].append((waits, fn, inc))
        self.nops += 1
        for vw in list(reads) + list(writes):
            a = vw.arena
            if a is None or not a.psum:
                continue
            for q in range(vw.pr[0] // 32, (vw.pr[1] + 31) // 32):
                a.qlast[q][eng] = tok
        for vw in reads:
            a = vw.arena
            if a is None or a.const:
                continue
            for (lo, hi) in vw.rngs:
                if not dma:
                    a.r = [x for x in a.r if not (x[2][0] == eng and lo <= x[0] and x[1] <= hi)]
                a.r.append((lo, hi, tok))
        for vw in writes:
            a = vw.arena
            if a is None:
                continue
            for (lo, hi) in vw.rngs:
                a.w = [x for x in a.w if not (lo <= x[0] and x[1] <= hi)]
                a.r = [x for x in a.r if not (lo <= x[0] and x[1] <= hi)]
                a.w.append((lo, hi, tok))
        return tok

    def freeze(self, arena):
        arena.const = True
        arena.r = []

    def matmul(self, out, lhsT, rhs, start=True, stop=True, **kw):
        return self.op("pe", lambda e: e.matmul(out.ap, lhsT.ap, rhs.ap, start=start, stop=stop, **kw),
                       [lhsT, rhs] + ([] if start else [out]), [out])

    def transpose(self, out, in_, ident):
        return self.op("pe", lambda e: e.transpose(out.ap, in_.ap, ident.ap), [in_, ident], [out])

    def act(self, out, in_, func, bias=None, scale=None, accum_out=None):
        rd = [in_]
        kw = {}
        if bias is not None:
            if isinstance(bias, View):
                rd.append(bias)
                kw["bias"] = bias.ap
            else:
                kw["bias"] = bias
        if scale is not None:
            if isinstance(scale, View):
                rd.append(scale)
                kw["scale"] = scale.ap
            else:
                kw["scale"] = scale
        wr = [out]
        if accum_out is not None:
            wr.append(accum_out)
            kw["accum_out"] = accum_out.ap
        return self.op("act", lambda e: e.activation(out.ap, in_.ap, func, **kw), rd, wr)

    def tt(self, out, in0, in1, op, eng="dve"):
        return self.op(eng, lambda e: e.tensor_tensor(out.ap, in0.ap, in1.ap, op), [in0, in1], [out])

    def ts(self, out, in0, s1, s2, op0, op1=None, eng="dve", accum_out=None):
        rd = [in0]
        a1 = s1
        a2 = s2
        if isinstance(s1, View):
            rd.append(s1)
            a1 = s1.ap
        if isinstance(s2, View):
            rd.append(s2)
            a2 = s2.ap
        kw = {}
        if op1 is not None:
            kw["op1"] = op1
        wr = [out]
        if accum_out is not None:
            kw["accum_out"] = accum_out.ap
            wr.append(accum_out)
        return self.op(eng, lambda e: e.tensor_scalar(out.ap, in0.ap, a1, a2, op0, **kw), rd, wr)

    def stt(self, out, in0, scalar, in1, op0, op1):
        rd = [in0, in1]
        sc = scalar
        if isinstance(scalar, View):
            rd.append(scalar)
            sc = scalar.ap
        return self.op("dve", lambda e: e.scalar_tensor_tensor(out.ap, in0.ap, sc, in1.ap, op0, op1), rd, [out])

    def copy(self, out, in_, eng="dve"):
        if eng == "act":
            return self.op("act", lambda e: e.copy(out.ap, in_.ap), [in_], [out])
        return self.op(eng, lambda e: e.tensor_copy(out.ap, in_.ap), [in_], [out])

    def reduce(self, out, in_, op, axis=AX.X, eng="dve"):
        return self.op(eng, lambda e: e.tensor_reduce(out.ap, in_.ap, axis, op), [in_], [out])

    def recip(self, out, in_):
        return self.op("dve", lambda e: e.reciprocal(out.ap, in_.ap), [in_], [out])

    def memset(self, out, val, eng="dve"):
        return self.op(eng, lambda e: e.memset(out.ap, val), [], [out])

    def scan(self, out, d0, d1, initial, op0, op1):
        rd = [d0, d1]
        ini = initial
        if isinstance(initial, View):
            rd.append(initial)
            ini = initial.ap
        return self.op("dve", lambda e: e.tensor_tensor_scan(out.ap, d0.ap, d1.ap, ini, op0, op1), rd, [out])

    def affine_select(self, out, in_, pattern, cmp, fill, base, cm):
        return self.op("pool", lambda e: e.affine_select(out.ap, in_.ap, pattern, cmp, fill, base=base,
                                                         channel_multiplier=cm), [in_], [out])

    def iota(self, out, pattern, base, cm):
        return self.op("pool", lambda e: e.iota(out.ap, pattern, base=base, channel_multiplier=cm), [], [out])

    def dma(self, out, in_, q="sp", **kw):
        if q == "pool":
            kw.setdefault("max_dma_last_dim", 4096)
        return self.op(q, lambda e: e.dma_start(out=out.ap, in_=in_.ap, **kw), [in_], [out], dma=True)

    def emit(self):
        nc = self.nc
        with ExitStack() as es:
            sems = {}
            for e in COMPUTE:
                sems[e] = es.enter_context(nc.semaphore("s_" + e))
            for i in range(self.ndma):
                sems[("d", i)] = es.enter_context(nc.semaphore("s_d%d" % i))
            for i in range(self.npool):
                sems[("p", i)] = es.enter_context(nc.semaphore("s_p%d" % i))
            fin = []
            for i in range(self.ndma):
                if self.dma_cnt[i] > 0:
                    fin.append((("d", i), self.dma_cnt[i]))
            for i in range(self.npool):
                fin.append((("p", i), 16))
            for e in COMPUTE:
                if self.count[e] > 0:
                    fin.append((e, self.count[e]))
            block = es.enter_context(nc.Block())

            def run(stream, final=None):
                def f(eng):
                    for waits, fn, inc in stream:
                        for k, v in waits:
                            eng.wait_ge(sems[k], v)
                        ins = fn(eng)
                        ins.then_inc(sems[inc[0]], inc[1])
                    if final:
                        for k, v in final:
                            eng.wait_ge(sems[k], v)
                return f

            block.tensor(run(self.ops["pe"]))
            block.scalar(run(self.ops["act"]))
            block.vector(run(self.ops["dve"]))
            block.gpsimd(run(self.ops["pool"]))
            block.sync(run(self.ops["sp"], fin))
from concourse.bass_utils import run_bass_kernel_spmd
import numpy as np

T = 2048
DM = 1024
NSEQ = 2
EPS = 1e-6
BIG = 30000.0
C_Q, C_K, C_V, C_G, C_Z, C_XBC, C_DT, C_QM, C_GM = 0, 512, 1024, 1536, 2048, 3072, 5120, 5136, 5648
CB_ID, CB_TRI, CB_MNEG, CB_ONES, CB_EH, NCB = 0, 128, 256, 384, 512, 2560
CF_ID, CF_ONES, CF_PRM, CF_CAP, CF_CMK, CF_OWN, CF_MISC, NCF = 0, 128, 256, 1424, 1488, 1552, 1616, 1624
P_NW, P_MNW, P_SNW, P_CVB, P_CVW, P_DSK, P_DTB, P_ALOG, P_FNW = 0, 8, 16, 24, 40, 104, 112, 128, 144
NPRM = 1168


def host_consts():
    cb = np.zeros((128, NCB), np.float32)
    cb[:, CB_ID:CB_ID + 128] = np.eye(128)
    s = np.arange(128)[:, None]
    t = np.arange(128)[None, :]
    cb[:, CB_TRI:CB_TRI + 128] = (t >= s)
    cb[:, CB_MNEG:CB_MNEG + 128] = np.where(t < s, -BIG, 0.0)
    cb[:, CB_ONES:CB_ONES + 128] = 1.0
    for h in range(16):
        cb[h, CB_EH + h * 128: CB_EH + (h + 1) * 128] = 1.0
    cf = np.zeros((128, NCF), np.float32)
    cf[:, CF_ID:CF_ID + 128] = np.eye(128)
    cf[:, CF_ONES:CF_ONES + 128] = 1.0
    cap = np.zeros((8, 8), np.float32)
    cmk = np.zeros((8, 8), np.float32)
    own = np.zeros((8, 8), np.float32)
    for ti in range(8):
        qb = 4 + ti // 2
        for j in range(8):
            cap[ti, j] = 1e30 if j < qb else -1e30
            cmk[ti, j] = 1.0 if j < qb else 0.0
            own[ti, j] = 1.0 if j == qb else 0.0
    cf[:, CF_CAP:CF_CAP + 64] = cap.reshape(-1)[None, :]
    cf[:, CF_CMK:CF_CMK + 64] = cmk.reshape(-1)[None, :]
    cf[:, CF_OWN:CF_OWN + 64] = own.reshape(-1)[None, :]
    cf[:, CF_MISC] = EPS
    cf[:, CF_MISC + 1] = 1.0
    kc = np.zeros((128, T), np.float32)
    pos = np.arange(T)
    for r in range(8):
        kc[64 + r] = (pos // 256 == r)
    kc[96] = pos // 16
    kc[97] = pos % 16
    kc[98] = 1.0
    kc[99] = 1.0
    qc = np.zeros((8, 4, T), np.float32)
    for h in range(8):
        sl = 2.0 ** (-8.0 * (h + 1) / 8)
        qc[h, 0] = 16 * sl * 8
        qc[h, 1] = sl * 8
        qc[h, 2] = -16 * sl * 8 * (pos // 16)
        qc[h, 3] = -sl * 8 * (pos % 16)
    return cb, cf, kc, qc


def host_params(norm_w, conv_w, conv_b, dt_bias, a_log, d_skip, ssd_norm_w, mem_norm_w, final_norm_w):
    prm = np.zeros((128, NPRM), np.float32)
    prm[:, P_NW:P_NW + 8] = norm_w.reshape(8, 128).T
    prm[:, P_MNW:P_MNW + 8] = mem_norm_w.reshape(8, 128).T
    prm[:, P_SNW:P_SNW + 8] = ssd_norm_w.reshape(8, 128).T
    prm[:, P_CVB:P_CVB + 16] = conv_b.reshape(16, 128).T
    prm[:, P_CVW:P_CVW + 64] = conv_w.T.reshape(16, 128, 4).transpose(1, 0, 2).reshape(128, 64)
    prm[:, P_DSK:P_DSK + 8] = np.repeat(d_skip, 64).reshape(8, 128).T
    prm[:, P_DTB:P_DTB + 16] = dt_bias[None, :]
    prm[:, P_ALOG:P_ALOG + 16] = a_log[None, :]
    prm[:, P_FNW:P_FNW + 1024] = final_norm_w[None, :]
    return prm
def build(nseq=NSEQ, dump=None, phases="ASBME"):
    nc = bass.Bass("TRN2", target_bir_lowering=False)
    x_d = nc.dram_tensor("x", [nseq, T, DM], F32, kind="ExternalInput").ap()
    mem_d = nc.dram_tensor("mem", [nseq, 256, DM], F32, kind="ExternalInput").ap()
    win_d = nc.dram_tensor("w_in", [DM, 6160], F32, kind="ExternalInput").ap()
    wkv_d = nc.dram_tensor("w_kv", [DM, 1024], F32, kind="ExternalInput").ap()
    wout_d = nc.dram_tensor("w_out", [2048, DM], F32, kind="ExternalInput").ap()
    cb_d = nc.dram_tensor("cstb", [128, NCB], F32, kind="ExternalInput").ap()
    cf_d = nc.dram_tensor("cstf", [128, NCF], F32, kind="ExternalInput").ap()
    kc_d = nc.dram_tensor("kconst", [128, T], F32, kind="ExternalInput").ap()
    qc_d = nc.dram_tensor("qconst", [8, 4, T], F32, kind="ExternalInput").ap()
    out_d = nc.dram_tensor("out", [nseq, T, DM], F32, kind="ExternalOutput").ap()
    dumps = {}
    wsi_d = nc.dram_tensor("wsc_in", [DM, 6160], BF16, kind="Internal").ap()
    wskv_d = nc.dram_tensor("wsc_kv", [DM, 1024], BF16, kind="Internal").ap()
    wso_d = nc.dram_tensor("wsc_out", [2048, DM], BF16, kind="Internal").ap()
    kcs_d = nc.dram_tensor("kc_sc", [128, T], BF16, kind="Internal").ap()
    qcs_d = nc.dram_tensor("qc_sc", [32, T], BF16, kind="Internal").ap()
    qc2_d = qc_d.rearrange("h r t -> (h r) t")
    wsi3 = wsi_d.rearrange("(k p) c -> p k c", p=128)
    wskv3 = wskv_d.rearrange("(k p) c -> p k c", p=128)
    wso3 = wso_d.rearrange("(k p) c -> p k c", p=128)
    win3 = win_d.rearrange("(k p) c -> p k c", p=128)
    wkv3 = wkv_d.rearrange("(k p) c -> p k c", p=128)
    wout3 = wout_d.rearrange("(k p) c -> p k c", p=128)

    with ExitStack() as es:
        def sb(name, n, dt):
            t = es.enter_context(nc.sbuf_tensor(name, [128, n], dt))
            return Arena(name, t, dt, n)

        UT = sb("UT", 8 * T, BF16)
        MIX = sb("MIX", 16 * T, BF16)
        WB = sb("WB", 8 * 3088, BF16)
        WKSZ = 47616
        WK = sb("WK", WKSZ // 2, BF16)
        CB = sb("CB", NCB, BF16)
        CF = sb("CF", NCF, F32)
        SM = sb("SM", 64, F32)
        PS = []
        for i in range(8):
            t = es.enter_context(nc.psum_tensor("ps%d" % i, [128, 512], F32))
            PS.append(Arena("ps%d" % i, t, F32, 512))
            PS[-1].psum = True
        p = Prog(nc)

        def dbg(name, view, shape):
            if dump is None or name not in dump:
                return
            d = nc.dram_tensor("dbg_" + name, list(shape), view.ap.dtype, kind="ExternalOutput").ap()
            dumps[name] = d
            p.dma(D(d), view)

        p.dma(CB.v(), D(cb_d), q="pool")
        p.dma(CF.v(), D(cf_d))
        ident = CB.v(CB_ID, 128)
        tri = CB.v(CB_TRI, 128)
        mneg = CB.v(CB_MNEG, 128)
        ones = CB.v(CB_ONES, 128)
        identf = CF.v(CF_ID, 128)
        epsv = CF.v(CF_MISC, 1)
        onev = CF.v(CF_MISC + 1, 1)

        def prm(off, n, p0=0, p1=128):
            return CF.v(CF_PRM + off, n, p0, p1)

        AB = SM.v(0, 16)
        p.act(AB, prm(P_ALOG, 16), AF.Exp)
        p.ts(AB, AB, -1.0, None, ALU.mult)

        SC = {n: Arena("sc_" + n, None, BF16, 1) for n in ("attn", "mem", "ssd", "kv", "out", "kc", "qc")}
        converted = [False]

        def scv(ap, name, k):
            return View(ap, SC[name], [(k, k + 1)])

        NHALF = {"attn": 4, "mem": 4, "ssd": 4, "kv": 4, "out": 8}

        def convert_all():
            p.dma(scv(kcs_d, "kc", 0), D(kc_d), q="pool")
            p.dma(scv(qcs_d, "qc", 0), D(qc2_d), q="pool")
            for name, src, dst, c0, nw, nrows in (("attn", win_d, wsi_d, 0, 2048, 1024), ("kv", wkv_d, wskv_d, 0, 1024, 1024),
                                                  ("mem", win_d, wsi_d, C_QM, 1024, 1024), ("out", wout_d, wso_d, 0, 1024, 2048),
                                                  ("ssd", win_d, wsi_d, C_Z, 3088, 1024)):
                hr = nrows // 2
                for h in range(2):
                    p.dma(scv(dst[h * hr:(h + 1) * hr, c0:c0 + nw], name, h), D(src[h * hr:(h + 1) * hr, c0:c0 + nw]),
                          q="pool")
            converted[0] = True

        def load_w(name, dst3, src3, c0, nw, nk, wb_off=0):
            for k in range(nk):
                if converted[0]:
                    p.dma(WB.v(wb_off + k * nw, nw), scv(dst3[:, k, c0:c0 + nw], name, k // NHALF[name]), q="sp")
                else:
                    p.dma(WB.v(wb_off + k * nw, nw), D(src3[:, k, c0:c0 + nw]), q="pool")

        pj_i = [0]

        def rms_transpose(src_tile, ntiles, nwoff, dst_fn, wk):
            XT = [wk.get(F32, 1024) for _ in range(2)]
            XS = [wk.get(BF16, 1024) for _ in range(2)]
            JNK = wk.get(BF16, 1024)
            SSQ = wk.get(F32, 16)
            RSTD = wk.get(F32, 16)
            for i in range(ntiles):
                xt = XT[i % 2]
                p.dma(xt.v(), D(src_tile(i)))
                p.act(JNK.v(), xt.v(), AF.Square, accum_out=SSQ.v(i, 1))
                p.act(RSTD.v(i, 1), SSQ.v(i, 1), AF.Ln, scale=1.0 / DM, bias=epsv)
                p.act(RSTD.v(i, 1), RSTD.v(i, 1), AF.Exp, scale=-0.5)
                xs = XS[i % 2]
                p.ts(xs.v(), xt.v(), RSTD.v(i, 1), None, ALU.mult)
                pb = PS[i % 2]
                for k in range(8):
                    p.transpose(pb.vb(BF16, k * 128, 128), xs.v(k * 128, 128), ident)
                p.tt(dst_fn(i), pb.vb(BF16, 0, 1024).re("p (k t) -> p k t", k=8),
                     prm(nwoff, 8).re("p (k o) -> p k o", o=1).bc([128, 8, 128]), ALU.mult)

        def utv(k, t0, n):
            return UT.v(k * T + t0, n)

        def proj_fm(nw, c0, ncols, t0, n, ps_view):
            for k in range(8):
                p.matmul(ps_view, WB.v(k * nw + c0, ncols), utv(k, t0, n), start=(k == 0), stop=(k == 7))

        def phase_ssd(b):
            NW = 3088
            load_w("ssd", wsi3, win3, C_Z, NW, 8)
            if not converted[0]:
                convert_all()
            b1 = Bump(MIX, 0, 16384)
            b2 = Bump(MIX, 12 * T * 2, 16384)
            wk = Bump(WK, 0, WKSZ)
            XPRE = b1.get(BF16, 16 * 259)
            XST = b1.get(BF16, 8 * 256)
            BMT = b1.get(BF16, 4 * 256)
            LT = [b1.get(BF16, 384) for _ in range(2)]
            SZ = b2.get(BF16, 8 * 256)
            PREV = b2.get(F32, 1024)
            XDD = b2.get(BF16, 2 * 1024)
            CMT = b2.get(BF16, 4 * 256)
            BTOK = b2.get(BF16, 2 * 512)
            DIAG = wk.get(BF16, 64 * 128)
            CBT = wk.get(BF16, 4 * 384)
            L0 = [wk.get(BF16, 256) for _ in range(2)]
            MT = [wk.get(BF16, 384) for _ in range(2)]
            GT = [wk.get(BF16, 256) for _ in range(2)]
            GG = [wk.get(F32, 256) for _ in range(4)]
            GSQ = [wk.get(BF16, 256) for _ in range(4)]
            RS = wk.get(F32, 256)
            XDT = wk.get(BF16, 2 * 2048)
            PREVB = wk.get(BF16, 2048)
            DTR = wk.get(F32, 32)
            DT = wk.get(F32, 32)
            DA = wk.get(F32, 256)
            NCS = wk.get(F32, 32)
            DEC = wk.get(F32, 32)
            W2 = wk.get(F32, 32)
            CST = wk.get(F32, 256)
            CSH = wk.get(BF16, 256)
            CSL = wk.get(BF16, 256)
            DECT = wk.get(F32, 256)
            DG16 = wk.get(F32, 16)
            CDB = wk.get(F32, 16)

            for zt in (XDT, PREVB, DA, CST, CSH, CSL, DECT, DG16):
                p.memset(zt.v(), 0.0, eng="pool")
            for j in range(64):
                p.ts(DIAG.v(j * 128, 128), ident, prm(P_CVW + j, 1), None, ALU.mult,
                     eng=("pool" if j % 2 else "dve"))
            p.memset(XPRE.v3(16, 259, 0, 3), 0.0)
            p.memset(PREV.v(), 0.0)

            for c in range(8):
                t0 = c * 256

                def zgroup(zc):
                    ps = PS[zc % 2].v((zc // 2 % 2) * 256, 256)
                    proj_fm(NW, zc * 128, 128, t0, 256, ps)
                    p.copy(SZ.v(zc * 256, 256), ps, eng="dve")

                def xgroup(cc):
                    ps = PS[cc % 2].v((cc // 2 % 2) * 256, 256)
                    proj_fm(NW, 1024 + cc * 128, 128, t0, 256, ps)
                    p.copy(XPRE.v(cc * 259 + 3, 256), ps, eng=("dve" if cc % 2 else "act"))

                if c > 0:
                    p.copy(XPRE.v3(16, 259, 0, 3), XPRE.v3(16, 259, 256, 3), eng="pool")
                for lt in range(2):
                    for k in range(8):
                        p.matmul(PS[5].v(384 + lt * 16, 16), utv(k, t0 + lt * 128, 128), WB.v(k * NW + 3072, 16),
                                 start=(k == 0), stop=(k == 7))
                p.tt(DTR.v(), PS[5].v(384, 32).re("p (l h) -> p l h", l=2),
                     prm(P_DTB, 16).re("p (o h) -> p o h", o=1).bc([128, 2, 16]), ALU.add)
                p.act(DTR.v(), DTR.v(), AF.Exp)
                p.act(DT.v(), DTR.v(), AF.Ln, bias=onev)
                p.tt(DA.v3(2, 128, 0, 16), DT.v().re("p (l h) -> p l h", l=2),
                     SM.v(0, 16).re("p (o h) -> p o h", o=1).bc([128, 2, 16]), ALU.mult)
                for zc in range(6):
                    zgroup(zc)
                for lt in range(2):
                    p.transpose(PS[6].v(lt * 128, 128), DA.v(lt * 128, 128), identf)
                p.scan(CST.v(0, 256, 0, 16), CF.v(CF_ONES, 1, 0, 16).bc([16, 256]), PS[6].v(0, 256, 0, 16), 0.0,
                       ALU.mult, ALU.add)
                p.copy(CSH.v(0, 256, 0, 16), CST.v(0, 256, 0, 16))
                p.tt(CSL.v(0, 256, 0, 16), CST.v(0, 256, 0, 16), CSH.v(0, 256, 0, 16), ALU.subtract)
                p.act(DECT.v(0, 256, 0, 16), CST.v(0, 256, 0, 16), AF.Exp, scale=-1.0, bias=CST.v(255, 1, 0, 16))
                for zc in range(6, 8):
                    zgroup(zc)
                for cc in range(8, 12):
                    xgroup(cc)
                for lt in range(2):
                    p.transpose(PS[7].v(lt * 128, 128), CST.v(lt * 128, 128), identf)
                    p.transpose(PS[7].v(256 + lt * 128, 128), DECT.v(lt * 128, 128), identf)
                p.ts(NCS.v().re("p (l h) -> p l h", l=2), PS[7].v3(F32, 2, 128, 0, 16), -1.0, None, ALU.mult)
                p.copy(DEC.v().re("p (l h) -> p l h", l=2), PS[7].v3(F32, 2, 128, 256, 16))
                p.tt(W2.v(), DT.v(), DEC.v(), ALU.mult)
                if c < 7:
                    p.ts(DG16.v(0, 16, 0, 16), CF.v(CF_ID, 16, 0, 16), CST.v(255, 1, 0, 16), None, ALU.mult)
                    p.matmul(PS[5].v(480, 16), CF.v(CF_ONES, 128), DG16.v())
                    p.act(CDB.v(), PS[5].v(480, 16), AF.Exp)
                for cc in list(range(12, 16)) + list(range(0, 8)):
                    xgroup(cc)

                def conv(cc):
                    ps = PS[cc % 2].v((cc // 2 % 2) * 256, 256)
                    for k in range(4):
                        p.matmul(ps, DIAG.v((cc * 4 + k) * 128, 128), XPRE.v(cc * 259 + k, 256),
                                 start=(k == 0), stop=(k == 3))
                    if cc < 8:
                        dst = XST.v(cc * 256, 256)
                    elif cc < 12:
                        dst = BMT.v((cc - 8) * 256, 256)
                    else:
                        dst = CMT.v((cc - 12) * 256, 256)
                    p.act(dst, ps, AF.Silu, bias=prm(P_CVB + cc, 1))

                for cc in range(8, 16):
                    conv(cc)
                for lt in range(2):
                    pb = PS[7]
                    for g in range(4):
                        p.transpose(pb.vb(BF16, g * 128, 128), BMT.v(g * 256 + lt * 128, 128), ident)
                    p.copy(BTOK.v(lt * 512, 512), pb.vb(BF16, 0, 512), eng="dve")
                for g in range(4):
                    ps = PS[5]
                    p.matmul(ps.v(0, 256), BMT.v(g * 256, 128), CMT.v(g * 256, 256))
                    p.matmul(ps.v(256, 128), BMT.v(g * 256 + 128, 128), CMT.v(g * 256 + 128, 128))
                    p.copy(CBT.v(g * 384, 384), ps.v(0, 384), eng="dve")
                for cc in range(0, 8):
                    conv(cc)
                p.act(SZ.v(), SZ.v(), AF.Silu)
                for lt in range(2):
                    pb = PS[6]
                    for cc in range(8):
                        p.transpose(pb.vb(BF16, cc * 128, 128), XST.v(cc * 256 + lt * 128, 128), ident)
                    src = pb.vb(BF16, 0, 1024).re("p (h q) -> p h q", h=16)
                    for hh in range(2):
                        p.tt(XDT.v(lt * 2048, 2048).re("p (c e q) -> p c e q", c=8, e=2)[:, :, hh, hh * 64:hh * 64 + 64],
                             src.re("p (c e) q -> p c e q", e=2)[:, :, hh, :],
                             DT.v(lt * 16, 16).re("p (c e o) -> p c e o", e=2, o=1)[:, :, hh, :].bc([128, 8, 64]),
                             ALU.mult)
                    p.tt(XDD.v(lt * 1024, 1024).re("p (h q) -> p h q", h=16), src,
                         W2.v(lt * 16, 16).re("p (h o) -> p h o", o=1).bc([128, 16, 64]), ALU.mult)
                def seg_stage(h):
                    g = h // 4
                    sg = PS[2 + h % 2]
                    eh = CB.v(CB_EH + h * 128, 128)
                    csh = lambda lo, n: CSH.v(lo, n)
                    csl = lambda lo, n: CSL.v(lo, n)
                    p.matmul(sg.v(0, 256), eh, csh(0, 256), start=True, stop=False)
                    p.matmul(sg.v(0, 256), eh, csl(0, 256), start=False, stop=True)
                    p.matmul(sg.v(256, 128), eh, csh(0, 128), start=True, stop=False)
                    p.matmul(sg.v(256, 128), eh, csl(0, 128), start=False, stop=False)
                    p.matmul(sg.v(256, 128), ident, mneg, start=False, stop=True)
                    p.matmul(sg.v(384, 128), eh, csh(128, 128), start=True, stop=False)
                    p.matmul(sg.v(384, 128), eh, csl(128, 128), start=False, stop=False)
                    p.matmul(sg.v(384, 128), ident, mneg, start=False, stop=True)
                    l0 = L0[h % 2]
                    lt_ = LT[h % 2]
                    mt = MT[h % 2]
                    gt = GT[h % 2]
                    if c > 0:
                        p.act(l0.v(), sg.v(0, 256), AF.Exp)
                    p.act(lt_.v(0, 128), sg.v(256, 128), AF.Exp, bias=NCS.v(h, 1))
                    p.act(lt_.v(128, 128), sg.v(128, 128), AF.Exp, bias=NCS.v(h, 1))
                    p.act(lt_.v(256, 128), sg.v(384, 128), AF.Exp, bias=NCS.v(16 + h, 1))
                    p.tt(mt.v(), lt_.v(), CBT.v(g * 384, 384), ALU.mult)
                    if c > 0:
                        p.tt(gt.v(), l0.v(), CMT.v(g * 256, 256), ALU.mult, eng="pool")

                def y_stage(h):
                    g = h // 4
                    pr = h % 2
                    pc = h // 2
                    mt = MT[h % 2]
                    gt = GT[h % 2]
                    ybank = PS[4 + 2 * (pc % 2)]
                    yt = ybank.v(0, 256)
                    yt2 = ybank.v(128, 128)
                    p.matmul(yt, XDT.v(h * 128, 128), mt.v(0, 256), start=(pr == 0), stop=False)
                    p.matmul(yt2, XDT.v(2048 + h * 128, 128), mt.v(256, 128), start=False, stop=(c == 0 and pr == 1))
                    if c > 0:
                        p.matmul(yt, PREVB.v(h * 128, 128), gt.v(), start=False, stop=(pr == 1))
                    if pr == 1:
                        gg = GG[pc % 4]
                        p.stt(gg.v(), XST.v(pc * 256, 256), prm(P_DSK + pc, 1), yt, ALU.mult, ALU.add)
                        p.tt(gg.v(), gg.v(), SZ.v(pc * 256, 256), ALU.mult)
                        p.act(GSQ[pc % 4].v(), gg.v(), AF.Square)
                def norm_stage(g):
                        pc = 2 * g + 1
                        pcs = [pc - 1, pc]
                        ss = PS[7].v(256, 256)
                        p.matmul(ss, ones, GSQ[pcs[0] % 4].v(), start=True, stop=False)
                        p.matmul(ss, ones, GSQ[pcs[1] % 4].v(), start=False, stop=True)
                        p.act(RS.v(), ss, AF.Ln, scale=1.0 / 256, bias=epsv)
                        p.act(RS.v(), RS.v(), AF.Exp, scale=-0.5)
                        for q in pcs:
                            p.stt(MIX.v((4 + q) * T + t0, 256), GG[q % 4].v(), prm(P_SNW + q, 1), RS.v(),
                                  ALU.mult, ALU.mult)

                for h in range(18):
                    if h < 16:
                        seg_stage(h)
                    if 1 <= h <= 16:
                        y_stage(h - 1)
                    if h >= 2 and (h - 2) % 4 == 3:
                        norm_stage((h - 2) // 4)
                if c < 7:
                    for g in range(4):
                        st = PS[7].v(0, 256)
                        for lt in range(2):
                            p.matmul(st, BTOK.v(lt * 512 + g * 128, 128), XDD.v(lt * 1024 + g * 256, 256),
                                     start=(lt == 0), stop=(lt == 1))
                        pv = PREV.v(g * 256, 256)
                        p.tt(pv.re("p (h q) -> p h q", h=4), pv.re("p (h q) -> p h q", h=4),
                             CDB.v(g * 4, 4).re("p (h o) -> p h o", o=1).bc([128, 4, 64]), ALU.mult)
                        p.tt(pv, pv, st, ALU.add)
                        for hh in range(2):
                            p.copy(PREVB.v(g * 512, 512).re("p (c e q) -> p c e q", c=2, e=2)[:, :, hh, hh * 64:hh * 64 + 64],
                                   pv.re("p (c e q) -> p c e q", c=2, e=2)[:, :, hh, :], eng="pool")
            dbg("ssd", MIX.v(4 * T, 8 * T), [128, 8 * T])

        def attn_core(kt_list_fn, nq, kv_lhsT, q_rhs, v_lhsT, scale, PTs, epilogue, diag_fn=None, den_lhsT=None):
            pass

        def phase_attn(b):
            NW = 2048
            load_w("attn", wsi3, win3, 0, NW, 8)
            wk = Bump(WK, 0, WKSZ)
            bm = Bump(MIX, 12 * T * 2, 16384)
            bw = Bump(WB, 8 * NW * 2, WB.ncols * 2 - 8 * NW * 2)
            sets = [dict(QK=[wk.get(BF16, T) for _ in range(4)], VA=wk.get(BF16, 2048), VB=wk.get(BF16, 2048),
                         SG=wk.get(BF16, T)),
                    dict(QK=[bm.get(BF16, T) for _ in range(4)], VA=bw.get(BF16, 2048), VB=bw.get(BF16, 2048),
                         SG=bw.get(BF16, T))]
            PT = [wk.get(BF16, 512) for _ in range(3)]
            R = [wk.get(F32, 512) for _ in range(2)]
            T1 = [wk.get(F32, 512) for _ in range(2)]
            PENB = wk.get(BF16, 8 * 128)
            GM = wk.get(F32, 64)
            CMP = wk.get(F32, 512)
            RANK = wk.get(F32, 64)
            KSUM = wk.get(F32, 8)
            KMT = wk.get(BF16, 8)
            p.memset(KMT.v(), 0.0, eng="pool")
            p.memset(PENB.v(), 0.0, eng="pool")
            for S in sets:
                for i in range(2):
                    p.memset(S["QK"][i].v(), 0.0, eng="pool")
                    p.dma(S["QK"][2 + i].v(), scv(kcs_d, "kc", 0), q="sp")
                p.memset(S["VA"].v(), 1.0, eng="pool")
                p.memset(S["VB"].v(), 1.0, eng="pool")

            def make_thunks(hp, S):
                QK, VA, VB, SG = S["QK"], S["VA"], S["VB"], S["SG"]
                th = []

                def t_qc():
                    for hh in range(2):
                        h_ = 2 * hp + hh
                        p.dma(QK[hh].v(0, T, 96, 100), scv(qcs_d[4 * h_:4 * h_ + 4, :], "qc", 0), q="sp")
                th.append(t_qc)
                def half_proj(c0, tq, half, evac):
                    def f():
                        ps = PS[2 + tq % 2]
                        if half == 1:
                            for k in range(8):
                                p.matmul(ps.v(), WB.v(k * NW + c0, 128), utv(k, tq * 512, 512), start=(k == 0), stop=(k == 7))
                            evac(ps)
                    return f

                for which, c0 in ((0, C_Q), (1, C_K)):
                    for tq in range(4):
                        def ev(ps, which=which, tq=tq):
                            p.copy(QK[2 * which].v(tq * 512, 512, 0, 64), ps.v(0, 512, 0, 64), eng="act")
                            p.copy(QK[2 * which + 1].v(tq * 512, 512, 0, 64), ps.v(0, 512, 64, 128), eng="dve")
                        th.append(half_proj(c0 + hp * 128, tq, 0, ev))
                        th.append(half_proj(c0 + hp * 128, tq, 1, ev))

                def gating(hh):
                    Q = QK[hh]
                    K = QK[2 + hh]
                    p.reduce(KSUM.v(0, 8, 0, 64), K.v(0, T, 0, 64).re("p (j s) -> p j s", j=8), ALU.add)
                    p.ts(KMT.v(0, 8, 0, 64), KSUM.v(0, 8, 0, 64), 1.0 / 256, None, ALU.mult)
                    gps = PS[7]
                    for ti in range(8):
                        p.matmul(gps.v(ti * 8, 8), Q.v(1024 + ti * 128, 128), KMT.v())
                    p.tt(GM.v(), gps.v(0, 64), CF.v(CF_CAP, 64), ALU.min)
                    g3 = GM.v().re("p (t j) -> p t j", t=8)
                    p.tt(CMP.v().re("p (t j k) -> p t j k", t=8, j=8),
                         g3.re("p t (o k) -> p t o k", o=1).bc([128, 8, 8, 8]),
                         g3.re("p t (j o) -> p t j o", o=1).bc([128, 8, 8, 8]), ALU.is_gt)
                    p.reduce(RANK.v(), CMP.v().re("p (a k) -> p a k", k=8), ALU.add)
                    p.ts(RANK.v(), RANK.v(), 3.0, None, ALU.is_lt)
                    p.tt(RANK.v(), RANK.v(), CF.v(CF_CMK, 64), ALU.mult)
                    p.tt(RANK.v(), RANK.v(), CF.v(CF_OWN, 64), ALU.add)
                    p.ts(PENB.v3(8, 128, 64, 8), RANK.v().re("p (t j) -> p t j", t=8), -1.0, BIG, ALU.add, ALU.mult)
                    pps = PS[7]
                    for ti in range(8):
                        p.transpose(pps.vb(BF16, ti * 128, 128), PENB.v(ti * 128, 128), ident)
                    p.copy(Q.v(1024, 1024, 64, 72), pps.vb(BF16, 0, 1024, 64, 72), eng="dve")

                for tq in range(4):
                    def ev(ps, tq=tq):
                        p.copy(SG.v(tq * 512, 512), ps.v(), eng="dve")
                    th.append(half_proj(C_G + hp * 128, tq, 0, ev))
                    th.append(half_proj(C_G + hp * 128, tq, 1, ev))
                th.append(lambda: gating(0))
                for tg in range(4):
                    for j in range(4):
                        def f(tg=tg, j=j):
                            ps = PS[2 + tg % 2]
                            ti = tg * 4 + j
                            for k in range(8):
                                p.matmul(ps.v(j * 128, 128), utv(k, ti * 128, 128),
                                         WB.v(k * NW + C_V + hp * 128, 128), start=(k == 0), stop=(k == 7))
                            if j == 3:
                                src = ps.v().re("p (j c) -> p j c", j=4)
                                p.copy(VA.v3(4, 128, tg * 512, 64), src[:, :, 0:64], eng="dve")
                                p.copy(VB.v3(4, 128, tg * 512 + 64, 64), src[:, :, 64:128], eng="dve")
                        th.append(f)
                th.append(lambda: gating(1))
                th.append(lambda: p.act(SG.v(), SG.v(), AF.Silu))
                return th

            def attention(hp, hh, S, bg, pt_base):
                Q = S["QK"][hh]
                K = S["QK"][2 + hh]
                V = S["VA"] if hh == 0 else S["VB"]
                SG = S["SG"]
                tiles = [(qt, kt) for qt in range(4) for kt in range(4 * qt + 4)]
                o0, d0 = (0, 64) if hh == 0 else (64, 0)

                def geom(idx):
                    qt, kt = tiles[idx]
                    tq0 = qt * 512
                    s0 = kt * 128
                    qlo = max(tq0, s0)
                    return qt, kt, tq0, s0, qlo, tq0 + 512 - qlo

                def emit_qk(idx):
                    qt, kt, tq0, s0, qlo, n = geom(idx)
                    gi = pt_base + idx
                    sp = PS[4 + gi % 3]
                    ptile = PT[gi % 3]
                    p.matmul(sp.v(0, n), K.v(s0, 128), Q.v(qlo, n))
                    p.act(ptile.v(0, n), sp.v(0, n), AF.Exp, scale=0.125)
                    if s0 >= tq0:
                        p.tt(ptile.v(0, 128), ptile.v(0, 128), tri, ALU.mult, eng="pool")

                def emit_pv(idx):
                    qt, kt, tq0, s0, qlo, n = geom(idx)
                    gi = pt_base + idx
                    nkt = 4 * qt + 4
                    po = PS[qt % 2]
                    ptile = PT[gi % 3]
                    p.matmul(po.v(qlo - tq0, n), V.v(kt * 128, 128), ptile.v(0, n),
                             start=(kt == 0), stop=(kt == nkt - 1))
                    if kt == nkt - 1:
                        r_ = R[qt % 2]
                        t1_ = T1[qt % 2]
                        p.recip(r_.v(0, 512, o0, o0 + 64), po.v(0, 512, d0, d0 + 64))
                        p.tt(t1_.v(0, 512, o0, o0 + 64), r_.v(0, 512, o0, o0 + 64), SG.v(tq0, 512, o0, o0 + 64),
                             ALU.mult, eng="pool")
                        p.tt(MIX.v(hp * T + tq0, 512, o0, o0 + 64), po.v(0, 512, o0, o0 + 64),
                             t1_.v(0, 512, o0, o0 + 64), ALU.mult)

                LA = 2
                for idx in range(len(tiles) + LA):
                    if idx < len(tiles):
                        emit_qk(idx)
                    if idx >= LA:
                        emit_pv(idx - LA)
                    if bg and idx % 2 == 1:
                        bg.pop(0)()
                return pt_base + len(tiles)

            pt_i = 0
            for f in make_thunks(0, sets[0]):
                f()
            for hp in range(4):
                bg = make_thunks(hp + 1, sets[(hp + 1) % 2]) if hp < 3 else []
                for hh in range(2):
                    pt_i = attention(hp, hh, sets[hp % 2], bg, pt_i)
                while bg:
                    bg.pop(0)()
            dbg("att", MIX.v(0, 4 * T), [128, 4 * T])

        def phase_mem(b):
            NW = 1024
            load_w("mem", wsi3, win3, C_QM, NW, 8)
            load_w("kv", wskv3, wkv3, 0, NW, 8, wb_off=8192)
            wk = Bump(WK, 0, WKSZ)
            MEMT = wk.get(BF16, 8 * 256)
            KM = wk.get(BF16, 4 * 256)
            VM = wk.get(BF16, 2 * 512)
            QM = [wk.get(BF16, T) for _ in range(2)]
            SGM = [wk.get(BF16, T) for _ in range(2)]
            PT = [wk.get(BF16, 512) for _ in range(2)]
            R = wk.get(F32, 512)
            T1 = wk.get(F32, 512)
            rms_transpose(lambda i: mem_d[b, i * 128:(i + 1) * 128, :], 2, P_MNW,
                          lambda i: MEMT.v3(8, 256, i * 128, 128), wk)
            for h in range(4):
                ps = PS[2 + h % 2]
                for k in range(8):
                    p.matmul(ps.v(0, 256), WB.v(8192 + k * NW + h * 128, 128), MEMT.v(k * 256, 256),
                             start=(k == 0), stop=(k == 7))
                p.copy(KM.v(h * 256, 256), ps.v(0, 256), eng="act")
            for mt in range(2):
                ps = PS[2 + mt % 2]
                for k in range(8):
                    p.matmul(ps.v(), MEMT.v(k * 256 + mt * 128, 128), WB.v(8192 + k * NW + 512, 512),
                             start=(k == 0), stop=(k == 7))
                p.copy(VM.v(mt * 512, 512), ps.v(), eng="dve")
            pt_i = 0
            for h in range(4):
                qm = QM[h % 2]
                sgm = SGM[h % 2]
                for tq in range(4):
                    ps = PS[2 + tq % 2]
                    proj_fm(NW, h * 128, 128, tq * 512, 512, ps.v())
                    p.copy(qm.v(tq * 512, 512), ps.v(), eng="act")
                for tq in range(4):
                    ps = PS[2 + tq % 2]
                    proj_fm(NW, 512 + h * 128, 128, tq * 512, 512, ps.v())
                    p.act(sgm.v(tq * 512, 512), ps.v(), AF.Silu)
                for qt in range(4):
                    tq0 = qt * 512
                    po = PS[qt % 2]
                    den = PS[6 + qt % 2]
                    for mt in range(2):
                        sp = PS[4 + pt_i % 2]
                        ptile = PT[pt_i % 2]
                        pt_i += 1
                        p.matmul(sp.v(), KM.v(h * 256 + mt * 128, 128), qm.v(tq0, 512))
                        p.act(ptile.v(), sp.v(), AF.Exp, scale=128 ** -0.5)
                        p.matmul(po.v(), VM.v(mt * 512 + h * 128, 128), ptile.v(), start=(mt == 0), stop=(mt == 1))
                        p.matmul(den.v(), ones, ptile.v(), start=(mt == 0), stop=(mt == 1))
                    p.recip(R.v(), den.v())
                    p.tt(T1.v(), R.v(), sgm.v(tq0, 512), ALU.mult, eng="pool")
                    p.tt(MIX.v((12 + h) * T + tq0, 512), po.v(), T1.v(), ALU.mult)
            dbg("memo", MIX.v(12 * T, 4 * T), [128, 4 * T])

        def phase_out(b):
            NW = 1024
            load_w("out", wso3, wout3, 0, NW, 16)
            wk = Bump(WK, 0, WKSZ)
            XT = [wk.get(F32, 1024) for _ in range(2)]
            HT = [wk.get(F32, 1024) for _ in range(2)]
            OT = [wk.get(F32, 1024) for _ in range(2)]
            JNK = wk.get(BF16, 1024)
            SSQ = wk.get(F32, 16)
            RSTD = wk.get(F32, 16)
            for i in range(16):
                xt = XT[i % 2]
                ht = HT[i % 2]
                ot = OT[i % 2]
                p.dma(xt.v(), D(x_d[b, i * 128:(i + 1) * 128, :]))
                for nh in range(2):
                    ps = PS[(2 * i + nh) % 4]
                    for m in range(16):
                        p.matmul(ps.v(), MIX.v(m * T + i * 128, 128), WB.v(m * NW + nh * 512, 512),
                                 start=(m == 0), stop=(m == 15))
                    p.tt(ht.v(nh * 512, 512), ps.v(), xt.v(nh * 512, 512), ALU.add)
                p.act(JNK.v(), ht.v(), AF.Square, accum_out=SSQ.v(i, 1))
                p.act(RSTD.v(i, 1), SSQ.v(i, 1), AF.Ln, scale=1.0 / DM, bias=epsv)
                p.act(RSTD.v(i, 1), RSTD.v(i, 1), AF.Exp, scale=-0.5)
                p.stt(ot.v(), ht.v(), RSTD.v(i, 1), prm(P_FNW, 1024), ALU.mult, ALU.mult)
                p.dma(D(out_d[b, i * 128:(i + 1) * 128, :]), ot.v())

        for b in range(nseq):
            wkA = Bump(WK, 16384, WKSZ - 16384)
            rms_transpose(lambda i: x_d[b, i * 128:(i + 1) * 128, :], 16, P_NW,
                          lambda i: UT.v3(BF16, 8, T, i * 128, 128), wkA)
            if b == 0:
                dbg("ut", UT.v(), [128, 8 * T])
            if "S" in phases:
                phase_ssd(b)
            if "B" in phases:
                phase_attn(b)
            if "M" in phases:
                phase_mem(b)
            if "E" in phases:
                phase_out(b)
        print("ops:", p.nops, {e: len(v) for e, v in p.ops.items()})
        p.emit()
    return nc, dumps


def kernel(x, mem, norm_w, w_in, conv_w, conv_b, dt_bias, a_log, d_skip, ssd_norm_w, mem_norm_w, w_mem_kv,
           w_out, final_norm_w):
    f = lambda a: np.ascontiguousarray(np.asarray(a, dtype=np.float32))
    x, mem = f(x), f(mem)
    cb, cf, kc, qc = host_consts()
    cf[:, CF_PRM:CF_PRM + NPRM] = host_params(f(norm_w)[0], f(conv_w)[0], f(conv_b)[0], f(dt_bias)[0], f(a_log)[0],
                                              f(d_skip)[0], f(ssd_norm_w)[0], f(mem_norm_w)[0], f(final_norm_w))
    nc, _ = build()
    shared = {"w_in": f(w_in)[0], "w_kv": f(w_mem_kv)[0], "w_out": f(w_out)[0], "cstb": cb, "cstf": cf,
              "kconst": kc, "qconst": qc}
    in_maps = []
    for c in range(8):
        m = dict(shared)
        m["x"] = x[c * NSEQ:(c + 1) * NSEQ]
        m["mem"] = mem[c * NSEQ:(c + 1) * NSEQ]
        in_maps.append(m)
    res = run_bass_kernel_spmd(nc, in_maps, core_ids=list(range(8)))
    return np.concatenate([r["out"] for r in res.results], axis=0)
```

```python
from contextlib import ExitStack
import concourse.bass as bass
import concourse.mybir as mybir

F32 = mybir.dt.float32
BF16 = mybir.dt.bfloat16
I32 = mybir.dt.int32
AF = mybir.ActivationFunctionType
ALU = mybir.AluOpType
AX = mybir.AxisListType
ESZ = {F32: 4, BF16: 2, I32: 4}


class View:
    __slots__ = ("ap", "arena", "rngs", "pr")

    def __init__(self, ap, arena, rngs, pr=(0, 128)):
        self.ap, self.arena, self.rngs, self.pr = ap, arena, rngs, pr

    def re(self, s, **kw):
        return View(self.ap.rearrange(s, **kw), self.arena, self.rngs, self.pr)

    def bc(self, shape):
        return View(self.ap.broadcast_to(shape), self.arena, self.rngs, self.pr)

    def __getitem__(self, key):
        return View(self.ap[key], self.arena, self.rngs, self.pr)


class Arena:
    def __init__(self, name, t, dtype, ncols, const=False):
        self.name, self.t, self.dtype, self.ncols = name, t, dtype, ncols
        self.esz = ESZ[dtype]
        self.w = []
        self.r = []
        self.const = const
        self.alt = {dtype: t}
        self.psum = False
        self.qlast = [dict() for _ in range(4)]

    def v(self, lo=0, n=None, p0=0, p1=128):
        if n is None:
            n = self.ncols - lo
        assert 0 <= lo and lo + n <= self.ncols, (self.name, lo, n, self.ncols)
        return View(self.t[p0:p1, lo:lo + n], self, [(lo * self.esz, (lo + n) * self.esz)], (p0, p1))

    def v3(self, dtype, nk, stride, lo, n, p0=0, p1=128):
        if dtype not in self.alt:
            self.alt[dtype] = self.t.bitcast(dtype)
        e = ESZ[dtype]
        assert ((nk - 1) * stride + lo + n) * e <= self.ncols * self.esz
        total = self.ncols * self.esz // e
        o = max(0, lo + nk * stride - total)
        assert o <= stride - n and o <= lo, (self.name, lo, nk, stride, n, total)
        ws = lo - o
        ap = self.alt[dtype][p0:p1, ws:ws + nk * stride].rearrange("p (k s) -> p k s", s=stride)[:, :, o:o + n]
        return View(ap, self, [((k * stride + lo) * e, (k * stride + lo + n) * e) for k in range(nk)], (p0, p1))

    def vb(self, dtype, lo, n, p0=0, p1=128):
        if dtype not in self.alt:
            self.alt[dtype] = self.t.bitcast(dtype)
        e = ESZ[dtype]
        assert (lo + n) * e <= self.ncols * self.esz
        return View(self.alt[dtype][p0:p1, lo:lo + n], self, [(lo * e, (lo + n) * e)], (p0, p1))


class Sub:
    def __init__(self, arena, dtype, boff, n):
        self.a, self.dt, self.e, self.n = arena, dtype, ESZ[dtype], n
        assert boff % self.e == 0
        self.base = boff // self.e

    def v(self, lo=0, n=None, p0=0, p1=128):
        if n is None:
            n = self.n - lo
        assert 0 <= lo and lo + n <= self.n, (lo, n, self.n)
        return self.a.vb(self.dt, self.base + lo, n, p0, p1)

    def v3(self, nk, stride, lo, n, p0=0, p1=128):
        assert (nk - 1) * stride + lo + n <= self.n
        return self.a.v3(self.dt, nk, stride, self.base + lo, n, p0, p1)


class Bump:
    def __init__(self, arena, boff, size):
        self.a, self.off, self.end = arena, boff, boff + size

    def get(self, dtype, n):
        self.off = (self.off + 3) // 4 * 4
        s = Sub(self.a, dtype, self.off, n)
        self.off += n * ESZ[dtype]
        assert self.off <= self.end, ("bump overflow", self.off, self.end)
        return s


def D(ap):
    return View(ap, None, [])


COMPUTE = ("pe", "act", "dve", "pool")


class Prog:
    def __init__(self, nc, ndma=40):
        self.nc = nc
        self.ops = {e: [] for e in COMPUTE + ("sp",)}
        self.count = {e: 0 for e in COMPUTE}
        self.waited = {e: {} for e in COMPUTE + ("sp",)}
        self.ndma = ndma
        self.dma_cnt = [0] * ndma
        self.dma_next = 0
        self.npool = 0
        self.nops = 0

    def op(self, eng, fn, reads=(), writes=(), dma=False):
        deps = {}

        def add(tok):
            k, v = tok
            if deps.get(k, 0) < v:
                deps[k] = v

        for vw in reads:
            a = vw.arena
            if a is None:
                continue
            for (lo, hi) in vw.rngs:
                for (l, h, t) in a.w:
                    if l < hi and lo < h:
                        add(t)
        for vw in writes:
            a = vw.arena
            if a is None:
                continue
            assert not a.const, a.name
            for (lo, hi) in vw.rngs:
                for (l, h, t) in a.w:
                    if l < hi and lo < h:
                        add(t)
                for (l, h, t) in a.r:
                    if l < hi and lo < h:
                        add(t)
        for vw in list(reads) + list(writes):
            a = vw.arena
            if a is None or not a.psum:
                continue
            for q in range(vw.pr[0] // 32, (vw.pr[1] + 31) // 32):
                for e2, t in a.qlast[q].items():
                    if e2 != eng:
                        add(t)
        if dma and eng == "pool":
            key = ("p", self.npool)
            self.npool += 1
            tok = (key, 16)
            inc = (key, 16)
        elif dma:
            i = self.dma_next
            self.dma_next = (i + 1) % self.ndma
            if self.dma_cnt[i] > 0:
                add((("d", i), self.dma_cnt[i]))
            self.dma_cnt[i] += 16
            tok = (("d", i), self.dma_cnt[i])
            inc = (("d", i), 16)
        else:
            self.count[eng] += 1
            tok = (eng, self.count[eng])
            inc = (eng, 1)
        waits = []
        wd = self.waited[eng]
        for k, v in deps.items():
            if k == eng and eng == "pe":
                continue
            if wd.get(k, 0) >= v:
                continue
            wd[k] = v
            waits.append((k, v))
        self.ops[eng].append((waits, fn, inc))
        self.nops += 1
        for vw in list(reads) + list(writes):
            a = vw.arena
            if a is None or not a.psum:
                continue
            for q in range(vw.pr[0] // 32, (vw.pr[1] + 31) // 32):
                a.qlast[q][eng] = tok
        for vw in reads:
            a = vw.arena
            if a is None or a.const:
                continue
            for (lo, hi) in vw.rngs:
                if not dma:
                    a.r = [x for x in a.r if not (x[2][0] == eng and lo <= x[0] and x[1] <= hi)]
                a.r.append((lo, hi, tok))
        for vw in writes:
            a = vw.arena
            if a is None:
                continue
            for (lo, hi) in vw.rngs:
                a.w = [x for x in a.w if not (lo <= x[0] and x[1] <= hi)]
                a.r = [x for x in a.r if not (lo <= x[0] and x[1] <= hi)]
                a.w.append((lo, hi, tok))
        return tok

    def freeze(self, arena):
        arena.const = True
        arena.r = []

    def matmul(self, out, lhsT, rhs, start=True, stop=True, **kw):
        return self.op("pe", lambda e: e.matmul(out.ap, lhsT.ap, rhs.ap, start=start, stop=stop, **kw),
                       [lhsT, rhs] + ([] if start else [out]), [out])

    def transpose(self, out, in_, ident):
        return self.op("pe", lambda e: e.transpose(out.ap, in_.ap, ident.ap), [in_, ident], [out])

    def act(self, out, in_, func, bias=None, scale=None, accum_out=None):
        rd = [in_]
        kw = {}
        if bias is not None:
            if isinstance(bias, View):
                rd.append(bias)
                kw["bias"] = bias.ap
            else:
                kw["bias"] = bias
        if scale is not None:
            if isinstance(scale, View):
                rd.append(scale)
                kw["scale"] = scale.ap
            else:
                kw["scale"] = scale
        wr = [out]
        if accum_out is not None:
            wr.append(accum_out)
            kw["accum_out"] = accum_out.ap
        return self.op("act", lambda e: e.activation(out.ap, in_.ap, func, **kw), rd, wr)

    def tt(self, out, in0, in1, op, eng="dve"):
        return self.op(eng, lambda e: e.tensor_tensor(out.ap, in0.ap, in1.ap, op), [in0, in1], [out])

    def ts(self, out, in0, s1, s2, op0, op1=None, eng="dve", accum_out=None):
        rd = [in0]
        a1 = s1
        a2 = s2
        if isinstance(s1, View):
            rd.append(s1)
            a1 = s1.ap
        if isinstance(s2, View):
            rd.append(s2)
            a2 = s2.ap
        kw = {}
        if op1 is not None:
            kw["op1"] = op1
        wr = [out]
        if accum_out is not None:
            kw["accum_out"] = accum_out.ap
            wr.append(accum_out)
        return self.op(eng, lambda e: e.tensor_scalar(out.ap, in0.ap, a1, a2, op0, **kw), rd, wr)

    def stt(self, out, in0, scalar, in1, op0, op1):
        rd = [in0, in1]
        sc = scalar
        if isinstance(scalar, View):
            rd.append(scalar)
            sc = scalar.ap
        return self.op("dve", lambda e: e.scalar_tensor_tensor(out.ap, in0.ap, sc, in1.ap, op0, op1), rd, [out])

    def copy(self, out, in_, eng="dve"):
        if eng == "act":
            return self.op("act", lambda e: e.copy(out.ap, in_.ap), [in_], [out])
        return self.op(eng, lambda e: e.tensor_copy(out.ap, in_.ap), [in_], [out])

    def reduce(self, out, in_, op, axis=AX.X, eng="dve"):
        return self.op(eng, lambda e: e.tensor_reduce(out.ap, in_.ap, axis, op), [in_], [out])

    def recip(self, out, in_):
        return self.op("dve", lambda e: e.reciprocal(out.ap, in_.ap), [in_], [out])

    def memset(self, out, val, eng="dve"):
        return self.op(eng, lambda e: e.memset(out.ap, val), [], [out])

    def scan(self, out, d0, d1, initial, op0, op1):
        rd = [d0, d1]
        ini = initial
        if isinstance(initial, View):
            rd.append(initial)
            ini = initial.ap
        return self.op("dve", lambda e: e.tensor_tensor_scan(out.ap, d0.ap, d1.ap, ini, op0, op1), rd, [out])

    def affine_select(self, out, in_, pattern, cmp, fill, base, cm):
        return self.op("pool", lambda e: e.affine_select(out.ap, in_.ap, pattern, cmp, fill, base=base,
                                                         channel_multiplier=cm), [in_], [out])

    def iota(self, out, pattern, base, cm):
        return self.op("pool", lambda e: e.iota(out.ap, pattern, base=base, channel_multiplier=cm), [], [out])

    def dma(self, out, in_, q="sp", **kw):
        if q == "pool":
            kw.setdefault("max_dma_last_dim", 4096)
        return self.op(q, lambda e: e.dma_start(out=out.ap, in_=in_.ap, **kw), [in_], [out], dma=True)

    def emit(self):
        nc = self.nc
        with ExitStack() as es:
            sems = {}
            for e in COMPUTE:
                sems[e] = es.enter_context(nc.semaphore("s_" + e))
            for i in range(self.ndma):
                sems[("d", i)] = es.enter_context(nc.semaphore("s_d%d" % i))
            for i in range(self.npool):
                sems[("p", i)] = es.enter_context(nc.semaphore("s_p%d" % i))
            fin = []
            for i in range(self.ndma):
                if self.dma_cnt[i] > 0:
                    fin.append((("d", i), self.dma_cnt[i]))
            for i in range(self.npool):
                fin.append((("p", i), 16))
            for e in COMPUTE:
                if self.count[e] > 0:
                    fin.append((e, self.count[e]))
            block = es.enter_context(nc.Block())

            def run(stream, final=None):
                def f(eng):
                    for waits, fn, inc in stream:
                        for k, v in waits:
                            eng.wait_ge(sems[k], v)
                        ins = fn(eng)
                        ins.then_inc(sems[inc[0]], inc[1])
                    if final:
                        for k, v in final:
                            eng.wait_ge(sems[k], v)
                return f

            block.tensor(run(self.ops["pe"]))
            block.scalar(run(self.ops["act"]))
            block.vector(run(self.ops["dve"]))
            block.gpsimd(run(self.ops["pool"]))
            block.sync(run(self.ops["sp"], fin))
from concourse.bass_utils import run_bass_kernel_spmd
import numpy as np

T = 2048
DM = 1024
NSEQ = 2
EPS = 1e-6
BIG = 30000.0
C_Q, C_K, C_V, C_G, C_Z, C_XBC, C_DT, C_QM, C_GM = 0, 512, 1024, 1536, 2048, 3072, 5120, 5136, 5648
CB_ID, CB_TRI, CB_MNEG, CB_ONES, CB_EH, NCB = 0, 128, 256, 384, 512, 2560
CF_ID, CF_ONES, CF_PRM, CF_CAP, CF_CMK, CF_OWN, CF_MISC, NCF = 0, 128, 256, 1424, 1488, 1552, 1616, 1624
P_NW, P_MNW, P_SNW, P_CVB, P_CVW, P_DSK, P_DTB, P_ALOG, P_FNW = 0, 8, 16, 24, 40, 104, 112, 128, 144
NPRM = 1168


def host_consts():
    cb = np.zeros((128, NCB), np.float32)
    cb[:, CB_ID:CB_ID + 128] = np.eye(128)
    s = np.arange(128)[:, None]
    t = np.arange(128)[None, :]
    cb[:, CB_TRI:CB_TRI + 128] = (t >= s)
    cb[:, CB_MNEG:CB_MNEG + 128] = np.where(t < s, -BIG, 0.0)
    cb[:, CB_ONES:CB_ONES + 128] = 1.0
    for h in range(16):
        cb[h, CB_EH + h * 128: CB_EH + (h + 1) * 128] = 1.0
    cf = np.zeros((128, NCF), np.float32)
    cf[:, CF_ID:CF_ID + 128] = np.eye(128)
    cf[:, CF_ONES:CF_ONES + 128] = 1.0
    cap = np.zeros((8, 8), np.float32)
    cmk = np.zeros((8, 8), np.float32)
    own = np.zeros((8, 8), np.float32)
    for ti in range(8):
        qb = 4 + ti // 2
        for j in range(8):
            cap[ti, j] = 1e30 if j < qb else -1e30
            cmk[ti, j] = 1.0 if j < qb else 0.0
            own[ti, j] = 1.0 if j == qb else 0.0
    cf[:, CF_CAP:CF_CAP + 64] = cap.reshape(-1)[None, :]
    cf[:, CF_CMK:CF_CMK + 64] = cmk.reshape(-1)[None, :]
    cf[:, CF_OWN:CF_OWN + 64] = own.reshape(-1)[None, :]
    cf[:, CF_MISC] = EPS
    cf[:, CF_MISC + 1] = 1.0
    kc = np.zeros((128, T), np.float32)
    pos = np.arange(T)
    for r in range(8):
        kc[64 + r] = (pos // 256 == r)
    kc[96] = pos // 16
    kc[97] = pos % 16
    kc[98] = 1.0
    kc[99] = 1.0
    qc = np.zeros((8, 4, T), np.float32)
    for h in range(8):
        sl = 2.0 ** (-8.0 * (h + 1) / 8)
        qc[h, 0] = 16 * sl * 8
        qc[h, 1] = sl * 8
        qc[h, 2] = -16 * sl * 8 * (pos // 16)
        qc[h, 3] = -sl * 8 * (pos % 16)
    return cb, cf, kc, qc


def host_params(norm_w, conv_w, conv_b, dt_bias, a_log, d_skip, ssd_norm_w, mem_norm_w, final_norm_w):
    prm = np.zeros((128, NPRM), np.float32)
    prm[:, P_NW:P_NW + 8] = norm_w.reshape(8, 128).T
    prm[:, P_MNW:P_MNW + 8] = mem_norm_w.reshape(8, 128).T
    prm[:, P_SNW:P_SNW + 8] = ssd_norm_w.reshape(8, 128).T
    prm[:, P_CVB:P_CVB + 16] = conv_b.reshape(16, 128).T
    prm[:, P_CVW:P_CVW + 64] = conv_w.T.reshape(16, 128, 4).transpose(1, 0, 2).reshape(128, 64)
    prm[:, P_DSK:P_DSK + 8] = np.repeat(d_skip, 64).reshape(8, 128).T
    prm[:, P_DTB:P_DTB + 16] = dt_bias[None, :]
    prm[:, P_ALOG:P_ALOG + 16] = a_log[None, :]
    prm[:, P_FNW:P_FNW + 1024] = final_norm_w[None, :]
    return prm
def build(nseq=NSEQ, dump=None, phases="ASBME"):
    nc = bass.Bass("TRN2", target_bir_lowering=False)
    x_d = nc.dram_tensor("x", [nseq, T, DM], F32, kind="ExternalInput").ap()
    mem_d = nc.dram_tensor("mem", [nseq, 256, DM], F32, kind="ExternalInput").ap()
    win_d = nc.dram_tensor("w_in", [DM, 6160], F32, kind="ExternalInput").ap()
    wkv_d = nc.dram_tensor("w_kv", [DM, 1024], F32, kind="ExternalInput").ap()
    wout_d = nc.dram_tensor("w_out", [2048, DM], F32, kind="ExternalInput").ap()
    cb_d = nc.dram_tensor("cstb", [128, NCB], F32, kind="ExternalInput").ap()
    cf_d = nc.dram_tensor("cstf", [128, NCF], F32, kind="ExternalInput").ap()
    kc_d = nc.dram_tensor("kconst", [128, T], F32, kind="ExternalInput").ap()
    qc_d = nc.dram_tensor("qconst", [8, 4, T], F32, kind="ExternalInput").ap()
    out_d = nc.dram_tensor("out", [nseq, T, DM], F32, kind="ExternalOutput").ap()
    dumps = {}
    wsi_d = nc.dram_tensor("wsc_in", [DM, 6160], BF16, kind="Internal").ap()
    wskv_d = nc.dram_tensor("wsc_kv", [DM, 1024], BF16, kind="Internal").ap()
    wso_d = nc.dram_tensor("wsc_out", [2048, DM], BF16, kind="Internal").ap()
    kcs_d = nc.dram_tensor("kc_sc", [128, T], BF16, kind="Internal").ap()
    qcs_d = nc.dram_tensor("qc_sc", [32, T], BF16, kind="Internal").ap()
    qc2_d = qc_d.rearrange("h r t -> (h r) t")
    wsi3 = wsi_d.rearrange("(k p) c -> p k c", p=128)
    wskv3 = wskv_d.rearrange("(k p) c -> p k c", p=128)
    wso3 = wso_d.rearrange("(k p) c -> p k c", p=128)
    win3 = win_d.rearrange("(k p) c -> p k c", p=128)
    wkv3 = wkv_d.rearrange("(k p) c -> p k c", p=128)
    wout3 = wout_d.rearrange("(k p) c -> p k c", p=128)

    with ExitStack() as es:
        def sb(name, n, dt):
            t = es.enter_context(nc.sbuf_tensor(name, [128, n], dt))
            return Arena(name, t, dt, n)

        UT = sb("UT", 8 * T, BF16)
        MIX = sb("MIX", 16 * T, BF16)
        WB = sb("WB", 8 * 3088, BF16)
        WKSZ = 47616
        WK = sb("WK", WKSZ // 2, BF16)
        CB = sb("CB", NCB, BF16)
        CF = sb("CF", NCF, F32)
        SM = sb("SM", 64, F32)
        PS = []
        for i in range(8):
            t = es.enter_context(nc.psum_tensor("ps%d" % i, [128, 512], F32))
            PS.append(Arena("ps%d" % i, t, F32, 512))
            PS[-1].psum = True
        p = Prog(nc)

        def dbg(name, view, shape):
            if dump is None or name not in dump:
                return
            d = nc.dram_tensor("dbg_" + name, list(shape), view.ap.dtype, kind="ExternalOutput").ap()
            dumps[name] = d
            p.dma(D(d), view)

        p.dma(CB.v(), D(cb_d), q="pool")
        p.dma(CF.v(), D(cf_d))
        ident = CB.v(CB_ID, 128)
        tri = CB.v(CB_TRI, 128)
        mneg = CB.v(CB_MNEG, 128)
        ones = CB.v(CB_ONES, 128)
        identf = CF.v(CF_ID, 128)
        epsv = CF.v(CF_MISC, 1)
        onev = CF.v(CF_MISC + 1, 1)

        def prm(off, n, p0=0, p1=128):
            return CF.v(CF_PRM + off, n, p0, p1)

        AB = SM.v(0, 16)
        p.act(AB, prm(P_ALOG, 16), AF.Exp)
        p.ts(AB, AB, -1.0, None, ALU.mult)

        SC = {n: Arena("sc_" + n, None, BF16, 1) for n in ("attn", "mem", "ssd", "kv", "out", "kc", "qc")}
        converted = [False]

        def scv(ap, name, k):
            return View(ap, SC[name], [(k, k + 1)])

        NHALF = {"attn": 4, "mem": 4, "ssd": 4, "kv": 4, "out": 8}

        def convert_all():
            p.dma(scv(kcs_d, "kc", 0), D(kc_d), q="pool")
            p.dma(scv(qcs_d, "qc", 0), D(qc2_d), q="pool")
            for name, src, dst, c0, nw, nrows in (("attn", win_d, wsi_d, 0, 2048, 1024), ("kv", wkv_d, wskv_d, 0, 1024, 1024),
                                                  ("mem", win_d, wsi_d, C_QM, 1024, 1024), ("out", wout_d, wso_d, 0, 1024, 2048),
                                                  ("ssd", win_d, wsi_d, C_Z, 3088, 1024)):
                hr = nrows // 2
                for h in range(2):
                    p.dma(scv(dst[h * hr:(h + 1) * hr, c0:c0 + nw], name, h), D(src[h * hr:(h + 1) * hr, c0:c0 + nw]),
                          q="pool")
            converted[0] = True

        def load_w(name, dst3, src3, c0, nw, nk, wb_off=0):
            for k in range(nk):
                if converted[0]:
                    p.dma(WB.v(wb_off + k * nw, nw), scv(dst3[:, k, c0:c0 + nw], name, k // NHALF[name]), q="sp")
                else:
                    p.dma(WB.v(wb_off + k * nw, nw), D(src3[:, k, c0:c0 + nw]), q="pool")

        pj_i = [0]

        def rms_transpose(src_tile, ntiles, nwoff, dst_fn, wk):
            XT = [wk.get(F32, 1024) for _ in range(2)]
            XS = [wk.get(BF16, 1024) for _ in range(2)]
            JNK = wk.get(BF16, 1024)
            SSQ = wk.get(F32, 16)
            RSTD = wk.get(F32, 16)
            for i in range(ntiles):
                xt = XT[i % 2]
                p.dma(xt.v(), D(src_tile(i)))
                p.act(JNK.v(), xt.v(), AF.Square, accum_out=SSQ.v(i, 1))
                p.act(RSTD.v(i, 1), SSQ.v(i, 1), AF.Ln, scale=1.0 / DM, bias=epsv)
                p.act(RSTD.v(i, 1), RSTD.v(i, 1), AF.Exp, scale=-0.5)
                xs = XS[i % 2]
                p.ts(xs.v(), xt.v(), RSTD.v(i, 1), None, ALU.mult)
                pb = PS[i % 2]
                for k in range(8):
                    p.transpose(pb.vb(BF16, k * 128, 128), xs.v(k * 128, 128), ident)
                p.tt(dst_fn(i), pb.vb(BF16, 0, 1024).re("p (k t) -> p k t", k=8),
                     prm(nwoff, 8).re("p (k o) -> p k o", o=1).bc([128, 8, 128]), ALU.mult)

        def utv(k, t0, n):
            return UT.v(k * T + t0, n)

        def proj_fm(nw, c0, ncols, t0, n, ps_view):
            for k in range(8):
                p.matmul(ps_view, WB.v(k * nw + c0, ncols), utv(k, t0, n), start=(k == 0), stop=(k == 7))

        def phase_ssd(b):
            NW = 3088
            load_w("ssd", wsi3, win3, C_Z, NW, 8)
            if not converted[0]:
                convert_all()
            b1 = Bump(MIX, 0, 16384)
            b2 = Bump(MIX, 12 * T * 2, 16384)
            wk = Bump(WK, 0, WKSZ)
            XPRE = b1.get(BF16, 16 * 259)
            XST = b1.get(BF16, 8 * 256)
            BMT = b1.get(BF16, 4 * 256)
            LT = [b1.get(BF16, 384) for _ in range(2)]
            SZ = b2.get(BF16, 8 * 256)
            PREV = b2.get(F32, 1024)
            XDD = b2.get(BF16, 2 * 1024)
            CMT = b2.get(BF16, 4 * 256)
            BTOK = b2.get(BF16, 2 * 512)
            DIAG = wk.get(BF16, 64 * 128)
            CBT = wk.get(BF16, 4 * 384)
            L0 = [wk.get(BF16, 256) for _ in range(2)]
            MT = [wk.get(BF16, 384) for _ in range(2)]
            GT = [wk.get(BF16, 256) for _ in range(2)]
            GG = [wk.get(F32, 256) for _ in range(4)]
            GSQ = [wk.get(BF16, 256) for _ in range(4)]
            RS = wk.get(F32, 256)
            XDT = wk.get(BF16, 2 * 2048)
            PREVB = wk.get(BF16, 2048)
            DTR = wk.get(F32, 32)
            DT = wk.get(F32, 32)
            DA = wk.get(F32, 256)
            NCS = wk.get(F32, 32)
            DEC = wk.get(F32, 32)
            W2 = wk.get(F32, 32)
            CST = wk.get(F32, 256)
            CSH = wk.get(BF16, 256)
            CSL = wk.get(BF16, 256)
            DECT = wk.get(F32, 256)
            DG16 = wk.get(F32, 16)
            CDB = wk.get(F32, 16)

            for zt in (XDT, PREVB, DA, CST, CSH, CSL, DECT, DG16):
                p.memset(zt.v(), 0.0, eng="pool")
            for j in range(64):
                p.ts(DIAG.v(j * 128, 128), ident, prm(P_CVW + j, 1), None, ALU.mult,
                     eng=("pool" if j % 2 else "dve"))
            p.memset(XPRE.v3(16, 259, 0, 3), 0.0)
            p.memset(PREV.v(), 0.0)

            for c in range(8):
                t0 = c * 256

                def zgroup(zc):
                    ps = PS[zc % 2].v((zc // 2 % 2) * 256, 256)
                    proj_fm(NW, zc * 128, 128, t0, 256, ps)
                    p.copy(SZ.v(zc * 256, 256), ps, eng="dve")

                def xgroup(cc):
                    ps = PS[cc % 2].v((cc // 2 % 2) * 256, 256)
                    proj_fm(NW, 1024 + cc * 128, 128, t0, 256, ps)
                    p.copy(XPRE.v(cc * 259 + 3, 256), ps, eng=("dve" if cc % 2 else "act"))

                if c > 0:
                    p.copy(XPRE.v3(16, 259, 0, 3), XPRE.v3(16, 259, 256, 3), eng="pool")
                for lt in range(2):
                    for k in range(8):
                        p.matmul(PS[5].v(384 + lt * 16, 16), utv(k, t0 + lt * 128, 128), WB.v(k * NW + 3072, 16),
                                 start=(k == 0), stop=(k == 7))
                p.tt(DTR.v(), PS[5].v(384, 32).re("p (l h) -> p l h", l=2),
                     prm(P_DTB, 16).re("p (o h) -> p o h", o=1).bc([128, 2, 16]), ALU.add)
                p.act(DTR.v(), DTR.v(), AF.Exp)
                p.act(DT.v(), DTR.v(), AF.Ln, bias=onev)
                p.tt(DA.v3(2, 128, 0, 16), DT.v().re("p (l h) -> p l h", l=2),
                     SM.v(0, 16).re("p (o h) -> p o h", o=1).bc([128, 2, 16]), ALU.mult)
                for zc in range(6):
                    zgroup(zc)
                for lt in range(2):
                    p.transpose(PS[6].v(lt * 128, 128), DA.v(lt * 128, 128), identf)
                p.scan(CST.v(0, 256, 0, 16), CF.v(CF_ONES, 1, 0, 16).bc([16, 256]), PS[6].v(0, 256, 0, 16), 0.0,
                       ALU.mult, ALU.add)
                p.copy(CSH.v(0, 256, 0, 16), CST.v(0, 256, 0, 16))
                p.tt(CSL.v(0, 256, 0, 16), CST.v(0, 256, 0, 16), CSH.v(0, 256, 0, 16), ALU.subtract)
                p.act(DECT.v(0, 256, 0, 16), CST.v(0, 256, 0, 16), AF.Exp, scale=-1.0, bias=CST.v(255, 1, 0, 16))
                for zc in range(6, 8):
                    zgroup(zc)
                for cc in range(8, 12):
                    xgroup(cc)
                for lt in range(2):
                    p.transpose(PS[7].v(lt * 128, 128), CST.v(lt * 128, 128), identf)
                    p.transpose(PS[7].v(256 + lt * 128, 128), DECT.v(lt * 128, 128), identf)
                p.ts(NCS.v().re("p (l h) -> p l h", l=2), PS[7].v3(F32, 2, 128, 0, 16), -1.0, None, ALU.mult)
                p.copy(DEC.v().re("p (l h) -> p l h", l=2), PS[7].v3(F32, 2, 128, 256, 16))
                p.tt(W2.v(), DT.v(), DEC.v(), ALU.mult)
                if c < 7:
                    p.ts(DG16.v(0, 16, 0, 16), CF.v(CF_ID, 16, 0, 16), CST.v(255, 1, 0, 16), None, ALU.mult)
                    p.matmul(PS[5].v(480, 16), CF.v(CF_ONES, 128), DG16.v())
                    p.act(CDB.v(), PS[5].v(480, 16), AF.Exp)
                for cc in list(range(12, 16)) + list(range(0, 8)):
                    xgroup(cc)

                def conv(cc):
                    ps = PS[cc % 2].v((cc // 2 % 2) * 256, 256)
                    for k in range(4):
                        p.matmul(ps, DIAG.v((cc * 4 + k) * 128, 128), XPRE.v(cc * 259 + k, 256),
                                 start=(k == 0), stop=(k == 3))
                    if cc < 8:
                        dst = XST.v(cc * 256, 256)
                    elif cc < 12:
                        dst = BMT.v((cc - 8) * 256, 256)
                    else:
                        dst = CMT.v((cc - 12) * 256, 256)
                    p.act(dst, ps, AF.Silu, bias=prm(P_CVB + cc, 1))

                for cc in range(8, 16):
                    conv(cc)
                for lt in range(2):
                    pb = PS[7]
                    for g in range(4):
                        p.transpose(pb.vb(BF16, g * 128, 128), BMT.v(g * 256 + lt * 128, 128), ident)
                    p.copy(BTOK.v(lt * 512, 512), pb.vb(BF16, 0, 512), eng="dve")
                for g in range(4):
                    ps = PS[5]
                    p.matmul(ps.v(0, 256), BMT.v(g * 256, 128), CMT.v(g * 256, 256))
                    p.matmul(ps.v(256, 128), BMT.v(g * 256 + 128, 128), CMT.v(g * 256 + 128, 128))
                    p.copy(CBT.v(g * 384, 384), ps.v(0, 384), eng="dve")
                for cc in range(0, 8):
                    conv(cc)
                p.act(SZ.v(), SZ.v(), AF.Silu)
                for lt in range(2):
                    pb = PS[6]
                    for cc in range(8):
                        p.transpose(pb.vb(BF16, cc * 128, 128), XST.v(cc * 256 + lt * 128, 128), ident)
                    src = pb.vb(BF16, 0, 1024).re("p (h q) -> p h q", h=16)
                    for hh in range(2):
                        p.tt(XDT.v(lt * 2048, 2048).re("p (c e q) -> p c e q", c=8, e=2)[:, :, hh, hh * 64:hh * 64 + 64],
                             src.re("p (c e) q -> p c e q", e=2)[:, :, hh, :],
                             DT.v(lt * 16, 16).re("p (c e o) -> p c e o", e=2, o=1)[:, :, hh, :].bc([128, 8, 64]),
                             ALU.mult)
                    p.tt(XDD.v(lt * 1024, 1024).re("p (h q) -> p h q", h=16), src,
                         W2.v(lt * 16, 16).re("p (h o) -> p h o", o=1).bc([128, 16, 64]), ALU.mult)
                def seg_stage(h):
                    g = h // 4
                    sg = PS[2 + h % 2]
                    eh = CB.v(CB_EH + h * 128, 128)
                    csh = lambda lo, n: CSH.v(lo, n)
                    csl = lambda lo, n: CSL.v(lo, n)
                    p.matmul(sg.v(0, 256), eh, csh(0, 256), start=True, stop=False)
                    p.matmul(sg.v(0, 256), eh, csl(0, 256), start=False, stop=True)
                    p.matmul(sg.v(256, 128), eh, csh(0, 128), start=True, stop=False)
                    p.matmul(sg.v(256, 128), eh, csl(0, 128), start=False, stop=False)
                    p.matmul(sg.v(256, 128), ident, mneg, start=False, stop=True)
                    p.matmul(sg.v(384, 128), eh, csh(128, 128), start=True, stop=False)
                    p.matmul(sg.v(384, 128), eh, csl(128, 128), start=False, stop=False)
                    p.matmul(sg.v(384, 128), ident, mneg, start=False, stop=True)
                    l0 = L0[h % 2]
                    lt_ = LT[h % 2]
                    mt = MT[h % 2]
                    gt = GT[h % 2]
                    if c > 0:
                        p.act(l0.v(), sg.v(0, 256), AF.Exp)
                    p.act(lt_.v(0, 128), sg.v(256, 128), AF.Exp, bias=NCS.v(h, 1))
                    p.act(lt_.v(128, 128), sg.v(128, 128), AF.Exp, bias=NCS.v(h, 1))
                    p.act(lt_.v(256, 128), sg.v(384, 128), AF.Exp, bias=NCS.v(16 + h, 1))
                    p.tt(mt.v(), lt_.v(), CBT.v(g * 384, 384), ALU.mult)
                    if c > 0:
                        p.tt(gt.v(), l0.v(), CMT.v(g * 256, 256), ALU.mult, eng="pool")

                def y_stage(h):
                    g = h // 4
                    pr = h % 2
                    pc = h // 2
                    mt = MT[h % 2]
                    gt = GT[h % 2]
                    ybank = PS[4 + 2 * (pc % 2)]
                    yt = ybank.v(0, 256)
                    yt2 = ybank.v(128, 128)
                    p.matmul(yt, XDT.v(h * 128, 128), mt.v(0, 256), start=(pr == 0), stop=False)
                    p.matmul(yt2, XDT.v(2048 + h * 128, 128), mt.v(256, 128), start=False, stop=(c == 0 and pr == 1))
                    if c > 0:
                        p.matmul(yt, PREVB.v(h * 128, 128), gt.v(), start=False, stop=(pr == 1))
                    if pr == 1:
                        gg = GG[pc % 4]
                        p.stt(gg.v(), XST.v(pc * 256, 256), prm(P_DSK + pc, 1), yt, ALU.mult, ALU.add)
                        p.tt(gg.v(), gg.v(), SZ.v(pc * 256, 256), ALU.mult)
                        p.act(GSQ[pc % 4].v(), gg.v(), AF.Square)
                def norm_stage(g):
                        pc = 2 * g + 1
                        pcs = [pc - 1, pc]
                        ss = PS[7].v(256, 256)
                        p.matmul(ss, ones, GSQ[pcs[0] % 4].v(), start=True, stop=False)
                        p.matmul(ss, ones, GSQ[pcs[1] % 4].v(), start=False, stop=True)
                        p.act(RS.v(), ss, AF.Ln, scale=1.0 / 256, bias=epsv)
                        p.act(RS.v(), RS.v(), AF.Exp, scale=-0.5)
                        for q in pcs:
                            p.stt(MIX.v((4 + q) * T + t0, 256), GG[q % 4].v(), prm(P_SNW + q, 1), RS.v(),
                                  ALU.mult, ALU.mult)

                for h in range(18):
                    if h < 16:
                        seg_stage(h)
                    if 1 <= h <= 16:
                        y_stage(h - 1)
                    if h >= 2 and (h - 2) % 4 == 3:
                        norm_stage((h - 2) // 4)
                if c < 7:
                    for g in range(4):
                        st = PS[7].v(0, 256)
                        for lt in range(2):
                            p.matmul(st, BTOK.v(lt * 512 + g * 128, 128), XDD.v(lt * 1024 + g * 256, 256),
                                     start=(lt == 0), stop=(lt == 1))
                        pv = PREV.v(g * 256, 256)
                        p.tt(pv.re("p (h q) -> p h q", h=4), pv.re("p (h q) -> p h q", h=4),
                             CDB.v(g * 4, 4).re("p (h o) -> p h o", o=1).bc([128, 4, 64]), ALU.mult)
                        p.tt(pv, pv, st, ALU.add)
                        for hh in range(2):
                            p.copy(PREVB.v(g * 512, 512).re("p (c e q) -> p c e q", c=2, e=2)[:, :, hh, hh * 64:hh * 64 + 64],
                                   pv.re("p (c e q) -> p c e q", c=2, e=2)[:, :, hh, :], eng="pool")
            dbg("ssd", MIX.v(4 * T, 8 * T), [128, 8 * T])

        def attn_core(kt_list_fn, nq, kv_lhsT, q_rhs, v_lhsT, scale, PTs, epilogue, diag_fn=None, den_lhsT=None):
            pass

        def phase_attn(b):
            NW = 2048
            load_w("attn", wsi3, win3, 0, NW, 8)
            wk = Bump(WK, 0, WKSZ)
            bm = Bump(MIX, 12 * T * 2, 16384)
            bw = Bump(WB, 8 * NW * 2, WB.ncols * 2 - 8 * NW * 2)
            sets = [dict(QK=[wk.get(BF16, T) for _ in range(4)], VA=wk.get(BF16, 2048), VB=wk.get(BF16, 2048),
                         SG=wk.get(BF16, T)),
                    dict(QK=[bm.get(BF16, T) for _ in range(4)], VA=bw.get(BF16, 2048), VB=bw.get(BF16, 2048),
                         SG=bw.get(BF16, T))]
            PT = [wk.get(BF16, 512) for _ in range(4)]
            R = [wk.get(F32, 512) for _ in range(2)]
            T1 = [wk.get(F32, 512) for _ in range(2)]
            PENB = wk.get(BF16, 8 * 128)
            GM = wk.get(F32, 64)
            CMP = wk.get(F32, 512)
            RANK = wk.get(F32, 64)
            KSUM = wk.get(F32, 8)
            KMT = wk.get(BF16, 8)
            p.memset(KMT.v(), 0.0, eng="pool")
            p.memset(PENB.v(), 0.0, eng="pool")
            for S in sets:
                for i in range(2):
                    p.memset(S["QK"][i].v(), 0.0, eng="pool")
                    p.dma(S["QK"][2 + i].v(), scv(kcs_d, "kc", 0), q="sp")
                p.memset(S["VA"].v(), 1.0, eng="pool")
                p.memset(S["VB"].v(), 1.0, eng="pool")

            def make_thunks(hp, S):
                QK, VA, VB, SG = S["QK"], S["VA"], S["VB"], S["SG"]
                th = []

                def t_qc():
                    for hh in range(2):
                        h_ = 2 * hp + hh
                        p.dma(QK[hh].v(0, T, 96, 100), scv(qcs_d[4 * h_:4 * h_ + 4, :], "qc", 0), q="sp")
                th.append(t_qc)
                def half_proj(c0, tq, half, evac):
                    def f():
                        ps = PS[2 + tq % 2]
                        if half == 1:
                            for k in range(8):
                                p.matmul(ps.v(), WB.v(k * NW + c0, 128), utv(k, tq * 512, 512), start=(k == 0), stop=(k == 7))
                            evac(ps)
                    return f

                for which, c0 in ((0, C_Q), (1, C_K)):
                    for tq in range(4):
                        def ev(ps, which=which, tq=tq):
                            p.copy(QK[2 * which].v(tq * 512, 512, 0, 64), ps.v(0, 512, 0, 64), eng="act")
                            p.copy(QK[2 * which + 1].v(tq * 512, 512, 0, 64), ps.v(0, 512, 64, 128), eng="dve")
                        th.append(half_proj(c0 + hp * 128, tq, 0, ev))
                        th.append(half_proj(c0 + hp * 128, tq, 1, ev))

                def gating(hh):
                    Q = QK[hh]
                    K = QK[2 + hh]
                    p.reduce(KSUM.v(0, 8, 0, 64), K.v(0, T, 0, 64).re("p (j s) -> p j s", j=8), ALU.add)
                    p.ts(KMT.v(0, 8, 0, 64), KSUM.v(0, 8, 0, 64), 1.0 / 256, None, ALU.mult)
                    gps = PS[2]
                    for ti in range(8):
                        p.matmul(gps.v(ti * 8, 8), Q.v(1024 + ti * 128, 128), KMT.v())
                    p.tt(GM.v(), gps.v(0, 64), CF.v(CF_CAP, 64), ALU.min)
                    g3 = GM.v().re("p (t j) -> p t j", t=8)
                    p.tt(CMP.v().re("p (t j k) -> p t j k", t=8, j=8),
                         g3.re("p t (o k) -> p t o k", o=1).bc([128, 8, 8, 8]),
                         g3.re("p t (j o) -> p t j o", o=1).bc([128, 8, 8, 8]), ALU.is_gt)
                    p.reduce(RANK.v(), CMP.v().re("p (a k) -> p a k", k=8), ALU.add)
                    p.ts(RANK.v(), RANK.v(), 3.0, None, ALU.is_lt)
                    p.tt(RANK.v(), RANK.v(), CF.v(CF_CMK, 64), ALU.mult)
                    p.tt(RANK.v(), RANK.v(), CF.v(CF_OWN, 64), ALU.add)
                    p.ts(PENB.v3(8, 128, 64, 8), RANK.v().re("p (t j) -> p t j", t=8), -1.0, BIG, ALU.add, ALU.mult)
                    pps = PS[3]
                    for ti in range(8):
                        p.transpose(pps.vb(BF16, ti * 128, 128), PENB.v(ti * 128, 128), ident)
                    p.copy(Q.v(1024, 1024, 64, 72), pps.vb(BF16, 0, 1024, 64, 72), eng="dve")

                for tq in range(4):
                    def ev(ps, tq=tq):
                        p.copy(SG.v(tq * 512, 512), ps.v(), eng="dve")
                    th.append(half_proj(C_G + hp * 128, tq, 0, ev))
                    th.append(half_proj(C_G + hp * 128, tq, 1, ev))
                th.append(lambda: gating(0))
                for tg in range(4):
                    for j in range(4):
                        def f(tg=tg, j=j):
                            ps = PS[2 + tg % 2]
                            ti = tg * 4 + j
                            for k in range(8):
                                p.matmul(ps.v(j * 128, 128), utv(k, ti * 128, 128),
                                         WB.v(k * NW + C_V + hp * 128, 128), start=(k == 0), stop=(k == 7))
                            if j == 3:
                                src = ps.v().re("p (j c) -> p j c", j=4)
                                p.copy(VA.v3(4, 128, tg * 512, 64), src[:, :, 0:64], eng="dve")
                                p.copy(VB.v3(4, 128, tg * 512 + 64, 64), src[:, :, 64:128], eng="dve")
                        th.append(f)
                th.append(lambda: gating(1))
                th.append(lambda: p.act(SG.v(), SG.v(), AF.Silu))
                return th

            def attention(hp, hh, S, bg, pt_base):
                Q = S["QK"][hh]
                K = S["QK"][2 + hh]
                V = S["VA"] if hh == 0 else S["VB"]
                SG = S["SG"]
                tiles = []
                for qt in range(4):
                    order = [4 * qt + r for r in range(4)] + list(range(4 * qt))
                    for j, kt in enumerate(order):
                        tiles.append((qt, kt, j == 0, j == len(order) - 1))
                o0, d0 = (0, 64) if hh == 0 else (64, 0)

                def geom(idx):
                    qt, kt, first, last = tiles[idx]
                    tq0 = qt * 512
                    s0 = kt * 128
                    qlo = max(tq0, s0)
                    return qt, kt, tq0, s0, qlo, tq0 + 512 - qlo

                def emit_qk(idx):
                    qt, kt, tq0, s0, qlo, n = geom(idx)
                    gi = pt_base + idx
                    sp = PS[4 + gi % 4]
                    ptile = PT[gi % 4]
                    p.matmul(sp.v(0, n), K.v(s0, 128), Q.v(qlo, n))
                    p.act(ptile.v(0, n), sp.v(0, n), AF.Exp, scale=0.125)
                    if s0 >= tq0:
                        p.tt(ptile.v(0, 128), ptile.v(0, 128), tri, ALU.mult, eng="pool")

                def emit_pv(idx):
                    qt, kt, tq0, s0, qlo, n = geom(idx)
                    gi = pt_base + idx
                    first, last = tiles[idx][2], tiles[idx][3]
                    po = PS[qt % 2]
                    ptile = PT[gi % 4]
                    p.matmul(po.v(qlo - tq0, n), V.v(kt * 128, 128), ptile.v(0, n), start=first, stop=last)
                    if last:
                        r_ = R[qt % 2]
                        t1_ = T1[qt % 2]
                        p.recip(r_.v(0, 512, o0, o0 + 64), po.v(0, 512, d0, d0 + 64))
                        p.tt(t1_.v(0, 512, o0, o0 + 64), r_.v(0, 512, o0, o0 + 64), SG.v(tq0, 512, o0, o0 + 64),
                             ALU.mult, eng="dve")
                        p.tt(MIX.v(hp * T + tq0, 512, o0, o0 + 64), po.v(0, 512, o0, o0 + 64),
                             t1_.v(0, 512, o0, o0 + 64), ALU.mult)

                LA = 3
                for idx in range(len(tiles) + LA):
                    if idx < len(tiles):
                        emit_qk(idx)
                    if idx >= LA:
                        emit_pv(idx - LA)
                    if bg and idx % 2 == 1:
                        bg.pop(0)()
                return pt_base + len(tiles)

            pt_i = 0
            for f in make_thunks(0, sets[0]):
                f()
            for hp in range(4):
                bg = make_thunks(hp + 1, sets[(hp + 1) % 2]) if hp < 3 else []
                for hh in range(2):
                    pt_i = attention(hp, hh, sets[hp % 2], bg, pt_i)
                while bg:
                    bg.pop(0)()
            dbg("att", MIX.v(0, 4 * T), [128, 4 * T])

        def phase_mem(b):
            NW = 1024
            load_w("mem", wsi3, win3, C_QM, NW, 8)
            load_w("kv", wskv3, wkv3, 0, NW, 8, wb_off=8192)
            wk = Bump(WK, 0, WKSZ)
            MEMT = wk.get(BF16, 8 * 256)
            KM = wk.get(BF16, 4 * 256)
            VM = wk.get(BF16, 2 * 512)
            QM = [wk.get(BF16, T) for _ in range(2)]
            SGM = [wk.get(BF16, T) for _ in range(2)]
            PT = [wk.get(BF16, 512) for _ in range(2)]
            R = wk.get(F32, 512)
            T1 = wk.get(F32, 512)
            rms_transpose(lambda i: mem_d[b, i * 128:(i + 1) * 128, :], 2, P_MNW,
                          lambda i: MEMT.v3(8, 256, i * 128, 128), wk)
            for h in range(4):
                ps = PS[2 + h % 2]
                for k in range(8):
                    p.matmul(ps.v(0, 256), WB.v(8192 + k * NW + h * 128, 128), MEMT.v(k * 256, 256),
                             start=(k == 0), stop=(k == 7))
                p.copy(KM.v(h * 256, 256), ps.v(0, 256), eng="act")
            for mt in range(2):
                ps = PS[2 + mt % 2]
                for k in range(8):
                    p.matmul(ps.v(), MEMT.v(k * 256 + mt * 128, 128), WB.v(8192 + k * NW + 512, 512),
                             start=(k == 0), stop=(k == 7))
                p.copy(VM.v(mt * 512, 512), ps.v(), eng="dve")
            pt_i = 0
            for h in range(4):
                qm = QM[h % 2]
                sgm = SGM[h % 2]
                for tq in range(4):
                    ps = PS[2 + tq % 2]
                    proj_fm(NW, h * 128, 128, tq * 512, 512, ps.v())
                    p.copy(qm.v(tq * 512, 512), ps.v(), eng="act")
                for tq in range(4):
                    ps = PS[2 + tq % 2]
                    proj_fm(NW, 512 + h * 128, 128, tq * 512, 512, ps.v())
                    p.act(sgm.v(tq * 512, 512), ps.v(), AF.Silu)
                for qt in range(4):
                    tq0 = qt * 512
                    po = PS[qt % 2]
                    den = PS[6 + qt % 2]
                    for mt in range(2):
                        sp = PS[4 + pt_i % 2]
                        ptile = PT[pt_i % 2]
                        pt_i += 1
                        p.matmul(sp.v(), KM.v(h * 256 + mt * 128, 128), qm.v(tq0, 512))
                        p.act(ptile.v(), sp.v(), AF.Exp, scale=128 ** -0.5)
                        p.matmul(po.v(), VM.v(mt * 512 + h * 128, 128), ptile.v(), start=(mt == 0), stop=(mt == 1))
                        p.matmul(den.v(), ones, ptile.v(), start=(mt == 0), stop=(mt == 1))
                    p.recip(R.v(), den.v())
                    p.tt(T1.v(), R.v(), sgm.v(tq0, 512), ALU.mult, eng="pool")
                    p.tt(MIX.v((12 + h) * T + tq0, 512), po.v(), T1.v(), ALU.mult)
            dbg("memo", MIX.v(12 * T, 4 * T), [128, 4 * T])

        def phase_out(b):
            NW = 1024
            load_w("out", wso3, wout3, 0, NW, 16)
            wk = Bump(WK, 0, WKSZ)
            XT = [wk.get(F32, 1024) for _ in range(2)]
            HT = [wk.get(F32, 1024) for _ in range(2)]
            OT = [wk.get(F32, 1024) for _ in range(2)]
            JNK = wk.get(BF16, 1024)
            SSQ = wk.get(F32, 16)
            RSTD = wk.get(F32, 16)
            for i in range(16):
                xt = XT[i % 2]
                ht = HT[i % 2]
                ot = OT[i % 2]
                p.dma(xt.v(), D(x_d[b, i * 128:(i + 1) * 128, :]))
                for nh in range(2):
                    ps = PS[(2 * i + nh) % 4]
                    for m in range(16):
                        p.matmul(ps.v(), MIX.v(m * T + i * 128, 128), WB.v(m * NW + nh * 512, 512),
                                 start=(m == 0), stop=(m == 15))
                    p.tt(ht.v(nh * 512, 512), ps.v(), xt.v(nh * 512, 512), ALU.add)
                p.act(JNK.v(), ht.v(), AF.Square, accum_out=SSQ.v(i, 1))
                p.act(RSTD.v(i, 1), SSQ.v(i, 1), AF.Ln, scale=1.0 / DM, bias=epsv)
                p.act(RSTD.v(i, 1), RSTD.v(i, 1), AF.Exp, scale=-0.5)
                p.stt(ot.v(), ht.v(), RSTD.v(i, 1), prm(P_FNW, 1024), ALU.mult, ALU.mult)
                p.dma(D(out_d[b, i * 128:(i + 1) * 128, :]), ot.v())

        for b in range(nseq):
            wkA = Bump(WK, 16384, WKSZ - 16384)
            rms_transpose(lambda i: x_d[b, i * 128:(i + 1) * 128, :], 16, P_NW,
                          lambda i: UT.v3(BF16, 8, T, i * 128, 128), wkA)
            if b == 0:
                dbg("ut", UT.v(), [128, 8 * T])
            if "S" in phases:
                phase_ssd(b)
            if "B" in phases:
                phase_attn(b)
            if "M" in phases:
                phase_mem(b)
            if "E" in phases:
                phase_out(b)
        print("ops:", p.nops, {e: len(v) for e, v in p.ops.items()})
        p.emit()
    return nc, dumps


def kernel(x, mem, norm_w, w_in, conv_w, conv_b, dt_bias, a_log, d_skip, ssd_norm_w, mem_norm_w, w_mem_kv,
           w_out, final_norm_w):
    f = lambda a: np.ascontiguousarray(np.asarray(a, dtype=np.float32))
    x, mem = f(x), f(mem)
    cb, cf, kc, qc = host_consts()
    cf[:, CF_PRM:CF_PRM + NPRM] = host_params(f(norm_w)[0], f(conv_w)[0], f(conv_b)[0], f(dt_bias)[0], f(a_log)[0],
                                              f(d_skip)[0], f(ssd_norm_w)[0], f(mem_norm_w)[0], f(final_norm_w))
    nc, _ = build()
    shared = {"w_in": f(w_in)[0], "w_kv": f(w_mem_kv)[0], "w_out": f(w_out)[0], "cstb": cb, "cstf": cf,
              "kconst": kc, "qconst": qc}
    in_maps = []
    for c in range(8):
        m = dict(shared)
        m["x"] = x[c * NSEQ:(c + 1) * NSEQ]
        m["mem"] = mem[c * NSEQ:(c + 1) * NSEQ]
        in_maps.append(m)
    res = run_bass_kernel_spmd(nc, in_maps, core_ids=list(range(8)))
    return np.concatenate([r["out"] for r in res.results], axis=0)
```

```python
from contextlib import ExitStack
import concourse.bass as bass
import concourse.mybir as mybir

F32 = mybir.dt.float32
BF16 = mybir.dt.bfloat16
I32 = mybir.dt.int32
AF = mybir.ActivationFunctionType
ALU = mybir.AluOpType
AX = mybir.AxisListType
ESZ = {F32: 4, BF16: 2, I32: 4}


class View:
    __slots__ = ("ap", "arena", "rngs", "pr")

    def __init__(self, ap, arena, rngs, pr=(0, 128)):
        self.ap, self.arena, self.rngs, self.pr = ap, arena, rngs, pr

    def re(self, s, **kw):
        return View(self.ap.rearrange(s, **kw), self.arena, self.rngs, self.pr)

    def bc(self, shape):
        return View(self.ap.broadcast_to(shape), self.arena, self.rngs, self.pr)

    def __getitem__(self, key):
        return View(self.ap[key], self.arena, self.rngs, self.pr)


class Arena:
    def __init__(self, name, t, dtype, ncols, const=False):
        self.name, self.t, self.dtype, self.ncols = name, t, dtype, ncols
        self.esz = ESZ[dtype]
        self.w = []
        self.r = []
        self.const = const
        self.alt = {dtype: t}
        self.psum = False
        self.qlast = [dict() for _ in range(4)]

    def v(self, lo=0, n=None, p0=0, p1=128):
        if n is None:
            n = self.ncols - lo
        assert 0 <= lo and lo + n <= self.ncols, (self.name, lo, n, self.ncols)
        return View(self.t[p0:p1, lo:lo + n], self, [(lo * self.esz, (lo + n) * self.esz)], (p0, p1))

    def v3(self, dtype, nk, stride, lo, n, p0=0, p1=128):
        if dtype not in self.alt:
            self.alt[dtype] = self.t.bitcast(dtype)
        e = ESZ[dtype]
        assert ((nk - 1) * stride + lo + n) * e <= self.ncols * self.esz
        total = self.ncols * self.esz // e
        o = max(0, lo + nk * stride - total)
        assert o <= stride - n and o <= lo, (self.name, lo, nk, stride, n, total)
        ws = lo - o
        ap = self.alt[dtype][p0:p1, ws:ws + nk * stride].rearrange("p (k s) -> p k s", s=stride)[:, :, o:o + n]
        return View(ap, self, [((k * stride + lo) * e, (k * stride + lo + n) * e) for k in range(nk)], (p0, p1))

    def vb(self, dtype, lo, n, p0=0, p1=128):
        if dtype not in self.alt:
            self.alt[dtype] = self.t.bitcast(dtype)
        e = ESZ[dtype]
        assert (lo + n) * e <= self.ncols * self.esz
        return View(self.alt[dtype][p0:p1, lo:lo + n], self, [(lo * e, (lo + n) * e)], (p0, p1))


class Sub:
    def __init__(self, arena, dtype, boff, n):
        self.a, self.dt, self.e, self.n = arena, dtype, ESZ[dtype], n
        assert boff % self.e == 0
        self.base = boff // self.e

    def v(self, lo=0, n=None, p0=0, p1=128):
        if n is None:
            n = self.n - lo
        assert 0 <= lo and lo + n <= self.n, (lo, n, self.n)
        return self.a.vb(self.dt, self.base + lo, n, p0, p1)

    def v3(self, nk, stride, lo, n, p0=0, p1=128):
        assert (nk - 1) * stride + lo + n <= self.n
        return self.a.v3(self.dt, nk, stride, self.base + lo, n, p0, p1)


class Bump:
    def __init__(self, arena, boff, size):
        self.a, self.off, self.end = arena, boff, boff + size

    def get(self, dtype, n):
        self.off = (self.off + 3) // 4 * 4
        s = Sub(self.a, dtype, self.off, n)
        self.off += n * ESZ[dtype]
        assert self.off <= self.end, ("bump overflow", self.off, self.end)
        return s


def D(ap):
    return View(ap, None, [])


COMPUTE = ("pe", "act", "dve", "pool")


class Prog:
    def __init__(self, nc, ndma=40):
        self.nc = nc
        self.ops = {e: [] for e in COMPUTE + ("sp",)}
        self.count = {e: 0 for e in COMPUTE}
        self.waited = {e: {} for e in COMPUTE + ("sp",)}
        self.ndma = ndma
        self.dma_cnt = [0] * ndma
        self.dma_next = 0
        self.npool = 0
        self.nops = 0

    def op(self, eng, fn, reads=(), writes=(), dma=False):
        deps = {}

        def add(tok):
            k, v = tok
            if deps.get(k, 0) < v:
                deps[k] = v

        for vw in reads:
            a = vw.arena
            if a is None:
                continue
            for (lo, hi) in vw.rngs:
                for (l, h, t) in a.w:
                    if l < hi and lo < h:
                        add(t)
        for vw in writes:
            a = vw.arena
            if a is None:
                continue
            assert not a.const, a.name
            for (lo, hi) in vw.rngs:
                for (l, h, t) in a.w:
                    if l < hi and lo < h:
                        add(t)
                for (l, h, t) in a.r:
                    if l < hi and lo < h:
                        add(t)
        for vw in list(reads) + list(writes):
            a = vw.arena
            if a is None or not a.psum:
                continue
            for q in range(vw.pr[0] // 32, (vw.pr[1] + 31) // 32):
                for e2, t in a.qlast[q].items():
                    if e2 != eng:
                        add(t)
        if dma and eng == "pool":
            key = ("p", self.npool)
            self.npool += 1
            tok = (key, 16)
            inc = (key, 16)
        elif dma:
            i = self.dma_next
            self.dma_next = (i + 1) % self.ndma
            if self.dma_cnt[i] > 0:
                add((("d", i), self.dma_cnt[i]))
            self.dma_cnt[i] += 16
            tok = (("d", i), self.dma_cnt[i])
            inc = (("d", i), 16)
        else:
            self.count[eng] += 1
            tok = (eng, self.count[eng])
            inc = (eng, 1)
        waits = []
        wd = self.waited[eng]
        for k, v in deps.items():
            if k == eng and eng == "pe":
                continue
            if wd.get(k, 0) >= v:
                continue
            wd[k] = v
            waits.append((k, v))
        self.ops[eng].append((waits, fn, inc))
        self.nops += 1
        for vw in list(reads) + list(writes):
            a = vw.arena
            if a is None or not a.psum:
                continue
            for q in range(vw.pr[0] // 32, (vw.pr[1] + 31) // 32):
                a.qlast[q][eng] = tok
        for vw in reads:
            a = vw.arena
            if a is None or a.const:
                continue
            for (lo, hi) in vw.rngs:
                if not dma:
                    a.r = [x for x in a.r if not (x[2][0] == eng and lo <= x[0] and x[1] <= hi)]
                a.r.append((lo, hi, tok))
        for vw in writes:
            a = vw.arena
            if a is None:
                continue
            for (lo, hi) in vw.rngs:
                a.w = [x for x in a.w if not (lo <= x[0] and x[1] <= hi)]
                a.r = [x for x in a.r if not (lo <= x[0] and x[1] <= hi)]
                a.w.append((lo, hi, tok))
        return tok

    def freeze(self, arena):
        arena.const = True
        arena.r = []

    def matmul(self, out, lhsT, rhs, start=True, stop=True, **kw):
        return self.op("pe", lambda e: e.matmul(out.ap, lhsT.ap, rhs.ap, start=start, stop=stop, **kw),
                       [lhsT, rhs] + ([] if start else [out]), [out])

    def transpose(self, out, in_, ident):
        return self.op("pe", lambda e: e.transpose(out.ap, in_.ap, ident.ap), [in_, ident], [out])

    def act(self, out, in_, func, bias=None, scale=None, accum_out=None):
        rd = [in_]
        kw = {}
        if bias is not None:
            if isinstance(bias, View):
                rd.append(bias)
                kw["bias"] = bias.ap
            else:
                kw["bias"] = bias
        if scale is not None:
            if isinstance(scale, View):
                rd.append(scale)
                kw["scale"] = scale.ap
            else:
                kw["scale"] = scale
        wr = [out]
        if accum_out is not None:
            wr.append(accum_out)
            kw["accum_out"] = accum_out.ap
        return self.op("act", lambda e: e.activation(out.ap, in_.ap, func, **kw), rd, wr)

    def tt(self, out, in0, in1, op, eng="dve"):
        return self.op(eng, lambda e: e.tensor_tensor(out.ap, in0.ap, in1.ap, op), [in0, in1], [out])

    def ts(self, out, in0, s1, s2, op0, op1=None, eng="dve", accum_out=None):
        rd = [in0]
        a1 = s1
        a2 = s2
        if isinstance(s1, View):
            rd.append(s1)
            a1 = s1.ap
        if isinstance(s2, View):
            rd.append(s2)
            a2 = s2.ap
        kw = {}
        if op1 is not None:
            kw["op1"] = op1
        wr = [out]
        if accum_out is not None:
            kw["accum_out"] = accum_out.ap
            wr.append(accum_out)
        return self.op(eng, lambda e: e.tensor_scalar(out.ap, in0.ap, a1, a2, op0, **kw), rd, wr)

    def stt(self, out, in0, scalar, in1, op0, op1):
        rd = [in0, in1]
        sc = scalar
        if isinstance(scalar, View):
            rd.append(scalar)
            sc = scalar.ap
        return self.op("dve", lambda e: e.scalar_tensor_tensor(out.ap, in0.ap, sc, in1.ap, op0, op1), rd, [out])

    def copy(self, out, in_, eng="dve"):
        if eng == "act":
            return self.op("act", lambda e: e.copy(out.ap, in_.ap), [in_], [out])
        return self.op(eng, lambda e: e.tensor_copy(out.ap, in_.ap), [in_], [out])

    def reduce(self, out, in_, op, axis=AX.X, eng="dve"):
        return self.op(eng, lambda e: e.tensor_reduce(out.ap, in_.ap, axis, op), [in_], [out])

    def recip(self, out, in_):
        return self.op("dve", lambda e: e.reciprocal(out.ap, in_.ap), [in_], [out])

    def memset(self, out, val, eng="dve"):
        return self.op(eng, lambda e: e.memset(out.ap, val), [], [out])

    def scan(self, out, d0, d1, initial, op0, op1):
        rd = [d0, d1]
        ini = initial
        if isinstance(initial, View):
            rd.append(initial)
            ini = initial.ap
        return self.op("dve", lambda e: e.tensor_tensor_scan(out.ap, d0.ap, d1.ap, ini, op0, op1), rd, [out])

    def affine_select(self, out, in_, pattern, cmp, fill, base, cm):
        return self.op("pool", lambda e: e.affine_select(out.ap, in_.ap, pattern, cmp, fill, base=base,
                                                         channel_multiplier=cm), [in_], [out])

    def iota(self, out, pattern, base, cm):
        return self.op("pool", lambda e: e.iota(out.ap, pattern, base=base, channel_multiplier=cm), [], [out])

    def dma(self, out, in_, q="sp", **kw):
        if q == "pool":
            kw.setdefault("max_dma_last_dim", 4096)
        return self.op(q, lambda e: e.dma_start(out=out.ap, in_=in_.ap, **kw), [in_], [out], dma=True)

    def emit(self):
        nc = self.nc
        with ExitStack() as es:
            sems = {}
            for e in COMPUTE:
                sems[e] = es.enter_context(nc.semaphore("s_" + e))
            for i in range(self.ndma):
                sems[("d", i)] = es.enter_context(nc.semaphore("s_d%d" % i))
            for i in range(self.npool):
                sems[("p", i)] = es.enter_context(nc.semaphore("s_p%d" % i))
            fin = []
            for i in range(self.ndma):
                if self.dma_cnt[i] > 0:
                    fin.append((("d", i), self.dma_cnt[i]))
            for i in range(self.npool):
                fin.append((("p", i), 16))
            for e in COMPUTE:
                if self.count[e] > 0:
                    fin.append((e, self.count[e]))
            block = es.enter_context(nc.Block())

            def run(stream, final=None):
                def f(eng):
                    for waits, fn, inc in stream:
                        for k, v in waits:
                            eng.wait_ge(sems[k], v)
                        ins = fn(eng)
                        ins.then_inc(sems[inc[0]], inc[1])
                    if final:
                        for k, v in final:
                            eng.wait_ge(sems[k], v)
                return f

            block.tensor(run(self.ops["pe"]))
            block.scalar(run(self.ops["act"]))
            block.vector(run(self.ops["dve"]))
            block.gpsimd(run(self.ops["pool"]))
            block.sync(run(self.ops["sp"], fin))
from concourse.bass_utils import run_bass_kernel_spmd
import numpy as np

T = 2048
DM = 1024
NSEQ = 2
EPS = 1e-6
BIG = 30000.0
C_Q, C_K, C_V, C_G, C_Z, C_XBC, C_DT, C_QM, C_GM = 0, 512, 1024, 1536, 2048, 3072, 5120, 5136, 5648
CB_ID, CB_TRI, CB_MNEG, CB_ONES, CB_EH, NCB = 0, 128, 256, 384, 512, 2560
CF_ID, CF_ONES, CF_PRM, CF_CAP, CF_CMK, CF_OWN, CF_MISC, NCF = 0, 128, 256, 1424, 1488, 1552, 1616, 1624
P_NW, P_MNW, P_SNW, P_CVB, P_CVW, P_DSK, P_DTB, P_ALOG, P_FNW = 0, 8, 16, 24, 40, 104, 112, 128, 144
NPRM = 1168


def host_consts():
    cb = np.zeros((128, NCB), np.float32)
    cb[:, CB_ID:CB_ID + 128] = np.eye(128)
    s = np.arange(128)[:, None]
    t = np.arange(128)[None, :]
    cb[:, CB_TRI:CB_TRI + 128] = (t >= s)
    cb[:, CB_MNEG:CB_MNEG + 128] = np.where(t < s, -BIG, 0.0)
    cb[:, CB_ONES:CB_ONES + 128] = 1.0
    for h in range(16):
        cb[h, CB_EH + h * 128: CB_EH + (h + 1) * 128] = 1.0
    cf = np.zeros((128, NCF), np.float32)
    cf[:, CF_ID:CF_ID + 128] = np.eye(128)
    cf[:, CF_ONES:CF_ONES + 128] = 1.0
    cap = np.zeros((8, 8), np.float32)
    cmk = np.zeros((8, 8), np.float32)
    own = np.zeros((8, 8), np.float32)
    for ti in range(8):
        qb = 4 + ti // 2
        for j in range(8):
            cap[ti, j] = 1e30 if j < qb else -1e30
            cmk[ti, j] = 1.0 if j < qb else 0.0
            own[ti, j] = 1.0 if j == qb else 0.0
    cf[:, CF_CAP:CF_CAP + 64] = cap.reshape(-1)[None, :]
    cf[:, CF_CMK:CF_CMK + 64] = cmk.reshape(-1)[None, :]
    cf[:, CF_OWN:CF_OWN + 64] = own.reshape(-1)[None, :]
    cf[:, CF_MISC] = EPS
    cf[:, CF_MISC + 1] = 1.0
    kc = np.zeros((128, T), np.float32)
    pos = np.arange(T)
    for r in range(8):
        kc[64 + r] = (pos // 256 == r)
    kc[96] = pos // 16
    kc[97] = pos % 16
    kc[98] = 1.0
    kc[99] = 1.0
    qc = np.zeros((8, 4, T), np.float32)
    for h in range(8):
        sl = 2.0 ** (-8.0 * (h + 1) / 8)
        qc[h, 0] = 16 * sl * 8
        qc[h, 1] = sl * 8
        qc[h, 2] = -16 * sl * 8 * (pos // 16)
        qc[h, 3] = -sl * 8 * (pos % 16)
    return cb, cf, kc, qc


def host_params(norm_w, conv_w, conv_b, dt_bias, a_log, d_skip, ssd_norm_w, mem_norm_w, final_norm_w):
    prm = np.zeros((128, NPRM), np.float32)
    prm[:, P_NW:P_NW + 8] = norm_w.reshape(8, 128).T
    prm[:, P_MNW:P_MNW + 8] = mem_norm_w.reshape(8, 128).T
    prm[:, P_SNW:P_SNW + 8] = ssd_norm_w.reshape(8, 128).T
    prm[:, P_CVB:P_CVB + 16] = conv_b.reshape(16, 128).T
    prm[:, P_CVW:P_CVW + 64] = conv_w.T.reshape(16, 128, 4).transpose(1, 0, 2).reshape(128, 64)
    prm[:, P_DSK:P_DSK + 8] = np.repeat(d_skip, 64).reshape(8, 128).T
    prm[:, P_DTB:P_DTB + 16] = dt_bias[None, :]
    prm[:, P_ALOG:P_ALOG + 16] = a_log[None, :]
    prm[:, P_FNW:P_FNW + 1024] = final_norm_w[None, :]
    return prm
def build(nseq=NSEQ, dump=None, phases="ASBME"):
    nc = bass.Bass("TRN2", target_bir_lowering=False)
    x_d = nc.dram_tensor("x", [nseq, T, DM], F32, kind="ExternalInput").ap()
    mem_d = nc.dram_tensor("mem", [nseq, 256, DM], F32, kind="ExternalInput").ap()
    win_d = nc.dram_tensor("w_in", [DM, 6160], F32, kind="ExternalInput").ap()
    wkv_d = nc.dram_tensor("w_kv", [DM, 1024], F32, kind="ExternalInput").ap()
    wout_d = nc.dram_tensor("w_out", [2048, DM], F32, kind="ExternalInput").ap()
    cb_d = nc.dram_tensor("cstb", [128, NCB], F32, kind="ExternalInput").ap()
    cf_d = nc.dram_tensor("cstf", [128, NCF], F32, kind="ExternalInput").ap()
    kc_d = nc.dram_tensor("kconst", [128, T], F32, kind="ExternalInput").ap()
    qc_d = nc.dram_tensor("qconst", [8, 4, T], F32, kind="ExternalInput").ap()
    out_d = nc.dram_tensor("out", [nseq, T, DM], F32, kind="ExternalOutput").ap()
    dumps = {}
    wsi_d = nc.dram_tensor("wsc_in", [DM, 6160], BF16, kind="Internal").ap()
    wskv_d = nc.dram_tensor("wsc_kv", [DM, 1024], BF16, kind="Internal").ap()
    wso_d = nc.dram_tensor("wsc_out", [2048, DM], BF16, kind="Internal").ap()
    kcs_d = nc.dram_tensor("kc_sc", [128, T], BF16, kind="Internal").ap()
    qcs_d = nc.dram_tensor("qc_sc", [32, T], BF16, kind="Internal").ap()
    qc2_d = qc_d.rearrange("h r t -> (h r) t")
    wsi3 = wsi_d.rearrange("(k p) c -> p k c", p=128)
    wskv3 = wskv_d.rearrange("(k p) c -> p k c", p=128)
    wso3 = wso_d.rearrange("(k p) c -> p k c", p=128)
    win3 = win_d.rearrange("(k p) c -> p k c", p=128)
    wkv3 = wkv_d.rearrange("(k p) c -> p k c", p=128)
    wout3 = wout_d.rearrange("(k p) c -> p k c", p=128)

    with ExitStack() as es:
        def sb(name, n, dt):
            t = es.enter_context(nc.sbuf_tensor(name, [128, n], dt))
            return Arena(name, t, dt, n)

        UT = sb("UT", 8 * T, BF16)
        MIX = sb("MIX", 16 * T, BF16)
        WB = sb("WB", 8 * 3088, BF16)
        WKSZ = 47616
        WK = sb("WK", WKSZ // 2, BF16)
        CB = sb("CB", NCB, BF16)
        CF = sb("CF", NCF, F32)
        SM = sb("SM", 64, F32)
        PS = []
        for i in range(8):
            t = es.enter_context(nc.psum_tensor("ps%d" % i, [128, 512], F32))
            PS.append(Arena("ps%d" % i, t, F32, 512))
            PS[-1].psum = True
        p = Prog(nc)

        def dbg(name, view, shape):
            if dump is None or name not in dump:
                return
            d = nc.dram_tensor("dbg_" + name, list(shape), view.ap.dtype, kind="ExternalOutput").ap()
            dumps[name] = d
            p.dma(D(d), view)

        p.dma(CB.v(), D(cb_d), q="pool")
        p.dma(CF.v(), D(cf_d))
        ident = CB.v(CB_ID, 128)
        tri = CB.v(CB_TRI, 128)
        mneg = CB.v(CB_MNEG, 128)
        ones = CB.v(CB_ONES, 128)
        identf = CF.v(CF_ID, 128)
        epsv = CF.v(CF_MISC, 1)
        onev = CF.v(CF_MISC + 1, 1)

        def prm(off, n, p0=0, p1=128):
            return CF.v(CF_PRM + off, n, p0, p1)

        AB = SM.v(0, 16)
        p.act(AB, prm(P_ALOG, 16), AF.Exp)
        p.ts(AB, AB, -1.0, None, ALU.mult)

        SC = {n: Arena("sc_" + n, None, BF16, 1) for n in ("attn", "mem", "ssd", "kv", "out", "kc", "qc")}
        converted = [False]

        def scv(ap, name, k):
            return View(ap, SC[name], [(k, k + 1)])

        NHALF = {"attn": 4, "mem": 4, "ssd": 4, "kv": 4, "out": 8}

        def convert_all():
            p.dma(scv(kcs_d, "kc", 0), D(kc_d), q="pool")
            p.dma(scv(qcs_d, "qc", 0), D(qc2_d), q="pool")
            for name, src, dst, c0, nw, nrows in (("attn", win_d, wsi_d, 0, 2048, 1024), ("kv", wkv_d, wskv_d, 0, 1024, 1024),
                                                  ("mem", win_d, wsi_d, C_QM, 1024, 1024), ("out", wout_d, wso_d, 0, 1024, 2048),
                                                  ("ssd", win_d, wsi_d, C_Z, 3088, 1024)):
                hr = nrows // 2
                for h in range(2):
                    p.dma(scv(dst[h * hr:(h + 1) * hr, c0:c0 + nw], name, h), D(src[h * hr:(h + 1) * hr, c0:c0 + nw]),
                          q="pool")
            converted[0] = True

        def load_w(name, dst3, src3, c0, nw, nk, wb_off=0):
            for k in range(nk):
                if converted[0]:
                    p.dma(WB.v(wb_off + k * nw, nw), scv(dst3[:, k, c0:c0 + nw], name, k // NHALF[name]), q="sp")
                else:
                    p.dma(WB.v(wb_off + k * nw, nw), D(src3[:, k, c0:c0 + nw]), q="pool")

        pj_i = [0]

        def rms_transpose(src_tile, ntiles, nwoff, dst_fn, wk):
            XT = [wk.get(F32, 1024) for _ in range(2)]
            XS = [wk.get(BF16, 1024) for _ in range(2)]
            JNK = wk.get(BF16, 1024)
            SSQ = wk.get(F32, 16)
            RSTD = wk.get(F32, 16)
            for i in range(ntiles):
                xt = XT[i % 2]
                p.dma(xt.v(), D(src_tile(i)))
                p.act(JNK.v(), xt.v(), AF.Square, accum_out=SSQ.v(i, 1))
                p.act(RSTD.v(i, 1), SSQ.v(i, 1), AF.Ln, scale=1.0 / DM, bias=epsv)
                p.act(RSTD.v(i, 1), RSTD.v(i, 1), AF.Exp, scale=-0.5)
                xs = XS[i % 2]
                p.ts(xs.v(), xt.v(), RSTD.v(i, 1), None, ALU.mult)
                pb = PS[i % 2]
                for k in range(8):
                    p.transpose(pb.vb(BF16, k * 128, 128), xs.v(k * 128, 128), ident)
                p.tt(dst_fn(i), pb.vb(BF16, 0, 1024).re("p (k t) -> p k t", k=8),
                     prm(nwoff, 8).re("p (k o) -> p k o", o=1).bc([128, 8, 128]), ALU.mult)

        def utv(k, t0, n):
            return UT.v(k * T + t0, n)

        def proj_fm(nw, c0, ncols, t0, n, ps_view):
            for k in range(8):
                p.matmul(ps_view, WB.v(k * nw + c0, ncols), utv(k, t0, n), start=(k == 0), stop=(k == 7))

        def phase_ssd(b):
            NW = 3088
            load_w("ssd", wsi3, win3, C_Z, NW, 8)
            if not converted[0]:
                convert_all()
            b1 = Bump(MIX, 0, 16384)
            b2 = Bump(MIX, 12 * T * 2, 16384)
            wk = Bump(WK, 0, WKSZ)
            XPRE = b1.get(BF16, 16 * 259)
            XST = b1.get(BF16, 8 * 256)
            BMT = b1.get(BF16, 4 * 256)
            LT = [b1.get(BF16, 384) for _ in range(2)]
            SZ = b2.get(BF16, 8 * 256)
            PREV = b2.get(F32, 1024)
            XDD = b2.get(BF16, 2 * 1024)
            CMT = b2.get(BF16, 4 * 256)
            BTOK = b2.get(BF16, 2 * 512)
            DIAG = wk.get(BF16, 64 * 128)
            CBT = wk.get(BF16, 4 * 384)
            L0 = [wk.get(BF16, 256) for _ in range(2)]
            MT = [wk.get(BF16, 384) for _ in range(2)]
            GT = [wk.get(BF16, 256) for _ in range(2)]
            GG = [wk.get(F32, 256) for _ in range(4)]
            GSQ = [wk.get(BF16, 256) for _ in range(4)]
            RS = wk.get(F32, 256)
            XDT = wk.get(BF16, 2 * 2048)
            PREVB = wk.get(BF16, 2048)
            DTR = wk.get(F32, 32)
            DT = wk.get(F32, 32)
            DA = wk.get(F32, 256)
            NCS = wk.get(F32, 32)
            DEC = wk.get(F32, 32)
            W2 = wk.get(F32, 32)
            CST = wk.get(F32, 256)
            CSH = wk.get(BF16, 256)
            CSL = wk.get(BF16, 256)
            DECT = wk.get(F32, 256)
            DG16 = wk.get(F32, 16)
            CDB = wk.get(F32, 16)

            for zt in (XDT, PREVB, DA, CST, CSH, CSL, DECT, DG16):
                p.memset(zt.v(), 0.0, eng="pool")
            for j in range(64):
                p.ts(DIAG.v(j * 128, 128), ident, prm(P_CVW + j, 1), None, ALU.mult,
                     eng=("pool" if j % 2 else "dve"))
            p.memset(XPRE.v3(16, 259, 0, 3), 0.0)
            p.memset(PREV.v(), 0.0)

            pending = []
            for c in range(8):
                t0 = c * 256

                def zgroup(zc):
                    ps = PS[zc % 2].v((zc // 2 % 2) * 256, 256)
                    proj_fm(NW, zc * 128, 128, t0, 256, ps)
                    p.copy(SZ.v(zc * 256, 256), ps, eng="dve")

                def xgroup(cc):
                    ps = PS[cc % 2].v((cc // 2 % 2) * 256, 256)
                    proj_fm(NW, 1024 + cc * 128, 128, t0, 256, ps)
                    p.copy(XPRE.v(cc * 259 + 3, 256), ps, eng=("dve" if cc % 2 else "act"))

                if c > 0:
                    p.copy(XPRE.v3(16, 259, 0, 3), XPRE.v3(16, 259, 256, 3), eng="pool")
                for lt in range(2):
                    for k in range(8):
                        p.matmul(PS[5].v(384 + lt * 16, 16), utv(k, t0 + lt * 128, 128), WB.v(k * NW + 3072, 16),
                                 start=(k == 0), stop=(k == 7))
                p.tt(DTR.v(), PS[5].v(384, 32).re("p (l h) -> p l h", l=2),
                     prm(P_DTB, 16).re("p (o h) -> p o h", o=1).bc([128, 2, 16]), ALU.add)
                p.act(DTR.v(), DTR.v(), AF.Exp)
                p.act(DT.v(), DTR.v(), AF.Ln, bias=onev)
                p.tt(DA.v3(2, 128, 0, 16), DT.v().re("p (l h) -> p l h", l=2),
                     SM.v(0, 16).re("p (o h) -> p o h", o=1).bc([128, 2, 16]), ALU.mult)
                for zc in range(6):
                    zgroup(zc)
                    if zc == 1 and pending:
                        pending.pop(0)()
                for lt in range(2):
                    p.transpose(PS[6].v(lt * 128, 128), DA.v(lt * 128, 128), identf)
                p.scan(CST.v(0, 256, 0, 16), CF.v(CF_ONES, 1, 0, 16).bc([16, 256]), PS[6].v(0, 256, 0, 16), 0.0,
                       ALU.mult, ALU.add)
                p.copy(CSH.v(0, 256, 0, 16), CST.v(0, 256, 0, 16))
                p.tt(CSL.v(0, 256, 0, 16), CST.v(0, 256, 0, 16), CSH.v(0, 256, 0, 16), ALU.subtract)
                p.act(DECT.v(0, 256, 0, 16), CST.v(0, 256, 0, 16), AF.Exp, scale=-1.0, bias=CST.v(255, 1, 0, 16))
                for zc in range(6, 8):
                    zgroup(zc)
                for cc in range(8, 12):
                    xgroup(cc)
                for lt in range(2):
                    p.transpose(PS[7].v(lt * 128, 128), CST.v(lt * 128, 128), identf)
                    p.transpose(PS[7].v(256 + lt * 128, 128), DECT.v(lt * 128, 128), identf)
                p.ts(NCS.v().re("p (l h) -> p l h", l=2), PS[7].v3(F32, 2, 128, 0, 16), -1.0, None, ALU.mult)
                p.copy(DEC.v().re("p (l h) -> p l h", l=2), PS[7].v3(F32, 2, 128, 256, 16))
                p.tt(W2.v(), DT.v(), DEC.v(), ALU.mult)
                if c < 7:
                    p.ts(DG16.v(0, 16, 0, 16), CF.v(CF_ID, 16, 0, 16), CST.v(255, 1, 0, 16), None, ALU.mult)
                    p.matmul(PS[5].v(480, 16), CF.v(CF_ONES, 128), DG16.v())
                    p.act(CDB.v(), PS[5].v(480, 16), AF.Exp)
                for cc in list(range(12, 16)) + list(range(0, 8)):
                    xgroup(cc)

                def conv(cc):
                    ps = PS[cc % 2].v((cc // 2 % 2) * 256, 256)
                    for k in range(4):
                        p.matmul(ps, DIAG.v((cc * 4 + k) * 128, 128), XPRE.v(cc * 259 + k, 256),
                                 start=(k == 0), stop=(k == 3))
                    if cc < 8:
                        dst = XST.v(cc * 256, 256)
                    elif cc < 12:
                        dst = BMT.v((cc - 8) * 256, 256)
                    else:
                        dst = CMT.v((cc - 12) * 256, 256)
                    p.act(dst, ps, AF.Silu, bias=prm(P_CVB + cc, 1))

                for cc in range(8, 16):
                    conv(cc)
                for lt in range(2):
                    pb = PS[7]
                    for g in range(4):
                        p.transpose(pb.vb(BF16, g * 128, 128), BMT.v(g * 256 + lt * 128, 128), ident)
                    p.copy(BTOK.v(lt * 512, 512), pb.vb(BF16, 0, 512), eng="dve")
                for g in range(4):
                    ps = PS[5]
                    p.matmul(ps.v(0, 256), BMT.v(g * 256, 128), CMT.v(g * 256, 256))
                    p.matmul(ps.v(256, 128), BMT.v(g * 256 + 128, 128), CMT.v(g * 256 + 128, 128))
                    p.copy(CBT.v(g * 384, 384), ps.v(0, 384), eng="dve")
                for cc in range(0, 8):
                    conv(cc)
                p.act(SZ.v(), SZ.v(), AF.Silu)
                for lt in range(2):
                    pb = PS[6]
                    for cc in range(8):
                        p.transpose(pb.vb(BF16, cc * 128, 128), XST.v(cc * 256 + lt * 128, 128), ident)
                    src = pb.vb(BF16, 0, 1024).re("p (h q) -> p h q", h=16)
                    for hh in range(2):
                        p.tt(XDT.v(lt * 2048, 2048).re("p (c e q) -> p c e q", c=8, e=2)[:, :, hh, hh * 64:hh * 64 + 64],
                             src.re("p (c e) q -> p c e q", e=2)[:, :, hh, :],
                             DT.v(lt * 16, 16).re("p (c e o) -> p c e o", e=2, o=1)[:, :, hh, :].bc([128, 8, 64]),
                             ALU.mult)
                    p.tt(XDD.v(lt * 1024, 1024).re("p (h q) -> p h q", h=16), src,
                         W2.v(lt * 16, 16).re("p (h o) -> p h o", o=1).bc([128, 16, 64]), ALU.mult)
                if c < 7:
                    for g in range(4):
                        st = PS[g // 2].v((g % 2) * 256, 256)
                        for lt in range(2):
                            p.matmul(st, BTOK.v(lt * 512 + g * 128, 128), XDD.v(lt * 1024 + g * 256, 256),
                                     start=(lt == 0), stop=(lt == 1))
                    for g in range(4):
                        st = PS[g // 2].v((g % 2) * 256, 256)
                        pv = PREV.v(g * 256, 256)
                        p.tt(pv.re("p (h q) -> p h q", h=4), pv.re("p (h q) -> p h q", h=4),
                             CDB.v(g * 4, 4).re("p (h o) -> p h o", o=1).bc([128, 4, 64]), ALU.mult)
                        p.tt(pv, pv, st, ALU.add)
                def seg_stage(h):
                    g = h // 4
                    sg = PS[2 + h % 2]
                    eh = CB.v(CB_EH + h * 128, 128)
                    csh = lambda lo, n: CSH.v(lo, n)
                    csl = lambda lo, n: CSL.v(lo, n)
                    p.matmul(sg.v(0, 256), eh, csh(0, 256), start=True, stop=False)
                    p.matmul(sg.v(0, 256), eh, csl(0, 256), start=False, stop=True)
                    p.matmul(sg.v(256, 128), eh, csh(0, 128), start=True, stop=False)
                    p.matmul(sg.v(256, 128), eh, csl(0, 128), start=False, stop=False)
                    p.matmul(sg.v(256, 128), ident, mneg, start=False, stop=True)
                    p.matmul(sg.v(384, 128), eh, csh(128, 128), start=True, stop=False)
                    p.matmul(sg.v(384, 128), eh, csl(128, 128), start=False, stop=False)
                    p.matmul(sg.v(384, 128), ident, mneg, start=False, stop=True)
                    l0 = L0[h % 2]
                    lt_ = LT[h % 2]
                    mt = MT[h % 2]
                    gt = GT[h % 2]
                    if c > 0:
                        p.act(l0.v(), sg.v(0, 256), AF.Exp)
                    p.act(lt_.v(0, 128), sg.v(256, 128), AF.Exp, bias=NCS.v(h, 1))
                    p.act(lt_.v(128, 128), sg.v(128, 128), AF.Exp, bias=NCS.v(h, 1))
                    p.act(lt_.v(256, 128), sg.v(384, 128), AF.Exp, bias=NCS.v(16 + h, 1))
                    p.tt(mt.v(), lt_.v(), CBT.v(g * 384, 384), ALU.mult)
                    if c > 0:
                        p.tt(gt.v(), l0.v(), CMT.v(g * 256, 256), ALU.mult, eng="pool")

                def y_stage(h):
                    g = h // 4
                    pr = h % 2
                    pc = h // 2
                    mt = MT[h % 2]
                    gt = GT[h % 2]
                    ybank = PS[4 + 2 * (pc % 2)]
                    yt = ybank.v(0, 256)
                    yt2 = ybank.v(128, 128)
                    p.matmul(yt, XDT.v(h * 128, 128), mt.v(0, 256), start=(pr == 0), stop=False)
                    p.matmul(yt2, XDT.v(2048 + h * 128, 128), mt.v(256, 128), start=False, stop=(c == 0 and pr == 1))
                    if c > 0:
                        p.matmul(yt, PREVB.v(h * 128, 128), gt.v(), start=False, stop=(pr == 1))
                    if pr == 1:
                        gg = GG[pc % 4]
                        p.stt(gg.v(), XST.v(pc * 256, 256), prm(P_DSK + pc, 1), yt, ALU.mult, ALU.add)
                        p.tt(gg.v(), gg.v(), SZ.v(pc * 256, 256), ALU.mult)
                        p.act(GSQ[pc % 4].v(), gg.v(), AF.Square)
                def norm_stage(g, t0=t0):
                        pc = 2 * g + 1
                        pcs = [pc - 1, pc]
                        ss = PS[7].v(256, 256)
                        p.matmul(ss, ones, GSQ[pcs[0] % 4].v(), start=True, stop=False)
                        p.matmul(ss, ones, GSQ[pcs[1] % 4].v(), start=False, stop=True)
                        p.act(RS.v(), ss, AF.Ln, scale=1.0 / 256, bias=epsv)
                        p.act(RS.v(), RS.v(), AF.Exp, scale=-0.5)
                        for q in pcs:
                            p.stt(MIX.v((4 + q) * T + t0, 256), GG[q % 4].v(), prm(P_SNW + q, 1), RS.v(),
                                  ALU.mult, ALU.mult)

                for h in range(17):
                    if h < 16:
                        seg_stage(h)
                    if 1 <= h <= 16:
                        y_stage(h - 1)
                    if h >= 2 and (h - 2) % 4 == 3:
                        norm_stage((h - 2) // 4)
                pending.append(lambda ns=norm_stage: ns(3))
                if c < 7:
                    for g in range(4):
                        pv = PREV.v(g * 256, 256)
                        for hh in range(2):
                            p.copy(PREVB.v(g * 512, 512).re("p (c e q) -> p c e q", c=2, e=2)[:, :, hh, hh * 64:hh * 64 + 64],
                                   pv.re("p (c e q) -> p c e q", c=2, e=2)[:, :, hh, :], eng="pool")
            while pending:
                pending.pop(0)()
            dbg("ssd", MIX.v(4 * T, 8 * T), [128, 8 * T])

        def attn_core(kt_list_fn, nq, kv_lhsT, q_rhs, v_lhsT, scale, PTs, epilogue, diag_fn=None, den_lhsT=None):
            pass

        def phase_attn(b):
            NW = 2048
            load_w("attn", wsi3, win3, 0, NW, 8)
            wk = Bump(WK, 0, WKSZ)
            bm = Bump(MIX, 12 * T * 2, 16384)
            bw = Bump(WB, 8 * NW * 2, WB.ncols * 2 - 8 * NW * 2)
            sets = [dict(QK=[wk.get(BF16, T) for _ in range(4)], VA=wk.get(BF16, 2048), VB=wk.get(BF16, 2048),
                         SG=wk.get(BF16, T)),
                    dict(QK=[bm.get(BF16, T) for _ in range(4)], VA=bw.get(BF16, 2048), VB=bw.get(BF16, 2048),
                         SG=bw.get(BF16, T))]
            PT = [wk.get(BF16, 512) for _ in range(4)]
            R = [wk.get(F32, 512) for _ in range(2)]
            T1 = [wk.get(F32, 512) for _ in range(2)]
            PENB = wk.get(BF16, 8 * 128)
            GM = wk.get(F32, 64)
            CMP = wk.get(F32, 512)
            RANK = wk.get(F32, 64)
            KSUM = wk.get(F32, 8)
            KMTS = [wk.get(BF16, 8) for _ in range(2)]
            for KMT in KMTS:
                p.memset(KMT.v(), 0.0, eng="pool")
            p.memset(PENB.v(), 0.0, eng="pool")
            for S in sets:
                for i in range(2):
                    p.memset(S["QK"][i].v(), 0.0, eng="pool")
                    p.dma(S["QK"][2 + i].v(), scv(kcs_d, "kc", 0), q="sp")
                p.memset(S["VA"].v(), 1.0, eng="pool")
                p.memset(S["VB"].v(), 1.0, eng="pool")

            def make_thunks(hp, S):
                QK, VA, VB, SG = S["QK"], S["VA"], S["VB"], S["SG"]
                th = []

                def t_qc():
                    for hh in range(2):
                        h_ = 2 * hp + hh
                        p.dma(QK[hh].v(0, T, 96, 100), scv(qcs_d[4 * h_:4 * h_ + 4, :], "qc", 0), q="sp")
                th.append(t_qc)
                def half_proj(c0, tq, half, evac):
                    def f():
                        ps = PS[2 + tq % 2]
                        if half == 1:
                            for k in range(8):
                                p.matmul(ps.v(), WB.v(k * NW + c0, 128), utv(k, tq * 512, 512), start=(k == 0), stop=(k == 7))
                            evac(ps)
                    return f

                for which, c0 in ((0, C_Q), (1, C_K)):
                    for tq in range(4):
                        def ev(ps, which=which, tq=tq):
                            p.copy(QK[2 * which].v(tq * 512, 512, 0, 64), ps.v(0, 512, 0, 64), eng="act")
                            p.copy(QK[2 * which + 1].v(tq * 512, 512, 0, 64), ps.v(0, 512, 64, 128), eng="dve")
                        th.append(half_proj(c0 + hp * 128, tq, 0, ev))
                        th.append(half_proj(c0 + hp * 128, tq, 1, ev))

                def gate1(hh):
                    K = QK[2 + hh]
                    KMT = KMTS[hh]
                    p.reduce(KSUM.v(0, 8, 0, 64), K.v(0, T, 0, 64).re("p (j s) -> p j s", j=8), ALU.add)
                    p.ts(KMT.v(0, 8, 0, 64), KSUM.v(0, 8, 0, 64), 1.0 / 256, None, ALU.mult)

                def gate2(hh):
                    Q = QK[hh]
                    KMT = KMTS[hh]
                    gps = PS[2]
                    for ti in range(8):
                        p.matmul(gps.v(ti * 8, 8), Q.v(1024 + ti * 128, 128), KMT.v())
                    p.tt(GM.v(), gps.v(0, 64), CF.v(CF_CAP, 64), ALU.min)
                    g3 = GM.v().re("p (t j) -> p t j", t=8)
                    p.tt(CMP.v().re("p (t j k) -> p t j k", t=8, j=8),
                         g3.re("p t (o k) -> p t o k", o=1).bc([128, 8, 8, 8]),
                         g3.re("p t (j o) -> p t j o", o=1).bc([128, 8, 8, 8]), ALU.is_gt)
                    p.reduce(RANK.v(), CMP.v().re("p (a k) -> p a k", k=8), ALU.add)
                    p.ts(RANK.v(), RANK.v(), 3.0, None, ALU.is_lt)
                    p.tt(RANK.v(), RANK.v(), CF.v(CF_CMK, 64), ALU.mult)
                    p.tt(RANK.v(), RANK.v(), CF.v(CF_OWN, 64), ALU.add)
                    p.ts(PENB.v3(8, 128, 64, 8), RANK.v().re("p (t j) -> p t j", t=8), -1.0, BIG, ALU.add, ALU.mult)

                def gate3(hh):
                    Q = QK[hh]
                    pps = PS[3]
                    for ti in range(8):
                        p.transpose(pps.vb(BF16, ti * 128, 128), PENB.v(ti * 128, 128), ident)
                    p.copy(Q.v(1024, 1024, 64, 72), pps.vb(BF16, 0, 1024, 64, 72), eng="dve")

                th.append(lambda: gate1(0))
                th.append(lambda: gate1(1))
                for tq in range(4):
                    def ev(ps, tq=tq):
                        p.copy(SG.v(tq * 512, 512), ps.v(), eng="dve")
                    th.append(half_proj(C_G + hp * 128, tq, 0, ev))
                    th.append(half_proj(C_G + hp * 128, tq, 1, ev))
                vth = []
                for tg in range(4):
                    for j in range(4):
                        def f(tg=tg, j=j):
                            ps = PS[2 + tg % 2]
                            ti = tg * 4 + j
                            for k in range(8):
                                p.matmul(ps.v(j * 128, 128), utv(k, ti * 128, 128),
                                         WB.v(k * NW + C_V + hp * 128, 128), start=(k == 0), stop=(k == 7))
                            if j == 3:
                                src = ps.v().re("p (j c) -> p j c", j=4)
                                p.copy(VA.v3(4, 128, tg * 512, 64), src[:, :, 0:64], eng="dve")
                                p.copy(VB.v3(4, 128, tg * 512 + 64, 64), src[:, :, 64:128], eng="dve")
                        vth.append(f)
                th.append(lambda: gate2(0))
                th += vth[0:4]
                th.append(lambda: gate3(0))
                th.append(lambda: gate2(1))
                th += vth[4:8]
                th.append(lambda: gate3(1))
                th += vth[8:16]
                th.append(lambda: p.act(SG.v(), SG.v(), AF.Silu))
                return th

            def attention(hp, hh, S, bg, pt_base):
                Q = S["QK"][hh]
                K = S["QK"][2 + hh]
                V = S["VA"] if hh == 0 else S["VB"]
                SG = S["SG"]
                tiles = []
                for qt in range(4):
                    order = [4 * qt + r for r in range(4)] + list(range(4 * qt))
                    for j, kt in enumerate(order):
                        tiles.append((qt, kt, j == 0, j == len(order) - 1))
                o0, d0 = (0, 64) if hh == 0 else (64, 0)

                def geom(idx):
                    qt, kt, first, last = tiles[idx]
                    tq0 = qt * 512
                    s0 = kt * 128
                    qlo = max(tq0, s0)
                    return qt, kt, tq0, s0, qlo, tq0 + 512 - qlo

                def emit_qk(idx):
                    qt, kt, tq0, s0, qlo, n = geom(idx)
                    gi = pt_base + idx
                    sp = PS[4 + gi % 4]
                    ptile = PT[gi % 4]
                    p.matmul(sp.v(0, n), K.v(s0, 128), Q.v(qlo, n))
                    p.act(ptile.v(0, n), sp.v(0, n), AF.Exp, scale=0.125)
                    if s0 >= tq0:
                        p.tt(ptile.v(0, 128), ptile.v(0, 128), tri, ALU.mult, eng="pool")

                def emit_pv(idx):
                    qt, kt, tq0, s0, qlo, n = geom(idx)
                    gi = pt_base + idx
                    first, last = tiles[idx][2], tiles[idx][3]
                    po = PS[qt % 2]
                    ptile = PT[gi % 4]
                    p.matmul(po.v(qlo - tq0, n), V.v(kt * 128, 128), ptile.v(0, n), start=first, stop=last)
                    if last:
                        r_ = R[qt % 2]
                        t1_ = T1[qt % 2]
                        p.recip(r_.v(0, 512, o0, o0 + 64), po.v(0, 512, d0, d0 + 64))
                        p.tt(t1_.v(0, 512, o0, o0 + 64), r_.v(0, 512, o0, o0 + 64), SG.v(tq0, 512, o0, o0 + 64),
                             ALU.mult, eng="dve")
                        p.tt(MIX.v(hp * T + tq0, 512, o0, o0 + 64), po.v(0, 512, o0, o0 + 64),
                             t1_.v(0, 512, o0, o0 + 64), ALU.mult)

                LA = 3
                for idx in range(len(tiles) + LA):
                    if idx < len(tiles):
                        emit_qk(idx)
                    if idx >= LA:
                        emit_pv(idx - LA)
                    if bg and idx % 2 == 1:
                        bg.pop(0)()
                return pt_base + len(tiles)

            pt_i = 0
            for f in make_thunks(0, sets[0]):
                f()
            for hp in range(4):
                bg = make_thunks(hp + 1, sets[(hp + 1) % 2]) if hp < 3 else []
                for hh in range(2):
                    pt_i = attention(hp, hh, sets[hp % 2], bg, pt_i)
                while bg:
                    bg.pop(0)()
            dbg("att", MIX.v(0, 4 * T), [128, 4 * T])

        def phase_mem(b):
            NW = 1024
            load_w("mem", wsi3, win3, C_QM, NW, 8)
            load_w("kv", wskv3, wkv3, 0, NW, 8, wb_off=8192)
            wk = Bump(WK, 0, WKSZ)
            MEMT = wk.get(BF16, 8 * 256)
            KM = wk.get(BF16, 4 * 256)
            VM = wk.get(BF16, 2 * 512)
            QM = [wk.get(BF16, T) for _ in range(2)]
            SGM = [wk.get(BF16, T) for _ in range(2)]
            PT = [wk.get(BF16, 512) for _ in range(2)]
            R = wk.get(F32, 512)
            T1 = wk.get(F32, 512)
            rms_transpose(lambda i: mem_d[b, i * 128:(i + 1) * 128, :], 2, P_MNW,
                          lambda i: MEMT.v3(8, 256, i * 128, 128), wk)
            for h in range(4):
                ps = PS[2 + h % 2]
                for k in range(8):
                    p.matmul(ps.v(0, 256), WB.v(8192 + k * NW + h * 128, 128), MEMT.v(k * 256, 256),
                             start=(k == 0), stop=(k == 7))
                p.copy(KM.v(h * 256, 256), ps.v(0, 256), eng="act")
            for mt in range(2):
                ps = PS[2 + mt % 2]
                for k in range(8):
                    p.matmul(ps.v(), MEMT.v(k * 256 + mt * 128, 128), WB.v(8192 + k * NW + 512, 512),
                             start=(k == 0), stop=(k == 7))
                p.copy(VM.v(mt * 512, 512), ps.v(), eng="dve")
            pt_i = 0
            for h in range(4):
                qm = QM[h % 2]
                sgm = SGM[h % 2]
                for tq in range(4):
                    ps = PS[2 + tq % 2]
                    proj_fm(NW, h * 128, 128, tq * 512, 512, ps.v())
                    p.copy(qm.v(tq * 512, 512), ps.v(), eng="act")
                for tq in range(4):
                    ps = PS[2 + tq % 2]
                    proj_fm(NW, 512 + h * 128, 128, tq * 512, 512, ps.v())
                    p.act(sgm.v(tq * 512, 512), ps.v(), AF.Silu)
                for qt in range(4):
                    tq0 = qt * 512
                    po = PS[qt % 2]
                    den = PS[6 + qt % 2]
                    for mt in range(2):
                        sp = PS[4 + pt_i % 2]
                        ptile = PT[pt_i % 2]
                        pt_i += 1
                        p.matmul(sp.v(), KM.v(h * 256 + mt * 128, 128), qm.v(tq0, 512))
                        p.act(ptile.v(), sp.v(), AF.Exp, scale=128 ** -0.5)
                        p.matmul(po.v(), VM.v(mt * 512 + h * 128, 128), ptile.v(), start=(mt == 0), stop=(mt == 1))
                        p.matmul(den.v(), ones, ptile.v(), start=(mt == 0), stop=(mt == 1))
                    p.recip(R.v(), den.v())
                    p.tt(T1.v(), R.v(), sgm.v(tq0, 512), ALU.mult, eng="pool")
                    p.tt(MIX.v((12 + h) * T + tq0, 512), po.v(), T1.v(), ALU.mult)
            dbg("memo", MIX.v(12 * T, 4 * T), [128, 4 * T])

        def phase_out(b):
            NW = 1024
            load_w("out", wso3, wout3, 0, NW, 16)
            wk = Bump(WK, 0, WKSZ)
            XT = [wk.get(F32, 1024) for _ in range(2)]
            HT = [wk.get(F32, 1024) for _ in range(2)]
            OT = [wk.get(F32, 1024) for _ in range(2)]
            JNK = wk.get(BF16, 1024)
            SSQ = wk.get(F32, 16)
            RSTD = wk.get(F32, 16)
            for i in range(16):
                xt = XT[i % 2]
                ht = HT[i % 2]
                ot = OT[i % 2]
                p.dma(xt.v(), D(x_d[b, i * 128:(i + 1) * 128, :]))
                for nh in range(2):
                    ps = PS[(2 * i + nh) % 4]
                    for m in range(16):
                        p.matmul(ps.v(), MIX.v(m * T + i * 128, 128), WB.v(m * NW + nh * 512, 512),
                                 start=(m == 0), stop=(m == 15))
                    p.tt(ht.v(nh * 512, 512), ps.v(), xt.v(nh * 512, 512), ALU.add)
                p.act(JNK.v(), ht.v(), AF.Square, accum_out=SSQ.v(i, 1))
                p.act(RSTD.v(i, 1), SSQ.v(i, 1), AF.Ln, scale=1.0 / DM, bias=epsv)
                p.act(RSTD.v(i, 1), RSTD.v(i, 1), AF.Exp, scale=-0.5)
                p.stt(ot.v(), ht.v(), RSTD.v(i, 1), prm(P_FNW, 1024), ALU.mult, ALU.mult)
                p.dma(D(out_d[b, i * 128:(i + 1) * 128, :]), ot.v())

        for b in range(nseq):
            wkA = Bump(WK, 16384, WKSZ - 16384)
            rms_transpose(lambda i: x_d[b, i * 128:(i + 1) * 128, :], 16, P_NW,
                          lambda i: UT.v3(BF16, 8, T, i * 128, 128), wkA)
            if b == 0:
                dbg("ut", UT.v(), [128, 8 * T])
            if "S" in phases:
                phase_ssd(b)
            if "B" in phases:
                phase_attn(b)
            if "M" in phases:
                phase_mem(b)
            if "E" in phases:
                phase_out(b)
        print("ops:", p.nops, {e: len(v) for e, v in p.ops.items()})
        p.emit()
    return nc, dumps


def kernel(x, mem, norm_w, w_in, conv_w, conv_b, dt_bias, a_log, d_skip, ssd_norm_w, mem_norm_w, w_mem_kv,
           w_out, final_norm_w):
    f = lambda a: np.ascontiguousarray(np.asarray(a, dtype=np.float32))
    x, mem = f(x), f(mem)
    cb, cf, kc, qc = host_consts()
    cf[:, CF_PRM:CF_PRM + NPRM] = host_params(f(norm_w)[0], f(conv_w)[0], f(conv_b)[0], f(dt_bias)[0], f(a_log)[0],
                                              f(d_skip)[0], f(ssd_norm_w)[0], f(mem_norm_w)[0], f(final_norm_w))
    nc, _ = build()
    shared = {"w_in": f(w_in)[0], "w_kv": f(w_mem_kv)[0], "w_out": f(w_out)[0], "cstb": cb, "cstf": cf,
              "kconst": kc, "qconst": qc}
    in_maps = []
    for c in range(8):
        m = dict(shared)
        m["x"] = x[c * NSEQ:(c + 1) * NSEQ]
        m["mem"] = mem[c * NSEQ:(c + 1) * NSEQ]
        in_maps.append(m)
    res = run_bass_kernel_spmd(nc, in_maps, core_ids=list(range(8)))
    return np.concatenate([r["out"] for r in res.results], axis=0)
```

```python
from contextlib import ExitStack
import concourse.bass as bass
import concourse.mybir as mybir

F32 = mybir.dt.float32
BF16 = mybir.dt.bfloat16
I32 = mybir.dt.int32
AF = mybir.ActivationFunctionType
ALU = mybir.AluOpType
AX = mybir.AxisListType
ESZ = {F32: 4, BF16: 2, I32: 4}


class View:
    __slots__ = ("ap", "arena", "rngs", "pr")

    def __init__(self, ap, arena, rngs, pr=(0, 128)):
        self.ap, self.arena, self.rngs, self.pr = ap, arena, rngs, pr

    def re(self, s, **kw):
        return View(self.ap.rearrange(s, **kw), self.arena, self.rngs, self.pr)

    def bc(self, shape):
        return View(self.ap.broadcast_to(shape), self.arena, self.rngs, self.pr)

    def __getitem__(self, key):
        return View(self.ap[key], self.arena, self.rngs, self.pr)


class Arena:
    def __init__(self, name, t, dtype, ncols, const=False):
        self.name, self.t, self.dtype, self.ncols = name, t, dtype, ncols
        self.esz = ESZ[dtype]
        self.w = []
        self.r = []
        self.const = const
        self.alt = {dtype: t}
        self.psum = False
        self.qlast = [dict() for _ in range(4)]

    def v(self, lo=0, n=None, p0=0, p1=128):
        if n is None:
            n = self.ncols - lo
        assert 0 <= lo and lo + n <= self.ncols, (self.name, lo, n, self.ncols)
        return View(self.t[p0:p1, lo:lo + n], self, [(lo * self.esz, (lo + n) * self.esz)], (p0, p1))

    def v3(self, dtype, nk, stride, lo, n, p0=0, p1=128):
        if dtype not in self.alt:
            self.alt[dtype] = self.t.bitcast(dtype)
        e = ESZ[dtype]
        assert ((nk - 1) * stride + lo + n) * e <= self.ncols * self.esz
        total = self.ncols * self.esz // e
        o = max(0, lo + nk * stride - total)
        assert o <= stride - n and o <= lo, (self.name, lo, nk, stride, n, total)
        ws = lo - o
        ap = self.alt[dtype][p0:p1, ws:ws + nk * stride].rearrange("p (k s) -> p k s", s=stride)[:, :, o:o + n]
        return View(ap, self, [((k * stride + lo) * e, (k * stride + lo + n) * e) for k in range(nk)], (p0, p1))

    def vb(self, dtype, lo, n, p0=0, p1=128):
        if dtype not in self.alt:
            self.alt[dtype] = self.t.bitcast(dtype)
        e = ESZ[dtype]
        assert (lo + n) * e <= self.ncols * self.esz
        return View(self.alt[dtype][p0:p1, lo:lo + n], self, [(lo * e, (lo + n) * e)], (p0, p1))


class Sub:
    def __init__(self, arena, dtype, boff, n):
        self.a, self.dt, self.e, self.n = arena, dtype, ESZ[dtype], n
        assert boff % self.e == 0
        self.base = boff // self.e

    def v(self, lo=0, n=None, p0=0, p1=128):
        if n is None:
            n = self.n - lo
        assert 0 <= lo and lo + n <= self.n, (lo, n, self.n)
        return self.a.vb(self.dt, self.base + lo, n, p0, p1)

    def v3(self, nk, stride, lo, n, p0=0, p1=128):
        assert (nk - 1) * stride + lo + n <= self.n
        return self.a.v3(self.dt, nk, stride, self.base + lo, n, p0, p1)


class Bump:
    def __init__(self, arena, boff, size):
        self.a, self.off, self.end = arena, boff, boff + size

    def get(self, dtype, n):
        self.off = (self.off + 3) // 4 * 4
        s = Sub(self.a, dtype, self.off, n)
        self.off += n * ESZ[dtype]
        assert self.off <= self.end, ("bump overflow", self.off, self.end)
        return s


def D(ap):
    return View(ap, None, [])


COMPUTE = ("pe", "act", "dve", "pool")


class Prog:
    def __init__(self, nc, ndma=40):
        self.nc = nc
        self.ops = {e: [] for e in COMPUTE + ("sp",)}
        self.count = {e: 0 for e in COMPUTE}
        self.waited = {e: {} for e in COMPUTE + ("sp",)}
        self.ndma = ndma
        self.dma_cnt = [0] * ndma
        self.dma_next = 0
        self.npool = 0
        self.nops = 0

    def op(self, eng, fn, reads=(), writes=(), dma=False):
        deps = {}

        def add(tok):
            k, v = tok
            if deps.get(k, 0) < v:
                deps[k] = v

        for vw in reads:
            a = vw.arena
            if a is None:
                continue
            for (lo, hi) in vw.rngs:
                for (l, h, t) in a.w:
                    if l < hi and lo < h:
                        add(t)
        for vw in writes:
            a = vw.arena
            if a is None:
                continue
            assert not a.const, a.name
            for (lo, hi) in vw.rngs:
                for (l, h, t) in a.w:
                    if l < hi and lo < h:
                        add(t)
                for (l, h, t) in a.r:
                    if l < hi and lo < h:
                        add(t)
        for vw in list(reads) + list(writes):
            a = vw.arena
            if a is None or not a.psum:
                continue
            for q in range(vw.pr[0] // 32, (vw.pr[1] + 31) // 32):
                for e2, t in a.qlast[q].items():
                    if e2 != eng:
                        add(t)
        if dma and eng == "pool":
            key = ("p", self.npool)
            self.npool += 1
            tok = (key, 16)
            inc = (key, 16)
        elif dma:
            i = self.dma_next
            self.dma_next = (i + 1) % self.ndma
            if self.dma_cnt[i] > 0:
                add((("d", i), self.dma_cnt[i]))
            self.dma_cnt[i] += 16
            tok = (("d", i), self.dma_cnt[i])
            inc = (("d", i), 16)
        else:
            self.count[eng] += 1
            tok = (eng, self.count[eng])
            inc = (eng, 1)
        waits = []
        wd = self.waited[eng]
        for k, v in deps.items():
            if k == eng and eng == "pe":
                continue
            if wd.get(k, 0) >= v:
                continue
            wd[k] = v
            waits.append((k, v))
        self.ops[eng].append((waits, fn, inc))
        self.nops += 1
        for vw in list(reads) + list(writes):
            a = vw.arena
            if a is None or not a.psum:
                continue
            for q in range(vw.pr[0] // 32, (vw.pr[1] + 31) // 32):
                a.qlast[q][eng] = tok
        for vw in reads:
            a = vw.arena
            if a is None or a.const:
                continue
            for (lo, hi) in vw.rngs:
                if not dma:
                    a.r = [x for x in a.r if not (x[2][0] == eng and lo <= x[0] and x[1] <= hi)]
                a.r.append((lo, hi, tok))
        for vw in writes:
            a = vw.arena
            if a is None:
                continue
            for (lo, hi) in vw.rngs:
                a.w = [x for x in a.w if not (lo <= x[0] and x[1] <= hi)]
                a.r = [x for x in a.r if not (lo <= x[0] and x[1] <= hi)]
                a.w.append((lo, hi, tok))
        return tok

    def freeze(self, arena):
        arena.const = True
        arena.r = []

    def matmul(self, out, lhsT, rhs, start=True, stop=True, **kw):
        return self.op("pe", lambda e: e.matmul(out.ap, lhsT.ap, rhs.ap, start=start, stop=stop, **kw),
                       [lhsT, rhs] + ([] if start else [out]), [out])

    def transpose(self, out, in_, ident):
        return self.op("pe", lambda e: e.transpose(out.ap, in_.ap, ident.ap), [in_, ident], [out])

    def act(self, out, in_, func, bias=None, scale=None, accum_out=None):
        rd = [in_]
        kw = {}
        if bias is not None:
            if isinstance(bias, View):
                rd.append(bias)
                kw["bias"] = bias.ap
            else:
                kw["bias"] = bias
        if scale is not None:
            if isinstance(scale, View):
                rd.append(scale)
                kw["scale"] = scale.ap
            else:
                kw["scale"] = scale
        wr = [out]
        if accum_out is not None:
            wr.append(accum_out)
            kw["accum_out"] = accum_out.ap
        return self.op("act", lambda e: e.activation(out.ap, in_.ap, func, **kw), rd, wr)

    def tt(self, out, in0, in1, op, eng="dve"):
        return self.op(eng, lambda e: e.tensor_tensor(out.ap, in0.ap, in1.ap, op), [in0, in1], [out])

    def ts(self, out, in0, s1, s2, op0, op1=None, eng="dve", accum_out=None):
        rd = [in0]
        a1 = s1
        a2 = s2
        if isinstance(s1, View):
            rd.append(s1)
            a1 = s1.ap
        if isinstance(s2, View):
            rd.append(s2)
            a2 = s2.ap
        kw = {}
        if op1 is not None:
            kw["op1"] = op1
        wr = [out]
        if accum_out is not None:
            kw["accum_out"] = accum_out.ap
            wr.append(accum_out)
        return self.op(eng, lambda e: e.tensor_scalar(out.ap, in0.ap, a1, a2, op0, **kw), rd, wr)

    def stt(self, out, in0, scalar, in1, op0, op1):
        rd = [in0, in1]
        sc = scalar
        if isinstance(scalar, View):
            rd.append(scalar)
            sc = scalar.ap
        return self.op("dve", lambda e: e.scalar_tensor_tensor(out.ap, in0.ap, sc, in1.ap, op0, op1), rd, [out])

    def copy(self, out, in_, eng="dve"):
        if eng == "act":
            return self.op("act", lambda e: e.copy(out.ap, in_.ap), [in_], [out])
        return self.op(eng, lambda e: e.tensor_copy(out.ap, in_.ap), [in_], [out])

    def reduce(self, out, in_, op, axis=AX.X, eng="dve"):
        return self.op(eng, lambda e: e.tensor_reduce(out.ap, in_.ap, axis, op), [in_], [out])

    def recip(self, out, in_):
        return self.op("dve", lambda e: e.reciprocal(out.ap, in_.ap), [in_], [out])

    def memset(self, out, val, eng="dve"):
        return self.op(eng, lambda e: e.memset(out.ap, val), [], [out])

    def scan(self, out, d0, d1, initial, op0, op1):
        rd = [d0, d1]
        ini = initial
        if isinstance(initial, View):
            rd.append(initial)
            ini = initial.ap
        return self.op("dve", lambda e: e.tensor_tensor_scan(out.ap, d0.ap, d1.ap, ini, op0, op1), rd, [out])

    def affine_select(self, out, in_, pattern, cmp, fill, base, cm):
        return self.op("pool", lambda e: e.affine_select(out.ap, in_.ap, pattern, cmp, fill, base=base,
                                                         channel_multiplier=cm), [in_], [out])

    def iota(self, out, pattern, base, cm):
        return self.op("pool", lambda e: e.iota(out.ap, pattern, base=base, channel_multiplier=cm), [], [out])

    def dma(self, out, in_, q="sp", **kw):
        if q == "pool":
            kw.setdefault("max_dma_last_dim", 4096)
        return self.op(q, lambda e: e.dma_start(out=out.ap, in_=in_.ap, **kw), [in_], [out], dma=True)

    def emit(self):
        nc = self.nc
        with ExitStack() as es:
            sems = {}
            for e in COMPUTE:
                sems[e] = es.enter_context(nc.semaphore("s_" + e))
            for i in range(self.ndma):
                sems[("d", i)] = es.enter_context(nc.semaphore("s_d%d" % i))
            for i in range(self.npool):
                sems[("p", i)] = es.enter_context(nc.semaphore("s_p%d" % i))
            fin = []
            for i in range(self.ndma):
                if self.dma_cnt[i] > 0:
                    fin.append((("d", i), self.dma_cnt[i]))
            for i in range(self.npool):
                fin.append((("p", i), 16))
            for e in COMPUTE:
                if self.count[e] > 0:
                    fin.append((e, self.count[e]))
            block = es.enter_context(nc.Block())

            def run(stream, final=None):
                def f(eng):
                    for waits, fn, inc in stream:
                        for k, v in waits:
                            eng.wait_ge(sems[k], v)
                        ins = fn(eng)
                        ins.then_inc(sems[inc[0]], inc[1])
                    if final:
                        for k, v in final:
                            eng.wait_ge(sems[k], v)
                return f

            block.tensor(run(self.ops["pe"]))
            block.scalar(run(self.ops["act"]))
            block.vector(run(self.ops["dve"]))
            block.gpsimd(run(self.ops["pool"]))
            block.sync(run(self.ops["sp"], fin))
from concourse.bass_utils import run_bass_kernel_spmd
import numpy as np

T = 2048
DM = 1024
NSEQ = 2
EPS = 1e-6
BIG = 30000.0
C_Q, C_K, C_V, C_G, C_Z, C_XBC, C_DT, C_QM, C_GM = 0, 512, 1024, 1536, 2048, 3072, 5120, 5136, 5648
CB_ID, CB_TRI, CB_MNEG, CB_ONES, CB_EH, NCB = 0, 128, 256, 384, 512, 2560
CF_ID, CF_ONES, CF_PRM, CF_CAP, CF_CMK, CF_OWN, CF_MISC, NCF = 0, 128, 256, 1424, 1488, 1552, 1616, 1624
P_NW, P_MNW, P_SNW, P_CVB, P_CVW, P_DSK, P_DTB, P_ALOG, P_FNW = 0, 8, 16, 24, 40, 104, 112, 128, 144
NPRM = 1168


def host_consts():
    cb = np.zeros((128, NCB), np.float32)
    cb[:, CB_ID:CB_ID + 128] = np.eye(128)
    s = np.arange(128)[:, None]
    t = np.arange(128)[None, :]
    cb[:, CB_TRI:CB_TRI + 128] = (t >= s)
    cb[:, CB_MNEG:CB_MNEG + 128] = np.where(t < s, -BIG, 0.0)
    cb[:, CB_ONES:CB_ONES + 128] = 1.0
    for h in range(16):
        cb[h, CB_EH + h * 128: CB_EH + (h + 1) * 128] = 1.0
    cf = np.zeros((128, NCF), np.float32)
    cf[:, CF_ID:CF_ID + 128] = np.eye(128)
    cf[:, CF_ONES:CF_ONES + 128] = 1.0
    cap = np.zeros((8, 8), np.float32)
    cmk = np.zeros((8, 8), np.float32)
    own = np.zeros((8, 8), np.float32)
    for ti in range(8):
        qb = 4 + ti // 2
        for j in range(8):
            cap[ti, j] = 1e30 if j < qb else -1e30
            cmk[ti, j] = 1.0 if j < qb else 0.0
            own[ti, j] = 1.0 if j == qb else 0.0
    cf[:, CF_CAP:CF_CAP + 64] = cap.reshape(-1)[None, :]
    cf[:, CF_CMK:CF_CMK + 64] = cmk.reshape(-1)[None, :]
    cf[:, CF_OWN:CF_OWN + 64] = own.reshape(-1)[None, :]
    cf[:, CF_MISC] = EPS
    cf[:, CF_MISC + 1] = 1.0
    kc = np.zeros((128, T), np.float32)
    pos = np.arange(T)
    for r in range(8):
        kc[64 + r] = (pos // 256 == r)
    kc[96] = pos // 16
    kc[97] = pos % 16
    kc[98] = 1.0
    kc[99] = 1.0
    qc = np.zeros((8, 4, T), np.float32)
    for h in range(8):
        sl = 2.0 ** (-8.0 * (h + 1) / 8)
        qc[h, 0] = 16 * sl * 8
        qc[h, 1] = sl * 8
        qc[h, 2] = -16 * sl * 8 * (pos // 16)
        qc[h, 3] = -sl * 8 * (pos % 16)
    return cb, cf, kc, qc


def host_params(norm_w, conv_w, conv_b, dt_bias, a_log, d_skip, ssd_norm_w, mem_norm_w, final_norm_w):
    prm = np.zeros((128, NPRM), np.float32)
    prm[:, P_NW:P_NW + 8] = norm_w.reshape(8, 128).T
    prm[:, P_MNW:P_MNW + 8] = mem_norm_w.reshape(8, 128).T
    prm[:, P_SNW:P_SNW + 8] = ssd_norm_w.reshape(8, 128).T
    prm[:, P_CVB:P_CVB + 16] = conv_b.reshape(16, 128).T
    prm[:, P_CVW:P_CVW + 64] = conv_w.T.reshape(16, 128, 4).transpose(1, 0, 2).reshape(128, 64)
    prm[:, P_DSK:P_DSK + 8] = np.repeat(d_skip, 64).reshape(8, 128).T
    prm[:, P_DTB:P_DTB + 16] = dt_bias[None, :]
    prm[:, P_ALOG:P_ALOG + 16] = a_log[None, :]
    prm[:, P_FNW:P_FNW + 1024] = final_norm_w[None, :]
    return prm
def build(nseq=NSEQ, dump=None, phases="ASBME"):
    nc = bass.Bass("TRN2", target_bir_lowering=False)
    x_d = nc.dram_tensor("x", [nseq, T, DM], F32, kind="ExternalInput").ap()
    mem_d = nc.dram_tensor("mem", [nseq, 256, DM], F32, kind="ExternalInput").ap()
    win_d = nc.dram_tensor("w_in", [DM, 6160], F32, kind="ExternalInput").ap()
    wkv_d = nc.dram_tensor("w_kv", [DM, 1024], F32, kind="ExternalInput").ap()
    wout_d = nc.dram_tensor("w_out", [2048, DM], F32, kind="ExternalInput").ap()
    cb_d = nc.dram_tensor("cstb", [128, NCB], F32, kind="ExternalInput").ap()
    cf_d = nc.dram_tensor("cstf", [128, NCF], F32, kind="ExternalInput").ap()
    kc_d = nc.dram_tensor("kconst", [128, T], F32, kind="ExternalInput").ap()
    qc_d = nc.dram_tensor("qconst", [8, 4, T], F32, kind="ExternalInput").ap()
    out_d = nc.dram_tensor("out", [nseq, T, DM], F32, kind="ExternalOutput").ap()
    dumps = {}
    wsi_d = nc.dram_tensor("wsc_in", [DM, 6160], BF16, kind="Internal").ap()
    wskv_d = nc.dram_tensor("wsc_kv", [DM, 1024], BF16, kind="Internal").ap()
    wso_d = nc.dram_tensor("wsc_out", [2048, DM], BF16, kind="Internal").ap()
    kcs_d = nc.dram_tensor("kc_sc", [128, T], BF16, kind="Internal").ap()
    qcs_d = nc.dram_tensor("qc_sc", [32, T], BF16, kind="Internal").ap()
    qc2_d = qc_d.rearrange("h r t -> (h r) t")
    wsi3 = wsi_d.rearrange("(k p) c -> p k c", p=128)
    wskv3 = wskv_d.rearrange("(k p) c -> p k c", p=128)
    wso3 = wso_d.rearrange("(k p) c -> p k c", p=128)
    win3 = win_d.rearrange("(k p) c -> p k c", p=128)
    wkv3 = wkv_d.rearrange("(k p) c -> p k c", p=128)
    wout3 = wout_d.rearrange("(k p) c -> p k c", p=128)

    with ExitStack() as es:
        def sb(name, n, dt):
            t = es.enter_context(nc.sbuf_tensor(name, [128, n], dt))
            return Arena(name, t, dt, n)

        UT = sb("UT", 8 * T, BF16)
        MIX = sb("MIX", 16 * T, BF16)
        WB = sb("WB", 8 * 3088, BF16)
        WKSZ = 47616
        WK = sb("WK", WKSZ // 2, BF16)
        CB = sb("CB", NCB, BF16)
        CF = sb("CF", NCF, F32)
        SM = sb("SM", 64, F32)
        PS = []
        for i in range(8):
            t = es.enter_context(nc.psum_tensor("ps%d" % i, [128, 512], F32))
            PS.append(Arena("ps%d" % i, t, F32, 512))
            PS[-1].psum = True
        p = Prog(nc)

        def dbg(name, view, shape):
            if dump is None or name not in dump:
                return
            d = nc.dram_tensor("dbg_" + name, list(shape), view.ap.dtype, kind="ExternalOutput").ap()
            dumps[name] = d
            p.dma(D(d), view)

        p.dma(CB.v(), D(cb_d), q="pool")
        p.dma(CF.v(), D(cf_d))
        ident = CB.v(CB_ID, 128)
        tri = CB.v(CB_TRI, 128)
        mneg = CB.v(CB_MNEG, 128)
        ones = CB.v(CB_ONES, 128)
        identf = CF.v(CF_ID, 128)
        epsv = CF.v(CF_MISC, 1)
        onev = CF.v(CF_MISC + 1, 1)

        def prm(off, n, p0=0, p1=128):
            return CF.v(CF_PRM + off, n, p0, p1)

        AB = SM.v(0, 16)
        p.act(AB, prm(P_ALOG, 16), AF.Exp)
        p.ts(AB, AB, -1.0, None, ALU.mult)

        SC = {n: Arena("sc_" + n, None, BF16, 1) for n in ("attn", "mem", "ssd", "kv", "out", "kc", "qc")}
        converted = [False]

        def scv(ap, name, k):
            return View(ap, SC[name], [(k, k + 1)])

        NHALF = {"attn": 4, "mem": 4, "ssd": 4, "kv": 4, "out": 8}

        def convert_all():
            p.dma(scv(kcs_d, "kc", 0), D(kc_d), q="pool")
            p.dma(scv(qcs_d, "qc", 0), D(qc2_d), q="pool")
            for name, src, dst, c0, nw, nrows in (("attn", win_d, wsi_d, 0, 2048, 1024), ("kv", wkv_d, wskv_d, 0, 1024, 1024),
                                                  ("mem", win_d, wsi_d, C_QM, 1024, 1024), ("out", wout_d, wso_d, 0, 1024, 2048),
                                                  ("ssd", win_d, wsi_d, C_Z, 3088, 1024)):
                hr = nrows // 2
                for h in range(2):
                    p.dma(scv(dst[h * hr:(h + 1) * hr, c0:c0 + nw], name, h), D(src[h * hr:(h + 1) * hr, c0:c0 + nw]),
                          q="pool")
            converted[0] = True

        def load_w(name, dst3, src3, c0, nw, nk, wb_off=0):
            for k in range(nk):
                if converted[0]:
                    p.dma(WB.v(wb_off + k * nw, nw), scv(dst3[:, k, c0:c0 + nw], name, k // NHALF[name]), q="sp")
                else:
                    p.dma(WB.v(wb_off + k * nw, nw), D(src3[:, k, c0:c0 + nw]), q="pool")

        pj_i = [0]

        def rms_transpose(src_tile, ntiles, nwoff, dst_fn, wk):
            XT = [wk.get(F32, 1024) for _ in range(2)]
            XS = [wk.get(BF16, 1024) for _ in range(2)]
            JNK = wk.get(BF16, 1024)
            SSQ = wk.get(F32, 16)
            RSTD = wk.get(F32, 16)
            for i in range(ntiles):
                xt = XT[i % 2]
                p.dma(xt.v(), D(src_tile(i)))
                p.act(JNK.v(), xt.v(), AF.Square, accum_out=SSQ.v(i, 1))
                p.act(RSTD.v(i, 1), SSQ.v(i, 1), AF.Ln, scale=1.0 / DM, bias=epsv)
                p.act(RSTD.v(i, 1), RSTD.v(i, 1), AF.Exp, scale=-0.5)
                xs = XS[i % 2]
                p.ts(xs.v(), xt.v(), RSTD.v(i, 1), None, ALU.mult)
                pb = PS[i % 2]
                for k in range(8):
                    p.transpose(pb.vb(BF16, k * 128, 128), xs.v(k * 128, 128), ident)
                p.tt(dst_fn(i), pb.vb(BF16, 0, 1024).re("p (k t) -> p k t", k=8),
                     prm(nwoff, 8).re("p (k o) -> p k o", o=1).bc([128, 8, 128]), ALU.mult)

        def utv(k, t0, n):
            return UT.v(k * T + t0, n)

        def proj_fm(nw, c0, ncols, t0, n, ps_view):
            for k in range(8):
                p.matmul(ps_view, WB.v(k * nw + c0, ncols), utv(k, t0, n), start=(k == 0), stop=(k == 7))

        def phase_ssd(b):
            NW = 3088
            load_w("ssd", wsi3, win3, C_Z, NW, 8)
            if not converted[0]:
                convert_all()
            b1 = Bump(MIX, 0, 16384)
            b2 = Bump(MIX, 12 * T * 2, 16384)
            wk = Bump(WK, 0, WKSZ)
            XPRE = b1.get(BF16, 16 * 259)
            XST = b1.get(BF16, 8 * 256)
            BMT = b1.get(BF16, 4 * 256)
            LT = [b1.get(BF16, 384) for _ in range(2)]
            SZ = b2.get(BF16, 8 * 256)
            PREV = b2.get(F32, 1024)
            XDD = b2.get(BF16, 2 * 1024)
            CMT = b2.get(BF16, 4 * 256)
            BTOK = b2.get(BF16, 2 * 512)
            DIAG = wk.get(BF16, 64 * 128)
            CBT = wk.get(BF16, 4 * 384)
            L0 = [wk.get(BF16, 256) for _ in range(2)]
            MT = [wk.get(BF16, 384) for _ in range(2)]
            GT = [wk.get(BF16, 256) for _ in range(2)]
            GG = [wk.get(F32, 256) for _ in range(4)]
            GSQ = [wk.get(BF16, 256) for _ in range(4)]
            RS = wk.get(F32, 256)
            XDT = wk.get(BF16, 2 * 2048)
            PREVB = wk.get(BF16, 2048)
            DTR = wk.get(F32, 32)
            DT = wk.get(F32, 32)
            DA = wk.get(F32, 256)
            NCS = wk.get(F32, 32)
            DEC = wk.get(F32, 32)
            W2 = wk.get(F32, 32)
            CST = wk.get(F32, 256)
            CSH = wk.get(BF16, 256)
            CSL = wk.get(BF16, 256)
            DECT = wk.get(F32, 256)
            DG16 = wk.get(F32, 16)
            CDB = wk.get(F32, 16)

            for zt in (XDT, PREVB, DA, CST, CSH, CSL, DECT, DG16):
                p.memset(zt.v(), 0.0, eng="pool")
            for j in range(64):
                p.ts(DIAG.v(j * 128, 128), ident, prm(P_CVW + j, 1), None, ALU.mult,
                     eng=("pool" if j % 2 else "dve"))
            p.memset(XPRE.v3(16, 259, 0, 3), 0.0)
            p.memset(PREV.v(), 0.0)

            pending = []
            for c in range(8):
                t0 = c * 256

                def zgroup(zc):
                    ps = PS[zc % 2].v((zc // 2 % 2) * 256, 256)
                    proj_fm(NW, zc * 128, 128, t0, 256, ps)
                    p.copy(SZ.v(zc * 256, 256), ps, eng="dve")

                def xgroup(cc):
                    ps = PS[cc % 2].v((cc // 2 % 2) * 256, 256)
                    proj_fm(NW, 1024 + cc * 128, 128, t0, 256, ps)
                    p.copy(XPRE.v(cc * 259 + 3, 256), ps, eng=("dve" if cc % 2 else "act"))

                if c > 0:
                    p.copy(XPRE.v3(16, 259, 0, 3), XPRE.v3(16, 259, 256, 3), eng="pool")
                for lt in range(2):
                    for k in range(8):
                        p.matmul(PS[5].v(384 + lt * 16, 16), utv(k, t0 + lt * 128, 128), WB.v(k * NW + 3072, 16),
                                 start=(k == 0), stop=(k == 7))
                p.tt(DTR.v(), PS[5].v(384, 32).re("p (l h) -> p l h", l=2),
                     prm(P_DTB, 16).re("p (o h) -> p o h", o=1).bc([128, 2, 16]), ALU.add)
                p.act(DTR.v(), DTR.v(), AF.Exp)
                p.act(DT.v(), DTR.v(), AF.Ln, bias=onev)
                p.tt(DA.v3(2, 128, 0, 16), DT.v().re("p (l h) -> p l h", l=2),
                     SM.v(0, 16).re("p (o h) -> p o h", o=1).bc([128, 2, 16]), ALU.mult)
                for zc in range(6):
                    zgroup(zc)
                    if zc == 1 and pending:
                        pending.pop(0)()
                for lt in range(2):
                    p.transpose(PS[6].v(lt * 128, 128), DA.v(lt * 128, 128), identf)
                p.scan(CST.v(0, 256, 0, 16), CF.v(CF_ONES, 1, 0, 16).bc([16, 256]), PS[6].v(0, 256, 0, 16), 0.0,
                       ALU.mult, ALU.add)
                p.copy(CSH.v(0, 256, 0, 16), CST.v(0, 256, 0, 16))
                p.tt(CSL.v(0, 256, 0, 16), CST.v(0, 256, 0, 16), CSH.v(0, 256, 0, 16), ALU.subtract)
                p.act(DECT.v(0, 256, 0, 16), CST.v(0, 256, 0, 16), AF.Exp, scale=-1.0, bias=CST.v(255, 1, 0, 16))
                for zc in range(6, 8):
                    zgroup(zc)
                for cc in range(8, 12):
                    xgroup(cc)
                for lt in range(2):
                    p.transpose(PS[7].v(lt * 128, 128), CST.v(lt * 128, 128), identf)
                    p.transpose(PS[7].v(256 + lt * 128, 128), DECT.v(lt * 128, 128), identf)
                p.ts(NCS.v().re("p (l h) -> p l h", l=2), PS[7].v3(F32, 2, 128, 0, 16), -1.0, None, ALU.mult)
                p.copy(DEC.v().re("p (l h) -> p l h", l=2), PS[7].v3(F32, 2, 128, 256, 16))
                p.tt(W2.v(), DT.v(), DEC.v(), ALU.mult)
                if c < 7:
                    p.ts(DG16.v(0, 16, 0, 16), CF.v(CF_ID, 16, 0, 16), CST.v(255, 1, 0, 16), None, ALU.mult)
                    p.matmul(PS[5].v(480, 16), CF.v(CF_ONES, 128), DG16.v())
                    p.act(CDB.v(), PS[5].v(480, 16), AF.Exp)
                for cc in list(range(12, 16)) + list(range(0, 8)):
                    xgroup(cc)

                def conv(cc):
                    ps = PS[cc % 2].v((cc // 2 % 2) * 256, 256)
                    for k in range(4):
                        p.matmul(ps, DIAG.v((cc * 4 + k) * 128, 128), XPRE.v(cc * 259 + k, 256),
                                 start=(k == 0), stop=(k == 3))
                    if cc < 8:
                        dst = XST.v(cc * 256, 256)
                    elif cc < 12:
                        dst = BMT.v((cc - 8) * 256, 256)
                    else:
                        dst = CMT.v((cc - 12) * 256, 256)
                    p.act(dst, ps, AF.Silu, bias=prm(P_CVB + cc, 1))

                for cc in range(0, 8):
                    conv(cc)
                for lt in range(2):
                    pb = PS[6] if lt == 0 else PS[4]
                    for cc in range(8):
                        p.transpose(pb.vb(BF16, cc * 128, 128), XST.v(cc * 256 + lt * 128, 128), ident)
                    src = pb.vb(BF16, 0, 1024).re("p (h q) -> p h q", h=16)
                    for hh in range(2):
                        p.tt(XDT.v(lt * 2048, 2048).re("p (c e q) -> p c e q", c=8, e=2)[:, :, hh, hh * 64:hh * 64 + 64],
                             src.re("p (c e) q -> p c e q", e=2)[:, :, hh, :],
                             DT.v(lt * 16, 16).re("p (c e o) -> p c e o", e=2, o=1)[:, :, hh, :].bc([128, 8, 64]),
                             ALU.mult)
                    p.tt(XDD.v(lt * 1024, 1024).re("p (h q) -> p h q", h=16), src,
                         W2.v(lt * 16, 16).re("p (h o) -> p h o", o=1).bc([128, 16, 64]), ALU.mult)
                for cc in range(8, 16):
                    conv(cc)
                p.act(SZ.v(), SZ.v(), AF.Silu)
                for lt in range(2):
                    pb = PS[7]
                    for g in range(4):
                        p.transpose(pb.vb(BF16, g * 128, 128), BMT.v(g * 256 + lt * 128, 128), ident)
                    p.copy(BTOK.v(lt * 512, 512), pb.vb(BF16, 0, 512), eng="dve")
                for g in range(4):
                    ps = PS[5] if g % 2 == 0 else PS[6]
                    p.matmul(ps.v(0, 256), BMT.v(g * 256, 128), CMT.v(g * 256, 256))
                    p.matmul(ps.v(256, 128), BMT.v(g * 256 + 128, 128), CMT.v(g * 256 + 128, 128))
                    p.copy(CBT.v(g * 384, 384), ps.v(0, 384), eng="dve")
                if c < 7:
                    for g in range(4):
                        st = PS[g // 2].v((g % 2) * 256, 256)
                        for lt in range(2):
                            p.matmul(st, BTOK.v(lt * 512 + g * 128, 128), XDD.v(lt * 1024 + g * 256, 256),
                                     start=(lt == 0), stop=(lt == 1))
                    for g in range(4):
                        st = PS[g // 2].v((g % 2) * 256, 256)
                        pv = PREV.v(g * 256, 256)
                        p.tt(pv.re("p (h q) -> p h q", h=4), pv.re("p (h q) -> p h q", h=4),
                             CDB.v(g * 4, 4).re("p (h o) -> p h o", o=1).bc([128, 4, 64]), ALU.mult)
                        p.tt(pv, pv, st, ALU.add)
                def seg_stage(h):
                    g = h // 4
                    sg = PS[2 + h % 2]
                    eh = CB.v(CB_EH + h * 128, 128)
                    csh = lambda lo, n: CSH.v(lo, n)
                    csl = lambda lo, n: CSL.v(lo, n)
                    p.matmul(sg.v(0, 256), eh, csh(0, 256), start=True, stop=False)
                    p.matmul(sg.v(0, 256), eh, csl(0, 256), start=False, stop=True)
                    p.matmul(sg.v(256, 128), eh, csh(0, 128), start=True, stop=False)
                    p.matmul(sg.v(256, 128), eh, csl(0, 128), start=False, stop=False)
                    p.matmul(sg.v(256, 128), ident, mneg, start=False, stop=True)
                    p.matmul(sg.v(384, 128), eh, csh(128, 128), start=True, stop=False)
                    p.matmul(sg.v(384, 128), eh, csl(128, 128), start=False, stop=False)
                    p.matmul(sg.v(384, 128), ident, mneg, start=False, stop=True)
                    l0 = L0[h % 2]
                    lt_ = LT[h % 2]
                    mt = MT[h % 2]
                    gt = GT[h % 2]
                    if c > 0:
                        p.act(l0.v(), sg.v(0, 256), AF.Exp)
                    p.act(lt_.v(0, 128), sg.v(256, 128), AF.Exp, bias=NCS.v(h, 1))
                    p.act(lt_.v(128, 128), sg.v(128, 128), AF.Exp, bias=NCS.v(h, 1))
                    p.act(lt_.v(256, 128), sg.v(384, 128), AF.Exp, bias=NCS.v(16 + h, 1))
                    p.tt(mt.v(), lt_.v(), CBT.v(g * 384, 384), ALU.mult)
                    if c > 0:
                        p.tt(gt.v(), l0.v(), CMT.v(g * 256, 256), ALU.mult, eng="pool")

                def y_stage(h):
                    g = h // 4
                    pr = h % 2
                    pc = h // 2
                    mt = MT[h % 2]
                    gt = GT[h % 2]
                    ybank = PS[4 + 2 * (pc % 2)]
                    yt = ybank.v(0, 256)
                    yt2 = ybank.v(128, 128)
                    p.matmul(yt, XDT.v(h * 128, 128), mt.v(0, 256), start=(pr == 0), stop=False)
                    p.matmul(yt2, XDT.v(2048 + h * 128, 128), mt.v(256, 128), start=False, stop=(c == 0 and pr == 1))
                    if c > 0:
                        p.matmul(yt, PREVB.v(h * 128, 128), gt.v(), start=False, stop=(pr == 1))
                    if pr == 1:
                        gg = GG[pc % 4]
                        p.stt(gg.v(), XST.v(pc * 256, 256), prm(P_DSK + pc, 1), yt, ALU.mult, ALU.add)
                        p.tt(gg.v(), gg.v(), SZ.v(pc * 256, 256), ALU.mult)
                        p.act(GSQ[pc % 4].v(), gg.v(), AF.Square)
                def norm_stage(g, t0=t0):
                        pc = 2 * g + 1
                        pcs = [pc - 1, pc]
                        ss = PS[7].v(256, 256)
                        p.matmul(ss, ones, GSQ[pcs[0] % 4].v(), start=True, stop=False)
                        p.matmul(ss, ones, GSQ[pcs[1] % 4].v(), start=False, stop=True)
                        p.act(RS.v(), ss, AF.Ln, scale=1.0 / 256, bias=epsv)
                        p.act(RS.v(), RS.v(), AF.Exp, scale=-0.5)
                        for q in pcs:
                            p.stt(MIX.v((4 + q) * T + t0, 256), GG[q % 4].v(), prm(P_SNW + q, 1), RS.v(),
                                  ALU.mult, ALU.mult)

                for h in range(17):
                    if h < 16:
                        seg_stage(h)
                    if 1 <= h <= 16:
                        y_stage(h - 1)
                    if h >= 2 and (h - 2) % 4 == 3:
                        norm_stage((h - 2) // 4)
                pending.append(lambda ns=norm_stage: ns(3))
                if c < 7:
                    for g in range(4):
                        pv = PREV.v(g * 256, 256)
                        for hh in range(2):
                            p.copy(PREVB.v(g * 512, 512).re("p (c e q) -> p c e q", c=2, e=2)[:, :, hh, hh * 64:hh * 64 + 64],
                                   pv.re("p (c e q) -> p c e q", c=2, e=2)[:, :, hh, :], eng="pool")
            while pending:
                pending.pop(0)()
            dbg("ssd", MIX.v(4 * T, 8 * T), [128, 8 * T])

        def attn_core(kt_list_fn, nq, kv_lhsT, q_rhs, v_lhsT, scale, PTs, epilogue, diag_fn=None, den_lhsT=None):
            pass

        def phase_attn(b):
            NW = 2048
            load_w("attn", wsi3, win3, 0, NW, 8)
            wk = Bump(WK, 0, WKSZ)
            bm = Bump(MIX, 12 * T * 2, 16384)
            bw = Bump(WB, 8 * NW * 2, WB.ncols * 2 - 8 * NW * 2)
            sets = [dict(QK=[wk.get(BF16, T) for _ in range(4)], VA=wk.get(BF16, 2048), VB=wk.get(BF16, 2048),
                         SG=wk.get(BF16, T)),
                    dict(QK=[bm.get(BF16, T) for _ in range(4)], VA=bw.get(BF16, 2048), VB=bw.get(BF16, 2048),
                         SG=bw.get(BF16, T))]
            PT = [wk.get(BF16, 512) for _ in range(4)]
            R = [wk.get(F32, 512) for _ in range(2)]
            T1 = [wk.get(F32, 512) for _ in range(2)]
            PENB = wk.get(BF16, 8 * 128)
            GM = wk.get(F32, 64)
            CMP = wk.get(F32, 512)
            RANK = wk.get(F32, 64)
            KSUM = wk.get(F32, 8)
            KMTS = [wk.get(BF16, 8) for _ in range(2)]
            for KMT in KMTS:
                p.memset(KMT.v(), 0.0, eng="pool")
            p.memset(PENB.v(), 0.0, eng="pool")
            for S in sets:
                for i in range(2):
                    p.memset(S["QK"][i].v(), 0.0, eng="pool")
                    p.dma(S["QK"][2 + i].v(), scv(kcs_d, "kc", 0), q="sp")
                p.memset(S["VA"].v(), 1.0, eng="pool")
                p.memset(S["VB"].v(), 1.0, eng="pool")

            def make_thunks(hp, S):
                QK, VA, VB, SG = S["QK"], S["VA"], S["VB"], S["SG"]
                th = []

                def t_qc():
                    for hh in range(2):
                        h_ = 2 * hp + hh
                        p.dma(QK[hh].v(0, T, 96, 100), scv(qcs_d[4 * h_:4 * h_ + 4, :], "qc", 0), q="sp")
                th.append(t_qc)
                def half_proj(c0, tq, half, evac):
                    def f():
                        ps = PS[2 + tq % 2]
                        if half == 1:
                            for k in range(8):
                                p.matmul(ps.v(), WB.v(k * NW + c0, 128), utv(k, tq * 512, 512), start=(k == 0), stop=(k == 7))
                            evac(ps)
                    return f

                for which, c0 in ((0, C_Q), (1, C_K)):
                    for tq in range(4):
                        def ev(ps, which=which, tq=tq):
                            p.copy(QK[2 * which].v(tq * 512, 512, 0, 64), ps.v(0, 512, 0, 64), eng="act")
                            p.copy(QK[2 * which + 1].v(tq * 512, 512, 0, 64), ps.v(0, 512, 64, 128), eng="dve")
                        th.append(half_proj(c0 + hp * 128, tq, 0, ev))
                        th.append(half_proj(c0 + hp * 128, tq, 1, ev))

                def gate1(hh):
                    K = QK[2 + hh]
                    KMT = KMTS[hh]
                    p.reduce(KSUM.v(0, 8, 0, 64), K.v(0, T, 0, 64).re("p (j s) -> p j s", j=8), ALU.add)
                    p.ts(KMT.v(0, 8, 0, 64), KSUM.v(0, 8, 0, 64), 1.0 / 256, None, ALU.mult)

                def gate2(hh):
                    Q = QK[hh]
                    KMT = KMTS[hh]
                    gps = PS[2]
                    for ti in range(8):
                        p.matmul(gps.v(ti * 8, 8), Q.v(1024 + ti * 128, 128), KMT.v())
                    p.tt(GM.v(), gps.v(0, 64), CF.v(CF_CAP, 64), ALU.min)
                    g3 = GM.v().re("p (t j) -> p t j", t=8)
                    p.tt(CMP.v().re("p (t j k) -> p t j k", t=8, j=8),
                         g3.re("p t (o k) -> p t o k", o=1).bc([128, 8, 8, 8]),
                         g3.re("p t (j o) -> p t j o", o=1).bc([128, 8, 8, 8]), ALU.is_gt)
                    p.reduce(RANK.v(), CMP.v().re("p (a k) -> p a k", k=8), ALU.add)
                    p.ts(RANK.v(), RANK.v(), 3.0, None, ALU.is_lt)
                    p.tt(RANK.v(), RANK.v(), CF.v(CF_CMK, 64), ALU.mult)
                    p.tt(RANK.v(), RANK.v(), CF.v(CF_OWN, 64), ALU.add)
                    p.ts(PENB.v3(8, 128, 64, 8), RANK.v().re("p (t j) -> p t j", t=8), -1.0, BIG, ALU.add, ALU.mult)

                def gate3(hh):
                    Q = QK[hh]
                    pps = PS[3]
                    for ti in range(8):
                        p.transpose(pps.vb(BF16, ti * 128, 128), PENB.v(ti * 128, 128), ident)
                    p.copy(Q.v(1024, 1024, 64, 72), pps.vb(BF16, 0, 1024, 64, 72), eng="dve")

                th.append(lambda: gate1(0))
                th.append(lambda: gate1(1))
                for tq in range(4):
                    def ev(ps, tq=tq):
                        p.copy(SG.v(tq * 512, 512), ps.v(), eng="dve")
                    th.append(half_proj(C_G + hp * 128, tq, 0, ev))
                    th.append(half_proj(C_G + hp * 128, tq, 1, ev))
                vth = []
                for tg in range(4):
                    for j in range(4):
                        def f(tg=tg, j=j):
                            ps = PS[2 + tg % 2]
                            ti = tg * 4 + j
                            for k in range(8):
                                p.matmul(ps.v(j * 128, 128), utv(k, ti * 128, 128),
                                         WB.v(k * NW + C_V + hp * 128, 128), start=(k == 0), stop=(k == 7))
                            if j == 3:
                                src = ps.v().re("p (j c) -> p j c", j=4)
                                p.copy(VA.v3(4, 128, tg * 512, 64), src[:, :, 0:64], eng="dve")
                                p.copy(VB.v3(4, 128, tg * 512 + 64, 64), src[:, :, 64:128], eng="dve")
                        vth.append(f)
                th.append(lambda: gate2(0))
                th += vth[0:4]
                th.append(lambda: gate3(0))
                th.append(lambda: gate2(1))
                th += vth[4:8]
                th.append(lambda: gate3(1))
                th += vth[8:16]
                th.append(lambda: p.act(SG.v(), SG.v(), AF.Silu))
                return th

            def attention(hp, hh, S, bg, pt_base):
                Q = S["QK"][hh]
                K = S["QK"][2 + hh]
                V = S["VA"] if hh == 0 else S["VB"]
                SG = S["SG"]
                tiles = []
                for qt in range(4):
                    order = [4 * qt + r for r in range(4)] + list(range(4 * qt))
                    for j, kt in enumerate(order):
                        tiles.append((qt, kt, j == 0, j == len(order) - 1))
                o0, d0 = (0, 64) if hh == 0 else (64, 0)

                def geom(idx):
                    qt, kt, first, last = tiles[idx]
                    tq0 = qt * 512
                    s0 = kt * 128
                    qlo = max(tq0, s0)
                    return qt, kt, tq0, s0, qlo, tq0 + 512 - qlo

                def emit_qk(idx):
                    qt, kt, tq0, s0, qlo, n = geom(idx)
                    gi = pt_base + idx
                    sp = PS[4 + gi % 4]
                    ptile = PT[gi % 4]
                    p.matmul(sp.v(0, n), K.v(s0, 128), Q.v(qlo, n))
                    p.act(ptile.v(0, n), sp.v(0, n), AF.Exp, scale=0.125)
                    if s0 >= tq0:
                        p.tt(ptile.v(0, 128), ptile.v(0, 128), tri, ALU.mult, eng="pool")

                def emit_pv(idx):
                    qt, kt, tq0, s0, qlo, n = geom(idx)
                    gi = pt_base + idx
                    first, last = tiles[idx][2], tiles[idx][3]
                    po = PS[qt % 2]
                    ptile = PT[gi % 4]
                    p.matmul(po.v(qlo - tq0, n), V.v(kt * 128, 128), ptile.v(0, n), start=first, stop=last)
                    if last:
                        r_ = R[qt % 2]
                        t1_ = T1[qt % 2]
                        p.recip(r_.v(0, 512, o0, o0 + 64), po.v(0, 512, d0, d0 + 64))
                        p.tt(t1_.v(0, 512, o0, o0 + 64), r_.v(0, 512, o0, o0 + 64), SG.v(tq0, 512, o0, o0 + 64),
                             ALU.mult, eng="dve")
                        p.tt(MIX.v(hp * T + tq0, 512, o0, o0 + 64), po.v(0, 512, o0, o0 + 64),
                             t1_.v(0, 512, o0, o0 + 64), ALU.mult)

                LA = 3
                for idx in range(len(tiles) + LA):
                    if idx < len(tiles):
                        emit_qk(idx)
                    if idx >= LA:
                        emit_pv(idx - LA)
                    if bg and idx % 2 == 1:
                        bg.pop(0)()
                return pt_base + len(tiles)

            pt_i = 0
            for f in make_thunks(0, sets[0]):
                f()
            for hp in range(4):
                bg = make_thunks(hp + 1, sets[(hp + 1) % 2]) if hp < 3 else []
                for hh in range(2):
                    pt_i = attention(hp, hh, sets[hp % 2], bg, pt_i)
                while bg:
                    bg.pop(0)()
            dbg("att", MIX.v(0, 4 * T), [128, 4 * T])

        def phase_mem(b):
            NW = 1024
            load_w("mem", wsi3, win3, C_QM, NW, 8)
            load_w("kv", wskv3, wkv3, 0, NW, 8, wb_off=8192)
            wk = Bump(WK, 0, WKSZ)
            MEMT = wk.get(BF16, 8 * 256)
            KM = wk.get(BF16, 4 * 256)
            VM = wk.get(BF16, 2 * 512)
            QM = [wk.get(BF16, T) for _ in range(2)]
            SGM = [wk.get(BF16, T) for _ in range(2)]
            PT = [wk.get(BF16, 512) for _ in range(2)]
            R = wk.get(F32, 512)
            T1 = wk.get(F32, 512)
            rms_transpose(lambda i: mem_d[b, i * 128:(i + 1) * 128, :], 2, P_MNW,
                          lambda i: MEMT.v3(8, 256, i * 128, 128), wk)
            for h in range(4):
                ps = PS[2 + h % 2]
                for k in range(8):
                    p.matmul(ps.v(0, 256), WB.v(8192 + k * NW + h * 128, 128), MEMT.v(k * 256, 256),
                             start=(k == 0), stop=(k == 7))
                p.copy(KM.v(h * 256, 256), ps.v(0, 256), eng="act")
            for mt in range(2):
                ps = PS[2 + mt % 2]
                for k in range(8):
                    p.matmul(ps.v(), MEMT.v(k * 256 + mt * 128, 128), WB.v(8192 + k * NW + 512, 512),
                             start=(k == 0), stop=(k == 7))
                p.copy(VM.v(mt * 512, 512), ps.v(), eng="dve")
            pt_i = 0
            for h in range(4):
                qm = QM[h % 2]
                sgm = SGM[h % 2]
                for tq in range(4):
                    ps = PS[2 + tq % 2]
                    proj_fm(NW, h * 128, 128, tq * 512, 512, ps.v())
                    p.copy(qm.v(tq * 512, 512), ps.v(), eng="act")
                for tq in range(4):
                    ps = PS[2 + tq % 2]
                    proj_fm(NW, 512 + h * 128, 128, tq * 512, 512, ps.v())
                    p.act(sgm.v(tq * 512, 512), ps.v(), AF.Silu)
                for qt in range(4):
                    tq0 = qt * 512
                    po = PS[qt % 2]
                    den = PS[6 + qt % 2]
                    for mt in range(2):
                        sp = PS[4 + pt_i % 2]
                        ptile = PT[pt_i % 2]
                        pt_i += 1
                        p.matmul(sp.v(), KM.v(h * 256 + mt * 128, 128), qm.v(tq0, 512))
                        p.act(ptile.v(), sp.v(), AF.Exp, scale=128 ** -0.5)
                        p.matmul(po.v(), VM.v(mt * 512 + h * 128, 128), ptile.v(), start=(mt == 0), stop=(mt == 1))
                        p.matmul(den.v(), ones, ptile.v(), start=(mt == 0), stop=(mt == 1))
                    p.recip(R.v(), den.v())
                    p.tt(T1.v(), R.v(), sgm.v(tq0, 512), ALU.mult, eng="pool")
                    p.tt(MIX.v((12 + h) * T + tq0, 512), po.v(), T1.v(), ALU.mult)
            dbg("memo", MIX.v(12 * T, 4 * T), [128, 4 * T])

        def phase_out(b):
            NW = 1024
            load_w("out", wso3, wout3, 0, NW, 16)
            wk = Bump(WK, 0, WKSZ)
            XT = [wk.get(F32, 1024) for _ in range(2)]
            HT = [wk.get(F32, 1024) for _ in range(2)]
            OT = [wk.get(F32, 1024) for _ in range(2)]
            JNK = wk.get(BF16, 1024)
            SSQ = wk.get(F32, 16)
            RSTD = wk.get(F32, 16)
            for i in range(16):
                xt = XT[i % 2]
                ht = HT[i % 2]
                ot = OT[i % 2]
                p.dma(xt.v(), D(x_d[b, i * 128:(i + 1) * 128, :]))
                for nh in range(2):
                    ps = PS[(2 * i + nh) % 4]
                    for m in range(16):
                        p.matmul(ps.v(), MIX.v(m * T + i * 128, 128), WB.v(m * NW + nh * 512, 512),
                                 start=(m == 0), stop=(m == 15))
                    p.tt(ht.v(nh * 512, 512), ps.v(), xt.v(nh * 512, 512), ALU.add)
                p.act(JNK.v(), ht.v(), AF.Square, accum_out=SSQ.v(i, 1))
                p.act(RSTD.v(i, 1), SSQ.v(i, 1), AF.Ln, scale=1.0 / DM, bias=epsv)
                p.act(RSTD.v(i, 1), RSTD.v(i, 1), AF.Exp, scale=-0.5)
                p.stt(ot.v(), ht.v(), RSTD.v(i, 1), prm(P_FNW, 1024), ALU.mult, ALU.mult)
                p.dma(D(out_d[b, i * 128:(i + 1) * 128, :]), ot.v())

        for b in range(nseq):
            wkA = Bump(WK, 16384, WKSZ - 16384)
            rms_transpose(lambda i: x_d[b, i * 128:(i + 1) * 128, :], 16, P_NW,
                          lambda i: UT.v3(BF16, 8, T, i * 128, 128), wkA)
            if b == 0:
                dbg("ut", UT.v(), [128, 8 * T])
            if "S" in phases:
                phase_ssd(b)
            if "B" in phases:
                phase_attn(b)
            if "M" in phases:
                phase_mem(b)
            if "E" in phases:
                phase_out(b)
        print("ops:", p.nops, {e: len(v) for e, v in p.ops.items()})
        p.emit()
    return nc, dumps


def kernel(x, mem, norm_w, w_in, conv_w, conv_b, dt_bias, a_log, d_skip, ssd_norm_w, mem_norm_w, w_mem_kv,
           w_out, final_norm_w):
    f = lambda a: np.ascontiguousarray(np.asarray(a, dtype=np.float32))
    x, mem = f(x), f(mem)
    cb, cf, kc, qc = host_consts()
    cf[:, CF_PRM:CF_PRM + NPRM] = host_params(f(norm_w)[0], f(conv_w)[0], f(conv_b)[0], f(dt_bias)[0], f(a_log)[0],
                                              f(d_skip)[0], f(ssd_norm_w)[0], f(mem_norm_w)[0], f(final_norm_w))
    nc, _ = build()
    shared = {"w_in": f(w_in)[0], "w_kv": f(w_mem_kv)[0], "w_out": f(w_out)[0], "cstb": cb, "cstf": cf,
              "kconst": kc, "qconst": qc}
    in_maps = []
    for c in range(8):
        m = dict(shared)
        m["x"] = x[c * NSEQ:(c + 1) * NSEQ]
        m["mem"] = mem[c * NSEQ:(c + 1) * NSEQ]
        in_maps.append(m)
    res = run_bass_kernel_spmd(nc, in_maps, core_ids=list(range(8)))
    return np.concatenate([r["out"] for r in res.results], axis=0)
```

```python
from contextlib import ExitStack
import concourse.bass as bass
import concourse.mybir as mybir

F32 = mybir.dt.float32
BF16 = mybir.dt.bfloat16
I32 = mybir.dt.int32
AF = mybir.ActivationFunctionType
ALU = mybir.AluOpType
AX = mybir.AxisListType
ESZ = {F32: 4, BF16: 2, I32: 4}


class View:
    __slots__ = ("ap", "arena", "rngs", "pr")

    def __init__(self, ap, arena, rngs, pr=(0, 128)):
        self.ap, self.arena, self.rngs, self.pr = ap, arena, rngs, pr

    def re(self, s, **kw):
        return View(self.ap.rearrange(s, **kw), self.arena, self.rngs, self.pr)

    def bc(self, shape):
        return View(self.ap.broadcast_to(shape), self.arena, self.rngs, self.pr)

    def __getitem__(self, key):
        return View(self.ap[key], self.arena, self.rngs, self.pr)


class Arena:
    def __init__(self, name, t, dtype, ncols, const=False):
        self.name, self.t, self.dtype, self.ncols = name, t, dtype, ncols
        self.esz = ESZ[dtype]
        self.w = []
        self.r = []
        self.const = const
        self.alt = {dtype: t}
        self.psum = False
        self.qlast = [dict() for _ in range(4)]

    def v(self, lo=0, n=None, p0=0, p1=128):
        if n is None:
            n = self.ncols - lo
        assert 0 <= lo and lo + n <= self.ncols, (self.name, lo, n, self.ncols)
        return View(self.t[p0:p1, lo:lo + n], self, [(lo * self.esz, (lo + n) * self.esz)], (p0, p1))

    def v3(self, dtype, nk, stride, lo, n, p0=0, p1=128):
        if dtype not in self.alt:
            self.alt[dtype] = self.t.bitcast(dtype)
        e = ESZ[dtype]
        assert ((nk - 1) * stride + lo + n) * e <= self.ncols * self.esz
        total = self.ncols * self.esz // e
        o = max(0, lo + nk * stride - total)
        assert o <= stride - n and o <= lo, (self.name, lo, nk, stride, n, total)
        ws = lo - o
        ap = self.alt[dtype][p0:p1, ws:ws + nk * stride].rearrange("p (k s) -> p k s", s=stride)[:, :, o:o + n]
        return View(ap, self, [((k * stride + lo) * e, (k * stride + lo + n) * e) for k in range(nk)], (p0, p1))

    def vb(self, dtype, lo, n, p0=0, p1=128):
        if dtype not in self.alt:
            self.alt[dtype] = self.t.bitcast(dtype)
        e = ESZ[dtype]
        assert (lo + n) * e <= self.ncols * self.esz
        return View(self.alt[dtype][p0:p1, lo:lo + n], self, [(lo * e, (lo + n) * e)], (p0, p1))


class Sub:
    def __init__(self, arena, dtype, boff, n):
        self.a, self.dt, self.e, self.n = arena, dtype, ESZ[dtype], n
        assert boff % self.e == 0
        self.base = boff // self.e

    def v(self, lo=0, n=None, p0=0, p1=128):
        if n is None:
            n = self.n - lo
        assert 0 <= lo and lo + n <= self.n, (lo, n, self.n)
        return self.a.vb(self.dt, self.base + lo, n, p0, p1)

    def v3(self, nk, stride, lo, n, p0=0, p1=128):
        assert (nk - 1) * stride + lo + n <= self.n
        return self.a.v3(self.dt, nk, stride, self.base + lo, n, p0, p1)


class Bump:
    def __init__(self, arena, boff, size):
        self.a, self.off, self.end = arena, boff, boff + size

    def get(self, dtype, n):
        self.off = (self.off + 3) // 4 * 4
        s = Sub(self.a, dtype, self.off, n)
        self.off += n * ESZ[dtype]
        assert self.off <= self.end, ("bump overflow", self.off, self.end)
        return s


def D(ap):
    return View(ap, None, [])


COMPUTE = ("pe", "act", "dve", "pool")


class Prog:
    def __init__(self, nc, ndma=40):
        self.nc = nc
        self.ops = {e: [] for e in COMPUTE + ("sp",)}
        self.count = {e: 0 for e in COMPUTE}
        self.waited = {e: {} for e in COMPUTE + ("sp",)}
        self.ndma = ndma
        self.dma_cnt = [0] * ndma
        self.dma_next = 0
        self.npool = 0
        self.nops = 0

    def op(self, eng, fn, reads=(), writes=(), dma=False):
        deps = {}

        def add(tok):
            k, v = tok
            if deps.get(k, 0) < v:
                deps[k] = v

        for vw in reads:
            a = vw.arena
            if a is None:
                continue
            for (lo, hi) in vw.rngs:
                for (l, h, t) in a.w:
                    if l < hi and lo < h:
                        add(t)
        for vw in writes:
            a = vw.arena
            if a is None:
                continue
            assert not a.const, a.name
            for (lo, hi) in vw.rngs:
                for (l, h, t) in a.w:
                    if l < hi and lo < h:
                        add(t)
                for (l, h, t) in a.r:
                    if l < hi and lo < h:
                        add(t)
        for vw in list(reads) + list(writes):
            a = vw.arena
            if a is None or not a.psum:
                continue
            for q in range(vw.pr[0] // 32, (vw.pr[1] + 31) // 32):
                for e2, t in a.qlast[q].items():
                    if e2 != eng:
                        add(t)
        if dma and eng == "pool":
            key = ("p", self.npool)
            self.npool += 1
            tok = (key, 16)
            inc = (key, 16)
        elif dma:
            i = self.dma_next
            self.dma_next = (i + 1) % self.ndma
            if self.dma_cnt[i] > 0:
                add((("d", i), self.dma_cnt[i]))
            self.dma_cnt[i] += 16
            tok = (("d", i), self.dma_cnt[i])
            inc = (("d", i), 16)
        else:
            self.count[eng] += 1
            tok = (eng, self.count[eng])
            inc = (eng, 1)
        waits = []
        wd = self.waited[eng]
        for k, v in deps.items():
            if k == eng and eng == "pe":
                continue
            if wd.get(k, 0) >= v:
                continue
            wd[k] = v
            waits.append((k, v))
        self.ops[eng].append((waits, fn, inc))
        self.nops += 1
        for vw in list(reads) + list(writes):
            a = vw.arena
            if a is None or not a.psum:
                continue
            for q in range(vw.pr[0] // 32, (vw.pr[1] + 31) // 32):
                a.qlast[q][eng] = tok
        for vw in reads:
            a = vw.arena
            if a is None or a.const:
                continue
            for (lo, hi) in vw.rngs:
                if not dma:
                    a.r = [x for x in a.r if not (x[2][0] == eng and lo <= x[0] and x[1] <= hi)]
                a.r.append((lo, hi, tok))
        for vw in writes:
            a = vw.arena
            if a is None:
                continue
            for (lo, hi) in vw.rngs:
                a.w = [x for x in a.w if not (lo <= x[0] and x[1] <= hi)]
                a.r = [x for x in a.r if not (lo <= x[0] and x[1] <= hi)]
                a.w.append((lo, hi, tok))
        return tok

    def freeze(self, arena):
        arena.const = True
        arena.r = []

    def matmul(self, out, lhsT, rhs, start=True, stop=True, **kw):
        return self.op("pe", lambda e: e.matmul(out.ap, lhsT.ap, rhs.ap, start=start, stop=stop, **kw),
                       [lhsT, rhs] + ([] if start else [out]), [out])

    def transpose(self, out, in_, ident):
        return self.op("pe", lambda e: e.transpose(out.ap, in_.ap, ident.ap), [in_, ident], [out])

    def act(self, out, in_, func, bias=None, scale=None, accum_out=None):
        rd = [in_]
        kw = {}
        if bias is not None:
            if isinstance(bias, View):
                rd.append(bias)
                kw["bias"] = bias.ap
            else:
                kw["bias"] = bias
        if scale is not None:
            if isinstance(scale, View):
                rd.append(scale)
                kw["scale"] = scale.ap
            else:
                kw["scale"] = scale
        wr = [out]
        if accum_out is not None:
            wr.append(accum_out)
            kw["accum_out"] = accum_out.ap
        return self.op("act", lambda e: e.activation(out.ap, in_.ap, func, **kw), rd, wr)

    def tt(self, out, in0, in1, op, eng="dve"):
        return self.op(eng, lambda e: e.tensor_tensor(out.ap, in0.ap, in1.ap, op), [in0, in1], [out])

    def ts(self, out, in0, s1, s2, op0, op1=None, eng="dve", accum_out=None):
        rd = [in0]
        a1 = s1
        a2 = s2
        if isinstance(s1, View):
            rd.append(s1)
            a1 = s1.ap
        if isinstance(s2, View):
            rd.append(s2)
            a2 = s2.ap
        kw = {}
        if op1 is not None:
            kw["op1"] = op1
        wr = [out]
        if accum_out is not None:
            kw["accum_out"] = accum_out.ap
            wr.append(accum_out)
        return self.op(eng, lambda e: e.tensor_scalar(out.ap, in0.ap, a1, a2, op0, **kw), rd, wr)

    def stt(self, out, in0, scalar, in1, op0, op1):
        rd = [in0, in1]
        sc = scalar
        if isinstance(scalar, View):
            rd.append(scalar)
            sc = scalar.ap
        return self.op("dve", lambda e: e.scalar_tensor_tensor(out.ap, in0.ap, sc, in1.ap, op0, op1), rd, [out])

    def copy(self, out, in_, eng="dve"):
        if eng == "act":
            return self.op("act", lambda e: e.copy(out.ap, in_.ap), [in_], [out])
        return self.op(eng, lambda e: e.tensor_copy(out.ap, in_.ap), [in_], [out])

    def reduce(self, out, in_, op, axis=AX.X, eng="dve"):
        return self.op(eng, lambda e: e.tensor_reduce(out.ap, in_.ap, axis, op), [in_], [out])

    def recip(self, out, in_):
        return self.op("dve", lambda e: e.reciprocal(out.ap, in_.ap), [in_], [out])

    def memset(self, out, val, eng="dve"):
        return self.op(eng, lambda e: e.memset(out.ap, val), [], [out])

    def scan(self, out, d0, d1, initial, op0, op1):
        rd = [d0, d1]
        ini = initial
        if isinstance(initial, View):
            rd.append(initial)
            ini = initial.ap
        return self.op("dve", lambda e: e.tensor_tensor_scan(out.ap, d0.ap, d1.ap, ini, op0, op1), rd, [out])

    def affine_select(self, out, in_, pattern, cmp, fill, base, cm):
        return self.op("pool", lambda e: e.affine_select(out.ap, in_.ap, pattern, cmp, fill, base=base,
                                                         channel_multiplier=cm), [in_], [out])

    def iota(self, out, pattern, base, cm):
        return self.op("pool", lambda e: e.iota(out.ap, pattern, base=base, channel_multiplier=cm), [], [out])

    def dma(self, out, in_, q="sp", **kw):
        if q == "pool":
            kw.setdefault("max_dma_last_dim", 4096)
        return self.op(q, lambda e: e.dma_start(out=out.ap, in_=in_.ap, **kw), [in_], [out], dma=True)

    def emit(self):
        nc = self.nc
        with ExitStack() as es:
            sems = {}
            for e in COMPUTE:
                sems[e] = es.enter_context(nc.semaphore("s_" + e))
            for i in range(self.ndma):
                sems[("d", i)] = es.enter_context(nc.semaphore("s_d%d" % i))
            for i in range(self.npool):
                sems[("p", i)] = es.enter_context(nc.semaphore("s_p%d" % i))
            fin = []
            for i in range(self.ndma):
                if self.dma_cnt[i] > 0:
                    fin.append((("d", i), self.dma_cnt[i]))
            for i in range(self.npool):
                fin.append((("p", i), 16))
            for e in COMPUTE:
                if self.count[e] > 0:
                    fin.append((e, self.count[e]))
            block = es.enter_context(nc.Block())

            def run(stream, final=None):
                def f(eng):
                    for waits, fn, inc in stream:
                        for k, v in waits:
                            eng.wait_ge(sems[k], v)
                        ins = fn(eng)
                        ins.then_inc(sems[inc[0]], inc[1])
                    if final:
                        for k, v in final:
                            eng.wait_ge(sems[k], v)
                return f

            block.tensor(run(self.ops["pe"]))
            block.scalar(run(self.ops["act"]))
            block.vector(run(self.ops["dve"]))
            block.gpsimd(run(self.ops["pool"]))
            block.sync(run(self.ops["sp"], fin))
from concourse.bass_utils import run_bass_kernel_spmd
import numpy as np

T = 2048
DM = 1024
NSEQ = 2
EPS = 1e-6
BIG = 30000.0
C_Q, C_K, C_V, C_G, C_Z, C_XBC, C_DT, C_QM, C_GM = 0, 512, 1024, 1536, 2048, 3072, 5120, 5136, 5648
CB_ID, CB_TRI, CB_MNEG, CB_ONES, CB_EH, NCB = 0, 128, 256, 384, 512, 2560
CF_ID, CF_ONES, CF_PRM, CF_CAP, CF_CMK, CF_OWN, CF_MISC, NCF = 0, 128, 256, 1424, 1488, 1552, 1616, 1624
P_NW, P_MNW, P_SNW, P_CVB, P_CVW, P_DSK, P_DTB, P_ALOG, P_FNW = 0, 8, 16, 24, 40, 104, 112, 128, 144
NPRM = 1168


def host_consts():
    cb = np.zeros((128, NCB), np.float32)
    cb[:, CB_ID:CB_ID + 128] = np.eye(128)
    s = np.arange(128)[:, None]
    t = np.arange(128)[None, :]
    cb[:, CB_TRI:CB_TRI + 128] = (t >= s)
    cb[:, CB_MNEG:CB_MNEG + 128] = np.where(t < s, -BIG, 0.0)
    cb[:, CB_ONES:CB_ONES + 128] = 1.0
    for h in range(16):
        cb[h, CB_EH + h * 128: CB_EH + (h + 1) * 128] = 1.0
    cf = np.zeros((128, NCF), np.float32)
    cf[:, CF_ID:CF_ID + 128] = np.eye(128)
    cf[:, CF_ONES:CF_ONES + 128] = 1.0
    cap = np.zeros((8, 8), np.float32)
    cmk = np.zeros((8, 8), np.float32)
    own = np.zeros((8, 8), np.float32)
    for ti in range(8):
        qb = 4 + ti // 2
        for j in range(8):
            cap[ti, j] = 1e30 if j < qb else -1e30
            cmk[ti, j] = 1.0 if j < qb else 0.0
            own[ti, j] = 1.0 if j == qb else 0.0
    cf[:, CF_CAP:CF_CAP + 64] = cap.reshape(-1)[None, :]
    cf[:, CF_CMK:CF_CMK + 64] = cmk.reshape(-1)[None, :]
    cf[:, CF_OWN:CF_OWN + 64] = own.reshape(-1)[None, :]
    cf[:, CF_MISC] = EPS
    cf[:, CF_MISC + 1] = 1.0
    kc = np.zeros((128, T), np.float32)
    pos = np.arange(T)
    for r in range(8):
        kc[64 + r] = (pos // 256 == r)
    kc[96] = pos // 16
    kc[97] = pos % 16
    kc[98] = 1.0
    kc[99] = 1.0
    qc = np.zeros((8, 4, T), np.float32)
    for h in range(8):
        sl = 2.0 ** (-8.0 * (h + 1) / 8)
        qc[h, 0] = 16 * sl * 8
        qc[h, 1] = sl * 8
        qc[h, 2] = -16 * sl * 8 * (pos // 16)
        qc[h, 3] = -sl * 8 * (pos % 16)
    return cb, cf, kc, qc


def host_params(norm_w, conv_w, conv_b, dt_bias, a_log, d_skip, ssd_norm_w, mem_norm_w, final_norm_w):
    prm = np.zeros((128, NPRM), np.float32)
    prm[:, P_NW:P_NW + 8] = norm_w.reshape(8, 128).T
    prm[:, P_MNW:P_MNW + 8] = mem_norm_w.reshape(8, 128).T
    prm[:, P_SNW:P_SNW + 8] = ssd_norm_w.reshape(8, 128).T
    prm[:, P_CVB:P_CVB + 16] = conv_b.reshape(16, 128).T
    prm[:, P_CVW:P_CVW + 64] = conv_w.T.reshape(16, 128, 4).transpose(1, 0, 2).reshape(128, 64)
    prm[:, P_DSK:P_DSK + 8] = np.repeat(d_skip, 64).reshape(8, 128).T
    prm[:, P_DTB:P_DTB + 16] = dt_bias[None, :]
    prm[:, P_ALOG:P_ALOG + 16] = a_log[None, :]
    prm[:, P_FNW:P_FNW + 1024] = final_norm_w[None, :]
    return prm
def build(nseq=NSEQ, dump=None, phases="ASBME"):
    nc = bass.Bass("TRN2", target_bir_lowering=False)
    x_d = nc.dram_tensor("x", [nseq, T, DM], F32, kind="ExternalInput").ap()
    mem_d = nc.dram_tensor("mem", [nseq, 256, DM], F32, kind="ExternalInput").ap()
    win_d = nc.dram_tensor("w_in", [DM, 6160], F32, kind="ExternalInput").ap()
    wkv_d = nc.dram_tensor("w_kv", [DM, 1024], F32, kind="ExternalInput").ap()
    wout_d = nc.dram_tensor("w_out", [2048, DM], F32, kind="ExternalInput").ap()
    cb_d = nc.dram_tensor("cstb", [128, NCB], F32, kind="ExternalInput").ap()
    cf_d = nc.dram_tensor("cstf", [128, NCF], F32, kind="ExternalInput").ap()
    kc_d = nc.dram_tensor("kconst", [128, T], F32, kind="ExternalInput").ap()
    qc_d = nc.dram_tensor("qconst", [8, 4, T], F32, kind="ExternalInput").ap()
    out_d = nc.dram_tensor("out", [nseq, T, DM], F32, kind="ExternalOutput").ap()
    dumps = {}
    wsi_d = nc.dram_tensor("wsc_in", [DM, 6160], BF16, kind="Internal").ap()
    wskv_d = nc.dram_tensor("wsc_kv", [DM, 1024], BF16, kind="Internal").ap()
    wso_d = nc.dram_tensor("wsc_out", [2048, DM], BF16, kind="Internal").ap()
    kcs_d = nc.dram_tensor("kc_sc", [128, T], BF16, kind="Internal").ap()
    qcs_d = nc.dram_tensor("qc_sc", [32, T], BF16, kind="Internal").ap()
    qc2_d = qc_d.rearrange("h r t -> (h r) t")
    wsi3 = wsi_d.rearrange("(k p) c -> p k c", p=128)
    wskv3 = wskv_d.rearrange("(k p) c -> p k c", p=128)
    wso3 = wso_d.rearrange("(k p) c -> p k c", p=128)
    win3 = win_d.rearrange("(k p) c -> p k c", p=128)
    wkv3 = wkv_d.rearrange("(k p) c -> p k c", p=128)
    wout3 = wout_d.rearrange("(k p) c -> p k c", p=128)

    with ExitStack() as es:
        def sb(name, n, dt):
            t = es.enter_context(nc.sbuf_tensor(name, [128, n], dt))
            return Arena(name, t, dt, n)

        UT = sb("UT", 8 * T, BF16)
        MIX = sb("MIX", 16 * T, BF16)
        WB = sb("WB", 8 * 3088, BF16)
        WKSZ = 47616
        WK = sb("WK", WKSZ // 2, BF16)
        CB = sb("CB", NCB, BF16)
        CF = sb("CF", NCF, F32)
        SM = sb("SM", 64, F32)
        PS = []
        for i in range(8):
            t = es.enter_context(nc.psum_tensor("ps%d" % i, [128, 512], F32))
            PS.append(Arena("ps%d" % i, t, F32, 512))
            PS[-1].psum = True
        p = Prog(nc)

        def dbg(name, view, shape):
            if dump is None or name not in dump:
                return
            d = nc.dram_tensor("dbg_" + name, list(shape), view.ap.dtype, kind="ExternalOutput").ap()
            dumps[name] = d
            p.dma(D(d), view)

        p.dma(CB.v(), D(cb_d), q="pool")
        p.dma(CF.v(), D(cf_d))
        ident = CB.v(CB_ID, 128)
        tri = CB.v(CB_TRI, 128)
        mneg = CB.v(CB_MNEG, 128)
        ones = CB.v(CB_ONES, 128)
        identf = CF.v(CF_ID, 128)
        epsv = CF.v(CF_MISC, 1)
        onev = CF.v(CF_MISC + 1, 1)

        def prm(off, n, p0=0, p1=128):
            return CF.v(CF_PRM + off, n, p0, p1)

        AB = SM.v(0, 16)
        p.act(AB, prm(P_ALOG, 16), AF.Exp)
        p.ts(AB, AB, -1.0, None, ALU.mult)

        SC = {n: Arena("sc_" + n, None, BF16, 1) for n in ("attn", "mem", "ssd", "kv", "out", "kc", "qc")}
        converted = [False]

        def scv(ap, name, k):
            return View(ap, SC[name], [(k, k + 1)])

        NHALF = {"attn": 4, "mem": 4, "ssd": 4, "kv": 4, "out": 8}

        def convert_all():
            p.dma(scv(kcs_d, "kc", 0), D(kc_d), q="pool")
            p.dma(scv(qcs_d, "qc", 0), D(qc2_d), q="pool")
            for name, src, dst, c0, nw, nrows in (("attn", win_d, wsi_d, 0, 2048, 1024), ("kv", wkv_d, wskv_d, 0, 1024, 1024),
                                                  ("mem", win_d, wsi_d, C_QM, 1024, 1024), ("out", wout_d, wso_d, 0, 1024, 2048),
                                                  ("ssd", win_d, wsi_d, C_Z, 3088, 1024)):
                hr = nrows // 2
                for h in range(2):
                    p.dma(scv(dst[h * hr:(h + 1) * hr, c0:c0 + nw], name, h), D(src[h * hr:(h + 1) * hr, c0:c0 + nw]),
                          q="pool")
            converted[0] = True

        def load_w(name, dst3, src3, c0, nw, nk, wb_off=0):
            for k in range(nk):
                if converted[0]:
                    p.dma(WB.v(wb_off + k * nw, nw), scv(dst3[:, k, c0:c0 + nw], name, k // NHALF[name]), q="sp")
                else:
                    p.dma(WB.v(wb_off + k * nw, nw), D(src3[:, k, c0:c0 + nw]), q="pool")

        pj_i = [0]

        def rms_transpose(src_tile, ntiles, nwoff, dst_fn, wk, nbuf=2):
            XT = [wk.get(F32, 1024) for _ in range(nbuf)]
            XS = [wk.get(BF16, 1024) for _ in range(nbuf)]
            JNK = wk.get(BF16, 1024)
            SSQ = wk.get(F32, 16)
            RSTD = wk.get(F32, 16)
            for i in range(ntiles):
                xt = XT[i % nbuf]
                p.dma(xt.v(), D(src_tile(i)))
                p.act(JNK.v(), xt.v(), AF.Square, accum_out=SSQ.v(i, 1))
                p.act(RSTD.v(i, 1), SSQ.v(i, 1), AF.Ln, scale=1.0 / DM, bias=epsv)
                p.act(RSTD.v(i, 1), RSTD.v(i, 1), AF.Exp, scale=-0.5)
                xs = XS[i % nbuf]
                p.ts(xs.v(), xt.v(), RSTD.v(i, 1), None, ALU.mult)
                pb = PS[i % 2]
                for k in range(8):
                    p.transpose(pb.vb(BF16, k * 128, 128), xs.v(k * 128, 128), ident)
                p.tt(dst_fn(i), pb.vb(BF16, 0, 1024).re("p (k t) -> p k t", k=8),
                     prm(nwoff, 8).re("p (k o) -> p k o", o=1).bc([128, 8, 128]), ALU.mult)

        def utv(k, t0, n):
            return UT.v(k * T + t0, n)

        def proj_fm(nw, c0, ncols, t0, n, ps_view):
            for k in range(8):
                p.matmul(ps_view, WB.v(k * nw + c0, ncols), utv(k, t0, n), start=(k == 0), stop=(k == 7))

        def phase_ssd(b):
            NW = 3088
            load_w("ssd", wsi3, win3, C_Z, NW, 8)
            if not converted[0]:
                convert_all()
            b1 = Bump(MIX, 0, 16384)
            b2 = Bump(MIX, 12 * T * 2, 16384)
            wk = Bump(WK, 0, WKSZ)
            XPRE = b1.get(BF16, 16 * 259)
            XST = b1.get(BF16, 8 * 256)
            BMT = b1.get(BF16, 4 * 256)
            LT = [b1.get(BF16, 384) for _ in range(2)]
            SZ = b2.get(BF16, 8 * 256)
            PREV = b2.get(F32, 1024)
            XDD = b2.get(BF16, 2 * 1024)
            CMT = b2.get(BF16, 4 * 256)
            BTOK = b2.get(BF16, 2 * 512)
            DIAG = wk.get(BF16, 64 * 128)
            CBT = wk.get(BF16, 4 * 384)
            L0 = [wk.get(BF16, 256) for _ in range(2)]
            MT = [wk.get(BF16, 384) for _ in range(2)]
            GT = [wk.get(BF16, 256) for _ in range(2)]
            GG = [wk.get(F32, 256) for _ in range(4)]
            GSQ = [wk.get(BF16, 256) for _ in range(4)]
            RS = wk.get(F32, 256)
            XDT = wk.get(BF16, 2 * 2048)
            PREVB = wk.get(BF16, 2048)
            DTR = wk.get(F32, 32)
            DT = wk.get(F32, 32)
            DA = wk.get(F32, 256)
            NCS = wk.get(F32, 32)
            DEC = wk.get(F32, 32)
            W2 = wk.get(F32, 32)
            CST = wk.get(F32, 256)
            CSH = wk.get(BF16, 256)
            CSL = wk.get(BF16, 256)
            DECT = wk.get(F32, 256)
            DG16 = wk.get(F32, 16)
            CDB = wk.get(F32, 16)

            for zt in (XDT, PREVB, DA, CST, CSH, CSL, DECT, DG16):
                p.memset(zt.v(), 0.0, eng="pool")
            for j in range(64):
                p.ts(DIAG.v(j * 128, 128), ident, prm(P_CVW + j, 1), None, ALU.mult,
                     eng=("pool" if j % 2 else "dve"))
            p.memset(XPRE.v3(16, 259, 0, 3), 0.0)
            p.memset(PREV.v(), 0.0)

            pending = []
            for c in range(8):
                t0 = c * 256

                def zgroup(zc):
                    ps = PS[zc % 2].v((zc // 2 % 2) * 256, 256)
                    proj_fm(NW, zc * 128, 128, t0, 256, ps)
                    p.copy(SZ.v(zc * 256, 256), ps, eng="dve")

                def xgroup(cc):
                    ps = PS[cc % 2].v((cc // 2 % 2) * 256, 256)
                    proj_fm(NW, 1024 + cc * 128, 128, t0, 256, ps)
                    p.copy(XPRE.v(cc * 259 + 3, 256), ps, eng=("dve" if cc % 2 else "act"))

                if c > 0:
                    p.copy(XPRE.v3(16, 259, 0, 3), XPRE.v3(16, 259, 256, 3), eng="pool")
                for lt in range(2):
                    for k in range(8):
                        p.matmul(PS[5].v(384 + lt * 16, 16), utv(k, t0 + lt * 128, 128), WB.v(k * NW + 3072, 16),
                                 start=(k == 0), stop=(k == 7))
                p.tt(DTR.v(), PS[5].v(384, 32).re("p (l h) -> p l h", l=2),
                     prm(P_DTB, 16).re("p (o h) -> p o h", o=1).bc([128, 2, 16]), ALU.add)
                p.act(DTR.v(), DTR.v(), AF.Exp)
                p.act(DT.v(), DTR.v(), AF.Ln, bias=onev)
                p.tt(DA.v3(2, 128, 0, 16), DT.v().re("p (l h) -> p l h", l=2),
                     SM.v(0, 16).re("p (o h) -> p o h", o=1).bc([128, 2, 16]), ALU.mult)
                for zc in range(6):
                    zgroup(zc)
                    if zc == 1 and pending:
                        pending.pop(0)()
                for lt in range(2):
                    p.transpose(PS[6].v(lt * 128, 128), DA.v(lt * 128, 128), identf)
                p.scan(CST.v(0, 256, 0, 16), CF.v(CF_ONES, 1, 0, 16).bc([16, 256]), PS[6].v(0, 256, 0, 16), 0.0,
                       ALU.mult, ALU.add)
                p.copy(CSH.v(0, 256, 0, 16), CST.v(0, 256, 0, 16))
                p.tt(CSL.v(0, 256, 0, 16), CST.v(0, 256, 0, 16), CSH.v(0, 256, 0, 16), ALU.subtract)
                p.act(DECT.v(0, 256, 0, 16), CST.v(0, 256, 0, 16), AF.Exp, scale=-1.0, bias=CST.v(255, 1, 0, 16))
                for zc in range(6, 8):
                    zgroup(zc)
                for cc in range(8, 12):
                    xgroup(cc)
                for lt in range(2):
                    p.transpose(PS[7].v(lt * 128, 128), CST.v(lt * 128, 128), identf)
                    p.transpose(PS[7].v(256 + lt * 128, 128), DECT.v(lt * 128, 128), identf)
                p.ts(NCS.v().re("p (l h) -> p l h", l=2), PS[7].v3(F32, 2, 128, 0, 16), -1.0, None, ALU.mult)
                p.copy(DEC.v().re("p (l h) -> p l h", l=2), PS[7].v3(F32, 2, 128, 256, 16))
                p.tt(W2.v(), DT.v(), DEC.v(), ALU.mult)
                if c < 7:
                    p.ts(DG16.v(0, 16, 0, 16), CF.v(CF_ID, 16, 0, 16), CST.v(255, 1, 0, 16), None, ALU.mult)
                    p.matmul(PS[5].v(480, 16), CF.v(CF_ONES, 128), DG16.v())
                    p.act(CDB.v(), PS[5].v(480, 16), AF.Exp)
                for cc in list(range(12, 16)) + list(range(0, 8)):
                    xgroup(cc)

                def conv(cc):
                    ps = PS[cc % 2].v((cc // 2 % 2) * 256, 256)
                    for k in range(4):
                        p.matmul(ps, DIAG.v((cc * 4 + k) * 128, 128), XPRE.v(cc * 259 + k, 256),
                                 start=(k == 0), stop=(k == 3))
                    if cc < 8:
                        dst = XST.v(cc * 256, 256)
                    elif cc < 12:
                        dst = BMT.v((cc - 8) * 256, 256)
                    else:
                        dst = CMT.v((cc - 12) * 256, 256)
                    p.act(dst, ps, AF.Silu, bias=prm(P_CVB + cc, 1))

                for cc in range(0, 8):
                    conv(cc)
                for lt in range(2):
                    pb = PS[6] if lt == 0 else PS[4]
                    for cc in range(8):
                        p.transpose(pb.vb(BF16, cc * 128, 128), XST.v(cc * 256 + lt * 128, 128), ident)
                    src = pb.vb(BF16, 0, 1024).re("p (h q) -> p h q", h=16)
                    for hh in range(2):
                        p.tt(XDT.v(lt * 2048, 2048).re("p (c e q) -> p c e q", c=8, e=2)[:, :, hh, hh * 64:hh * 64 + 64],
                             src.re("p (c e) q -> p c e q", e=2)[:, :, hh, :],
                             DT.v(lt * 16, 16).re("p (c e o) -> p c e o", e=2, o=1)[:, :, hh, :].bc([128, 8, 64]),
                             ALU.mult)
                    p.tt(XDD.v(lt * 1024, 1024).re("p (h q) -> p h q", h=16), src,
                         W2.v(lt * 16, 16).re("p (h o) -> p h o", o=1).bc([128, 16, 64]), ALU.mult)
                for cc in range(8, 16):
                    conv(cc)
                p.act(SZ.v(), SZ.v(), AF.Silu)
                for lt in range(2):
                    pb = PS[7]
                    for g in range(4):
                        p.transpose(pb.vb(BF16, g * 128, 128), BMT.v(g * 256 + lt * 128, 128), ident)
                    p.copy(BTOK.v(lt * 512, 512), pb.vb(BF16, 0, 512), eng="dve")
                for g in range(4):
                    ps = PS[5] if g % 2 == 0 else PS[6]
                    p.matmul(ps.v(0, 256), BMT.v(g * 256, 128), CMT.v(g * 256, 256))
                    p.matmul(ps.v(256, 128), BMT.v(g * 256 + 128, 128), CMT.v(g * 256 + 128, 128))
                    p.copy(CBT.v(g * 384, 384), ps.v(0, 384), eng="dve")
                if c < 7:
                    for g in range(4):
                        st = PS[g // 2].v((g % 2) * 256, 256)
                        for lt in range(2):
                            p.matmul(st, BTOK.v(lt * 512 + g * 128, 128), XDD.v(lt * 1024 + g * 256, 256),
                                     start=(lt == 0), stop=(lt == 1))
                    for g in range(4):
                        st = PS[g // 2].v((g % 2) * 256, 256)
                        pv = PREV.v(g * 256, 256)
                        p.tt(pv.re("p (h q) -> p h q", h=4), pv.re("p (h q) -> p h q", h=4),
                             CDB.v(g * 4, 4).re("p (h o) -> p h o", o=1).bc([128, 4, 64]), ALU.mult)
                        p.tt(pv, pv, st, ALU.add)
                def seg_stage(h):
                    g = h // 4
                    sg = PS[2 + h % 2]
                    eh = CB.v(CB_EH + h * 128, 128)
                    csh = lambda lo, n: CSH.v(lo, n)
                    csl = lambda lo, n: CSL.v(lo, n)
                    p.matmul(sg.v(0, 256), eh, csh(0, 256), start=True, stop=False)
                    p.matmul(sg.v(0, 256), eh, csl(0, 256), start=False, stop=True)
                    p.matmul(sg.v(256, 128), eh, csh(0, 128), start=True, stop=False)
                    p.matmul(sg.v(256, 128), eh, csl(0, 128), start=False, stop=False)
                    p.matmul(sg.v(256, 128), ident, mneg, start=False, stop=True)
                    p.matmul(sg.v(384, 128), eh, csh(128, 128), start=True, stop=False)
                    p.matmul(sg.v(384, 128), eh, csl(128, 128), start=False, stop=False)
                    p.matmul(sg.v(384, 128), ident, mneg, start=False, stop=True)
                    l0 = L0[h % 2]
                    lt_ = LT[h % 2]
                    mt = MT[h % 2]
                    gt = GT[h % 2]
                    if c > 0:
                        p.act(l0.v(), sg.v(0, 256), AF.Exp)
                    p.act(lt_.v(0, 128), sg.v(256, 128), AF.Exp, bias=NCS.v(h, 1))
                    p.act(lt_.v(128, 128), sg.v(128, 128), AF.Exp, bias=NCS.v(h, 1))
                    p.act(lt_.v(256, 128), sg.v(384, 128), AF.Exp, bias=NCS.v(16 + h, 1))
                    p.tt(mt.v(), lt_.v(), CBT.v(g * 384, 384), ALU.mult)
                    if c > 0:
                        p.tt(gt.v(), l0.v(), CMT.v(g * 256, 256), ALU.mult, eng="pool")

                def y_stage(h):
                    g = h // 4
                    pr = h % 2
                    pc = h // 2
                    mt = MT[h % 2]
                    gt = GT[h % 2]
                    ybank = PS[4 + 2 * (pc % 2)]
                    yt = ybank.v(0, 256)
                    yt2 = ybank.v(128, 128)
                    p.matmul(yt, XDT.v(h * 128, 128), mt.v(0, 256), start=(pr == 0), stop=False)
                    p.matmul(yt2, XDT.v(2048 + h * 128, 128), mt.v(256, 128), start=False, stop=(c == 0 and pr == 1))
                    if c > 0:
                        p.matmul(yt, PREVB.v(h * 128, 128), gt.v(), start=False, stop=(pr == 1))
                    if pr == 1:
                        gg = GG[pc % 4]
                        p.stt(gg.v(), XST.v(pc * 256, 256), prm(P_DSK + pc, 1), yt, ALU.mult, ALU.add)
                        p.tt(gg.v(), gg.v(), SZ.v(pc * 256, 256), ALU.mult)
                        p.act(GSQ[pc % 4].v(), gg.v(), AF.Square)
                def norm_stage(g, t0=t0):
                        pc = 2 * g + 1
                        pcs = [pc - 1, pc]
                        ss = PS[7].v(256, 256)
                        p.matmul(ss, ones, GSQ[pcs[0] % 4].v(), start=True, stop=False)
                        p.matmul(ss, ones, GSQ[pcs[1] % 4].v(), start=False, stop=True)
                        p.act(RS.v(), ss, AF.Ln, scale=1.0 / 256, bias=epsv)
                        p.act(RS.v(), RS.v(), AF.Exp, scale=-0.5)
                        for q in pcs:
                            p.stt(MIX.v((4 + q) * T + t0, 256), GG[q % 4].v(), prm(P_SNW + q, 1), RS.v(),
                                  ALU.mult, ALU.mult)

                for h in range(17):
                    if h < 16:
                        seg_stage(h)
                    if 1 <= h <= 16:
                        y_stage(h - 1)
                    if h >= 2 and (h - 2) % 4 == 3:
                        norm_stage((h - 2) // 4)
                pending.append(lambda ns=norm_stage: ns(3))
                if c < 7:
                    for g in range(4):
                        pv = PREV.v(g * 256, 256)
                        for hh in range(2):
                            p.copy(PREVB.v(g * 512, 512).re("p (c e q) -> p c e q", c=2, e=2)[:, :, hh, hh * 64:hh * 64 + 64],
                                   pv.re("p (c e q) -> p c e q", c=2, e=2)[:, :, hh, :], eng="pool")
            while pending:
                pending.pop(0)()
            dbg("ssd", MIX.v(4 * T, 8 * T), [128, 8 * T])

        def attn_core(kt_list_fn, nq, kv_lhsT, q_rhs, v_lhsT, scale, PTs, epilogue, diag_fn=None, den_lhsT=None):
            pass

        def phase_attn(b):
            NW = 2048
            load_w("attn", wsi3, win3, 0, NW, 8)
            wk = Bump(WK, 0, WKSZ)
            bm = Bump(MIX, 12 * T * 2, 16384)
            bw = Bump(WB, 8 * NW * 2, WB.ncols * 2 - 8 * NW * 2)
            sets = [dict(QK=[wk.get(BF16, T) for _ in range(4)], VA=wk.get(BF16, 2048), VB=wk.get(BF16, 2048),
                         SG=wk.get(BF16, T)),
                    dict(QK=[bm.get(BF16, T) for _ in range(4)], VA=bw.get(BF16, 2048), VB=bw.get(BF16, 2048),
                         SG=bw.get(BF16, T))]
            PT = [wk.get(BF16, 512) for _ in range(4)]
            R = [wk.get(F32, 512) for _ in range(2)]
            T1 = [wk.get(F32, 512) for _ in range(2)]
            PENB = wk.get(BF16, 8 * 128)
            GM = wk.get(F32, 64)
            CMP = wk.get(F32, 512)
            RANK = wk.get(F32, 64)
            KSUM = wk.get(F32, 8)
            KMTS = [wk.get(BF16, 8) for _ in range(2)]
            for KMT in KMTS:
                p.memset(KMT.v(), 0.0, eng="pool")
            p.memset(PENB.v(), 0.0, eng="pool")
            for S in sets:
                for i in range(2):
                    p.memset(S["QK"][i].v(), 0.0, eng="pool")
                    p.dma(S["QK"][2 + i].v(), scv(kcs_d, "kc", 0), q="sp")
                p.memset(S["VA"].v(), 1.0, eng="pool")
                p.memset(S["VB"].v(), 1.0, eng="pool")

            def make_thunks(hp, S):
                QK, VA, VB, SG = S["QK"], S["VA"], S["VB"], S["SG"]
                th = []

                def t_qc():
                    for hh in range(2):
                        h_ = 2 * hp + hh
                        p.dma(QK[hh].v(0, T, 96, 100), scv(qcs_d[4 * h_:4 * h_ + 4, :], "qc", 0), q="sp")
                th.append(t_qc)
                def half_proj(c0, tq, half, evac):
                    def f():
                        ps = PS[2 + tq % 2]
                        if half == 1:
                            for k in range(8):
                                p.matmul(ps.v(), WB.v(k * NW + c0, 128), utv(k, tq * 512, 512), start=(k == 0), stop=(k == 7))
                            evac(ps)
                    return f

                for which, c0 in ((0, C_Q), (1, C_K)):
                    for tq in range(4):
                        def ev(ps, which=which, tq=tq):
                            p.copy(QK[2 * which].v(tq * 512, 512, 0, 64), ps.v(0, 512, 0, 64), eng="act")
                            p.copy(QK[2 * which + 1].v(tq * 512, 512, 0, 64), ps.v(0, 512, 64, 128), eng="dve")
                        th.append(half_proj(c0 + hp * 128, tq, 0, ev))
                        th.append(half_proj(c0 + hp * 128, tq, 1, ev))

                def gate1(hh):
                    K = QK[2 + hh]
                    KMT = KMTS[hh]
                    p.reduce(KSUM.v(0, 8, 0, 64), K.v(0, T, 0, 64).re("p (j s) -> p j s", j=8), ALU.add)
                    p.ts(KMT.v(0, 8, 0, 64), KSUM.v(0, 8, 0, 64), 1.0 / 256, None, ALU.mult)

                def gate2(hh):
                    Q = QK[hh]
                    KMT = KMTS[hh]
                    gps = PS[2]
                    for ti in range(8):
                        p.matmul(gps.v(ti * 8, 8), Q.v(1024 + ti * 128, 128), KMT.v())
                    p.tt(GM.v(), gps.v(0, 64), CF.v(CF_CAP, 64), ALU.min)
                    g3 = GM.v().re("p (t j) -> p t j", t=8)
                    p.tt(CMP.v().re("p (t j k) -> p t j k", t=8, j=8),
                         g3.re("p t (o k) -> p t o k", o=1).bc([128, 8, 8, 8]),
                         g3.re("p t (j o) -> p t j o", o=1).bc([128, 8, 8, 8]), ALU.is_gt)
                    p.reduce(RANK.v(), CMP.v().re("p (a k) -> p a k", k=8), ALU.add)
                    p.ts(RANK.v(), RANK.v(), 3.0, None, ALU.is_lt)
                    p.tt(RANK.v(), RANK.v(), CF.v(CF_CMK, 64), ALU.mult)
                    p.tt(RANK.v(), RANK.v(), CF.v(CF_OWN, 64), ALU.add)
                    p.ts(PENB.v3(8, 128, 64, 8), RANK.v().re("p (t j) -> p t j", t=8), -1.0, BIG, ALU.add, ALU.mult)

                def gate3(hh):
                    Q = QK[hh]
                    pps = PS[3]
                    for ti in range(8):
                        p.transpose(pps.vb(BF16, ti * 128, 128), PENB.v(ti * 128, 128), ident)
                    p.copy(Q.v(1024, 1024, 64, 72), pps.vb(BF16, 0, 1024, 64, 72), eng="dve")

                th.append(lambda: gate1(0))
                th.append(lambda: gate1(1))
                for tq in range(4):
                    def ev(ps, tq=tq):
                        p.copy(SG.v(tq * 512, 512), ps.v(), eng="dve")
                    th.append(half_proj(C_G + hp * 128, tq, 0, ev))
                    th.append(half_proj(C_G + hp * 128, tq, 1, ev))
                vth = []
                for tg in range(4):
                    for j in range(4):
                        def f(tg=tg, j=j):
                            ps = PS[2 + tg % 2]
                            ti = tg * 4 + j
                            for k in range(8):
                                p.matmul(ps.v(j * 128, 128), utv(k, ti * 128, 128),
                                         WB.v(k * NW + C_V + hp * 128, 128), start=(k == 0), stop=(k == 7))
                            if j == 3:
                                src = ps.v().re("p (j c) -> p j c", j=4)
                                p.copy(VA.v3(4, 128, tg * 512, 64), src[:, :, 0:64], eng="dve")
                                p.copy(VB.v3(4, 128, tg * 512 + 64, 64), src[:, :, 64:128], eng="dve")
                        vth.append(f)
                th.append(lambda: gate2(0))
                th += vth[0:4]
                th.append(lambda: gate3(0))
                th.append(lambda: gate2(1))
                th += vth[4:8]
                th.append(lambda: gate3(1))
                th += vth[8:16]
                th.append(lambda: p.act(SG.v(), SG.v(), AF.Silu))
                return th

            def attention(hp, hh, S, bg, pt_base):
                Q = S["QK"][hh]
                K = S["QK"][2 + hh]
                V = S["VA"] if hh == 0 else S["VB"]
                SG = S["SG"]
                tiles = []
                for qt in range(4):
                    order = [4 * qt + r for r in range(4)] + list(range(4 * qt))
                    for j, kt in enumerate(order):
                        tiles.append((qt, kt, j == 0, j == len(order) - 1))
                o0, d0 = (0, 64) if hh == 0 else (64, 0)

                def geom(idx):
                    qt, kt, first, last = tiles[idx]
                    tq0 = qt * 512
                    s0 = kt * 128
                    qlo = max(tq0, s0)
                    return qt, kt, tq0, s0, qlo, tq0 + 512 - qlo

                def emit_qk(idx):
                    qt, kt, tq0, s0, qlo, n = geom(idx)
                    gi = pt_base + idx
                    sp = PS[4 + gi % 4]
                    ptile = PT[gi % 4]
                    p.matmul(sp.v(0, n), K.v(s0, 128), Q.v(qlo, n))
                    p.act(ptile.v(0, n), sp.v(0, n), AF.Exp, scale=0.125)
                    if s0 >= tq0:
                        p.tt(ptile.v(0, 128), ptile.v(0, 128), tri, ALU.mult, eng="pool")

                def emit_pv(idx):
                    qt, kt, tq0, s0, qlo, n = geom(idx)
                    gi = pt_base + idx
                    first, last = tiles[idx][2], tiles[idx][3]
                    po = PS[qt % 2]
                    ptile = PT[gi % 4]
                    p.matmul(po.v(qlo - tq0, n), V.v(kt * 128, 128), ptile.v(0, n), start=first, stop=last)
                    if last:
                        r_ = R[qt % 2]
                        t1_ = T1[qt % 2]
                        p.act(r_.v(0, 512, o0, o0 + 64), po.v(0, 512, d0, d0 + 64), AF.Ln)
                        p.act(r_.v(0, 512, o0, o0 + 64), r_.v(0, 512, o0, o0 + 64), AF.Exp, scale=-1.0)
                        p.tt(t1_.v(0, 512, o0, o0 + 64), r_.v(0, 512, o0, o0 + 64), SG.v(tq0, 512, o0, o0 + 64),
                             ALU.mult, eng="dve")
                        p.tt(MIX.v(hp * T + tq0, 512, o0, o0 + 64), po.v(0, 512, o0, o0 + 64),
                             t1_.v(0, 512, o0, o0 + 64), ALU.mult)

                LA = 3
                for idx in range(len(tiles) + LA):
                    if idx < len(tiles):
                        emit_qk(idx)
                    if idx >= LA:
                        emit_pv(idx - LA)
                    if bg and idx % 2 == 1:
                        bg.pop(0)()
                return pt_base + len(tiles)

            pt_i = 0
            for f in make_thunks(0, sets[0]):
                f()
            for hp in range(4):
                bg = make_thunks(hp + 1, sets[(hp + 1) % 2]) if hp < 3 else []
                for hh in range(2):
                    pt_i = attention(hp, hh, sets[hp % 2], bg, pt_i)
                while bg:
                    bg.pop(0)()
            dbg("att", MIX.v(0, 4 * T), [128, 4 * T])

        def phase_mem(b):
            NW = 1024
            load_w("mem", wsi3, win3, C_QM, NW, 8)
            load_w("kv", wskv3, wkv3, 0, NW, 8, wb_off=8192)
            wk = Bump(WK, 0, WKSZ)
            MEMT = wk.get(BF16, 8 * 256)
            KM = wk.get(BF16, 4 * 256)
            VM = wk.get(BF16, 2 * 512)
            QM = [wk.get(BF16, T) for _ in range(2)]
            SGM = [wk.get(BF16, T) for _ in range(2)]
            PT = [wk.get(BF16, 512) for _ in range(2)]
            R = wk.get(F32, 512)
            T1 = wk.get(F32, 512)
            rms_transpose(lambda i: mem_d[b, i * 128:(i + 1) * 128, :], 2, P_MNW,
                          lambda i: MEMT.v3(8, 256, i * 128, 128), wk)
            for h in range(4):
                ps = PS[2 + h % 2]
                for k in range(8):
                    p.matmul(ps.v(0, 256), WB.v(8192 + k * NW + h * 128, 128), MEMT.v(k * 256, 256),
                             start=(k == 0), stop=(k == 7))
                p.copy(KM.v(h * 256, 256), ps.v(0, 256), eng="act")
            for mt in range(2):
                ps = PS[2 + mt % 2]
                for k in range(8):
                    p.matmul(ps.v(), MEMT.v(k * 256 + mt * 128, 128), WB.v(8192 + k * NW + 512, 512),
                             start=(k == 0), stop=(k == 7))
                p.copy(VM.v(mt * 512, 512), ps.v(), eng="dve")
            pt_i = 0
            for h in range(4):
                qm = QM[h % 2]
                sgm = SGM[h % 2]
                for tq in range(4):
                    ps = PS[2 + tq % 2]
                    proj_fm(NW, h * 128, 128, tq * 512, 512, ps.v())
                    p.copy(qm.v(tq * 512, 512), ps.v(), eng="act")
                for tq in range(4):
                    ps = PS[2 + tq % 2]
                    proj_fm(NW, 512 + h * 128, 128, tq * 512, 512, ps.v())
                    p.act(sgm.v(tq * 512, 512), ps.v(), AF.Silu)
                for qt in range(4):
                    tq0 = qt * 512
                    po = PS[qt % 2]
                    den = PS[6 + qt % 2]
                    for mt in range(2):
                        sp = PS[4 + pt_i % 2]
                        ptile = PT[pt_i % 2]
                        pt_i += 1
                        p.matmul(sp.v(), KM.v(h * 256 + mt * 128, 128), qm.v(tq0, 512))
                        p.act(ptile.v(), sp.v(), AF.Exp, scale=128 ** -0.5)
                        p.matmul(po.v(), VM.v(mt * 512 + h * 128, 128), ptile.v(), start=(mt == 0), stop=(mt == 1))
                        p.matmul(den.v(), ones, ptile.v(), start=(mt == 0), stop=(mt == 1))
                    p.act(R.v(), den.v(), AF.Ln)
                    p.act(R.v(), R.v(), AF.Exp, scale=-1.0)
                    p.tt(T1.v(), R.v(), sgm.v(tq0, 512), ALU.mult, eng="pool")
                    p.tt(MIX.v((12 + h) * T + tq0, 512), po.v(), T1.v(), ALU.mult)
            dbg("memo", MIX.v(12 * T, 4 * T), [128, 4 * T])

        def phase_out(b):
            NW = 1024
            load_w("out", wso3, wout3, 0, NW, 16)
            wk = Bump(WK, 0, WKSZ)
            XT = [wk.get(F32, 1024) for _ in range(2)]
            HT = [wk.get(F32, 1024) for _ in range(2)]
            OT = [wk.get(F32, 1024) for _ in range(2)]
            JNK = wk.get(BF16, 1024)
            SSQ = wk.get(F32, 16)
            RSTD = wk.get(F32, 16)
            for i in range(16):
                xt = XT[i % 2]
                ht = HT[i % 2]
                ot = OT[i % 2]
                p.dma(xt.v(), D(x_d[b, i * 128:(i + 1) * 128, :]))
                for nh in range(2):
                    ps = PS[(2 * i + nh) % 4]
                    for m in range(16):
                        p.matmul(ps.v(), MIX.v(m * T + i * 128, 128), WB.v(m * NW + nh * 512, 512),
                                 start=(m == 0), stop=(m == 15))
                    p.tt(ht.v(nh * 512, 512), ps.v(), xt.v(nh * 512, 512), ALU.add)
                p.act(JNK.v(), ht.v(), AF.Square, accum_out=SSQ.v(i, 1))
                p.act(RSTD.v(i, 1), SSQ.v(i, 1), AF.Ln, scale=1.0 / DM, bias=epsv)
                p.act(RSTD.v(i, 1), RSTD.v(i, 1), AF.Exp, scale=-0.5)
                p.stt(ot.v(), ht.v(), RSTD.v(i, 1), prm(P_FNW, 1024), ALU.mult, ALU.mult)
                p.dma(D(out_d[b, i * 128:(i + 1) * 128, :]), ot.v())

        for b in range(nseq):
            wkA = Bump(WK, 16384, WKSZ - 16384)
            rms_transpose(lambda i: x_d[b, i * 128:(i + 1) * 128, :], 16, P_NW,
                          lambda i: UT.v3(BF16, 8, T, i * 128, 128), wkA, nbuf=3)
            if b == 0:
                dbg("ut", UT.v(), [128, 8 * T])
            if "S" in phases:
                phase_ssd(b)
            if "B" in phases:
                phase_attn(b)
            if "M" in phases:
                phase_mem(b)
            if "E" in phases:
                phase_out(b)
        print("ops:", p.nops, {e: len(v) for e, v in p.ops.items()})
        p.emit()
    return nc, dumps


def kernel(x, mem, norm_w, w_in, conv_w, conv_b, dt_bias, a_log, d_skip, ssd_norm_w, mem_norm_w, w_mem_kv,
           w_out, final_norm_w):
    f = lambda a: np.ascontiguousarray(np.asarray(a, dtype=np.float32))
    x, mem = f(x), f(mem)
    cb, cf, kc, qc = host_consts()
    cf[:, CF_PRM:CF_PRM + NPRM] = host_params(f(norm_w)[0], f(conv_w)[0], f(conv_b)[0], f(dt_bias)[0], f(a_log)[0],
                                              f(d_skip)[0], f(ssd_norm_w)[0], f(mem_norm_w)[0], f(final_norm_w))
    nc, _ = build()
    shared = {"w_in": f(w_in)[0], "w_kv": f(w_mem_kv)[0], "w_out": f(w_out)[0], "cstb": cb, "cstf": cf,
              "kconst": kc, "qconst": qc}
    in_maps = []
    for c in range(8):
        m = dict(shared)
        m["x"] = x[c * NSEQ:(c + 1) * NSEQ]
        m["mem"] = mem[c * NSEQ:(c + 1) * NSEQ]
        in_maps.append(m)
    res = run_bass_kernel_spmd(nc, in_maps, core_ids=list(range(8)))
    return np.concatenate([r["out"] for r in res.results], axis=0)
```
